# Optimizing a Trainium2 kernel written in Bass

```python
import math
import jax, jax.numpy as jnp
from jax import lax
import numpy as np

D_MODEL = 2048
BATCH = 4
SEQ = 4096
DEPTH = 4

M_HEADS = 8
M_QK_DIM = 128
M_V_DIM = 256
M_CHUNK = 64
CONV_K = 4
M_QK = M_HEADS * M_QK_DIM
M_V = M_HEADS * M_V_DIM
D_HEADS = 8
D_QK_DIM = 128
D_V_DIM = 2 * D_QK_DIM
D_Q = D_HEADS * 2 * D_QK_DIM
D_V = D_HEADS * D_V_DIM
Q_BLOCK = 128
D_FF = -(-8 * D_MODEL // 768) * 256
EPS = 1e-6
IN_SIZES = (M_QK, M_QK, M_V, M_V, M_HEADS, M_HEADS, D_Q, D_Q, D_V, D_MODEL, D_MODEL)
N_IN = sum(IN_SIZES)

kernel_name = "hybrid_mlstm_diffattn_gated_block"


def rmsnorm(x, w):
    xf = x.astype(jnp.float32)
    y = xf * lax.rsqrt(jnp.mean(xf * xf, axis=-1, keepdims=True) + EPS)
    return (y * w.astype(jnp.float32)).astype(x.dtype)


def head_rmsnorm(x, w):
    B, S, H, d = x.shape
    xf = x.astype(jnp.float32)
    y = xf * lax.rsqrt(jnp.mean(xf * xf, axis=-1, keepdims=True) + EPS)
    return y.reshape(B, S, H * d) * w.astype(jnp.float32)


def split_in(proj):
    outs, off = [], 0
    for n in IN_SIZES:
        outs.append(proj[..., off:off + n])
        off += n
    return outs


def causal_dwconv(u, w, b):
    C = u.shape[-1]
    y = lax.conv_general_dilated(u, w[:, None, :].astype(u.dtype), window_strides=(1,),
                                 padding=[(CONV_K - 1, 0)],
                                 dimension_numbers=('NWC', 'WIO', 'NWC'),
                                 feature_group_count=C)
    return y + b.astype(u.dtype)


def mlstm_chunkwise(q, k, v, ig, lf):
    B, S, H, dk = q.shape
    dv = v.shape[-1]
    nc = S // M_CHUNK

    def chunks(t):
        t = t.reshape((B, nc, M_CHUNK) + t.shape[2:])
        return t.transpose((1, 0, 3, 2) + tuple(range(4, t.ndim)))

    tril = jnp.tril(jnp.ones((M_CHUNK, M_CHUNK), dtype=bool))

    def step(carry, xs):
        C, n, m = carry
        qc, kc, vc, igc, lfc = xs
        b = jnp.cumsum(lfc, axis=-1)
        log_d = jnp.where(tril, b[..., :, None] - b[..., None, :] + igc[..., None, :], -jnp.inf)
        log_inter = b + m[..., None]
        m_row = jnp.maximum(log_inter, jnp.max(log_d, axis=-1))
        dmat = jnp.exp(log_d - m_row[..., None])
        inter = jnp.exp(log_inter - m_row)
        s = jnp.einsum('bhld,bhsd->bhls', qc, kc) * dmat
        num = jnp.einsum('bhls,bhsv->bhlv', s, vc) + inter[..., None] * jnp.einsum('bhvd,bhld->bhlv', C, qc)
        den = jnp.sum(s, axis=-1) + inter * jnp.einsum('bhd,bhld->bhl', n, qc)
        h = num / jnp.maximum(jnp.abs(den), jnp.exp(-m_row))[..., None]
        bL = b[..., -1]
        log_w = bL[..., None] - b + igc
        m_new = jnp.maximum(bL + m, jnp.max(log_w, axis=-1))
        w = jnp.exp(log_w - m_new[..., None])
        decay = jnp.exp(bL + m - m_new)
        C_new = decay[..., None, None] * C + jnp.einsum('bhl,bhlv,bhld->bhvd', w, vc, kc)
        n_new = decay[..., None] * n + jnp.einsum('bhl,bhld->bhd', w, kc)
        return (C_new, n_new, m_new), h

    init = (jnp.zeros((B, H, dv, dk), jnp.float32), jnp.zeros((B, H, dk), jnp.float32),
            jnp.zeros((B, H), jnp.float32))
    _, hs = lax.scan(step, init, (chunks(q), chunks(k), chunks(v), chunks(ig), chunks(lf)))
    return hs.transpose(1, 0, 3, 2, 4).reshape(B, S, H, dv)


def diff_attention(q, k, v, lam):
    B, S, H, _, d = q.shape
    dv = v.shape[-1]
    nb = S // Q_BLOCK
    kt = k.transpose(0, 2, 3, 1, 4)
    vt = v.transpose(0, 2, 1, 3)
    qb = (q * (d ** -0.5)).reshape(B, nb, Q_BLOCK, H, 2, d).transpose(1, 0, 3, 4, 2, 5)
    kpos = jnp.arange(S)

    def block(args):
        qblk, start = args
        s = jnp.einsum('bhcqd,bhckd->bhcqk', qblk, kt).astype(jnp.float32)
        qpos = start + jnp.arange(Q_BLOCK)
        mask = kpos[None, :] <= qpos[:, None]
        p = jax.nn.softmax(jnp.where(mask, s, -jnp.inf), axis=-1)
        a = p[:, :, 0] - lam * p[:, :, 1]
        return jnp.einsum('bhqk,bhkv->bhqv', a.astype(v.dtype), vt)

    out = lax.map(block, (qb, jnp.arange(nb, dtype=jnp.int32) * Q_BLOCK))
    return out.transpose(1, 0, 3, 2, 4).reshape(B, S, H, dv)


def hybrid_mixer(h, w_in, conv_w, conv_b, b_ig, b_fg, m_norm_w, lq1, lk1, lq2, lk2,
                 d_norm_w, w_bm, w_bd, w_out, lam_init):
    B, S, _ = h.shape
    proj = h @ w_in
    mq, mk, mv, mo, mi, mf, dq, dk, dv, gm, gd = split_in(proj)
    qk = jax.nn.silu(causal_dwconv(jnp.concatenate([mq, mk], axis=-1), conv_w, conv_b))
    q = qk[..., :M_QK].reshape(B, S, M_HEADS, M_QK_DIM).astype(jnp.float32)
    k = qk[..., M_QK:].reshape(B, S, M_HEADS, M_QK_DIM).astype(jnp.float32) * (M_QK_DIM ** -0.5)
    v = mv.reshape(B, S, M_HEADS, M_V_DIM).astype(jnp.float32)
    ig = (mi + b_ig).astype(jnp.float32)
    lf = jax.nn.log_sigmoid((mf + b_fg).astype(jnp.float32))
    hm = mlstm_chunkwise(q, k, v, ig, lf)
    hm = jax.nn.sigmoid(mo) * head_rmsnorm(hm, m_norm_w).astype(h.dtype)
    lam = (jnp.exp(jnp.sum(lq1.astype(jnp.float32) * lk1.astype(jnp.float32)))
           - jnp.exp(jnp.sum(lq2.astype(jnp.float32) * lk2.astype(jnp.float32))) + lam_init)
    hd = diff_attention(dq.reshape(B, S, D_HEADS, 2, D_QK_DIM), dk.reshape(B, S, D_HEADS, 2, D_QK_DIM),
                        dv.reshape(B, S, D_HEADS, D_V_DIM), lam)
    hd = (head_rmsnorm(hd, d_norm_w) * (1.0 - lam_init)).astype(h.dtype)
    y = jax.nn.sigmoid(gm) * (hm @ w_bm) + jax.nn.sigmoid(gd) * (hd @ w_bd)
    return y @ w_out


def swiglu(h, w_gate, w_up, w_down):
    return (jax.nn.silu(h @ w_gate) * (h @ w_up)) @ w_down


def setup_inputs(seed: int = 0) -> dict:
    key = jax.random.key(seed)
    ks = jax.random.split(key, 24)
    nrm = lambda k, shape, s: jax.random.normal(k, shape, jnp.float32) * s
    L = DEPTH
    f_bias = jnp.linspace(3.0, 6.0, M_HEADS, dtype=jnp.float32)[None, :] + nrm(ks[6], (L, M_HEADS), 0.1)
    return {
        "x": nrm(ks[0], (BATCH, SEQ, D_MODEL), 1.0),
        "attn_norm_w": 1.0 + nrm(ks[1], (L, D_MODEL), 0.02),
        "w_in": nrm(ks[2], (L, D_MODEL, N_IN), D_MODEL ** -0.5),
        "conv_w": nrm(ks[3], (L, CONV_K, 2 * M_QK), CONV_K ** -0.5),
        "conv_b": nrm(ks[4], (L, 2 * M_QK), 0.01),
        "b_igate": nrm(ks[5], (L, M_HEADS), 0.1),
        "b_fgate": f_bias,
        "mlstm_norm_w": 1.0 + nrm(ks[7], (L, M_V), 0.02),
        "lambda_q1": nrm(ks[8], (L, D_QK_DIM), 0.1),
        "lambda_k1": nrm(ks[9], (L, D_QK_DIM), 0.1),
        "lambda_q2": nrm(ks[10], (L, D_QK_DIM), 0.1),
        "lambda_k2": nrm(ks[11], (L, D_QK_DIM), 0.1),
        "diff_norm_w": 1.0 + nrm(ks[12], (L, D_V), 0.02),
        "w_branch_m": nrm(ks[13], (L, M_V, D_MODEL), M_V ** -0.5),
        "w_branch_d": nrm(ks[14], (L, D_V, D_MODEL), D_V ** -0.5),
        "w_out": nrm(ks[15], (L, D_MODEL, D_MODEL), D_MODEL ** -0.5),
        "ffn_norm_w": 1.0 + nrm(ks[16], (L, D_MODEL), 0.02),
        "w_ffn_gate": nrm(ks[17], (L, D_MODEL, D_FF), D_MODEL ** -0.5),
        "w_ffn_up": nrm(ks[18], (L, D_MODEL, D_FF), D_MODEL ** -0.5),
        "w_ffn_down": nrm(ks[19], (L, D_FF, D_MODEL), D_FF ** -0.5),
        "final_norm_w": 1.0 + nrm(ks[20], (D_MODEL,), 0.02),
    }


def reference(x, attn_norm_w, w_in, conv_w, conv_b, b_igate, b_fgate, mlstm_norm_w,
              lambda_q1, lambda_k1, lambda_q2, lambda_k2, diff_norm_w, w_branch_m, w_branch_d,
              w_out, ffn_norm_w, w_ffn_gate, w_ffn_up, w_ffn_down, final_norm_w):
    for l in range(DEPTH):
        lam_init = 0.8 - 0.6 * math.exp(-0.3 * l)
        h = rmsnorm(x, attn_norm_w[l])
        x = x + hybrid_mixer(h, w_in[l], conv_w[l], conv_b[l], b_igate[l], b_fgate[l], mlstm_norm_w[l],
                             lambda_q1[l], lambda_k1[l], lambda_q2[l], lambda_k2[l], diff_norm_w[l],
                             w_branch_m[l], w_branch_d[l], w_out[l], lam_init)
        h = rmsnorm(x, ffn_norm_w[l])
        x = x + swiglu(h, w_ffn_gate[l], w_ffn_up[l], w_ffn_down[l])
    return rmsnorm(x, final_norm_w)
```

```python
import contextlib
import math
import numpy as np
import ml_dtypes
import concourse.bass as bass
import concourse.mybir as mybir
from concourse.bass_utils import run_bass_kernel_spmd

F32 = mybir.dt.float32
BF16 = mybir.dt.bfloat16
AF = mybir.ActivationFunctionType
ALU = mybir.AluOpType
AX = mybir.AxisListType
NPBF = ml_dtypes.bfloat16

D_MODEL = 2048
BATCH = 4
SEQ = 4096
DEPTH = 4
NCORES = 8
TOK = 2048
D_FF = 5632
N_IN = 16400
EPS = 1e-6
KC = D_MODEL // 128

PE, ACT, DVE, POOL, SP = "pe", "act", "dve", "pool", "sp"
COMPUTE = (PE, ACT, DVE, POOL)


class Buf:
    __slots__ = ("name", "last_write", "reads", "dma_sem")

    def __init__(self, name):
        self.name = name
        self.last_write = None
        self.reads = {}
        self.dma_sem = None


class Instr:
    __slots__ = ("eng", "fn", "deps", "is_dma", "sem_key", "sig_val", "needs_sig")

    def __init__(self, eng, fn, is_dma, sem_key):
        self.eng = eng
        self.fn = fn
        self.deps = []
        self.is_dma = is_dma
        self.sem_key = sem_key
        self.sig_val = None
        self.needs_sig = False


class Prog:
    NDMA = 72

    def __init__(self, nc, es):
        self.nc = nc
        self.esem = {e: es.enter_context(nc.semaphore("s_" + e)) for e in COMPUTE}
        self.dsem = [es.enter_context(nc.semaphore("d_%d" % i)) for i in range(self.NDMA)]
        self.cnt = {e: 0 for e in COMPUTE}
        self.dcnt = [0] * self.NDMA
        self.barrier = []
        self.n_total = 0
        self._reset()

    def _reset(self):
        self.q = {e: [] for e in (PE, ACT, DVE, POOL, SP)}
        self.started = set()

    def op(self, eng, fn, reads=(), writes=(), dma=False, sem_key=None, pe_accum=False):
        if dma and sem_key is None:
            sem_key = writes[0] if len(writes) else reads[0]
        ins = Instr(eng, fn, dma, sem_key)
        deps = []
        if eng not in self.started:
            self.started.add(eng)
            ins.deps.extend(self.barrier)
        for b in reads:
            if b.last_write is not None:
                deps.append(b.last_write)
        for b in writes:
            if b.last_write is not None:
                deps.append(b.last_write)
            deps.extend(b.reads.values())
        seen = set()
        for d in deps:
            if d is ins or id(d) in seen:
                continue
            seen.add(id(d))
            if (not d.is_dma) and d.eng == PE and eng == PE and not dma:
                continue
            ins.deps.append(d)
            d.needs_sig = True
        for b in reads:
            b.reads[("d", id(sem_key)) if dma else eng] = ins
        for b in writes:
            b.last_write = ins
            b.reads = {}
        self.q[eng].append(ins)
        return ins

    def end_phase(self, last=False):
        nc = self.nc
        bar = {}
        for e in self.q:
            if self.q[e]:
                bar[id(self.q[e][-1])] = self.q[e][-1]
            for ins in self.q[e]:
                if ins.is_dma:
                    bar["k%d" % id(ins.sem_key)] = ins
        barrier = []
        seen = set()
        for ins in bar.values():
            if id(ins) not in seen:
                seen.add(id(ins))
                barrier.append(ins)
                ins.needs_sig = True
        nkeys = 0
        for e in self.q:
            for ins in self.q[e]:
                if ins.is_dma and ins.needs_sig and ins.sem_key.dma_sem is None:
                    ins.sem_key.dma_sem = nkeys
                    nkeys += 1
        assert nkeys <= self.NDMA, nkeys
        for e in self.q:
            for ins in self.q[e]:
                self.n_total += 1
                if not ins.needs_sig:
                    continue
                if ins.is_dma:
                    k = ins.sem_key.dma_sem
                    self.dcnt[k] += 16
                    ins.sig_val = (self.dsem[k], self.dcnt[k], 16)
                else:
                    self.cnt[ins.eng] += 1
                    ins.sig_val = (self.esem[ins.eng], self.cnt[ins.eng], 1)
        q = self.q
        with nc.Block() as block:
            def run(e, eng_obj):
                waited = {}
                for ins in q[e]:
                    for d in ins.deps:
                        sem, val, _ = d.sig_val
                        key = id(sem)
                        if waited.get(key, 0) >= val:
                            continue
                        waited[key] = val
                        eng_obj.wait_ge(sem, val)
                    bi = ins.fn(eng_obj)
                    if ins.needs_sig:
                        sem, val, inc = ins.sig_val
                        bi.then_inc(sem, inc)
                if e == SP and last:
                    for ins in barrier:
                        sem, val, _ = ins.sig_val
                        if waited.get(id(sem), 0) >= val:
                            continue
                        waited[id(sem)] = val
                        eng_obj.wait_ge(sem, val)

            if q[PE]:
                @block.tensor
                def _(eng):
                    run(PE, eng)
            if q[ACT]:
                @block.scalar
                def _(eng):
                    run(ACT, eng)
            if q[DVE]:
                @block.vector
                def _(eng):
                    run(DVE, eng)
            if q[POOL]:
                @block.gpsimd
                def _(eng):
                    run(POOL, eng)
            if q[SP] or last:
                @block.sync
                def _(eng):
                    run(SP, eng)
        for ins in barrier:
            ins.fn = None
        for e in q:
            for ins in q[e]:
                ins.fn = None
                ins.deps = None
        self.barrier = barrier
        self._reset()


class Ctx:
    def __init__(self, nc, es):
        self.nc = nc
        self.ges = es
        self.es = None
        self.P = Prog(nc, es)
        self.uid = 0
        self.banks = []
        self.bank_bufs = []
        self.bank_i = 0
        self.evac_i = 0

    def sb(self, name, shape, dt):
        self.uid += 1
        return self.es.enter_context(self.nc.sbuf_tensor("%s_%d" % (name, self.uid), list(shape), dt))

    def begin(self):
        self.es = contextlib.ExitStack()
        self.es.__enter__()
        self.bank_bufs = [Buf("bank%d" % i) for i in range(len(self.banks))]

    def end(self, last=False):
        self.P.end_phase(last)
        self.es.__exit__(None, None, None)
        self.es = None

    def alloc_banks(self, n=8):
        for i in range(n):
            self.banks.append(self.ges.enter_context(self.nc.psum_tensor("bank%d" % i, [128, 512], F32)))
            self.bank_bufs.append(Buf("bank%d" % i))

    def next_bank(self):
        i = self.bank_i % len(self.banks)
        self.bank_i += 1
        return self.banks[i], self.bank_bufs[i]

    def mm(self, out, lhsT, rhs, start, stop, reads, writes):
        return self.P.op(PE, lambda e: e.matmul(out, lhsT, rhs, start=start, stop=stop),
                         reads=reads, writes=writes)

    def act(self, out, in_, func, reads, writes, bias=None, scale=None, accum_out=None):
        kw = {}
        if bias is not None:
            kw["bias"] = bias
        if scale is not None:
            kw["scale"] = scale
        if accum_out is not None:
            kw["accum_out"] = accum_out
        return self.P.op(ACT, lambda e: e.activation(out=out, in_=in_, func=func, **kw),
                         reads=reads, writes=writes)

    def dma(self, eng, out, in_, reads, writes, sem_key=None):
        return self.P.op(eng, lambda e: e.dma_start(out=out, in_=in_), reads=reads, writes=writes,
                         dma=True, sem_key=sem_key)

    def tt(self, eng, out, in0, in1, op, reads, writes):
        return self.P.op(eng, lambda e: e.tensor_tensor(out=out, in0=in0, in1=in1, op=op),
                         reads=reads, writes=writes)

    def ts(self, eng, out, in0, s1, s2, op0, op1, reads, writes, accum_out=None):
        if op1 is None:
            return self.P.op(eng, lambda e: e.tensor_scalar(out=out, in0=in0, scalar1=s1, scalar2=None, op0=op0),
                             reads=reads, writes=writes)
        if accum_out is not None:
            return self.P.op(eng, lambda e: e.tensor_scalar(out=out, in0=in0, scalar1=s1, scalar2=s2, op0=op0,
                                                            op1=op1, accum_out=accum_out),
                             reads=reads, writes=writes)
        return self.P.op(eng, lambda e: e.tensor_scalar(out=out, in0=in0, scalar1=s1, scalar2=s2, op0=op0, op1=op1),
                         reads=reads, writes=writes)

    def stt(self, out, in0, scalar, in1, op0, op1, reads, writes):
        return self.P.op(DVE, lambda e: e.scalar_tensor_tensor(out=out, in0=in0, scalar=scalar, in1=in1,
                                                               op0=op0, op1=op1),
                         reads=reads, writes=writes)

    def copy(self, eng, out, in_, reads, writes):
        if eng == ACT:
            return self.P.op(ACT, lambda e: e.copy(out=out, in_=in_), reads=reads, writes=writes)
        return self.P.op(eng, lambda e: e.tensor_copy(out=out, in_=in_), reads=reads, writes=writes)

    def evac_copy(self, out, in_, reads, writes):
        self.evac_i += 1
        return self.copy(ACT if self.evac_i % 2 else DVE, out, in_, reads, writes)


def rmsnorm_to_T(c, xt, xbuf, scratch, hT, hT_buf, tok0, nw, nw_buf, ident, ident_buf, pfx):
    junk, junk_b = scratch["junk"]
    ss, ss_b = scratch["ss"]
    hn, hn_b = scratch["hn"]
    c.act(junk[:], xt[:], AF.Square, reads=[xbuf], writes=[junk_b, ss_b], accum_out=ss[:, 0:1])
    c.act(ss[:, 1:2], ss[:, 0:1], AF.Sqrt, reads=[ss_b, scratch["eps_b"]], writes=[ss_b], scale=1.0 / D_MODEL, bias=scratch["eps"][:, 0:1])
    c.P.op(DVE, lambda e: e.reciprocal(out=ss[:, 2:3], in_=ss[:, 1:2]), reads=[ss_b], writes=[ss_b])
    c.act(hn[:, 0:1024], xt[:, 0:1024], AF.Copy, reads=[xbuf, ss_b], writes=[hn_b], scale=ss[:, 2:3])
    c.ts(DVE, hn[:, 1024:2048], xt[:, 1024:2048], ss[:, 2:3], None, ALU.mult, None, reads=[xbuf, ss_b], writes=[hn_b])
    for kq in range(KC // 4):
        bank, bb = c.next_bank()
        for j in range(4):
            kc = kq * 4 + j
            c.mm(bank[:, j * 128:(j + 1) * 128], hn[:, kc * 128:(kc + 1) * 128], ident[:], True, True,
                 reads=[hn_b, ident_buf], writes=[bb])
        for j in range(4):
            kc = kq * 4 + j
            if j % 2 == 0:
                c.act(hT[:, kc, tok0:tok0 + 128], bank[:, j * 128:(j + 1) * 128], AF.Copy,
                      reads=[bb, nw_buf], writes=[hT_buf], scale=nw[:, kc:kc + 1])
            else:
                c.ts(DVE, hT[:, kc, tok0:tok0 + 128], bank[:, j * 128:(j + 1) * 128], nw[:, kc:kc + 1], None,
                     ALU.mult, None, reads=[bb, nw_buf], writes=[hT_buf])


def norm_scratch(c, pfx):
    eps = c.sb(pfx + "eps", [128, 1], F32)
    eb = Buf(pfx + "eps")
    c.P.op(POOL, lambda e: e.memset(eps[:], EPS), writes=[eb])
    return {
        "junk": (c.sb(pfx + "junk", [128, 2048], BF16), Buf(pfx + "junk")),
        "ss": (c.sb(pfx + "ss", [128, 4], F32), Buf(pfx + "ss")),
        "hn": (c.sb(pfx + "hn", [128, 2048], BF16), Buf(pfx + "hn")),
        "eps": eps, "eps_b": eb,
    }


A_SECTIONS = [
    ("qk", 0, 2048, "F", F32, False),
    ("mv", 2048, 2048, "T", BF16, False),
    ("mo", 4096, 2048, "T", BF16, True),
    ("gt", 6144, 16, "F", F32, False),
    ("dqk", 6160, 4096, "F", BF16, False),
    ("dv", 10256, 2048, "T", BF16, False),
    ("gmd", 12304, 4096, "F", BF16, True),
]


def load_wblock(c, wslot, wbuf, w_dram, row0, nkc, col0, ncols):
    src = w_dram[row0:row0 + nkc * 128, col0:col0 + ncols].rearrange("(kc p) c -> p kc c", p=128)
    c.dma(POOL, wslot[:, 0:nkc, 0:ncols], src, reads=[], writes=[wbuf])


def phase_A(c, x, nw_d, w, ident_d, outs):
    if True:
        c.begin()
        hT = c.sb("hT", [128, KC, TOK], BF16)
        hT_b = Buf("hT")
        nw = c.sb("nw_s", [128, KC], F32)
        nw_b = Buf("nw")
        ident = c.sb("ident_s", [128, 128], BF16)
        ident_b = Buf("ident")
        c.dma(SP, nw[:], nw_d[:, :], [], [nw_b])
        c.dma(POOL, ident[:], ident_d[:, :], [], [ident_b])
        NW = 3
        wslots = [(c.sb("w%d" % i, [128, KC, 512], BF16), Buf("w%d" % i)) for i in range(NW)]
        xs = [(c.sb("x%d" % i, [128, D_MODEL], F32), Buf("x%d" % i)) for i in range(2)]
        scr = norm_scratch(c, "n_")
        of32 = [(c.sb("of%d" % i, [128, TOK], F32), Buf("of%d" % i)) for i in range(2)]
        obf = [(c.sb("ob%d" % i, [128, TOK], BF16), Buf("ob%d" % i)) for i in range(2)]
        otk = [(c.sb("ot%d" % i, [128, 512], BF16), Buf("ot%d" % i)) for i in range(3)]

        blocks = []
        for name, c0, n, mode, dt, sg in A_SECTIONS:
            for o in range(0, n, 512):
                blocks.append((name, c0, o, min(512, n - o), mode, dt, sg))
        PRE = NW - 1

        def issue_load(bi):
            name, c0, o, n, mode, dt, sg = blocks[bi]
            ws, wb = wslots[bi % NW]
            load_wblock(c, ws, wb, w, 0, KC, c0 + o, n)

        for bi in range(min(PRE, len(blocks))):
            issue_load(bi)

        for tt in range(TOK // 128):
            xt, xb = xs[tt % 2]
            c.dma(SP, xt[:], x[tt * 128:(tt + 1) * 128, :], [], [xb])
            rmsnorm_to_T(c, xt, xb, scr, hT, hT_b, tt * 128, nw, nw_b, ident, ident_b, "n_")

        finals = []
        cnt = {"f": 0, "b": 0, "t": 0}
        for bi, (name, c0, o, n, mode, dt, sg) in enumerate(blocks):
            if bi + PRE < len(blocks):
                issue_load(bi + PRE)
            ws, wb = wslots[bi % NW]
            od = outs[name]
            if mode == "F":
                for cc in range(0, n, 128):
                    m = min(128, n - cc)
                    if dt == F32:
                        ot, ob = of32[cnt["f"] % 2]
                        cnt["f"] += 1
                    else:
                        ot, ob = obf[cnt["b"] % 2]
                        cnt["b"] += 1
                    for tg in range(TOK // 512):
                        bank, bb = c.next_bank()
                        for kc in range(KC):
                            c.mm(bank[0:m, :], ws[:, kc, cc:cc + m], hT[:, kc, tg * 512:(tg + 1) * 512],
                                 kc == 0, kc == KC - 1, reads=[wb, hT_b], writes=[bb])
                        dst = ot[0:m, tg * 512:(tg + 1) * 512]
                        if sg:
                            c.act(dst, bank[0:m, :], AF.Sigmoid, reads=[bb], writes=[ob])
                        else:
                            c.evac_copy(dst, bank[0:m, :], reads=[bb], writes=[ob])
                    finals.append(c.dma(SP, od[o + cc:o + cc + m, :], ot[0:m, :], [ob], []))
            else:
                for tt in range(TOK // 128):
                    bank, bb = c.next_bank()
                    for kc in range(KC):
                        c.mm(bank[:, 0:n], hT[:, kc, tt * 128:(tt + 1) * 128], ws[:, kc, 0:n],
                             kc == 0, kc == KC - 1, reads=[wb, hT_b], writes=[bb])
                    ot, ob = otk[cnt["t"] % 3]
                    cnt["t"] += 1
                    if sg:
                        c.act(ot[:, 0:n], bank[:, 0:n], AF.Sigmoid, reads=[bb], writes=[ob])
                    else:
                        c.evac_copy(ot[:, 0:n], bank[:, 0:n], reads=[bb], writes=[ob])
                    finals.append(c.dma(SP, od[tt * 128:(tt + 1) * 128, o:o + n], ot[:, 0:n], [ob], []))
        c.end()


def nw_layout(v):
    return np.ascontiguousarray(v.reshape(KC, 128).T)


NH = 4
NCH = SEQ // 128
MASKNEG = -30000.0
Q_R, Q_C, Q_INTER, Q_W, Q_EM = 0, 1, 2, 3, 4
NSB = 3
MSKEW = 1


def phase_BC(c, d, do_m=True, do_a=True):
    cw_d, mv_d, mo_d, gb_d, mnw_d, dv_d, dnw_d = d["cw"], d["mv"], d["mo"], d["gb"], d["mnw"], d["dv"], d["dnw"]
    lam_d, lami_d, ident_d, mask_d, hmT_d, hdT_d = d["lam"], d["lami"], d["ident"], d["maskneg"], d["hmT"], d["hdT"]
    gi_d, gf_d = d["gi"], d["gf"]
    if True:
        c.begin()
        if "pre" in d:
            d["pre"](c)
        P = c.P
        identf = c.sb("identf", [128, 128], F32); identf_b = Buf("identf")
        identb = c.sb("identb", [128, 128], BF16); identb_b = Buf("identb")
        maskf = c.sb("maskf", [128, 128], F32); maskf_b = Buf("maskf")
        maskb = c.sb("maskb", [128, 128], BF16); maskb_b = Buf("maskb")
        onesf = c.sb("onesf", [128, 128], F32); onesf_b = Buf("onesf")
        onesb = c.sb("onesb", [128, 128], BF16); onesb_b = Buf("onesb")
        epst = c.sb("epst", [128, 1], F32); eps_b = Buf("epst")
        c.dma(SP, identf[:], ident_d[:, :], [], [identf_b])
        c.dma(POOL, identb[:], ident_d[:, :], [], [identb_b])
        c.dma(SP, maskf[:], mask_d[:, :], [], [maskf_b])
        c.dma(POOL, maskb[:], mask_d[:, :], [], [maskb_b])
        P.op(POOL, lambda e: e.memset(onesf[:], 1.0), writes=[onesf_b])
        P.op(POOL, lambda e: e.memset(onesb[:], 1.0), writes=[onesb_b])
        P.op(POOL, lambda e: e.memset(epst[:], EPS), writes=[eps_b])
        cw = c.sb("cw_s", [128, 2 * NH, 5], F32); cw_b = Buf("cw")
        c.dma(SP, cw[:], cw_d, [], [cw_b])
        gb = c.sb("gb_s", [128, 2], F32); gb_b = Buf("gb")
        c.dma(SP, gb[:], gb_d, [], [gb_b])
        mnw = c.sb("mnw_s", [128, NH * 256], F32); mnw_b = Buf("mnw")
        c.dma(SP, mnw[:], mnw_d, [], [mnw_b])
        dnw = c.sb("dnw_s", [128, NH * 256], F32); dnw_b = Buf("dnw")
        c.dma(SP, dnw[:], dnw_d, [], [dnw_b])
        lamt = c.sb("lamt", [128, 4, 128], F32); lamt_b = Buf("lamt")
        c.dma(SP, lamt[:], lam_d, [], [lamt_b])
        lami = c.sb("lami_s", [128, 2], F32); lami_b = Buf("lami")
        c.dma(SP, lami[:], lami_d, [], [lami_b])

        g_i = c.sb("g_i", [128, 128], F32); g_f = c.sb("g_f", [128, 128], F32)
        gi_b, gf_b = Buf("g_i"), Buf("g_f")
        c.dma(SP, g_i[:], gi_d.rearrange("j (ci l) -> (j ci) l", l=128), [], [gi_b])
        c.dma(SP, g_f[:], gf_d.rearrange("j (ci l) -> (j ci) l", l=128), [], [gf_b])
        sm = c.sb("gsm", [128, 16], F32); sm_b = Buf("gsm")
        NBF, MPREV, DEC, RLAST = 0, 1, 2, 3
        gq = c.sb("gq", [128, 5, 128], F32); gq_b = Buf("gq")
        g_b = c.sb("g_bb", [128, 128], F32); gbb_b = Buf("g_bb")
        g_ml = c.sb("g_ml", [128, 128], F32); gml_b = Buf("g_ml")
        g_m = c.sb("g_m", [128, 128], F32); gm_b = Buf("g_m")
        c.ts(DVE, g_i[:], g_i[:], gb[:, 0:1], None, ALU.add, None, [gi_b, gb_b], [gi_b])
        c.ts(DVE, sm[:, NBF:NBF + 1], gb[:, 1:2], -1.0, None, ALU.mult, None, [gb_b], [sm_b])
        c.act(g_f[:], g_f[:], AF.Exp, [gf_b, sm_b], [gf_b], bias=sm[:, NBF:NBF + 1], scale=-1.0)
        c.act(g_f[:], g_f[:], AF.Ln, [gf_b, onesf_b], [gf_b], bias=onesf[:, 0:1], scale=1.0)
        c.ts(DVE, g_f[:], g_f[:], -1.0, None, ALU.mult, None, [gf_b], [gf_b])
        P.op(DVE, lambda e: e.tensor_tensor_scan(out=g_b[:], data0=g_f[:], data1=g_f[:], initial=0.0,
                                                 op0=ALU.add, op1=ALU.min), reads=[gf_b], writes=[gbb_b])
        P.op(DVE, lambda e: e.tensor_tensor_scan(out=g_ml[:], data0=g_f[:], data1=g_i[:], initial=-1e30,
                                                 op0=ALU.add, op1=ALU.max), reads=[gf_b, gi_b], writes=[gml_b])
        bankr, bankr_b = c.next_bank()
        c.mm(bankr[0:1, 0:128], g_b[:, 127:128], identf[:], True, True, [gbb_b, identf_b], [bankr_b])
        c.mm(bankr[0:1, 128:256], g_ml[:, 127:128], identf[:], True, True, [gml_b, identf_b], [bankr_b])
        erow = c.sb("erow", [1, 512], F32); erow_b = Buf("erow")
        c.copy(DVE, erow[0:1, 0:256], bankr[0:1, 0:256], [bankr_b], [erow_b])
        P.op(POOL, lambda e: e.memset(erow[0:1, 256:512], 0.0), writes=[erow_b])
        for j in range(NH):
            P.op(DVE, lambda e, j=j: e.tensor_tensor_scan(
                out=erow[0:1, 384 + j * 32:384 + (j + 1) * 32], data0=erow[0:1, j * 32:(j + 1) * 32],
                data1=erow[0:1, 128 + j * 32:128 + (j + 1) * 32], initial=0.0, op0=ALU.add, op1=ALU.max),
                reads=[erow_b], writes=[erow_b])
            c.copy(DVE, erow[0:1, 256 + j * 32 + 1:256 + (j + 1) * 32], erow[0:1, 384 + j * 32:384 + (j + 1) * 32 - 1],
                   [erow_b], [erow_b])
        c.mm(bankr[:, 256:257], erow[0:1, 256:384], onesf[0:1, 0:1], True, True, [erow_b, onesf_b], [bankr_b])
        c.copy(DVE, sm[:, MPREV:MPREV + 1], bankr[:, 256:257], [bankr_b], [sm_b])
        c.stt(g_m[:], g_b[:], sm[:, MPREV:MPREV + 1], g_ml[:], ALU.add, ALU.max, [gbb_b, sm_b, gml_b], [gm_b])
        c.tt(DVE, gq[:, Q_R, :], g_b[:], g_m[:], ALU.subtract, [gbb_b, gm_b], [gq_b])
        c.tt(DVE, gq[:, Q_C, :], g_i[:], g_b[:], ALU.subtract, [gi_b, gbb_b], [gq_b])
        c.copy(DVE, sm[:, RLAST:RLAST + 1], gq[:, Q_R, 127:128], [gq_b], [sm_b])
        c.act(gq[:, Q_INTER, :], gq[:, Q_R, :], AF.Exp, [gq_b, sm_b], [gq_b], bias=sm[:, MPREV:MPREV + 1], scale=1.0)
        c.act(gq[:, Q_W, :], gq[:, Q_C, :], AF.Exp, [gq_b, sm_b], [gq_b], bias=sm[:, RLAST:RLAST + 1], scale=1.0)
        c.act(gq[:, Q_EM, :], g_m[:], AF.Exp, [gm_b], [gq_b], scale=-1.0)
        c.act(sm[:, DEC:DEC + 1], sm[:, RLAST:RLAST + 1], AF.Exp, [sm_b], [sm_b], bias=sm[:, MPREV:MPREV + 1], scale=1.0)
        tq = c.sb("tq", [128, 5, 128], F32); tq_b = Buf("tq")
        for n in range(5):
            bk, bkb = c.next_bank()
            c.mm(bk[:, 0:128], gq[:, n, :], identf[:], True, True, [gq_b, identf_b], [bkb])
            c.copy(DVE, tq[:, n, :], bk[:, 0:128], [bkb], [tq_b])
        decm = c.sb("decm", [128, 128], F32); decm_b = Buf("decm")
        c.ts(DVE, decm[:], onesf[:], sm[:, DEC:DEC + 1], None, ALU.mult, None, [onesf_b, sm_b], [decm_b])
        bk, bkb = c.next_bank()
        c.mm(bk[:, 0:128], decm[:], identf[:], True, True, [decm_b, identf_b], [bkb])
        decb = c.sb("decb", [128, 128], F32); decb_b = Buf("decb")
        c.copy(DVE, decb[:], bk[:, 0:128], [bkb], [decb_b])

        lsm = c.sb("lsm", [128, 8], F32); lsm_b = Buf("lsm")
        lpr = c.sb("lpr", [128, 2, 128], F32); lpr_b = Buf("lpr")
        c.tt(DVE, lpr[:, 0, :], lamt[:, 0, :], lamt[:, 1, :], ALU.mult, [lamt_b], [lpr_b])
        c.tt(DVE, lpr[:, 1, :], lamt[:, 2, :], lamt[:, 3, :], ALU.mult, [lamt_b], [lpr_b])
        P.op(DVE, lambda e: e.reduce_sum(out=lsm[:, 0:2], in_=lpr[:], axis=AX.X), reads=[lpr_b], writes=[lsm_b])
        c.act(lsm[:, 2:4], lsm[:, 0:2], AF.Exp, [lsm_b], [lsm_b])
        c.tt(DVE, lsm[:, 4:5], lsm[:, 2:3], lsm[:, 3:4], ALU.subtract, [lsm_b], [lsm_b])
        c.ts(DVE, lsm[:, 5:6], lsm[:, 4:5], lami[:, 0:1], -1.0, ALU.add, ALU.mult, [lsm_b, lami_b], [lsm_b])
        NLAM = 5

        rawq = c.sb("rawq", [128, SEQ + 3], F32); rawq_b = Buf("rawq")
        rawk = c.sb("rawk", [128, SEQ + 3], F32); rawk_b = Buf("rawk")
        acc = c.sb("acc", [128, SEQ], F32); acc_b = Buf("acc")
        qT = c.sb("qT", [128, SEQ], BF16); qT_b = Buf("qT")
        kT = c.sb("kT", [128, SEQ], BF16); kT_b = Buf("kT")
        va = c.sb("va", [128, NCH, 257], BF16); va_b = Buf("va")
        P.op(POOL, lambda e: e.memset(rawq[:, 0:3], 0.0), writes=[rawq_b])
        P.op(POOL, lambda e: e.memset(rawk[:, 0:3], 0.0), writes=[rawk_b])
        P.op(POOL, lambda e: e.memset(va[:, :, 256:257], 1.0), writes=[va_b])
        CT = c.sb("CT", [128, 257], F32); CT_b = Buf("CT")
        CTb = c.sb("CTb", [128, 257], BF16); CTb_b = Buf("CTb")
        diagR = [(c.sb("diagR%d" % i, [128, 128], F32), Buf("diagR%d" % i)) for i in range(2)]
        Dm = [(c.sb("Dm%d" % i, [128, 128], F32), Buf("Dm%d" % i)) for i in range(2)]
        sdT = [(c.sb("sdT%d" % i, [128, 128], BF16), Buf("sdT%d" % i)) for i in range(2)]
        kw = [(c.sb("kw%d" % i, [128, 128], BF16), Buf("kw%d" % i)) for i in range(2)]
        numS = [(c.sb("numS%d" % i, [128, 257], F32), Buf("numS%d" % i)) for i in range(2)]
        tot = [(c.sb("tot%d" % i, [128, 257], F32), Buf("tot%d" % i)) for i in range(2)]
        junk = c.sb("junk", [128, 256], BF16); junk_b = Buf("junk")
        hs = [(c.sb("hs%d" % i, [128, 8], F32), Buf("hs%d" % i)) for i in range(2)]
        g2 = [(c.sb("g2_%d" % i, [128, 256], F32), Buf("g2_%d" % i)) for i in range(2)]
        hmt = [(c.sb("hm%d" % i, [128, 256], BF16), Buf("hm%d" % i)) for i in range(2)]
        mos = [(c.sb("mos%d" % i, [128, 4, 256], BF16), Buf("mos%d" % i)) for i in range(2)]
        hout = [(c.sb("hout%d" % i, [128, 2, 512], BF16), Buf("hout%d" % i)) for i in range(2)]
        kscale = 128.0 ** -0.5

        def conv_silu(raw, raw_b, idx, dst, dst_b, post_scale):
            c.ts(DVE, acc[:], raw[:, 0:SEQ], cw[:, idx, 0:1], cw[:, idx, 4:5], ALU.mult, ALU.add,
                 [raw_b, cw_b], [acc_b])
            for t in range(1, 4):
                c.stt(acc[:], raw[:, t:t + SEQ], cw[:, idx, t:t + 1], acc[:], ALU.mult, ALU.add,
                      [raw_b, cw_b, acc_b], [acc_b])
            if post_scale is None:
                c.act(dst[:], acc[:], AF.Silu, [acc_b], [dst_b])
            else:
                c.act(acc[:], acc[:], AF.Silu, [acc_b], [acc_b])
                c.ts(POOL, dst[:], acc[:], post_scale, None, ALU.mult, None, [acc_b], [dst_b])

        grp = 0
        for j in range(NH if do_m else 0):
            c.dma(SP, rawq[:, 3:SEQ + 3], d["qraw"](j), [], [rawq_b])
            c.dma(SP, rawk[:, 3:SEQ + 3], d["kraw"](j), [], [rawk_b])
            c.dma(SP, va[:, :, 0:256], mv_d[:, j * 256:(j + 1) * 256].rearrange("(ci p) v -> p ci v", p=128),
                  [], [va_b])
            conv_silu(rawq, rawq_b, j, qT, qT_b, None)
            conv_silu(rawk, rawk_b, NH + j, kT, kT_b, kscale)
            P.op(POOL, lambda e: e.memset(CT[:], 0.0), writes=[CT_b])
            P.op(POOL, lambda e: e.memset(CTb[:], 0.0), writes=[CTb_b])
            def stage1(ci):
                p = j * NCH + ci
                par = ci % 2
                cs = slice(ci * 128, (ci + 1) * 128)
                bA, bA_b = c.banks[par * 4 + 0], c.bank_bufs[par * 4 + 0]
                dR, dR_b = diagR[par]
                c.ts(POOL, dR[:], identf[:], tq[:, Q_R, p:p + 1], None, ALU.mult, None, [identf_b, tq_b], [dR_b])
                c.mm(bA[:, 0:128], kT[:, cs], qT[:, cs], True, True, [kT_b, qT_b], [bA_b])
                c.mm(bA[:, 128:256], onesf[:], dR[:], True, False, [onesf_b, dR_b], [bA_b])
                c.mm(bA[:, 128:256], identf[:], maskf[:], False, True, [identf_b, maskf_b], [bA_b])
                c.mm(bA[:, 256:384], kT[:, cs], identb[:], True, True, [kT_b, identb_b], [bA_b])
                dm, dm_b = Dm[par]
                c.act(dm[:], bA[:, 128:256], AF.Exp, [bA_b, tq_b], [dm_b], bias=tq[:, Q_C, p:p + 1], scale=1.0)
                sd, sd_b = sdT[par]
                c.tt(DVE, sd[:], bA[:, 0:128], dm[:], ALU.mult, [bA_b, dm_b], [sd_b])
                kwt, kw_b = kw[par]
                c.ts(DVE, kwt[:], bA[:, 256:384], tq[:, Q_W, p:p + 1], None, ALU.mult, None, [bA_b, tq_b], [kw_b])

            def stage2(ci):
                p = j * NCH + ci
                par = ci % 2
                cs = slice(ci * 128, (ci + 1) * 128)
                if ci % 4 == 0:
                    mo_t, mo_b = mos[(ci // 4) % 2]
                    c.dma(SP, mo_t[:], mo_d[ci * 128:(ci + 4) * 128, j * 256:(j + 1) * 256]
                          .rearrange("(cc p) v -> p cc v", p=128), [], [mo_b])
                mo_t, mo_b = mos[(ci // 4) % 2]
                bB, bB_b = c.banks[par * 4 + 1], c.bank_bufs[par * 4 + 1]
                bC, bC_b = c.banks[par * 4 + 2], c.bank_bufs[par * 4 + 2]
                bD, bD_b = c.banks[par * 4 + 3], c.bank_bufs[par * 4 + 3]
                sd, sd_b = sdT[par]
                kwt, kw_b = kw[par]
                c.mm(bB[:, 0:257], sd[:], va[:, ci, :], True, True, [sd_b, va_b], [bB_b])
                c.mm(bC[:, 0:257], qT[:, cs], CTb[:], True, True, [qT_b, CTb_b], [bC_b])
                c.mm(bD[:, 0:257], kwt[:], va[:, ci, :], True, True, [kw_b, va_b], [bD_b])
                ns, ns_b = numS[par]
                c.copy(ACT, ns[:], bB[:, 0:257], [bB_b], [ns_b])
                c.stt(CT[:], CT[:], decb[:, p:p + 1], bD[:, 0:257], ALU.mult, ALU.add, [CT_b, decb_b, bD_b], [CT_b])
                c.copy(POOL, CTb[:], CT[:], [CT_b], [CTb_b])
                tt_, tt_b = tot[par]
                c.stt(tt_[:], bC[:, 0:257], tq[:, Q_INTER, p:p + 1], ns[:], ALU.mult, ALU.add,
                      [bC_b, tq_b, ns_b], [tt_b])
                h_, h_b = hs[par]
                c.act(h_[:, 7:8], tt_[:, 256:257], AF.Abs, [tt_b], [h_b])
                c.ts(DVE, h_[:, 0:1], h_[:, 7:8], tq[:, Q_EM, p:p + 1], None, ALU.max, None, [h_b, tq_b], [h_b])
                c.act(h_[:, 1:2], h_[:, 0:1], AF.Square, [h_b], [h_b], scale=EPS ** 0.5)
                c.act(junk[:], tt_[:, 0:256], AF.Square, [tt_b], [junk_b, h_b], accum_out=h_[:, 2:3])
                c.act(h_[:, 4:5], h_[:, 2:3], AF.Sqrt, [h_b], [h_b], bias=h_[:, 1:2], scale=1.0 / 256)
                P.op(DVE, lambda e, h_=h_: e.reciprocal(out=h_[:, 6:7], in_=h_[:, 4:5]), reads=[h_b], writes=[h_b])
                g2t, g2_b = g2[par]
                c.tt(POOL, g2t[:], mnw[:, j * 256:(j + 1) * 256], mo_t[:, ci % 4, :], ALU.mult, [mnw_b, mo_b], [g2_b])
                hm_, hm_b = hmt[par]
                c.stt(hm_[:], tt_[:, 0:256], h_[:, 6:7], g2t[:], ALU.mult, ALU.mult, [tt_b, h_b, g2_b], [hm_b])

            def stage3(ci):
                par = ci % 2
                bB, bB_b = c.banks[par * 4 + 1], c.bank_bufs[par * 4 + 1]
                hm_, hm_b = hmt[par]
                ho_t, ho_b = hout[(ci // 4) % 2]
                for vc in range(2):
                    c.mm(bB[:, vc * 128:(vc + 1) * 128], hm_[:, vc * 128:(vc + 1) * 128], identb[:], True, True,
                         [hm_b, identb_b], [bB_b])
                c.copy(ACT, ho_t[:, :, (ci % 4) * 128:(ci % 4 + 1) * 128],
                       bB[:, 0:256].rearrange("p (a b) -> p a b", a=2), [bB_b], [ho_b])
                if ci % 4 == 3:
                    c.dma(SP, hmT_d[j * 256:(j + 1) * 256, (ci - 3) * 128:(ci + 1) * 128]
                          .rearrange("(a p) t -> p a t", p=128), ho_t[:], [ho_b], [])

            for it in range(NCH + MSKEW * 2):
                if it < NCH:
                    stage1(it)
                if 0 <= it - MSKEW < NCH:
                    stage2(it - MSKEW)
                if 0 <= it - 2 * MSKEW < NCH:
                    stage3(it - 2 * MSKEW)


        qc = [(c.sb("dq%d" % i, [128, SEQ], BF16), Buf("dq%d" % i)) for i in range(2)]
        kc_ = [(c.sb("dk%d" % i, [128, SEQ], BF16), Buf("dk%d" % i)) for i in range(2)]
        sq = c.sb("sq", [128, SEQ], BF16); sq_b = Buf("sq")
        mx = c.sb("mx", [1, 64], F32); mx_b = Buf("mx")
        nG = c.sb("nG", [128, 1], F32); nG_b = Buf("nG")
        Et = [(c.sb("E%d" % i, [128, 512], BF16), Buf("E%d" % i)) for i in range(NSB + 2)]
        ds = [(c.sb("ds%d" % i, [128, 8], F32), Buf("ds%d" % i)) for i in range(2)]
        dtm = [(c.sb("dt%d" % i, [128, 256], F32), Buf("dt%d" % i)) for i in range(2)]
        dhd = [(c.sb("dhd%d" % i, [128, 256], F32), Buf("dhd%d" % i)) for i in range(2)]
        dhn = [(c.sb("dhn%d" % i, [128, 256], BF16), Buf("dhn%d" % i)) for i in range(2)]
        ascale = 128.0 ** -0.5
        Obanks = [(c.banks[i], c.bank_bufs[i]) for i in range(4)]
        Sbanks = [(c.banks[4 + i], c.bank_bufs[4 + i]) for i in range(NSB)]
        Tbanks = [(c.banks[4 + NSB + i], c.bank_bufs[4 + NSB + i]) for i in range(4 - NSB)]
        e_i = 0
        s_i = 0
        t_i = 0
        for j in range(NH if do_a else 0):
            for cc in range(2):
                c.dma(SP, qc[cc][0][:], d["dq"](j, cc), [], [qc[cc][1]])
                c.dma(SP, kc_[cc][0][:], d["dk"](j, cc), [], [kc_[cc][1]])
            c.dma(SP, va[:, :, 0:256], dv_d[:, j * 256:(j + 1) * 256].rearrange("(ci p) v -> p ci v", p=128),
                  [], [va_b])
            tb, tb_b = Tbanks[0]
            for ti, (tns, tns_b) in enumerate([qc[0], qc[1], kc_[0], kc_[1]]):
                c.act(sq[:], tns[:], AF.Square, [tns_b], [sq_b])
                for s8 in range(8):
                    c.mm(tb[0:1, 0:512], onesb[:, 0:1], sq[:, s8 * 512:(s8 + 1) * 512], True, True,
                         [onesb_b, sq_b], [tb_b])
                    P.op(DVE, lambda e, ti=ti, s8=s8: e.reduce_max(out=mx[0:1, ti * 8 + s8:ti * 8 + s8 + 1],
                                                                   in_=tb[0:1, 0:512], axis=AX.X),
                         reads=[tb_b], writes=[mx_b])
            P.op(DVE, lambda e: e.reduce_max(out=mx[0:1, 32:34], in_=mx[0:1, 0:32].rearrange("p (a b) -> p a b", a=2),
                                             axis=AX.X), reads=[mx_b], writes=[mx_b])
            c.tt(DVE, mx[0:1, 34:35], mx[0:1, 32:33], mx[0:1, 33:34], ALU.mult, [mx_b], [mx_b])
            c.act(mx[0:1, 35:36], mx[0:1, 34:35], AF.Sqrt, [mx_b], [mx_b], scale=ascale * ascale)
            c.ts(DVE, mx[0:1, 36:37], mx[0:1, 35:36], -1.0, None, ALU.mult, None, [mx_b], [mx_b])
            c.mm(tb[:, 0:1], onesf[0:1, :], mx[0:1, 36:37], True, True, [onesf_b, mx_b], [tb_b])
            c.copy(DVE, nG[:], tb[:, 0:1], [tb_b], [nG_b])
            if "dbg" in d:
                c.dma(SP, d["dbg"][j, 0:1, 0:64], mx[0:1, :], [mx_b], [])

            def qk_exp(g, kb):
                nonlocal s_i, e_i
                sb_, sb_b = Sbanks[s_i % NSB]; s_i += 1
                et, et_b = Et[e_i % (NSB + 2)]; e_i += 1
                ks = slice(kb * 128, (kb + 1) * 128)
                if kb <= 2 * g:
                    for cc in range(2):
                        diag = (kb == 2 * g)
                        c.mm(sb_[:, cc * 256:(cc + 1) * 256], kc_[cc][0][:, ks], qc[cc][0][:, g * 256:(g + 1) * 256],
                             True, not diag, [kc_[cc][1], qc[cc][1]], [sb_b])
                        if diag:
                            c.mm(sb_[:, cc * 256:cc * 256 + 128], identb[:], maskb[:], False, True,
                                 [identb_b, maskb_b], [sb_b])
                    c.act(et[:], sb_[:], AF.Exp, [sb_b, nG_b], [et_b], bias=nG[:, 0:1], scale=ascale)
                    ilist = (0, 1)
                else:
                    for cc in range(2):
                        c.mm(sb_[:, cc * 256 + 128:(cc + 1) * 256], kc_[cc][0][:, ks],
                             qc[cc][0][:, g * 256 + 128:(g + 1) * 256], True, False, [kc_[cc][1], qc[cc][1]], [sb_b])
                        c.mm(sb_[:, cc * 256 + 128:(cc + 1) * 256], identb[:], maskb[:], False, True,
                             [identb_b, maskb_b], [sb_b])
                    c.act(et[:].rearrange("p (a b) -> p a b", a=2)[:, :, 128:256],
                          sb_[:].rearrange("p (a b) -> p a b", a=2)[:, :, 128:256], AF.Exp,
                          [sb_b, nG_b], [et_b], bias=nG[:, 0:1], scale=ascale)
                    ilist = (1,)
                return (g, kb, et, et_b, ilist)

            def pv(st):
                g, kb, et, et_b, ilist = st
                for cc in range(2):
                    for i in ilist:
                        ob, ob_b = Obanks[cc * 2 + i]
                        last = (kb == 2 * g + i)
                        c.mm(ob[:, 0:257], et[:, cc * 256 + i * 128:cc * 256 + (i + 1) * 128], va[:, kb, :],
                             kb == 0, last, [et_b, va_b], [ob_b])
                if kb == 2 * g + 1:
                    epilogue(g)

            def epilogue(g):
                nonlocal t_i
                for i in range(2):
                    qb = 2 * g + i
                    o1, o1_b = Obanks[i]
                    o2, o2_b = Obanks[2 + i]
                    d_, d_b = ds[i]
                    P.op(DVE, lambda e, d_=d_, o1=o1: e.reciprocal(out=d_[:, 0:1], in_=o1[:, 256:257]),
                         reads=[o1_b], writes=[d_b])
                    P.op(DVE, lambda e, d_=d_, o2=o2: e.reciprocal(out=d_[:, 1:2], in_=o2[:, 256:257]),
                         reads=[o2_b], writes=[d_b])
                    c.ts(DVE, d_[:, 2:3], d_[:, 1:2], lsm[:, NLAM:NLAM + 1], None, ALU.mult, None, [d_b, lsm_b], [d_b])
                    t1, t1_b = dtm[i]
                    c.act(t1[:], o1[:, 0:256], AF.Copy, [o1_b, d_b], [t1_b], scale=d_[:, 0:1])
                    hd_, hd_b = dhd[i]
                    c.stt(hd_[:], o2[:, 0:256], d_[:, 2:3], t1[:], ALU.mult, ALU.add, [o2_b, d_b, t1_b], [hd_b])
                    c.act(junk[:], hd_[:], AF.Square, [hd_b], [junk_b, d_b], accum_out=d_[:, 3:4])
                    c.act(d_[:, 4:5], d_[:, 3:4], AF.Sqrt, [d_b, eps_b], [d_b], bias=epst[:, 0:1], scale=1.0 / 256)
                    P.op(DVE, lambda e, d_=d_: e.reciprocal(out=d_[:, 5:6], in_=d_[:, 4:5]), reads=[d_b], writes=[d_b])
                    c.ts(DVE, d_[:, 6:7], d_[:, 5:6], lami[:, 1:2], None, ALU.mult, None, [d_b, lami_b], [d_b])
                    hn_, hn_b = dhn[i]
                    c.stt(hn_[:], hd_[:], d_[:, 6:7], dnw[:, j * 256:(j + 1) * 256], ALU.mult, ALU.mult,
                          [hd_b, d_b, dnw_b], [hn_b])
                    tb, tb_b = Tbanks[t_i % (4 - NSB)]; t_i += 1
                    for vc in range(2):
                        c.mm(tb[:, vc * 128:(vc + 1) * 128], hn_[:, vc * 128:(vc + 1) * 128], identb[:], True, True,
                             [hn_b, identb_b], [tb_b])
                    ho_t, ho_b = hout[(qb // 4) % 2]
                    c.copy(ACT if qb % 2 else DVE, ho_t[:, :, (qb % 4) * 128:(qb % 4 + 1) * 128],
                           tb[:, 0:256].rearrange("p (a b) -> p a b", a=2), [tb_b], [ho_b])
                    if qb % 4 == 3:
                        c.dma(SP, hdT_d[j * 256:(j + 1) * 256, (qb - 3) * 128:(qb + 1) * 128]
                              .rearrange("(a p) t -> p a t", p=128), ho_t[:], [ho_b], [])

            pairs = [(g, kb) for g in range(NCH // 2) for kb in range(2 * g + 2)]
            pend = []
            for (g, kb) in pairs:
                pend.append(qk_exp(g, kb))
                if len(pend) > NSB - 1:
                    pv(pend.pop(0))
            while pend:
                pv(pend.pop(0))
        c.end()


IDENT = np.eye(128, dtype=np.float32)
MASKNEG_NP = np.where(np.arange(128)[:, None] <= np.arange(128)[None, :], 0.0, MASKNEG).astype(np.float32)


def lam_init_of(l):
    return 0.8 - 0.6 * math.exp(-0.3 * l)


TG = 512
NFC = D_FF // 128


def phase_D(c, d, final, last):
    x_d, hm_d, hd_d, gmd_d, xo_d = d["x"], d["hmT"], d["hdT"], d["gmd"], d["xo"]
    wbm_d, wbd_d, wout_d, wg_d, wu_d, wd_d = d["wbm"], d["wbd"], d["wout"], d["wg"], d["wu"], d["wd"]
    nw2_d, ident_d = d["nw2"], d["ident"]
    if final:
        fnw_d = d["fnw"]
    if True:
        c.begin()
        P = c.P
        ident = c.sb("ident_s", [128, 128], BF16); ident_b = Buf("ident")
        c.dma(POOL, ident[:], ident_d[:, :], [], [ident_b])
        nw2 = c.sb("nw2_s", [128, KC], F32); nw2_b = Buf("nw2")
        c.dma(SP, nw2[:], nw2_d, [], [nw2_b])
        if final:
            fnw = c.sb("fnw_s", [128, D_MODEL], F32); fnw_b = Buf("fnw")
            c.dma(SP, fnw[:], fnw_d, [], [fnw_b])
        hmg = c.sb("hmg", [128, KC, TG], BF16); hmg_b = Buf("hmg")
        hdg = c.sb("hdg", [128, KC, TG], BF16); hdg_b = Buf("hdg")
        yT = c.sb("yT", [128, KC, TG], BF16); yT_b = Buf("yT")
        xg = c.sb("xg", [128, TG // 128, D_MODEL], F32)
        xg_b = [Buf("xg%d" % i) for i in range(TG // 128)]
        aT = c.sb("aT", [128, NFC, TG], BF16); aT_b = Buf("aT")
        NW = 3
        wslots = [(c.sb("w%d" % i, [128, KC, 512], BF16), Buf("w%d" % i)) for i in range(NW)]
        t1 = [(c.sb("t1_%d" % i, [128, TG], F32), Buf("t1_%d" % i)) for i in range(2)]
        t2 = [(c.sb("t2_%d" % i, [128, TG], F32), Buf("t2_%d" % i)) for i in range(2)]
        sgm = [(c.sb("sgm%d" % i, [128, TG], BF16), Buf("sgm%d" % i)) for i in range(2)]
        sgd = [(c.sb("sgd%d" % i, [128, TG], BF16), Buf("sgd%d" % i)) for i in range(2)]
        scr = norm_scratch(c, "n_")

        wlist = []
        for g in range(TOK // TG):
            for blk in range(4):
                wlist.append((wbm_d, 0, KC, blk * 512, 512))
                wlist.append((wbd_d, 0, KC, blk * 512, 512))
            for cg in range(4):
                wlist.append((wout_d, 0, KC, cg * 512, 512))
            for blk in range(D_FF // 512):
                wlist.append((wg_d, 0, KC, blk * 512, 512))
                wlist.append((wu_d, 0, KC, blk * 512, 512))
            for cg in range(4):
                for fb in range(4):
                    wlist.append((wd_d, fb * 11 * 128, 11, cg * 512, 512))
        wstate = {"issued": 0, "used": 0, "done": 0}

        def issue_one():
            i = wstate["issued"]
            wdr, r0, nkc, c0, ncol = wlist[i]
            ws, wb = wslots[i % NW]
            load_wblock(c, ws, wb, wdr, r0, nkc, c0, ncol)
            wstate["issued"] += 1

        def next_w():
            i = wstate["used"]
            while wstate["issued"] <= i:
                assert wstate["issued"] < wstate["done"] + NW
                issue_one()
            wstate["used"] += 1
            return wslots[i % NW]

        def release_w():
            wstate["done"] = wstate["used"]
            while wstate["issued"] < len(wlist) and wstate["issued"] < wstate["done"] + NW:
                issue_one()

        k2 = 0
        for g in range(TOK // TG):
            ts0 = g * TG
            c.dma(SP, hmg[:], hm_d[:, ts0:ts0 + TG].rearrange("(kc p) t -> p kc t", p=128), [], [hmg_b])
            c.dma(SP, hdg[:], hd_d[:, ts0:ts0 + TG].rearrange("(kc p) t -> p kc t", p=128), [], [hdg_b])
            for tt in range(TG // 128):
                c.dma(SP, xg[:, tt, :], x_d[ts0 + tt * 128:ts0 + (tt + 1) * 128, :], [], [xg_b[tt]])
            for blk in range(4):
                wm, wm_b = next_w()
                wd_, wd_b = next_w()
                for cc in range(4):
                    col = blk * 4 + cc
                    sm_, sm_b = sgm[k2 % 2]
                    sd_, sd_b = sgd[k2 % 2]
                    c.dma(SP, sm_[:], gmd_d[col * 128:(col + 1) * 128, ts0:ts0 + TG], [], [sm_b])
                    c.dma(SP, sd_[:], gmd_d[D_MODEL + col * 128:D_MODEL + (col + 1) * 128, ts0:ts0 + TG], [], [sd_b])
                    bA, bA_b = c.next_bank()
                    for kc in range(KC):
                        c.mm(bA[:, :], wm[:, kc, cc * 128:(cc + 1) * 128], hmg[:, kc, :], kc == 0, kc == KC - 1,
                             [wm_b, hmg_b], [bA_b])
                    bB, bB_b = c.next_bank()
                    for kc in range(KC):
                        c.mm(bB[:, :], wd_[:, kc, cc * 128:(cc + 1) * 128], hdg[:, kc, :], kc == 0, kc == KC - 1,
                             [wd_b, hdg_b], [bB_b])
                    a1, a1_b = t1[k2 % 2]
                    a2, a2_b = t2[k2 % 2]
                    k2 += 1
                    c.tt(DVE, a1[:], bA[:, :], sm_[:], ALU.mult, [bA_b, sm_b], [a1_b])
                    c.tt(DVE, a2[:], bB[:, :], sd_[:], ALU.mult, [bB_b, sd_b], [a2_b])
                    c.tt(POOL, yT[:, col, :], a1[:], a2[:], ALU.add, [a1_b, a2_b], [yT_b])
                release_w()
            for cg in range(4):
                wo, wo_b = next_w()
                for tt in range(TG // 128):
                    bk, bk_b = c.next_bank()
                    for kc in range(KC):
                        c.mm(bk[:, :], yT[:, kc, tt * 128:(tt + 1) * 128], wo[:, kc, :], kc == 0, kc == KC - 1,
                             [yT_b, wo_b], [bk_b])
                    c.tt(DVE, xg[:, tt, cg * 512:(cg + 1) * 512], bk[:, :], xg[:, tt, cg * 512:(cg + 1) * 512], ALU.add,
                         [bk_b, xg_b[tt]], [xg_b[tt]])
                release_w()
            for tt in range(TG // 128):
                rmsnorm_to_T(c, xg[:, tt, :], xg_b[tt], scr, hmg, hmg_b, tt * 128, nw2, nw2_b, ident, ident_b, "n_")
            for blk in range(D_FF // 512):
                wg_, wg_b = next_w()
                wu_, wu_b = next_w()
                for cc in range(4):
                    fc = blk * 4 + cc
                    bG, bG_b = c.next_bank()
                    for kc in range(KC):
                        c.mm(bG[:, :], wg_[:, kc, cc * 128:(cc + 1) * 128], hmg[:, kc, :], kc == 0, kc == KC - 1,
                             [wg_b, hmg_b], [bG_b])
                    bU, bU_b = c.next_bank()
                    for kc in range(KC):
                        c.mm(bU[:, :], wu_[:, kc, cc * 128:(cc + 1) * 128], hmg[:, kc, :], kc == 0, kc == KC - 1,
                             [wu_b, hmg_b], [bU_b])
                    a1, a1_b = t1[k2 % 2]
                    k2 += 1
                    c.act(a1[:], bG[:, :], AF.Silu, [bG_b], [a1_b])
                    c.tt(DVE, aT[:, fc, :], bU[:, :], a1[:], ALU.mult, [bU_b, a1_b], [aT_b])
                release_w()
            for cg in range(4):
                bks = [c.next_bank() for _ in range(TG // 128)]
                for fb in range(4):
                    wdn, wdn_b = next_w()
                    for tt in range(TG // 128):
                        bk, bk_b = bks[tt]
                        for i in range(11):
                            fc = fb * 11 + i
                            c.mm(bk[:, :], aT[:, fc, tt * 128:(tt + 1) * 128], wdn[:, i, :], fc == 0, fc == NFC - 1,
                                 [aT_b, wdn_b], [bk_b])
                    release_w()
                for tt in range(TG // 128):
                    bk, bk_b = bks[tt]
                    c.tt(DVE, xg[:, tt, cg * 512:(cg + 1) * 512], bk[:, :], xg[:, tt, cg * 512:(cg + 1) * 512], ALU.add,
                         [bk_b, xg_b[tt]], [xg_b[tt]])
            for tt in range(TG // 128):
                if final:
                    junk, junk_b = scr["junk"]
                    ss, ss_b = scr["ss"]
                    c.act(junk[:], xg[:, tt, :], AF.Square, [xg_b[tt]], [junk_b, ss_b], accum_out=ss[:, 0:1])
                    c.act(ss[:, 1:2], ss[:, 0:1], AF.Sqrt, [ss_b, scr["eps_b"]], [ss_b], scale=1.0 / D_MODEL,
                          bias=scr["eps"][:, 0:1])
                    P.op(DVE, lambda e, ss=ss: e.reciprocal(out=ss[:, 2:3], in_=ss[:, 1:2]), reads=[ss_b], writes=[ss_b])
                    c.stt(xg[:, tt, :], xg[:, tt, :], ss[:, 2:3], fnw[:], ALU.mult, ALU.mult,
                          [xg_b[tt], ss_b, fnw_b], [xg_b[tt]])
                c.dma(SP, xo_d[ts0 + tt * 128:ts0 + (tt + 1) * 128, :], xg[:, tt, :], [xg_b[tt]], [], sem_key=xg_b[tt])
        c.end(last)


NUSED = 4


def build_fused(depth=DEPTH):
    nc = bass.Bass("TRN2", target_bir_lowering=False)
    di = lambda n, s, dt: nc.dram_tensor(n, s, dt, kind="ExternalInput").ap()
    x_in = di("x", [SEQ, D_MODEL], F32)
    w_in = di("w_in", [DEPTH, D_MODEL, N_IN], F32)
    w_bm = di("w_branch_m", [DEPTH, D_MODEL, D_MODEL], F32)
    w_bd = di("w_branch_d", [DEPTH, D_MODEL, D_MODEL], F32)
    w_out = di("w_out", [DEPTH, D_MODEL, D_MODEL], F32)
    w_g = di("w_ffn_gate", [DEPTH, D_MODEL, D_FF], F32)
    w_u = di("w_ffn_up", [DEPTH, D_MODEL, D_FF], F32)
    w_d = di("w_ffn_down", [DEPTH, D_FF, D_MODEL], F32)
    anw = di("anw", [DEPTH, 128, KC], F32)
    fnw2 = di("fnw2", [DEPTH, 128, KC], F32)
    fnw = di("fnw", [128, D_MODEL], F32)
    cw = di("cw", [DEPTH, 2, 128, 2 * NH, 5], F32)
    gb = di("gb", [DEPTH, 2, 128, 2], F32)
    mnw = di("mnw", [DEPTH, 128, D_MODEL], F32)
    dnw = di("dnw", [DEPTH, 128, D_MODEL], F32)
    lam = di("lam", [DEPTH, 128, 4, 128], F32)
    lami = di("lami", [DEPTH, 128, 2], F32)
    ident = di("ident", [128, 128], F32)
    maskneg = di("maskneg", [128, 128], F32)
    y_out = nc.dram_tensor("y", [SEQ, D_MODEL], F32, kind="ExternalOutput").ap()
    sc = lambda n, s, dt: nc.dram_tensor(n, s, dt).ap()
    qk_s = sc("qk_s", [2048, SEQ], F32)
    mv_s = sc("mv_s", [SEQ, 2048], BF16)
    mo_s = sc("mo_s", [SEQ, 2048], BF16)
    gt_s = sc("gt_s", [16, SEQ], F32)
    dqk_s = sc("dqk_s", [4096, SEQ], BF16)
    dv_s = sc("dv_s", [SEQ, 2048], BF16)
    gmd_s = sc("gmd_s", [4096, SEQ], BF16)
    hm_s = sc("hm_s", [2048, SEQ], BF16)
    hd_s = sc("hd_s", [2048, SEQ], BF16)
    x_s = sc("x_s", [SEQ, D_MODEL], F32)
    scr = {"qk": qk_s, "mv": mv_s, "mo": mo_s, "gt": gt_s, "dqk": dqk_s, "dv": dv_s, "gmd": gmd_s}
    wb = {"wbm": sc("wbm_b", [D_MODEL, D_MODEL], BF16), "wbd": sc("wbd_b", [D_MODEL, D_MODEL], BF16),
          "wout": sc("wout_b", [D_MODEL, D_MODEL], BF16), "wg": sc("wg_b", [D_MODEL, D_FF], BF16),
          "wu": sc("wu_b", [D_MODEL, D_FF], BF16), "wd": sc("wd_b", [D_FF, D_MODEL], BF16)}

    def make_cast(l):
        def pre(c):
            srcs = {"wbm": w_bm[l], "wbd": w_bd[l], "wout": w_out[l], "wg": w_g[l], "wu": w_u[l], "wd": w_d[l]}
            for k in ("wbm", "wbd", "wout", "wg", "wu", "wd"):
                src, dst = srcs[k], wb[k]
                if k in ("wg", "wu"):
                    src = src.rearrange("r (a b) -> r a b", b=1408)
                    dst = dst.rearrange("r (a b) -> r a b", b=1408)
                c.dma(POOL, dst, src, [], [Buf("cast_" + k)])
        return pre

    with contextlib.ExitStack() as es:
        c = Ctx(nc, es)
        c.alloc_banks(8)
        for l in range(depth):
            x_src = x_in if l == 0 else x_s
            final = (l == depth - 1)
            for th in range(2):
                ts = slice(th * TOK, (th + 1) * TOK)
                outs = {}
                for name, c0, n, mode, dt, sg in A_SECTIONS:
                    outs[name] = scr[name][:, ts] if mode == "F" else scr[name][ts, :]
                phase_A(c, x_src[ts, :], anw[l], w_in[l], ident, outs)
            for hh in range(2):
                hs = slice(hh * NH * 256, (hh + 1) * NH * 256)
                d = {
                    "cw": cw[l, hh], "gb": gb[l, hh], "mnw": mnw[l][:, hs], "dnw": dnw[l][:, hs],
                    "lam": lam[l], "lami": lami[l], "ident": ident, "maskneg": maskneg,
                    "mv": mv_s[:, hs], "mo": mo_s[:, hs], "dv": dv_s[:, hs],
                    "gi": gt_s[hh * NH:(hh + 1) * NH, :], "gf": gt_s[8 + hh * NH:8 + (hh + 1) * NH, :],
                    "hmT": hm_s[hs, :], "hdT": hd_s[hs, :],
                    "qraw": (lambda j, hh=hh: qk_s[(hh * NH + j) * 128:(hh * NH + j + 1) * 128, :]),
                    "kraw": (lambda j, hh=hh: qk_s[1024 + (hh * NH + j) * 128:1024 + (hh * NH + j + 1) * 128, :]),
                    "dq": (lambda j, cc, hh=hh: dqk_s[(hh * NH + j) * 256 + cc * 128:(hh * NH + j) * 256 + (cc + 1) * 128, :]),
                    "dk": (lambda j, cc, hh=hh: dqk_s[2048 + (hh * NH + j) * 256 + cc * 128:
                                                      2048 + (hh * NH + j) * 256 + (cc + 1) * 128, :]),
                }
                if hh == 0:
                    d["pre"] = make_cast(l)
                phase_BC(c, d)
            for th in range(2):
                ts = slice(th * TOK, (th + 1) * TOK)
                d = {"x": x_src[ts, :], "hmT": hm_s[:, ts], "hdT": hd_s[:, ts], "gmd": gmd_s[:, ts],
                     "xo": (y_out if final else x_s)[ts, :],
                     "wbm": wb["wbm"], "wbd": wb["wbd"], "wout": wb["wout"], "wg": wb["wg"], "wu": wb["wu"],
                     "wd": wb["wd"],
                     "nw2": fnw2[l], "ident": ident, "fnw": fnw}
                phase_D(c, d, final, last=(l == depth - 1 and th == 1))
        build_fused.stats = (c.P.n_total, dict(c.P.cnt))
    return nc


def host_params(prm):
    L = DEPTH
    anw = np.stack([nw_layout(prm["attn_norm_w"][l]) for l in range(L)])
    fnw2 = np.stack([nw_layout(prm["ffn_norm_w"][l]) for l in range(L)])
    fnw = np.ascontiguousarray(np.broadcast_to(prm["final_norm_w"][None], (128, D_MODEL))).astype(np.float32)
    cw = np.zeros((L, 2, 128, 2 * NH, 5), np.float32)
    gb = np.zeros((L, 2, 128, 2), np.float32)
    for l in range(L):
        cwl, cbl = prm["conv_w"][l], prm["conv_b"][l]
        for hh in range(2):
            for i in range(NH):
                h = hh * NH + i
                cw[l, hh, :, i, 0:4] = cwl[:, h * 128:(h + 1) * 128].T
                cw[l, hh, :, i, 4] = cbl[h * 128:(h + 1) * 128]
                cw[l, hh, :, NH + i, 0:4] = cwl[:, 1024 + h * 128:1024 + (h + 1) * 128].T
                cw[l, hh, :, NH + i, 4] = cbl[1024 + h * 128:1024 + (h + 1) * 128]
            heads = slice(hh * NH, (hh + 1) * NH)
            gb[l, hh, :, 0] = np.repeat(prm["b_igate"][l][heads], NCH)
            gb[l, hh, :, 1] = np.repeat(prm["b_fgate"][l][heads], NCH)
    mnw = np.ascontiguousarray(np.broadcast_to(prm["mlstm_norm_w"][:, None, :], (L, 128, D_MODEL))).astype(np.float32)
    dnw = np.ascontiguousarray(np.broadcast_to(prm["diff_norm_w"][:, None, :], (L, 128, D_MODEL))).astype(np.float32)
    lam = np.stack([np.stack([prm["lambda_q1"][l], prm["lambda_k1"][l], prm["lambda_q2"][l], prm["lambda_k2"][l]])
                    for l in range(L)])
    lam = np.ascontiguousarray(np.broadcast_to(lam[:, None], (L, 128, 4, 128))).astype(np.float32)
    lami = np.zeros((L, 128, 2), np.float32)
    for l in range(L):
        lami[l, :, 0] = lam_init_of(l)
        lami[l, :, 1] = 1.0 - lam_init_of(l)
    return {"anw": anw, "fnw2": fnw2, "fnw": fnw, "cw": cw, "gb": gb, "mnw": mnw, "dnw": dnw, "lam": lam,
            "lami": lami, "ident": IDENT, "maskneg": MASKNEG_NP}


_NC = {}


def kernel(**inputs):
    x = np.ascontiguousarray(inputs["x"], dtype=np.float32)
    prm = {k: np.asarray(v, dtype=np.float32) for k, v in inputs.items() if k != "x"}
    if "nc" not in _NC:
        _NC["nc"] = build_fused()
    hp = host_params(prm)
    big = {k: np.ascontiguousarray(prm[k]) for k in ("w_in", "w_branch_m", "w_branch_d", "w_out",
                                                     "w_ffn_gate", "w_ffn_up", "w_ffn_down")}
    maps = []
    for b in range(NUSED):
        m = {"x": np.ascontiguousarray(x[b])}
        m.update(big)
        m.update(hp)
        maps.append(m)
    res = run_bass_kernel_spmd(_NC["nc"], maps, core_ids=list(range(NUSED)))
    return np.stack([np.asarray(res.results[b]["y"]) for b in range(NUSED)]).astype(np.float32)
```

```python
import contextlib
import math
import numpy as np
import ml_dtypes
import concourse.bass as bass
import concourse.mybir as mybir
from concourse.bass_utils import run_bass_kernel_spmd

F32 = mybir.dt.float32
BF16 = mybir.dt.bfloat16
AF = mybir.ActivationFunctionType
ALU = mybir.AluOpType
AX = mybir.AxisListType
NPBF = ml_dtypes.bfloat16

D_MODEL = 2048
BATCH = 4
SEQ = 4096
DEPTH = 4
NCORES = 8
TOK = 2048
D_FF = 5632
N_IN = 16400
EPS = 1e-6
KC = D_MODEL // 128

PE, ACT, DVE, POOL, SP = "pe", "act", "dve", "pool", "sp"
COMPUTE = (PE, ACT, DVE, POOL)


class Buf:
    __slots__ = ("name", "last_write", "reads", "dma_sem")

    def __init__(self, name):
        self.name = name
        self.last_write = None
        self.reads = {}
        self.dma_sem = None


class Instr:
    __slots__ = ("eng", "fn", "deps", "is_dma", "sem_key", "sig_val", "needs_sig")

    def __init__(self, eng, fn, is_dma, sem_key):
        self.eng = eng
        self.fn = fn
        self.deps = []
        self.is_dma = is_dma
        self.sem_key = sem_key
        self.sig_val = None
        self.needs_sig = False


class Prog:
    NDMA = 72

    def __init__(self, nc, es):
        self.nc = nc
        self.esem = {e: es.enter_context(nc.semaphore("s_" + e)) for e in COMPUTE}
        self.dsem = [es.enter_context(nc.semaphore("d_%d" % i)) for i in range(self.NDMA)]
        self.cnt = {e: 0 for e in COMPUTE}
        self.dcnt = [0] * self.NDMA
        self.barrier = []
        self.n_total = 0
        self._reset()

    def _reset(self):
        self.q = {e: [] for e in (PE, ACT, DVE, POOL, SP)}
        self.started = set()

    def op(self, eng, fn, reads=(), writes=(), dma=False, sem_key=None, pe_accum=False):
        if dma and sem_key is None:
            sem_key = writes[0] if len(writes) else reads[0]
        ins = Instr(eng, fn, dma, sem_key)
        deps = []
        if eng not in self.started:
            self.started.add(eng)
            ins.deps.extend(self.barrier)
        for b in reads:
            if b.last_write is not None:
                deps.append(b.last_write)
        for b in writes:
            if b.last_write is not None:
                deps.append(b.last_write)
            deps.extend(b.reads.values())
        seen = set()
        for d in deps:
            if d is ins or id(d) in seen:
                continue
            seen.add(id(d))
            if (not d.is_dma) and d.eng == PE and eng == PE and not dma:
                continue
            ins.deps.append(d)
            d.needs_sig = True
        for b in reads:
            b.reads[("d", id(sem_key)) if dma else eng] = ins
        for b in writes:
            b.last_write = ins
            b.reads = {}
        self.q[eng].append(ins)
        return ins

    def end_phase(self, last=False):
        nc = self.nc
        bar = {}
        for e in self.q:
            if self.q[e]:
                bar[id(self.q[e][-1])] = self.q[e][-1]
            for ins in self.q[e]:
                if ins.is_dma:
                    bar["k%d" % id(ins.sem_key)] = ins
        barrier = []
        seen = set()
        for ins in bar.values():
            if id(ins) not in seen:
                seen.add(id(ins))
                barrier.append(ins)
                ins.needs_sig = True
        nkeys = 0
        for e in self.q:
            for ins in self.q[e]:
                if ins.is_dma and ins.needs_sig and ins.sem_key.dma_sem is None:
                    ins.sem_key.dma_sem = nkeys
                    nkeys += 1
        assert nkeys <= self.NDMA, nkeys
        for e in self.q:
            for ins in self.q[e]:
                self.n_total += 1
                if not ins.needs_sig:
                    continue
                if ins.is_dma:
                    k = ins.sem_key.dma_sem
                    self.dcnt[k] += 16
                    ins.sig_val = (self.dsem[k], self.dcnt[k], 16)
                else:
                    self.cnt[ins.eng] += 1
                    ins.sig_val = (self.esem[ins.eng], self.cnt[ins.eng], 1)
        q = self.q
        with nc.Block() as block:
            def run(e, eng_obj):
                waited = {}
                for ins in q[e]:
                    for d in ins.deps:
                        sem, val, _ = d.sig_val
                        key = id(sem)
                        if waited.get(key, 0) >= val:
                            continue
                        waited[key] = val
                        eng_obj.wait_ge(sem, val)
                    bi = ins.fn(eng_obj)
                    if ins.needs_sig:
                        sem, val, inc = ins.sig_val
                        bi.then_inc(sem, inc)
                if e == SP and last:
                    for ins in barrier:
                        sem, val, _ = ins.sig_val
                        if waited.get(id(sem), 0) >= val:
                            continue
                        waited[id(sem)] = val
                        eng_obj.wait_ge(sem, val)

            if q[PE]:
                @block.tensor
                def _(eng):
                    run(PE, eng)
            if q[ACT]:
                @block.scalar
                def _(eng):
                    run(ACT, eng)
            if q[DVE]:
                @block.vector
                def _(eng):
                    run(DVE, eng)
            if q[POOL]:
                @block.gpsimd
                def _(eng):
                    run(POOL, eng)
            if q[SP] or last:
                @block.sync
                def _(eng):
                    run(SP, eng)
        for ins in barrier:
            ins.fn = None
        for e in q:
            for ins in q[e]:
                ins.fn = None
                ins.deps = None
        self.barrier = barrier
        self._reset()


class Ctx:
    def __init__(self, nc, es):
        self.nc = nc
        self.ges = es
        self.es = None
        self.P = Prog(nc, es)
        self.uid = 0
        self.banks = []
        self.bank_bufs = []
        self.bank_i = 0
        self.evac_i = 0

    def sb(self, name, shape, dt):
        self.uid += 1
        return self.es.enter_context(self.nc.sbuf_tensor("%s_%d" % (name, self.uid), list(shape), dt))

    def begin(self):
        self.es = contextlib.ExitStack()
        self.es.__enter__()
        self.bank_bufs = [Buf("bank%d" % i) for i in range(len(self.banks))]

    def end(self, last=False):
        self.P.end_phase(last)
        self.es.__exit__(None, None, None)
        self.es = None

    def alloc_banks(self, n=8):
        for i in range(n):
            self.banks.append(self.ges.enter_context(self.nc.psum_tensor("bank%d" % i, [128, 512], F32)))
            self.bank_bufs.append(Buf("bank%d" % i))

    def next_bank(self):
        i = self.bank_i % len(self.banks)
        self.bank_i += 1
        return self.banks[i], self.bank_bufs[i]

    def mm(self, out, lhsT, rhs, start, stop, reads, writes):
        return self.P.op(PE, lambda e: e.matmul(out, lhsT, rhs, start=start, stop=stop),
                         reads=reads, writes=writes)

    def act(self, out, in_, func, reads, writes, bias=None, scale=None, accum_out=None):
        kw = {}
        if bias is not None:
            kw["bias"] = bias
        if scale is not None:
            kw["scale"] = scale
        if accum_out is not None:
            kw["accum_out"] = accum_out
        return self.P.op(ACT, lambda e: e.activation(out=out, in_=in_, func=func, **kw),
                         reads=reads, writes=writes)

    def dma(self, eng, out, in_, reads, writes, sem_key=None):
        return self.P.op(eng, lambda e: e.dma_start(out=out, in_=in_), reads=reads, writes=writes,
                         dma=True, sem_key=sem_key)

    def tt(self, eng, out, in0, in1, op, reads, writes):
        return self.P.op(eng, lambda e: e.tensor_tensor(out=out, in0=in0, in1=in1, op=op),
                         reads=reads, writes=writes)

    def ts(self, eng, out, in0, s1, s2, op0, op1, reads, writes, accum_out=None):
        if op1 is None:
            return self.P.op(eng, lambda e: e.tensor_scalar(out=out, in0=in0, scalar1=s1, scalar2=None, op0=op0),
                             reads=reads, writes=writes)
        if accum_out is not None:
            return self.P.op(eng, lambda e: e.tensor_scalar(out=out, in0=in0, scalar1=s1, scalar2=s2, op0=op0,
                                                            op1=op1, accum_out=accum_out),
                             reads=reads, writes=writes)
        return self.P.op(eng, lambda e: e.tensor_scalar(out=out, in0=in0, scalar1=s1, scalar2=s2, op0=op0, op1=op1),
                         reads=reads, writes=writes)

    def stt(self, out, in0, scalar, in1, op0, op1, reads, writes):
        return self.P.op(DVE, lambda e: e.scalar_tensor_tensor(out=out, in0=in0, scalar=scalar, in1=in1,
                                                               op0=op0, op1=op1),
                         reads=reads, writes=writes)

    def copy(self, eng, out, in_, reads, writes):
        if eng == ACT:
            return self.P.op(ACT, lambda e: e.copy(out=out, in_=in_), reads=reads, writes=writes)
        return self.P.op(eng, lambda e: e.tensor_copy(out=out, in_=in_), reads=reads, writes=writes)

    def evac_copy(self, out, in_, reads, writes):
        self.evac_i += 1
        return self.copy(ACT if self.evac_i % 2 else DVE, out, in_, reads, writes)


def rmsnorm_to_T(c, xt, xbuf, scratch, hT, hT_buf, tok0, nw, nw_buf, ident, ident_buf, pfx):
    junk, junk_b = scratch["junk"]
    ss, ss_b = scratch["ss"]
    hn, hn_b = scratch["hn"]
    c.act(junk[:], xt[:], AF.Square, reads=[xbuf], writes=[junk_b, ss_b], accum_out=ss[:, 0:1])
    c.act(ss[:, 1:2], ss[:, 0:1], AF.Sqrt, reads=[ss_b, scratch["eps_b"]], writes=[ss_b], scale=1.0 / D_MODEL, bias=scratch["eps"][:, 0:1])
    c.P.op(DVE, lambda e: e.reciprocal(out=ss[:, 2:3], in_=ss[:, 1:2]), reads=[ss_b], writes=[ss_b])
    c.act(hn[:, 0:1024], xt[:, 0:1024], AF.Copy, reads=[xbuf, ss_b], writes=[hn_b], scale=ss[:, 2:3])
    c.ts(DVE, hn[:, 1024:2048], xt[:, 1024:2048], ss[:, 2:3], None, ALU.mult, None, reads=[xbuf, ss_b], writes=[hn_b])
    for kq in range(KC // 4):
        bank, bb = c.next_bank()
        for j in range(4):
            kc = kq * 4 + j
            c.mm(bank[:, j * 128:(j + 1) * 128], hn[:, kc * 128:(kc + 1) * 128], ident[:], True, True,
                 reads=[hn_b, ident_buf], writes=[bb])
        for j in range(4):
            kc = kq * 4 + j
            if j % 2 == 0:
                c.act(hT[:, kc, tok0:tok0 + 128], bank[:, j * 128:(j + 1) * 128], AF.Copy,
                      reads=[bb, nw_buf], writes=[hT_buf], scale=nw[:, kc:kc + 1])
            else:
                c.ts(DVE, hT[:, kc, tok0:tok0 + 128], bank[:, j * 128:(j + 1) * 128], nw[:, kc:kc + 1], None,
                     ALU.mult, None, reads=[bb, nw_buf], writes=[hT_buf])


def norm_scratch(c, pfx):
    eps = c.sb(pfx + "eps", [128, 1], F32)
    eb = Buf(pfx + "eps")
    c.P.op(POOL, lambda e: e.memset(eps[:], EPS), writes=[eb])
    return {
        "junk": (c.sb(pfx + "junk", [128, 2048], BF16), Buf(pfx + "junk")),
        "ss": (c.sb(pfx + "ss", [128, 4], F32), Buf(pfx + "ss")),
        "hn": (c.sb(pfx + "hn", [128, 2048], BF16), Buf(pfx + "hn")),
        "eps": eps, "eps_b": eb,
    }


A_SECTIONS = [
    ("qk", 0, 2048, "F", F32, False),
    ("mv", 2048, 2048, "T", BF16, False),
    ("mo", 4096, 2048, "T", BF16, True),
    ("gt", 6144, 16, "F", F32, False),
    ("dqk", 6160, 4096, "F", BF16, False),
    ("dv", 10256, 2048, "T", BF16, False),
    ("gmd", 12304, 4096, "F", BF16, True),
]


def load_wblock(c, wslot, wbuf, w_dram, row0, nkc, col0, ncols):
    src = w_dram[row0:row0 + nkc * 128, col0:col0 + ncols].rearrange("(kc p) c -> p kc c", p=128)
    c.dma(POOL, wslot[:, 0:nkc, 0:ncols], src, reads=[], writes=[wbuf])


def phase_A(c, x, nw_d, w, ident_d, outs):
    if True:
        c.begin()
        hT = c.sb("hT", [128, KC, TOK], BF16)
        hT_b = Buf("hT")
        nw = c.sb("nw_s", [128, KC], F32)
        nw_b = Buf("nw")
        ident = c.sb("ident_s", [128, 128], BF16)
        ident_b = Buf("ident")
        c.dma(SP, nw[:], nw_d[:, :], [], [nw_b])
        c.dma(POOL, ident[:], ident_d[:, :], [], [ident_b])
        NW = 3
        wslots = [(c.sb("w%d" % i, [128, KC, 512], BF16), Buf("w%d" % i)) for i in range(NW)]
        xs = [(c.sb("x%d" % i, [128, D_MODEL], F32), Buf("x%d" % i)) for i in range(2)]
        scr = norm_scratch(c, "n_")
        of32 = [(c.sb("of%d" % i, [128, TOK], F32), Buf("of%d" % i)) for i in range(2)]
        obf = [(c.sb("ob%d" % i, [128, TOK], BF16), Buf("ob%d" % i)) for i in range(2)]
        otk = [(c.sb("ot%d" % i, [128, 512], BF16), Buf("ot%d" % i)) for i in range(3)]

        blocks = []
        for name, c0, n, mode, dt, sg in A_SECTIONS:
            for o in range(0, n, 512):
                blocks.append((name, c0, o, min(512, n - o), mode, dt, sg))
        PRE = NW - 1

        def issue_load(bi):
            name, c0, o, n, mode, dt, sg = blocks[bi]
            ws, wb = wslots[bi % NW]
            load_wblock(c, ws, wb, w, 0, KC, c0 + o, n)

        for bi in range(min(PRE, len(blocks))):
            issue_load(bi)

        for tt in range(TOK // 128):
            xt, xb = xs[tt % 2]
            c.dma(SP, xt[:], x[tt * 128:(tt + 1) * 128, :], [], [xb])
            rmsnorm_to_T(c, xt, xb, scr, hT, hT_b, tt * 128, nw, nw_b, ident, ident_b, "n_")

        finals = []
        cnt = {"f": 0, "b": 0, "t": 0}
        for bi, (name, c0, o, n, mode, dt, sg) in enumerate(blocks):
            if bi + PRE < len(blocks):
                issue_load(bi + PRE)
            ws, wb = wslots[bi % NW]
            od = outs[name]
            if mode == "F":
                for cc in range(0, n, 128):
                    m = min(128, n - cc)
                    if dt == F32:
                        ot, ob = of32[cnt["f"] % 2]
                        cnt["f"] += 1
                    else:
                        ot, ob = obf[cnt["b"] % 2]
                        cnt["b"] += 1
                    for tg in range(TOK // 512):
                        bank, bb = c.next_bank()
                        for kc in range(KC):
                            c.mm(bank[0:m, :], ws[:, kc, cc:cc + m], hT[:, kc, tg * 512:(tg + 1) * 512],
                                 kc == 0, kc == KC - 1, reads=[wb, hT_b], writes=[bb])
                        dst = ot[0:m, tg * 512:(tg + 1) * 512]
                        if sg:
                            c.act(dst, bank[0:m, :], AF.Sigmoid, reads=[bb], writes=[ob])
                        else:
                            c.evac_copy(dst, bank[0:m, :], reads=[bb], writes=[ob])
                    finals.append(c.dma(SP, od[o + cc:o + cc + m, :], ot[0:m, :], [ob], []))
            else:
                for tt in range(TOK // 128):
                    bank, bb = c.next_bank()
                    for kc in range(KC):
                        c.mm(bank[:, 0:n], hT[:, kc, tt * 128:(tt + 1) * 128], ws[:, kc, 0:n],
                             kc == 0, kc == KC - 1, reads=[wb, hT_b], writes=[bb])
                    ot, ob = otk[cnt["t"] % 3]
                    cnt["t"] += 1
                    if sg:
                        c.act(ot[:, 0:n], bank[:, 0:n], AF.Sigmoid, reads=[bb], writes=[ob])
                    else:
                        c.evac_copy(ot[:, 0:n], bank[:, 0:n], reads=[bb], writes=[ob])
                    finals.append(c.dma(SP, od[tt * 128:(tt + 1) * 128, o:o + n], ot[:, 0:n], [ob], []))
        c.end()


def nw_layout(v):
    return np.ascontiguousarray(v.reshape(KC, 128).T)


NH = 4
NCH = SEQ // 128
MASKNEG = -30000.0
Q_R, Q_C, Q_INTER, Q_W, Q_EM = 0, 1, 2, 3, 4
NSB = 3
MSKEW = 1


def phase_BC(c, d, do_m=True, do_a=True):
    cw_d, mv_d, mo_d, gb_d, mnw_d, dv_d, dnw_d = d["cw"], d["mv"], d["mo"], d["gb"], d["mnw"], d["dv"], d["dnw"]
    lam_d, lami_d, ident_d, mask_d, hmT_d, hdT_d = d["lam"], d["lami"], d["ident"], d["maskneg"], d["hmT"], d["hdT"]
    gi_d, gf_d = d["gi"], d["gf"]
    if True:
        c.begin()
        if "pre" in d:
            d["pre"](c)
        P = c.P
        identf = c.sb("identf", [128, 128], F32); identf_b = Buf("identf")
        identb = c.sb("identb", [128, 128], BF16); identb_b = Buf("identb")
        maskf = c.sb("maskf", [128, 128], F32); maskf_b = Buf("maskf")
        maskb = c.sb("maskb", [128, 128], BF16); maskb_b = Buf("maskb")
        onesf = c.sb("onesf", [128, 128], F32); onesf_b = Buf("onesf")
        onesb = c.sb("onesb", [128, 128], BF16); onesb_b = Buf("onesb")
        epst = c.sb("epst", [128, 1], F32); eps_b = Buf("epst")
        c.dma(SP, identf[:], ident_d[:, :], [], [identf_b])
        c.dma(POOL, identb[:], ident_d[:, :], [], [identb_b])
        c.dma(SP, maskf[:], mask_d[:, :], [], [maskf_b])
        c.dma(POOL, maskb[:], mask_d[:, :], [], [maskb_b])
        P.op(POOL, lambda e: e.memset(onesf[:], 1.0), writes=[onesf_b])
        P.op(POOL, lambda e: e.memset(onesb[:], 1.0), writes=[onesb_b])
        P.op(POOL, lambda e: e.memset(epst[:], EPS), writes=[eps_b])
        cw = c.sb("cw_s", [128, 2 * NH, 5], F32); cw_b = Buf("cw")
        c.dma(SP, cw[:], cw_d, [], [cw_b])
        gb = c.sb("gb_s", [128, 2], F32); gb_b = Buf("gb")
        c.dma(SP, gb[:], gb_d, [], [gb_b])
        mnw = c.sb("mnw_s", [128, NH * 256], F32); mnw_b = Buf("mnw")
        c.dma(SP, mnw[:], mnw_d, [], [mnw_b])
        dnw = c.sb("dnw_s", [128, NH * 256], F32); dnw_b = Buf("dnw")
        c.dma(SP, dnw[:], dnw_d, [], [dnw_b])
        lamt = c.sb("lamt", [128, 4, 128], F32); lamt_b = Buf("lamt")
        c.dma(SP, lamt[:], lam_d, [], [lamt_b])
        lami = c.sb("lami_s", [128, 2], F32); lami_b = Buf("lami")
        c.dma(SP, lami[:], lami_d, [], [lami_b])

        g_i = c.sb("g_i", [128, 128], F32); g_f = c.sb("g_f", [128, 128], F32)
        gi_b, gf_b = Buf("g_i"), Buf("g_f")
        c.dma(SP, g_i[:], gi_d.rearrange("j (ci l) -> (j ci) l", l=128), [], [gi_b])
        c.dma(SP, g_f[:], gf_d.rearrange("j (ci l) -> (j ci) l", l=128), [], [gf_b])
        sm = c.sb("gsm", [128, 16], F32); sm_b = Buf("gsm")
        NBF, MPREV, DEC, RLAST = 0, 1, 2, 3
        gq = c.sb("gq", [128, 5, 128], F32); gq_b = Buf("gq")
        g_b = c.sb("g_bb", [128, 128], F32); gbb_b = Buf("g_bb")
        g_ml = c.sb("g_ml", [128, 128], F32); gml_b = Buf("g_ml")
        g_m = c.sb("g_m", [128, 128], F32); gm_b = Buf("g_m")
        c.ts(DVE, g_i[:], g_i[:], gb[:, 0:1], None, ALU.add, None, [gi_b, gb_b], [gi_b])
        c.ts(DVE, sm[:, NBF:NBF + 1], gb[:, 1:2], -1.0, None, ALU.mult, None, [gb_b], [sm_b])
        c.act(g_f[:], g_f[:], AF.Exp, [gf_b, sm_b], [gf_b], bias=sm[:, NBF:NBF + 1], scale=-1.0)
        c.act(g_f[:], g_f[:], AF.Ln, [gf_b, onesf_b], [gf_b], bias=onesf[:, 0:1], scale=1.0)
        c.ts(DVE, g_f[:], g_f[:], -1.0, None, ALU.mult, None, [gf_b], [gf_b])
        P.op(DVE, lambda e: e.tensor_tensor_scan(out=g_b[:], data0=g_f[:], data1=g_f[:], initial=0.0,
                                                 op0=ALU.add, op1=ALU.min), reads=[gf_b], writes=[gbb_b])
        P.op(DVE, lambda e: e.tensor_tensor_scan(out=g_ml[:], data0=g_f[:], data1=g_i[:], initial=-1e30,
                                                 op0=ALU.add, op1=ALU.max), reads=[gf_b, gi_b], writes=[gml_b])
        bankr, bankr_b = c.next_bank()
        c.mm(bankr[0:1, 0:128], g_b[:, 127:128], identf[:], True, True, [gbb_b, identf_b], [bankr_b])
        c.mm(bankr[0:1, 128:256], g_ml[:, 127:128], identf[:], True, True, [gml_b, identf_b], [bankr_b])
        erow = c.sb("erow", [1, 512], F32); erow_b = Buf("erow")
        c.copy(DVE, erow[0:1, 0:256], bankr[0:1, 0:256], [bankr_b], [erow_b])
        P.op(POOL, lambda e: e.memset(erow[0:1, 256:512], 0.0), writes=[erow_b])
        for j in range(NH):
            P.op(DVE, lambda e, j=j: e.tensor_tensor_scan(
                out=erow[0:1, 384 + j * 32:384 + (j + 1) * 32], data0=erow[0:1, j * 32:(j + 1) * 32],
                data1=erow[0:1, 128 + j * 32:128 + (j + 1) * 32], initial=0.0, op0=ALU.add, op1=ALU.max),
                reads=[erow_b], writes=[erow_b])
            c.copy(DVE, erow[0:1, 256 + j * 32 + 1:256 + (j + 1) * 32], erow[0:1, 384 + j * 32:384 + (j + 1) * 32 - 1],
                   [erow_b], [erow_b])
        c.mm(bankr[:, 256:257], erow[0:1, 256:384], onesf[0:1, 0:1], True, True, [erow_b, onesf_b], [bankr_b])
        c.copy(DVE, sm[:, MPREV:MPREV + 1], bankr[:, 256:257], [bankr_b], [sm_b])
        c.stt(g_m[:], g_b[:], sm[:, MPREV:MPREV + 1], g_ml[:], ALU.add, ALU.max, [gbb_b, sm_b, gml_b], [gm_b])
        c.tt(DVE, gq[:, Q_R, :], g_b[:], g_m[:], ALU.subtract, [gbb_b, gm_b], [gq_b])
        c.tt(DVE, gq[:, Q_C, :], g_i[:], g_b[:], ALU.subtract, [gi_b, gbb_b], [gq_b])
        c.copy(DVE, sm[:, RLAST:RLAST + 1], gq[:, Q_R, 127:128], [gq_b], [sm_b])
        c.act(gq[:, Q_INTER, :], gq[:, Q_R, :], AF.Exp, [gq_b, sm_b], [gq_b], bias=sm[:, MPREV:MPREV + 1], scale=1.0)
        c.act(gq[:, Q_W, :], gq[:, Q_C, :], AF.Exp, [gq_b, sm_b], [gq_b], bias=sm[:, RLAST:RLAST + 1], scale=1.0)
        c.act(gq[:, Q_EM, :], g_m[:], AF.Exp, [gm_b], [gq_b], scale=-1.0)
        c.act(sm[:, DEC:DEC + 1], sm[:, RLAST:RLAST + 1], AF.Exp, [sm_b], [sm_b], bias=sm[:, MPREV:MPREV + 1], scale=1.0)
        tq = c.sb("tq", [128, 5, 128], F32); tq_b = Buf("tq")
        for n in range(5):
            bk, bkb = c.next_bank()
            c.mm(bk[:, 0:128], gq[:, n, :], identf[:], True, True, [gq_b, identf_b], [bkb])
            c.copy(DVE, tq[:, n, :], bk[:, 0:128], [bkb], [tq_b])
        decm = c.sb("decm", [128, 128], F32); decm_b = Buf("decm")
        c.ts(DVE, decm[:], onesf[:], sm[:, DEC:DEC + 1], None, ALU.mult, None, [onesf_b, sm_b], [decm_b])
        bk, bkb = c.next_bank()
        c.mm(bk[:, 0:128], decm[:], identf[:], True, True, [decm_b, identf_b], [bkb])
        decb = c.sb("decb", [128, 128], F32); decb_b = Buf("decb")
        c.copy(DVE, decb[:], bk[:, 0:128], [bkb], [decb_b])

        lsm = c.sb("lsm", [128, 8], F32); lsm_b = Buf("lsm")
        lpr = c.sb("lpr", [128, 2, 128], F32); lpr_b = Buf("lpr")
        c.tt(DVE, lpr[:, 0, :], lamt[:, 0, :], lamt[:, 1, :], ALU.mult, [lamt_b], [lpr_b])
        c.tt(DVE, lpr[:, 1, :], lamt[:, 2, :], lamt[:, 3, :], ALU.mult, [lamt_b], [lpr_b])
        P.op(DVE, lambda e: e.reduce_sum(out=lsm[:, 0:2], in_=lpr[:], axis=AX.X), reads=[lpr_b], writes=[lsm_b])
        c.act(lsm[:, 2:4], lsm[:, 0:2], AF.Exp, [lsm_b], [lsm_b])
        c.tt(DVE, lsm[:, 4:5], lsm[:, 2:3], lsm[:, 3:4], ALU.subtract, [lsm_b], [lsm_b])
        c.ts(DVE, lsm[:, 5:6], lsm[:, 4:5], lami[:, 0:1], -1.0, ALU.add, ALU.mult, [lsm_b, lami_b], [lsm_b])
        NLAM = 5

        rawq = c.sb("rawq", [128, SEQ + 3], F32); rawq_b = Buf("rawq")
        rawk = c.sb("rawk", [128, SEQ + 3], F32); rawk_b = Buf("rawk")
        acc = c.sb("acc", [128, SEQ], F32); acc_b = Buf("acc")
        qT = c.sb("qT", [128, SEQ], BF16); qT_b = Buf("qT")
        kT = c.sb("kT", [128, SEQ], BF16); kT_b = Buf("kT")
        va = c.sb("va", [128, NCH, 257], BF16); va_b = Buf("va")
        P.op(POOL, lambda e: e.memset(rawq[:, 0:3], 0.0), writes=[rawq_b])
        P.op(POOL, lambda e: e.memset(rawk[:, 0:3], 0.0), writes=[rawk_b])
        P.op(POOL, lambda e: e.memset(va[:, :, 256:257], 1.0), writes=[va_b])
        CT = c.sb("CT", [128, 257], F32); CT_b = Buf("CT")
        CTb = c.sb("CTb", [128, 257], BF16); CTb_b = Buf("CTb")
        diagR = [(c.sb("diagR%d" % i, [128, 128], F32), Buf("diagR%d" % i)) for i in range(2)]
        Dm = [(c.sb("Dm%d" % i, [128, 128], F32), Buf("Dm%d" % i)) for i in range(2)]
        sdT = [(c.sb("sdT%d" % i, [128, 128], BF16), Buf("sdT%d" % i)) for i in range(2)]
        kw = [(c.sb("kw%d" % i, [128, 128], BF16), Buf("kw%d" % i)) for i in range(2)]
        numS = [(c.sb("numS%d" % i, [128, 257], F32), Buf("numS%d" % i)) for i in range(2)]
        tot = [(c.sb("tot%d" % i, [128, 257], F32), Buf("tot%d" % i)) for i in range(2)]
        junk = c.sb("junk", [128, 256], BF16); junk_b = Buf("junk")
        hs = [(c.sb("hs%d" % i, [128, 8], F32), Buf("hs%d" % i)) for i in range(2)]
        g2 = [(c.sb("g2_%d" % i, [128, 256], F32), Buf("g2_%d" % i)) for i in range(2)]
        hmt = [(c.sb("hm%d" % i, [128, 256], BF16), Buf("hm%d" % i)) for i in range(2)]
        mos = [(c.sb("mos%d" % i, [128, 4, 256], BF16), Buf("mos%d" % i)) for i in range(2)]
        hout = [(c.sb("hout%d" % i, [128, 2, 512], BF16), Buf("hout%d" % i)) for i in range(2)]
        kscale = 128.0 ** -0.5

        def conv_silu(raw, raw_b, idx, dst, dst_b, post_scale):
            c.ts(DVE, acc[:], raw[:, 0:SEQ], cw[:, idx, 0:1], cw[:, idx, 4:5], ALU.mult, ALU.add,
                 [raw_b, cw_b], [acc_b])
            for t in range(1, 4):
                c.stt(acc[:], raw[:, t:t + SEQ], cw[:, idx, t:t + 1], acc[:], ALU.mult, ALU.add,
                      [raw_b, cw_b, acc_b], [acc_b])
            if post_scale is None:
                c.act(dst[:], acc[:], AF.Silu, [acc_b], [dst_b])
            else:
                c.act(acc[:], acc[:], AF.Silu, [acc_b], [acc_b])
                c.ts(POOL, dst[:], acc[:], post_scale, None, ALU.mult, None, [acc_b], [dst_b])

        grp = 0
        for j in range(NH if do_m else 0):
            if j == 0:
                c.dma(SP, rawq[:, 3:SEQ + 3], d["qraw"](j), [], [rawq_b])
                c.dma(SP, rawk[:, 3:SEQ + 3], d["kraw"](j), [], [rawk_b])
            c.dma(SP, va[:, :, 0:256], mv_d[:, j * 256:(j + 1) * 256].rearrange("(ci p) v -> p ci v", p=128),
                  [], [va_b])
            conv_silu(rawq, rawq_b, j, qT, qT_b, None)
            conv_silu(rawk, rawk_b, NH + j, kT, kT_b, kscale)
            if j + 1 < NH:
                c.dma(SP, rawq[:, 3:SEQ + 3], d["qraw"](j + 1), [], [rawq_b])
                c.dma(SP, rawk[:, 3:SEQ + 3], d["kraw"](j + 1), [], [rawk_b])
            P.op(POOL, lambda e: e.memset(CT[:], 0.0), writes=[CT_b])
            P.op(POOL, lambda e: e.memset(CTb[:], 0.0), writes=[CTb_b])
            def stage1(ci):
                p = j * NCH + ci
                par = ci % 2
                cs = slice(ci * 128, (ci + 1) * 128)
                bA, bA_b = c.banks[par * 4 + 0], c.bank_bufs[par * 4 + 0]
                dR, dR_b = diagR[par]
                c.ts(POOL, dR[:], identf[:], tq[:, Q_R, p:p + 1], None, ALU.mult, None, [identf_b, tq_b], [dR_b])
                c.mm(bA[:, 0:128], kT[:, cs], qT[:, cs], True, True, [kT_b, qT_b], [bA_b])
                c.mm(bA[:, 128:256], onesf[:], dR[:], True, False, [onesf_b, dR_b], [bA_b])
                c.mm(bA[:, 128:256], identf[:], maskf[:], False, True, [identf_b, maskf_b], [bA_b])
                c.mm(bA[:, 256:384], kT[:, cs], identb[:], True, True, [kT_b, identb_b], [bA_b])
                dm, dm_b = Dm[par]
                c.act(dm[:], bA[:, 128:256], AF.Exp, [bA_b, tq_b], [dm_b], bias=tq[:, Q_C, p:p + 1], scale=1.0)
                sd, sd_b = sdT[par]
                c.tt(DVE, sd[:], bA[:, 0:128], dm[:], ALU.mult, [bA_b, dm_b], [sd_b])
                kwt, kw_b = kw[par]
                c.ts(DVE, kwt[:], bA[:, 256:384], tq[:, Q_W, p:p + 1], None, ALU.mult, None, [bA_b, tq_b], [kw_b])

            def stage2(ci):
                p = j * NCH + ci
                par = ci % 2
                cs = slice(ci * 128, (ci + 1) * 128)
                if ci % 4 == 0:
                    mo_t, mo_b = mos[(ci // 4) % 2]
                    c.dma(SP, mo_t[:], mo_d[ci * 128:(ci + 4) * 128, j * 256:(j + 1) * 256]
                          .rearrange("(cc p) v -> p cc v", p=128), [], [mo_b])
                mo_t, mo_b = mos[(ci // 4) % 2]
                bB, bB_b = c.banks[par * 4 + 1], c.bank_bufs[par * 4 + 1]
                bC, bC_b = c.banks[par * 4 + 2], c.bank_bufs[par * 4 + 2]
                bD, bD_b = c.banks[par * 4 + 3], c.bank_bufs[par * 4 + 3]
                sd, sd_b = sdT[par]
                kwt, kw_b = kw[par]
                c.mm(bB[:, 0:257], sd[:], va[:, ci, :], True, True, [sd_b, va_b], [bB_b])
                c.mm(bC[:, 0:257], qT[:, cs], CTb[:], True, True, [qT_b, CTb_b], [bC_b])
                c.mm(bD[:, 0:257], kwt[:], va[:, ci, :], True, True, [kw_b, va_b], [bD_b])
                ns, ns_b = numS[par]
                c.copy(ACT, ns[:], bB[:, 0:257], [bB_b], [ns_b])
                c.stt(CT[:], CT[:], decb[:, p:p + 1], bD[:, 0:257], ALU.mult, ALU.add, [CT_b, decb_b, bD_b], [CT_b])
                c.copy(POOL, CTb[:], CT[:], [CT_b], [CTb_b])
                tt_, tt_b = tot[par]
                c.stt(tt_[:], bC[:, 0:257], tq[:, Q_INTER, p:p + 1], ns[:], ALU.mult, ALU.add,
                      [bC_b, tq_b, ns_b], [tt_b])
                h_, h_b = hs[par]
                c.act(h_[:, 7:8], tt_[:, 256:257], AF.Abs, [tt_b], [h_b])
                c.ts(DVE, h_[:, 0:1], h_[:, 7:8], tq[:, Q_EM, p:p + 1], None, ALU.max, None, [h_b, tq_b], [h_b])
                c.act(h_[:, 1:2], h_[:, 0:1], AF.Square, [h_b], [h_b], scale=EPS ** 0.5)
                c.act(junk[:], tt_[:, 0:256], AF.Square, [tt_b], [junk_b, h_b], accum_out=h_[:, 2:3])
                c.act(h_[:, 4:5], h_[:, 2:3], AF.Sqrt, [h_b], [h_b], bias=h_[:, 1:2], scale=1.0 / 256)
                P.op(DVE, lambda e, h_=h_: e.reciprocal(out=h_[:, 6:7], in_=h_[:, 4:5]), reads=[h_b], writes=[h_b])
                g2t, g2_b = g2[par]
                c.tt(POOL, g2t[:], mnw[:, j * 256:(j + 1) * 256], mo_t[:, ci % 4, :], ALU.mult, [mnw_b, mo_b], [g2_b])
                hm_, hm_b = hmt[par]
                c.stt(hm_[:], tt_[:, 0:256], h_[:, 6:7], g2t[:], ALU.mult, ALU.mult, [tt_b, h_b, g2_b], [hm_b])

            def stage3(ci):
                par = ci % 2
                bB, bB_b = c.banks[par * 4 + 1], c.bank_bufs[par * 4 + 1]
                hm_, hm_b = hmt[par]
                ho_t, ho_b = hout[(ci // 4) % 2]
                for vc in range(2):
                    c.mm(bB[:, vc * 128:(vc + 1) * 128], hm_[:, vc * 128:(vc + 1) * 128], identb[:], True, True,
                         [hm_b, identb_b], [bB_b])
                c.copy(ACT, ho_t[:, :, (ci % 4) * 128:(ci % 4 + 1) * 128],
                       bB[:, 0:256].rearrange("p (a b) -> p a b", a=2), [bB_b], [ho_b])
                if ci % 4 == 3:
                    c.dma(SP, hmT_d[j * 256:(j + 1) * 256, (ci - 3) * 128:(ci + 1) * 128]
                          .rearrange("(a p) t -> p a t", p=128), ho_t[:], [ho_b], [])

            for it in range(NCH + MSKEW * 2):
                if it < NCH:
                    stage1(it)
                if 0 <= it - MSKEW < NCH:
                    stage2(it - MSKEW)
                if 0 <= it - 2 * MSKEW < NCH:
                    stage3(it - 2 * MSKEW)


        qc = [(c.sb("dq%d" % i, [128, SEQ], BF16), Buf("dq%d" % i)) for i in range(2)]
        kc_ = [(c.sb("dk%d" % i, [128, SEQ], BF16), Buf("dk%d" % i)) for i in range(2)]
        sq = c.sb("sq", [128, SEQ], BF16); sq_b = Buf("sq")
        mx = c.sb("mx", [1, 64], F32); mx_b = Buf("mx")
        nG = c.sb("nG", [128, 1], F32); nG_b = Buf("nG")
        Et = [(c.sb("E%d" % i, [128, 512], BF16), Buf("E%d" % i)) for i in range(NSB + 2)]
        ds = [(c.sb("ds%d" % i, [128, 8], F32), Buf("ds%d" % i)) for i in range(4)]
        dtm = [(c.sb("dt%d" % i, [128, 256], F32), Buf("dt%d" % i)) for i in range(4)]
        dhd = [(c.sb("dhd%d" % i, [128, 256], F32), Buf("dhd%d" % i)) for i in range(4)]
        dhn = [(c.sb("dhn%d" % i, [128, 256], BF16), Buf("dhn%d" % i)) for i in range(4)]
        Os1 = [(c.sb("os1_%d" % i, [128, 257], F32), Buf("os1_%d" % i)) for i in range(4)]
        Os2 = [(c.sb("os2_%d" % i, [128, 257], F32), Buf("os2_%d" % i)) for i in range(4)]
        junkf = c.sb("junkf", [128, 256], F32); junkf_b = Buf("junkf")
        ascale = 128.0 ** -0.5
        Obanks = [(c.banks[i], c.bank_bufs[i]) for i in range(4)]
        Sbanks = [(c.banks[4 + i], c.bank_bufs[4 + i]) for i in range(NSB)]
        Tbanks = [(c.banks[4 + NSB + i], c.bank_bufs[4 + NSB + i]) for i in range(4 - NSB)]
        e_i = 0
        s_i = 0
        t_i = 0
        for j in range(NH if do_a else 0):
            for cc in range(2):
                c.dma(SP, qc[cc][0][:], d["dq"](j, cc), [], [qc[cc][1]])
                c.dma(SP, kc_[cc][0][:], d["dk"](j, cc), [], [kc_[cc][1]])
            c.dma(SP, va[:, :, 0:256], dv_d[:, j * 256:(j + 1) * 256].rearrange("(ci p) v -> p ci v", p=128),
                  [], [va_b])
            tb, tb_b = Tbanks[0]
            for ti, (tns, tns_b) in enumerate([qc[0], qc[1], kc_[0], kc_[1]]):
                c.act(sq[:], tns[:], AF.Square, [tns_b], [sq_b])
                for s8 in range(8):
                    c.mm(tb[0:1, 0:512], onesb[:, 0:1], sq[:, s8 * 512:(s8 + 1) * 512], True, True,
                         [onesb_b, sq_b], [tb_b])
                    P.op(DVE, lambda e, ti=ti, s8=s8: e.reduce_max(out=mx[0:1, ti * 8 + s8:ti * 8 + s8 + 1],
                                                                   in_=tb[0:1, 0:512], axis=AX.X),
                         reads=[tb_b], writes=[mx_b])
            P.op(DVE, lambda e: e.reduce_max(out=mx[0:1, 32:34], in_=mx[0:1, 0:32].rearrange("p (a b) -> p a b", a=2),
                                             axis=AX.X), reads=[mx_b], writes=[mx_b])
            c.tt(DVE, mx[0:1, 34:35], mx[0:1, 32:33], mx[0:1, 33:34], ALU.mult, [mx_b], [mx_b])
            c.act(mx[0:1, 35:36], mx[0:1, 34:35], AF.Sqrt, [mx_b], [mx_b], scale=ascale * ascale)
            c.ts(DVE, mx[0:1, 36:37], mx[0:1, 35:36], -1.0, None, ALU.mult, None, [mx_b], [mx_b])
            c.mm(tb[:, 0:1], onesf[0:1, :], mx[0:1, 36:37], True, True, [onesf_b, mx_b], [tb_b])
            c.copy(DVE, nG[:], tb[:, 0:1], [tb_b], [nG_b])
            if "dbg" in d:
                c.dma(SP, d["dbg"][j, 0:1, 0:64], mx[0:1, :], [mx_b], [])

            def qk_exp(g, kb):
                nonlocal s_i, e_i
                sb_, sb_b = Sbanks[s_i % NSB]; s_i += 1
                et, et_b = Et[e_i % (NSB + 2)]; e_i += 1
                ks = slice(kb * 128, (kb + 1) * 128)
                if kb <= 2 * g:
                    for cc in range(2):
                        diag = (kb == 2 * g)
                        c.mm(sb_[:, cc * 256:(cc + 1) * 256], kc_[cc][0][:, ks], qc[cc][0][:, g * 256:(g + 1) * 256],
                             True, not diag, [kc_[cc][1], qc[cc][1]], [sb_b])
                        if diag:
                            c.mm(sb_[:, cc * 256:cc * 256 + 128], identb[:], maskb[:], False, True,
                                 [identb_b, maskb_b], [sb_b])
                    c.act(et[:], sb_[:], AF.Exp, [sb_b, nG_b], [et_b], bias=nG[:, 0:1], scale=ascale)
                    ilist = (0, 1)
                else:
                    for cc in range(2):
                        c.mm(sb_[:, cc * 256 + 128:(cc + 1) * 256], kc_[cc][0][:, ks],
                             qc[cc][0][:, g * 256 + 128:(g + 1) * 256], True, False, [kc_[cc][1], qc[cc][1]], [sb_b])
                        c.mm(sb_[:, cc * 256 + 128:(cc + 1) * 256], identb[:], maskb[:], False, True,
                             [identb_b, maskb_b], [sb_b])
                    c.act(et[:].rearrange("p (a b) -> p a b", a=2)[:, :, 128:256],
                          sb_[:].rearrange("p (a b) -> p a b", a=2)[:, :, 128:256], AF.Exp,
                          [sb_b, nG_b], [et_b], bias=nG[:, 0:1], scale=ascale)
                    ilist = (1,)
                return (g, kb, et, et_b, ilist)

            def pv(st):
                g, kb, et, et_b, ilist = st
                for cc in range(2):
                    for i in ilist:
                        ob, ob_b = Obanks[cc * 2 + i]
                        last = (kb == 2 * g + i)
                        c.mm(ob[:, 0:257], et[:, cc * 256 + i * 128:cc * 256 + (i + 1) * 128], va[:, kb, :],
                             kb == 0, last, [et_b, va_b], [ob_b])
                if kb == 2 * g + 1:
                    epilogue(g)

            def epilogue(g):
                nonlocal t_i
                for i in range(2):
                    qb = 2 * g + i
                    r = qb % 4
                    o1, o1_b = Obanks[i]
                    o2, o2_b = Obanks[2 + i]
                    os1, os1_b = Os1[r]
                    os2, os2_b = Os2[r]
                    c.copy(DVE, os1[:], o1[:, 0:257], [o1_b], [os1_b])
                    c.copy(DVE, os2[:], o2[:, 0:257], [o2_b], [os2_b])
                R = [(2 * g + i) % 4 for i in range(2)]
                for r in R:
                    P.op(DVE, lambda e, d_=ds[r][0], os1=Os1[r][0]: e.reciprocal(out=d_[:, 0:1], in_=os1[:, 256:257]),
                         reads=[Os1[r][1]], writes=[ds[r][1]])
                for r in R:
                    P.op(DVE, lambda e, d_=ds[r][0], os2=Os2[r][0]: e.reciprocal(out=d_[:, 1:2], in_=os2[:, 256:257]),
                         reads=[Os2[r][1]], writes=[ds[r][1]])
                for r in R:
                    d_, d_b = ds[r]
                    c.ts(DVE, d_[:, 2:3], d_[:, 1:2], lsm[:, NLAM:NLAM + 1], None, ALU.mult, None, [d_b, lsm_b], [d_b])
                for r in R:
                    d_, d_b = ds[r]
                    c.ts(DVE, dtm[r][0][:], Os1[r][0][:, 0:256], d_[:, 0:1], None, ALU.mult, None,
                         [Os1[r][1], d_b], [dtm[r][1]])
                for r in R:
                    d_, d_b = ds[r]
                    c.stt(dhd[r][0][:], Os2[r][0][:, 0:256], d_[:, 2:3], dtm[r][0][:], ALU.mult, ALU.add,
                          [Os2[r][1], d_b, dtm[r][1]], [dhd[r][1]])
                for r in R:
                    P.op(DVE, lambda e, hd_=dhd[r][0], d_=ds[r][0]: e.scalar_tensor_tensor(
                        out=junkf[:], in0=hd_[:], scalar=1.0, in1=hd_[:], op0=ALU.mult, op1=ALU.mult,
                        accum_out=d_[:, 3:4]), reads=[dhd[r][1]], writes=[junkf_b, ds[r][1]])
                defer.append([it_no[0] + 3, stage_b, (g,)])

            def stage_b(g):
                R = [(2 * g + i) % 4 for i in range(2)]
                for r in R:
                    d_, d_b = ds[r]
                    c.act(d_[:, 4:5], d_[:, 3:4], AF.Sqrt, [d_b, eps_b], [d_b], bias=epst[:, 0:1], scale=1.0 / 256)
                for r in R:
                    P.op(DVE, lambda e, d_=ds[r][0]: e.reciprocal(out=d_[:, 5:6], in_=d_[:, 4:5]),
                         reads=[ds[r][1]], writes=[ds[r][1]])
                for r in R:
                    d_, d_b = ds[r]
                    c.ts(DVE, d_[:, 6:7], d_[:, 5:6], lami[:, 1:2], None, ALU.mult, None, [d_b, lami_b], [d_b])
                for r in R:
                    d_, d_b = ds[r]
                    c.stt(dhn[r][0][:], dhd[r][0][:], d_[:, 6:7], dnw[:, j * 256:(j + 1) * 256], ALU.mult, ALU.mult,
                          [dhd[r][1], d_b, dnw_b], [dhn[r][1]])
                for i in range(2):
                    defer.append([it_no[0] + 3, stage_c, (g, i)])

            def stage_c(g, i):
                nonlocal t_i
                qb = 2 * g + i
                r = qb % 4
                hn_, hn_b = dhn[r]
                tb, tb_b = Tbanks[t_i % (4 - NSB)]; t_i += 1
                for vc in range(2):
                    c.mm(tb[:, vc * 128:(vc + 1) * 128], hn_[:, vc * 128:(vc + 1) * 128], identb[:], True, True,
                         [hn_b, identb_b], [tb_b])
                ho_t, ho_b = hout[(qb // 4) % 2]
                c.copy(DVE, ho_t[:, :, (qb % 4) * 128:(qb % 4 + 1) * 128],
                       tb[:, 0:256].rearrange("p (a b) -> p a b", a=2), [tb_b], [ho_b])
                if qb % 4 == 3:
                    c.dma(SP, hdT_d[j * 256:(j + 1) * 256, (qb - 3) * 128:(qb + 1) * 128]
                          .rearrange("(a p) t -> p a t", p=128), ho_t[:], [ho_b], [])

            defer = []
            it_no = [0]

            def run_deferred(flush=False):
                k = 0
                while k < len(defer):
                    if flush or defer[k][0] <= it_no[0]:
                        _, fn, args = defer.pop(k)
                        fn(*args)
                    else:
                        k += 1

            pairs = [(g, kb) for g in range(NCH // 2) for kb in range(2 * g + 2)]
            pend = []
            for (g, kb) in pairs:
                pend.append(qk_exp(g, kb))
                if len(pend) > NSB - 1:
                    pv(pend.pop(0))
                it_no[0] += 1
                run_deferred()
            while pend:
                pv(pend.pop(0))
            while defer:
                run_deferred(flush=True)
        c.end()


IDENT = np.eye(128, dtype=np.float32)
MASKNEG_NP = np.where(np.arange(128)[:, None] <= np.arange(128)[None, :], 0.0, MASKNEG).astype(np.float32)


def lam_init_of(l):
    return 0.8 - 0.6 * math.exp(-0.3 * l)


TG = 512
NFC = D_FF // 128


def phase_D(c, d, final, last):
    x_d, hm_d, hd_d, gmd_d, xo_d = d["x"], d["hmT"], d["hdT"], d["gmd"], d["xo"]
    wbm_d, wbd_d, wout_d, wg_d, wu_d, wd_d = d["wbm"], d["wbd"], d["wout"], d["wg"], d["wu"], d["wd"]
    nw2_d, ident_d = d["nw2"], d["ident"]
    if final:
        fnw_d = d["fnw"]
    if True:
        c.begin()
        P = c.P
        ident = c.sb("ident_s", [128, 128], BF16); ident_b = Buf("ident")
        c.dma(POOL, ident[:], ident_d[:, :], [], [ident_b])
        nw2 = c.sb("nw2_s", [128, KC], F32); nw2_b = Buf("nw2")
        c.dma(SP, nw2[:], nw2_d, [], [nw2_b])
        if final:
            fnw = c.sb("fnw_s", [128, D_MODEL], F32); fnw_b = Buf("fnw")
            c.dma(SP, fnw[:], fnw_d, [], [fnw_b])
        hmg = c.sb("hmg", [128, KC, TG], BF16); hmg_b = Buf("hmg")
        hdg = c.sb("hdg", [128, KC, TG], BF16); hdg_b = Buf("hdg")
        yT = c.sb("yT", [128, KC, TG], BF16); yT_b = Buf("yT")
        xg = c.sb("xg", [128, TG // 128, D_MODEL], F32)
        xg_b = [Buf("xg%d" % i) for i in range(TG // 128)]
        aT = c.sb("aT", [128, NFC, TG], BF16); aT_b = Buf("aT")
        NW = 3
        wslots = [(c.sb("w%d" % i, [128, KC, 512], BF16), Buf("w%d" % i)) for i in range(NW)]
        t1 = [(c.sb("t1_%d" % i, [128, TG], F32), Buf("t1_%d" % i)) for i in range(2)]
        t2 = [(c.sb("t2_%d" % i, [128, TG], F32), Buf("t2_%d" % i)) for i in range(2)]
        sgm = [(c.sb("sgm%d" % i, [128, TG], BF16), Buf("sgm%d" % i)) for i in range(2)]
        sgd = [(c.sb("sgd%d" % i, [128, TG], BF16), Buf("sgd%d" % i)) for i in range(2)]
        scr = norm_scratch(c, "n_")

        wlist = []
        for g in range(TOK // TG):
            for blk in range(4):
                wlist.append((wbm_d, 0, KC, blk * 512, 512))
                wlist.append((wbd_d, 0, KC, blk * 512, 512))
            for cg in range(4):
                wlist.append((wout_d, 0, KC, cg * 512, 512))
            for blk in range(D_FF // 512):
                wlist.append((wg_d, 0, KC, blk * 512, 512))
                wlist.append((wu_d, 0, KC, blk * 512, 512))
            for cg in range(4):
                for fb in range(4):
                    wlist.append((wd_d, fb * 11 * 128, 11, cg * 512, 512))
        wstate = {"issued": 0, "used": 0, "done": 0}

        def issue_one():
            i = wstate["issued"]
            wdr, r0, nkc, c0, ncol = wlist[i]
            ws, wb = wslots[i % NW]
            load_wblock(c, ws, wb, wdr, r0, nkc, c0, ncol)
            wstate["issued"] += 1

        def next_w():
            i = wstate["used"]
            while wstate["issued"] <= i:
                assert wstate["issued"] < wstate["done"] + NW
                issue_one()
            wstate["used"] += 1
            return wslots[i % NW]

        def release_w():
            wstate["done"] = wstate["used"]
            while wstate["issued"] < len(wlist) and wstate["issued"] < wstate["done"] + NW:
                issue_one()

        k2 = 0
        for g in range(TOK // TG):
            ts0 = g * TG
            c.dma(SP, hmg[:], hm_d[:, ts0:ts0 + TG].rearrange("(kc p) t -> p kc t", p=128), [], [hmg_b])
            c.dma(SP, hdg[:], hd_d[:, ts0:ts0 + TG].rearrange("(kc p) t -> p kc t", p=128), [], [hdg_b])
            for tt in range(TG // 128):
                c.dma(SP, xg[:, tt, :], x_d[ts0 + tt * 128:ts0 + (tt + 1) * 128, :], [], [xg_b[tt]])
            for blk in range(4):
                wm, wm_b = next_w()
                wd_, wd_b = next_w()
                for cc in range(4):
                    col = blk * 4 + cc
                    sm_, sm_b = sgm[k2 % 2]
                    sd_, sd_b = sgd[k2 % 2]
                    c.dma(SP, sm_[:], gmd_d[col * 128:(col + 1) * 128, ts0:ts0 + TG], [], [sm_b])
                    c.dma(SP, sd_[:], gmd_d[D_MODEL + col * 128:D_MODEL + (col + 1) * 128, ts0:ts0 + TG], [], [sd_b])
                    bA, bA_b = c.next_bank()
                    for kc in range(KC):
                        c.mm(bA[:, :], wm[:, kc, cc * 128:(cc + 1) * 128], hmg[:, kc, :], kc == 0, kc == KC - 1,
                             [wm_b, hmg_b], [bA_b])
                    bB, bB_b = c.next_bank()
                    for kc in range(KC):
                        c.mm(bB[:, :], wd_[:, kc, cc * 128:(cc + 1) * 128], hdg[:, kc, :], kc == 0, kc == KC - 1,
                             [wd_b, hdg_b], [bB_b])
                    a1, a1_b = t1[k2 % 2]
                    a2, a2_b = t2[k2 % 2]
                    k2 += 1
                    c.tt(DVE, a1[:], bA[:, :], sm_[:], ALU.mult, [bA_b, sm_b], [a1_b])
                    c.tt(DVE, a2[:], bB[:, :], sd_[:], ALU.mult, [bB_b, sd_b], [a2_b])
                    c.tt(POOL, yT[:, col, :], a1[:], a2[:], ALU.add, [a1_b, a2_b], [yT_b])
                release_w()
            for cg in range(4):
                wo, wo_b = next_w()
                for tt in range(TG // 128):
                    bk, bk_b = c.next_bank()
                    for kc in range(KC):
                        c.mm(bk[:, :], yT[:, kc, tt * 128:(tt + 1) * 128], wo[:, kc, :], kc == 0, kc == KC - 1,
                             [yT_b, wo_b], [bk_b])
                    c.tt(DVE, xg[:, tt, cg * 512:(cg + 1) * 512], bk[:, :], xg[:, tt, cg * 512:(cg + 1) * 512], ALU.add,
                         [bk_b, xg_b[tt]], [xg_b[tt]])
                release_w()
            for tt in range(TG // 128):
                rmsnorm_to_T(c, xg[:, tt, :], xg_b[tt], scr, hmg, hmg_b, tt * 128, nw2, nw2_b, ident, ident_b, "n_")
            for blk in range(D_FF // 512):
                wg_, wg_b = next_w()
                wu_, wu_b = next_w()
                for cc in range(4):
                    fc = blk * 4 + cc
                    bG, bG_b = c.next_bank()
                    for kc in range(KC):
                        c.mm(bG[:, :], wg_[:, kc, cc * 128:(cc + 1) * 128], hmg[:, kc, :], kc == 0, kc == KC - 1,
                             [wg_b, hmg_b], [bG_b])
                    bU, bU_b = c.next_bank()
                    for kc in range(KC):
                        c.mm(bU[:, :], wu_[:, kc, cc * 128:(cc + 1) * 128], hmg[:, kc, :], kc == 0, kc == KC - 1,
                             [wu_b, hmg_b], [bU_b])
                    a1, a1_b = t1[k2 % 2]
                    k2 += 1
                    c.act(a1[:], bG[:, :], AF.Silu, [bG_b], [a1_b])
                    c.tt(DVE, aT[:, fc, :], bU[:, :], a1[:], ALU.mult, [bU_b, a1_b], [aT_b])
                release_w()
            for cg in range(4):
                bks = [c.next_bank() for _ in range(TG // 128)]
                for fb in range(4):
                    wdn, wdn_b = next_w()
                    for tt in range(TG // 128):
                        bk, bk_b = bks[tt]
                        for i in range(11):
                            fc = fb * 11 + i
                            c.mm(bk[:, :], aT[:, fc, tt * 128:(tt + 1) * 128], wdn[:, i, :], fc == 0, fc == NFC - 1,
                                 [aT_b, wdn_b], [bk_b])
                    release_w()
                for tt in range(TG // 128):
                    bk, bk_b = bks[tt]
                    c.tt(DVE, xg[:, tt, cg * 512:(cg + 1) * 512], bk[:, :], xg[:, tt, cg * 512:(cg + 1) * 512], ALU.add,
                         [bk_b, xg_b[tt]], [xg_b[tt]])
            for tt in range(TG // 128):
                if final:
                    junk, junk_b = scr["junk"]
                    ss, ss_b = scr["ss"]
                    c.act(junk[:], xg[:, tt, :], AF.Square, [xg_b[tt]], [junk_b, ss_b], accum_out=ss[:, 0:1])
                    c.act(ss[:, 1:2], ss[:, 0:1], AF.Sqrt, [ss_b, scr["eps_b"]], [ss_b], scale=1.0 / D_MODEL,
                          bias=scr["eps"][:, 0:1])
                    P.op(DVE, lambda e, ss=ss: e.reciprocal(out=ss[:, 2:3], in_=ss[:, 1:2]), reads=[ss_b], writes=[ss_b])
                    c.stt(xg[:, tt, :], xg[:, tt, :], ss[:, 2:3], fnw[:], ALU.mult, ALU.mult,
                          [xg_b[tt], ss_b, fnw_b], [xg_b[tt]])
                c.dma(SP, xo_d[ts0 + tt * 128:ts0 + (tt + 1) * 128, :], xg[:, tt, :], [xg_b[tt]], [], sem_key=xg_b[tt])
        c.end(last)


NUSED = 4


def build_fused(depth=DEPTH):
    nc = bass.Bass("TRN2", target_bir_lowering=False)
    di = lambda n, s, dt: nc.dram_tensor(n, s, dt, kind="ExternalInput").ap()
    x_in = di("x", [SEQ, D_MODEL], F32)
    w_in = di("w_in", [DEPTH, D_MODEL, N_IN], F32)
    w_bm = di("w_branch_m", [DEPTH, D_MODEL, D_MODEL], F32)
    w_bd = di("w_branch_d", [DEPTH, D_MODEL, D_MODEL], F32)
    w_out = di("w_out", [DEPTH, D_MODEL, D_MODEL], F32)
    w_g = di("w_ffn_gate", [DEPTH, D_MODEL, D_FF], F32)
    w_u = di("w_ffn_up", [DEPTH, D_MODEL, D_FF], F32)
    w_d = di("w_ffn_down", [DEPTH, D_FF, D_MODEL], F32)
    anw = di("anw", [DEPTH, 128, KC], F32)
    fnw2 = di("fnw2", [DEPTH, 128, KC], F32)
    fnw = di("fnw", [128, D_MODEL], F32)
    cw = di("cw", [DEPTH, 2, 128, 2 * NH, 5], F32)
    gb = di("gb", [DEPTH, 2, 128, 2], F32)
    mnw = di("mnw", [DEPTH, 128, D_MODEL], F32)
    dnw = di("dnw", [DEPTH, 128, D_MODEL], F32)
    lam = di("lam", [DEPTH, 128, 4, 128], F32)
    lami = di("lami", [DEPTH, 128, 2], F32)
    ident = di("ident", [128, 128], F32)
    maskneg = di("maskneg", [128, 128], F32)
    y_out = nc.dram_tensor("y", [SEQ, D_MODEL], F32, kind="ExternalOutput").ap()
    sc = lambda n, s, dt: nc.dram_tensor(n, s, dt).ap()
    qk_s = sc("qk_s", [2048, SEQ], F32)
    mv_s = sc("mv_s", [SEQ, 2048], BF16)
    mo_s = sc("mo_s", [SEQ, 2048], BF16)
    gt_s = sc("gt_s", [16, SEQ], F32)
    dqk_s = sc("dqk_s", [4096, SEQ], BF16)
    dv_s = sc("dv_s", [SEQ, 2048], BF16)
    gmd_s = sc("gmd_s", [4096, SEQ], BF16)
    hm_s = sc("hm_s", [2048, SEQ], BF16)
    hd_s = sc("hd_s", [2048, SEQ], BF16)
    x_s = sc("x_s", [SEQ, D_MODEL], F32)
    scr = {"qk": qk_s, "mv": mv_s, "mo": mo_s, "gt": gt_s, "dqk": dqk_s, "dv": dv_s, "gmd": gmd_s}
    wb = {"wbm": sc("wbm_b", [D_MODEL, D_MODEL], BF16), "wbd": sc("wbd_b", [D_MODEL, D_MODEL], BF16),
          "wout": sc("wout_b", [D_MODEL, D_MODEL], BF16), "wg": sc("wg_b", [D_MODEL, D_FF], BF16),
          "wu": sc("wu_b", [D_MODEL, D_FF], BF16), "wd": sc("wd_b", [D_FF, D_MODEL], BF16)}

    def make_cast(l):
        def pre(c):
            srcs = {"wbm": w_bm[l], "wbd": w_bd[l], "wout": w_out[l], "wg": w_g[l], "wu": w_u[l], "wd": w_d[l]}
            for k in ("wbm", "wbd", "wout", "wg", "wu", "wd"):
                src, dst = srcs[k], wb[k]
                if k in ("wg", "wu"):
                    src = src.rearrange("r (a b) -> r a b", b=1408)
                    dst = dst.rearrange("r (a b) -> r a b", b=1408)
                c.dma(POOL, dst, src, [], [Buf("cast_" + k)])
        return pre

    with contextlib.ExitStack() as es:
        c = Ctx(nc, es)
        c.alloc_banks(8)
        for l in range(depth):
            x_src = x_in if l == 0 else x_s
            final = (l == depth - 1)
            for th in range(2):
                ts = slice(th * TOK, (th + 1) * TOK)
                outs = {}
                for name, c0, n, mode, dt, sg in A_SECTIONS:
                    outs[name] = scr[name][:, ts] if mode == "F" else scr[name][ts, :]
                phase_A(c, x_src[ts, :], anw[l], w_in[l], ident, outs)
            for hh in range(2):
                hs = slice(hh * NH * 256, (hh + 1) * NH * 256)
                d = {
                    "cw": cw[l, hh], "gb": gb[l, hh], "mnw": mnw[l][:, hs], "dnw": dnw[l][:, hs],
                    "lam": lam[l], "lami": lami[l], "ident": ident, "maskneg": maskneg,
                    "mv": mv_s[:, hs], "mo": mo_s[:, hs], "dv": dv_s[:, hs],
                    "gi": gt_s[hh * NH:(hh + 1) * NH, :], "gf": gt_s[8 + hh * NH:8 + (hh + 1) * NH, :],
                    "hmT": hm_s[hs, :], "hdT": hd_s[hs, :],
                    "qraw": (lambda j, hh=hh: qk_s[(hh * NH + j) * 128:(hh * NH + j + 1) * 128, :]),
                    "kraw": (lambda j, hh=hh: qk_s[1024 + (hh * NH + j) * 128:1024 + (hh * NH + j + 1) * 128, :]),
                    "dq": (lambda j, cc, hh=hh: dqk_s[(hh * NH + j) * 256 + cc * 128:(hh * NH + j) * 256 + (cc + 1) * 128, :]),
                    "dk": (lambda j, cc, hh=hh: dqk_s[2048 + (hh * NH + j) * 256 + cc * 128:
                                                      2048 + (hh * NH + j) * 256 + (cc + 1) * 128, :]),
                }
                if hh == 0:
                    d["pre"] = make_cast(l)
                phase_BC(c, d)
            for th in range(2):
                ts = slice(th * TOK, (th + 1) * TOK)
                d = {"x": x_src[ts, :], "hmT": hm_s[:, ts], "hdT": hd_s[:, ts], "gmd": gmd_s[:, ts],
                     "xo": (y_out if final else x_s)[ts, :],
                     "wbm": wb["wbm"], "wbd": wb["wbd"], "wout": wb["wout"], "wg": wb["wg"], "wu": wb["wu"],
                     "wd": wb["wd"],
                     "nw2": fnw2[l], "ident": ident, "fnw": fnw}
                phase_D(c, d, final, last=(l == depth - 1 and th == 1))
        build_fused.stats = (c.P.n_total, dict(c.P.cnt))
    return nc


def host_params(prm):
    L = DEPTH
    anw = np.stack([nw_layout(prm["attn_norm_w"][l]) for l in range(L)])
    fnw2 = np.stack([nw_layout(prm["ffn_norm_w"][l]) for l in range(L)])
    fnw = np.ascontiguousarray(np.broadcast_to(prm["final_norm_w"][None], (128, D_MODEL))).astype(np.float32)
    cw = np.zeros((L, 2, 128, 2 * NH, 5), np.float32)
    gb = np.zeros((L, 2, 128, 2), np.float32)
    for l in range(L):
        cwl, cbl = prm["conv_w"][l], prm["conv_b"][l]
        for hh in range(2):
            for i in range(NH):
                h = hh * NH + i
                cw[l, hh, :, i, 0:4] = cwl[:, h * 128:(h + 1) * 128].T
                cw[l, hh, :, i, 4] = cbl[h * 128:(h + 1) * 128]
                cw[l, hh, :, NH + i, 0:4] = cwl[:, 1024 + h * 128:1024 + (h + 1) * 128].T
                cw[l, hh, :, NH + i, 4] = cbl[1024 + h * 128:1024 + (h + 1) * 128]
            heads = slice(hh * NH, (hh + 1) * NH)
            gb[l, hh, :, 0] = np.repeat(prm["b_igate"][l][heads], NCH)
            gb[l, hh, :, 1] = np.repeat(prm["b_fgate"][l][heads], NCH)
    mnw = np.ascontiguousarray(np.broadcast_to(prm["mlstm_norm_w"][:, None, :], (L, 128, D_MODEL))).astype(np.float32)
    dnw = np.ascontiguousarray(np.broadcast_to(prm["diff_norm_w"][:, None, :], (L, 128, D_MODEL))).astype(np.float32)
    lam = np.stack([np.stack([prm["lambda_q1"][l], prm["lambda_k1"][l], prm["lambda_q2"][l], prm["lambda_k2"][l]])
                    for l in range(L)])
    lam = np.ascontiguousarray(np.broadcast_to(lam[:, None], (L, 128, 4, 128))).astype(np.float32)
    lami = np.zeros((L, 128, 2), np.float32)
    for l in range(L):
        lami[l, :, 0] = lam_init_of(l)
        lami[l, :, 1] = 1.0 - lam_init_of(l)
    return {"anw": anw, "fnw2": fnw2, "fnw": fnw, "cw": cw, "gb": gb, "mnw": mnw, "dnw": dnw, "lam": lam,
            "lami": lami, "ident": IDENT, "maskneg": MASKNEG_NP}


_NC = {}


def kernel(**inputs):
    x = np.ascontiguousarray(inputs["x"], dtype=np.float32)
    prm = {k: np.asarray(v, dtype=np.float32) for k, v in inputs.items() if k != "x"}
    if "nc" not in _NC:
        _NC["nc"] = build_fused()
    hp = host_params(prm)
    big = {k: np.ascontiguousarray(prm[k]) for k in ("w_in", "w_branch_m", "w_branch_d", "w_out",
                                                     "w_ffn_gate", "w_ffn_up", "w_ffn_down")}
    maps = []
    for b in range(NUSED):
        m = {"x": np.ascontiguousarray(x[b])}
        m.update(big)
        m.update(hp)
        maps.append(m)
    res = run_bass_kernel_spmd(_NC["nc"], maps, core_ids=list(range(NUSED)))
    return np.stack([np.asarray(res.results[b]["y"]) for b in range(NUSED)]).astype(np.float32)
```

```python
import contextlib
import math
import numpy as np
import ml_dtypes
import concourse.bass as bass
import concourse.mybir as mybir
from concourse.bass_utils import run_bass_kernel_spmd

F32 = mybir.dt.float32
BF16 = mybir.dt.bfloat16
AF = mybir.ActivationFunctionType
ALU = mybir.AluOpType
AX = mybir.AxisListType
NPBF = ml_dtypes.bfloat16

D_MODEL = 2048
BATCH = 4
SEQ = 4096
DEPTH = 4
NCORES = 8
TOK = 2048
D_FF = 5632
N_IN = 16400
EPS = 1e-6
KC = D_MODEL // 128

PE, ACT, DVE, POOL, SP = "pe", "act", "dve", "pool", "sp"
COMPUTE = (PE, ACT, DVE, POOL)


class Buf:
    __slots__ = ("name", "last_write", "reads", "dma_sem")

    def __init__(self, name):
        self.name = name
        self.last_write = None
        self.reads = {}
        self.dma_sem = None


class Instr:
    __slots__ = ("eng", "fn", "deps", "is_dma", "sem_key", "sig_val", "needs_sig")

    def __init__(self, eng, fn, is_dma, sem_key):
        self.eng = eng
        self.fn = fn
        self.deps = []
        self.is_dma = is_dma
        self.sem_key = sem_key
        self.sig_val = None
        self.needs_sig = False


class Prog:
    NDMA = 72

    def __init__(self, nc, es):
        self.nc = nc
        self.esem = {e: es.enter_context(nc.semaphore("s_" + e)) for e in COMPUTE}
        self.dsem = [es.enter_context(nc.semaphore("d_%d" % i)) for i in range(self.NDMA)]
        self.cnt = {e: 0 for e in COMPUTE}
        self.dcnt = [0] * self.NDMA
        self.barrier = []
        self.n_total = 0
        self._reset()

    def _reset(self):
        self.q = {e: [] for e in (PE, ACT, DVE, POOL, SP)}
        self.started = set()

    def op(self, eng, fn, reads=(), writes=(), dma=False, sem_key=None, pe_accum=False):
        if dma and sem_key is None:
            sem_key = writes[0] if len(writes) else reads[0]
        ins = Instr(eng, fn, dma, sem_key)
        deps = []
        if eng not in self.started:
            self.started.add(eng)
            ins.deps.extend(self.barrier)
        for b in reads:
            if b.last_write is not None:
                deps.append(b.last_write)
        for b in writes:
            if b.last_write is not None:
                deps.append(b.last_write)
            deps.extend(b.reads.values())
        seen = set()
        for d in deps:
            if d is ins or id(d) in seen:
                continue
            seen.add(id(d))
            if (not d.is_dma) and d.eng == PE and eng == PE and not dma:
                continue
            ins.deps.append(d)
            d.needs_sig = True
        for b in reads:
            b.reads[("d", id(sem_key)) if dma else eng] = ins
        for b in writes:
            b.last_write = ins
            b.reads = {}
        self.q[eng].append(ins)
        return ins

    def end_phase(self, last=False):
        nc = self.nc
        bar = {}
        for e in self.q:
            if self.q[e]:
                bar[id(self.q[e][-1])] = self.q[e][-1]
            for ins in self.q[e]:
                if ins.is_dma:
                    bar["k%d" % id(ins.sem_key)] = ins
        barrier = []
        seen = set()
        for ins in bar.values():
            if id(ins) not in seen:
                seen.add(id(ins))
                barrier.append(ins)
                ins.needs_sig = True
        nkeys = 0
        for e in self.q:
            for ins in self.q[e]:
                if ins.is_dma and ins.needs_sig and ins.sem_key.dma_sem is None:
                    ins.sem_key.dma_sem = nkeys
                    nkeys += 1
        assert nkeys <= self.NDMA, nkeys
        for e in self.q:
            for ins in self.q[e]:
                self.n_total += 1
                if not ins.needs_sig:
                    continue
                if ins.is_dma:
                    k = ins.sem_key.dma_sem
                    self.dcnt[k] += 16
                    ins.sig_val = (self.dsem[k], self.dcnt[k], 16)
                else:
                    self.cnt[ins.eng] += 1
                    ins.sig_val = (self.esem[ins.eng], self.cnt[ins.eng], 1)
        q = self.q
        with nc.Block() as block:
            def run(e, eng_obj):
                waited = {}
                for ins in q[e]:
                    for d in ins.deps:
                        sem, val, _ = d.sig_val
                        key = id(sem)
                        if waited.get(key, 0) >= val:
                            continue
                        waited[key] = val
                        eng_obj.wait_ge(sem, val)
                    bi = ins.fn(eng_obj)
                    if ins.needs_sig:
                        sem, val, inc = ins.sig_val
                        bi.then_inc(sem, inc)
                if e == SP and last:
                    for ins in barrier:
                        sem, val, _ = ins.sig_val
                        if waited.get(id(sem), 0) >= val:
                            continue
                        waited[id(sem)] = val
                        eng_obj.wait_ge(sem, val)

            if q[PE]:
                @block.tensor
                def _(eng):
                    run(PE, eng)
            if q[ACT]:
                @block.scalar
                def _(eng):
                    run(ACT, eng)
            if q[DVE]:
                @block.vector
                def _(eng):
                    run(DVE, eng)
            if q[POOL]:
                @block.gpsimd
                def _(eng):
                    run(POOL, eng)
            if q[SP] or last:
                @block.sync
                def _(eng):
                    run(SP, eng)
        for ins in barrier:
            ins.fn = None
        for e in q:
            for ins in q[e]:
                ins.fn = None
                ins.deps = None
        self.barrier = barrier
        self._reset()


class Ctx:
    def __init__(self, nc, es):
        self.nc = nc
        self.ges = es
        self.es = None
        self.P = Prog(nc, es)
        self.uid = 0
        self.banks = []
        self.bank_bufs = []
        self.bank_i = 0
        self.evac_i = 0

    def sb(self, name, shape, dt):
        self.uid += 1
        return self.es.enter_context(self.nc.sbuf_tensor("%s_%d" % (name, self.uid), list(shape), dt))

    def begin(self):
        self.es = contextlib.ExitStack()
        self.es.__enter__()
        self.bank_bufs = [Buf("bank%d" % i) for i in range(len(self.banks))]

    def end(self, last=False):
        self.P.end_phase(last)
        self.es.__exit__(None, None, None)
        self.es = None

    def alloc_banks(self, n=8):
        for i in range(n):
            self.banks.append(self.ges.enter_context(self.nc.psum_tensor("bank%d" % i, [128, 512], F32)))
            self.bank_bufs.append(Buf("bank%d" % i))

    def next_bank(self):
        i = self.bank_i % len(self.banks)
        self.bank_i += 1
        return self.banks[i], self.bank_bufs[i]

    def mm(self, out, lhsT, rhs, start, stop, reads, writes):
        return self.P.op(PE, lambda e: e.matmul(out, lhsT, rhs, start=start, stop=stop),
                         reads=reads, writes=writes)

    def act(self, out, in_, func, reads, writes, bias=None, scale=None, accum_out=None):
        kw = {}
        if bias is not None:
            kw["bias"] = bias
        if scale is not None:
            kw["scale"] = scale
        if accum_out is not None:
            kw["accum_out"] = accum_out
        return self.P.op(ACT, lambda e: e.activation(out=out, in_=in_, func=func, **kw),
                         reads=reads, writes=writes)

    def dma(self, eng, out, in_, reads, writes, sem_key=None):
        return self.P.op(eng, lambda e: e.dma_start(out=out, in_=in_), reads=reads, writes=writes,
                         dma=True, sem_key=sem_key)

    def tt(self, eng, out, in0, in1, op, reads, writes):
        return self.P.op(eng, lambda e: e.tensor_tensor(out=out, in0=in0, in1=in1, op=op),
                         reads=reads, writes=writes)

    def ts(self, eng, out, in0, s1, s2, op0, op1, reads, writes, accum_out=None):
        if op1 is None:
            return self.P.op(eng, lambda e: e.tensor_scalar(out=out, in0=in0, scalar1=s1, scalar2=None, op0=op0),
                             reads=reads, writes=writes)
        if accum_out is not None:
            return self.P.op(eng, lambda e: e.tensor_scalar(out=out, in0=in0, scalar1=s1, scalar2=s2, op0=op0,
                                                            op1=op1, accum_out=accum_out),
                             reads=reads, writes=writes)
        return self.P.op(eng, lambda e: e.tensor_scalar(out=out, in0=in0, scalar1=s1, scalar2=s2, op0=op0, op1=op1),
                         reads=reads, writes=writes)

    def stt(self, out, in0, scalar, in1, op0, op1, reads, writes):
        return self.P.op(DVE, lambda e: e.scalar_tensor_tensor(out=out, in0=in0, scalar=scalar, in1=in1,
                                                               op0=op0, op1=op1),
                         reads=reads, writes=writes)

    def copy(self, eng, out, in_, reads, writes):
        if eng == ACT:
            return self.P.op(ACT, lambda e: e.copy(out=out, in_=in_), reads=reads, writes=writes)
        return self.P.op(eng, lambda e: e.tensor_copy(out=out, in_=in_), reads=reads, writes=writes)

    def evac_copy(self, out, in_, reads, writes):
        self.evac_i += 1
        return self.copy(ACT if self.evac_i % 2 else DVE, out, in_, reads, writes)


def rmsnorm_to_T(c, xt, xbuf, scratch, hT, hT_buf, tok0, nw, nw_buf, ident, ident_buf, pfx):
    junk, junk_b = scratch["junk"]
    ss, ss_b = scratch["ss"]
    hn, hn_b = scratch["hn"]
    c.act(junk[:], xt[:], AF.Square, reads=[xbuf], writes=[junk_b, ss_b], accum_out=ss[:, 0:1])
    c.act(ss[:, 1:2], ss[:, 0:1], AF.Sqrt, reads=[ss_b, scratch["eps_b"]], writes=[ss_b], scale=1.0 / D_MODEL, bias=scratch["eps"][:, 0:1])
    c.P.op(DVE, lambda e: e.reciprocal(out=ss[:, 2:3], in_=ss[:, 1:2]), reads=[ss_b], writes=[ss_b])
    c.act(hn[:, 0:1024], xt[:, 0:1024], AF.Copy, reads=[xbuf, ss_b], writes=[hn_b], scale=ss[:, 2:3])
    c.ts(DVE, hn[:, 1024:2048], xt[:, 1024:2048], ss[:, 2:3], None, ALU.mult, None, reads=[xbuf, ss_b], writes=[hn_b])
    for kq in range(KC // 4):
        bank, bb = c.next_bank()
        for j in range(4):
            kc = kq * 4 + j
            c.mm(bank[:, j * 128:(j + 1) * 128], hn[:, kc * 128:(kc + 1) * 128], ident[:], True, True,
                 reads=[hn_b, ident_buf], writes=[bb])
        for j in range(4):
            kc = kq * 4 + j
            if j % 2 == 0:
                c.act(hT[:, kc, tok0:tok0 + 128], bank[:, j * 128:(j + 1) * 128], AF.Copy,
                      reads=[bb, nw_buf], writes=[hT_buf], scale=nw[:, kc:kc + 1])
            else:
                c.ts(DVE, hT[:, kc, tok0:tok0 + 128], bank[:, j * 128:(j + 1) * 128], nw[:, kc:kc + 1], None,
                     ALU.mult, None, reads=[bb, nw_buf], writes=[hT_buf])


def norm_scratch(c, pfx):
    eps = c.sb(pfx + "eps", [128, 1], F32)
    eb = Buf(pfx + "eps")
    c.P.op(POOL, lambda e: e.memset(eps[:], EPS), writes=[eb])
    return {
        "junk": (c.sb(pfx + "junk", [128, 2048], BF16), Buf(pfx + "junk")),
        "ss": (c.sb(pfx + "ss", [128, 4], F32), Buf(pfx + "ss")),
        "hn": (c.sb(pfx + "hn", [128, 2048], BF16), Buf(pfx + "hn")),
        "eps": eps, "eps_b": eb,
    }


A_SECTIONS = [
    ("qk", 0, 2048, "F", F32, False),
    ("mv", 2048, 2048, "T", BF16, False),
    ("mo", 4096, 2048, "T", BF16, True),
    ("gt", 6144, 16, "F", F32, False),
    ("dqk", 6160, 4096, "F", BF16, False),
    ("dv", 10256, 2048, "T", BF16, False),
    ("gmd", 12304, 4096, "F", BF16, True),
]


def load_wblock(c, wslot, wbuf, w_dram, row0, nkc, col0, ncols):
    src = w_dram[row0:row0 + nkc * 128, col0:col0 + ncols].rearrange("(kc p) c -> p kc c", p=128)
    c.dma(POOL, wslot[:, 0:nkc, 0:ncols], src, reads=[], writes=[wbuf])


def phase_A(c, x, nw_d, w, ident_d, outs):
    if True:
        c.begin()
        hT = c.sb("hT", [128, KC, TOK], BF16)
        hT_b = Buf("hT")
        nw = c.sb("nw_s", [128, KC], F32)
        nw_b = Buf("nw")
        ident = c.sb("ident_s", [128, 128], BF16)
        ident_b = Buf("ident")
        c.dma(SP, nw[:], nw_d[:, :], [], [nw_b])
        c.dma(POOL, ident[:], ident_d[:, :], [], [ident_b])
        NW = 3
        wslots = [(c.sb("w%d" % i, [128, KC, 512], BF16), Buf("w%d" % i)) for i in range(NW)]
        xs = [(c.sb("x%d" % i, [128, D_MODEL], F32), Buf("x%d" % i)) for i in range(2)]
        scr = norm_scratch(c, "n_")
        of32 = [(c.sb("of%d" % i, [128, TOK], F32), Buf("of%d" % i)) for i in range(2)]
        obf = [(c.sb("ob%d" % i, [128, TOK], BF16), Buf("ob%d" % i)) for i in range(2)]
        otk = [(c.sb("ot%d" % i, [128, 512], BF16), Buf("ot%d" % i)) for i in range(3)]

        blocks = []
        for name, c0, n, mode, dt, sg in A_SECTIONS:
            for o in range(0, n, 512):
                blocks.append((name, c0, o, min(512, n - o), mode, dt, sg))
        PRE = NW - 1

        def issue_load(bi):
            name, c0, o, n, mode, dt, sg = blocks[bi]
            ws, wb = wslots[bi % NW]
            load_wblock(c, ws, wb, w, 0, KC, c0 + o, n)

        for bi in range(min(PRE, len(blocks))):
            issue_load(bi)

        for tt in range(TOK // 128):
            xt, xb = xs[tt % 2]
            c.dma(SP, xt[:], x[tt * 128:(tt + 1) * 128, :], [], [xb])
            rmsnorm_to_T(c, xt, xb, scr, hT, hT_b, tt * 128, nw, nw_b, ident, ident_b, "n_")

        finals = []
        cnt = {"f": 0, "b": 0, "t": 0}
        for bi, (name, c0, o, n, mode, dt, sg) in enumerate(blocks):
            if bi + PRE < len(blocks):
                issue_load(bi + PRE)
            ws, wb = wslots[bi % NW]
            od = outs[name]
            if mode == "F":
                for cc in range(0, n, 128):
                    m = min(128, n - cc)
                    if dt == F32:
                        ot, ob = of32[cnt["f"] % 2]
                        cnt["f"] += 1
                    else:
                        ot, ob = obf[cnt["b"] % 2]
                        cnt["b"] += 1
                    for tg in range(TOK // 512):
                        bank, bb = c.next_bank()
                        for kc in range(KC):
                            c.mm(bank[0:m, :], ws[:, kc, cc:cc + m], hT[:, kc, tg * 512:(tg + 1) * 512],
                                 kc == 0, kc == KC - 1, reads=[wb, hT_b], writes=[bb])
                        dst = ot[0:m, tg * 512:(tg + 1) * 512]
                        if sg:
                            c.act(dst, bank[0:m, :], AF.Sigmoid, reads=[bb], writes=[ob])
                        else:
                            c.evac_copy(dst, bank[0:m, :], reads=[bb], writes=[ob])
                    finals.append(c.dma(SP, od[o + cc:o + cc + m, :], ot[0:m, :], [ob], []))
            else:
                for tt in range(TOK // 128):
                    bank, bb = c.next_bank()
                    for kc in range(KC):
                        c.mm(bank[:, 0:n], hT[:, kc, tt * 128:(tt + 1) * 128], ws[:, kc, 0:n],
                             kc == 0, kc == KC - 1, reads=[wb, hT_b], writes=[bb])
                    ot, ob = otk[cnt["t"] % 3]
                    cnt["t"] += 1
                    if sg:
                        c.act(ot[:, 0:n], bank[:, 0:n], AF.Sigmoid, reads=[bb], writes=[ob])
                    else:
                        c.evac_copy(ot[:, 0:n], bank[:, 0:n], reads=[bb], writes=[ob])
                    finals.append(c.dma(SP, od[tt * 128:(tt + 1) * 128, o:o + n], ot[:, 0:n], [ob], []))
        c.end()


def nw_layout(v):
    return np.ascontiguousarray(v.reshape(KC, 128).T)


NH = 4
NCH = SEQ // 128
MASKNEG = -30000.0
Q_R, Q_C, Q_INTER, Q_W, Q_EM = 0, 1, 2, 3, 4
NSB = 3
MSKEW = 1


def phase_BC(c, d, do_m=True, do_a=True):
    cw_d, mv_d, mo_d, gb_d, mnw_d, dv_d, dnw_d = d["cw"], d["mv"], d["mo"], d["gb"], d["mnw"], d["dv"], d["dnw"]
    lam_d, lami_d, ident_d, mask_d, hmT_d, hdT_d = d["lam"], d["lami"], d["ident"], d["maskneg"], d["hmT"], d["hdT"]
    gi_d, gf_d = d["gi"], d["gf"]
    if True:
        c.begin()
        if "pre" in d:
            d["pre"](c)
        P = c.P
        identf = c.sb("identf", [128, 128], F32); identf_b = Buf("identf")
        identb = c.sb("identb", [128, 128], BF16); identb_b = Buf("identb")
        maskf = c.sb("maskf", [128, 128], F32); maskf_b = Buf("maskf")
        maskb = c.sb("maskb", [128, 128], BF16); maskb_b = Buf("maskb")
        onesf = c.sb("onesf", [128, 128], F32); onesf_b = Buf("onesf")
        onesb = c.sb("onesb", [128, 128], BF16); onesb_b = Buf("onesb")
        epst = c.sb("epst", [128, 1], F32); eps_b = Buf("epst")
        c.dma(SP, identf[:], ident_d[:, :], [], [identf_b])
        c.dma(POOL, identb[:], ident_d[:, :], [], [identb_b])
        c.dma(SP, maskf[:], mask_d[:, :], [], [maskf_b])
        c.dma(POOL, maskb[:], mask_d[:, :], [], [maskb_b])
        P.op(POOL, lambda e: e.memset(onesf[:], 1.0), writes=[onesf_b])
        P.op(POOL, lambda e: e.memset(onesb[:], 1.0), writes=[onesb_b])
        P.op(POOL, lambda e: e.memset(epst[:], EPS), writes=[eps_b])
        cw = c.sb("cw_s", [128, 2 * NH, 5], F32); cw_b = Buf("cw")
        c.dma(SP, cw[:], cw_d, [], [cw_b])
        gb = c.sb("gb_s", [128, 2], F32); gb_b = Buf("gb")
        c.dma(SP, gb[:], gb_d, [], [gb_b])
        mnw = c.sb("mnw_s", [128, NH * 256], F32); mnw_b = Buf("mnw")
        c.dma(SP, mnw[:], mnw_d, [], [mnw_b])
        dnw = c.sb("dnw_s", [128, NH * 256], F32); dnw_b = Buf("dnw")
        c.dma(SP, dnw[:], dnw_d, [], [dnw_b])
        lamt = c.sb("lamt", [128, 4, 128], F32); lamt_b = Buf("lamt")
        c.dma(SP, lamt[:], lam_d, [], [lamt_b])
        lami = c.sb("lami_s", [128, 2], F32); lami_b = Buf("lami")
        c.dma(SP, lami[:], lami_d, [], [lami_b])

        g_i = c.sb("g_i", [128, 128], F32); g_f = c.sb("g_f", [128, 128], F32)
        gi_b, gf_b = Buf("g_i"), Buf("g_f")
        c.dma(SP, g_i[:], gi_d.rearrange("j (ci l) -> (j ci) l", l=128), [], [gi_b])
        c.dma(SP, g_f[:], gf_d.rearrange("j (ci l) -> (j ci) l", l=128), [], [gf_b])
        sm = c.sb("gsm", [128, 16], F32); sm_b = Buf("gsm")
        NBF, MPREV, DEC, RLAST = 0, 1, 2, 3
        gq = c.sb("gq", [128, 5, 128], F32); gq_b = Buf("gq")
        g_b = c.sb("g_bb", [128, 128], F32); gbb_b = Buf("g_bb")
        g_ml = c.sb("g_ml", [128, 128], F32); gml_b = Buf("g_ml")
        g_m = c.sb("g_m", [128, 128], F32); gm_b = Buf("g_m")
        c.ts(DVE, g_i[:], g_i[:], gb[:, 0:1], None, ALU.add, None, [gi_b, gb_b], [gi_b])
        c.ts(DVE, sm[:, NBF:NBF + 1], gb[:, 1:2], -1.0, None, ALU.mult, None, [gb_b], [sm_b])
        c.act(g_f[:], g_f[:], AF.Exp, [gf_b, sm_b], [gf_b], bias=sm[:, NBF:NBF + 1], scale=-1.0)
        c.act(g_f[:], g_f[:], AF.Ln, [gf_b, onesf_b], [gf_b], bias=onesf[:, 0:1], scale=1.0)
        c.ts(DVE, g_f[:], g_f[:], -1.0, None, ALU.mult, None, [gf_b], [gf_b])
        P.op(DVE, lambda e: e.tensor_tensor_scan(out=g_b[:], data0=g_f[:], data1=g_f[:], initial=0.0,
                                                 op0=ALU.add, op1=ALU.min), reads=[gf_b], writes=[gbb_b])
        P.op(DVE, lambda e: e.tensor_tensor_scan(out=g_ml[:], data0=g_f[:], data1=g_i[:], initial=-1e30,
                                                 op0=ALU.add, op1=ALU.max), reads=[gf_b, gi_b], writes=[gml_b])
        bankr, bankr_b = c.next_bank()
        c.mm(bankr[0:1, 0:128], g_b[:, 127:128], identf[:], True, True, [gbb_b, identf_b], [bankr_b])
        c.mm(bankr[0:1, 128:256], g_ml[:, 127:128], identf[:], True, True, [gml_b, identf_b], [bankr_b])
        erow = c.sb("erow", [1, 512], F32); erow_b = Buf("erow")
        c.copy(DVE, erow[0:1, 0:256], bankr[0:1, 0:256], [bankr_b], [erow_b])
        P.op(POOL, lambda e: e.memset(erow[0:1, 256:512], 0.0), writes=[erow_b])
        for j in range(NH):
            P.op(DVE, lambda e, j=j: e.tensor_tensor_scan(
                out=erow[0:1, 384 + j * 32:384 + (j + 1) * 32], data0=erow[0:1, j * 32:(j + 1) * 32],
                data1=erow[0:1, 128 + j * 32:128 + (j + 1) * 32], initial=0.0, op0=ALU.add, op1=ALU.max),
                reads=[erow_b], writes=[erow_b])
            c.copy(DVE, erow[0:1, 256 + j * 32 + 1:256 + (j + 1) * 32], erow[0:1, 384 + j * 32:384 + (j + 1) * 32 - 1],
                   [erow_b], [erow_b])
        c.mm(bankr[:, 256:257], erow[0:1, 256:384], onesf[0:1, 0:1], True, True, [erow_b, onesf_b], [bankr_b])
        c.copy(DVE, sm[:, MPREV:MPREV + 1], bankr[:, 256:257], [bankr_b], [sm_b])
        c.stt(g_m[:], g_b[:], sm[:, MPREV:MPREV + 1], g_ml[:], ALU.add, ALU.max, [gbb_b, sm_b, gml_b], [gm_b])
        c.tt(DVE, gq[:, Q_R, :], g_b[:], g_m[:], ALU.subtract, [gbb_b, gm_b], [gq_b])
        c.tt(DVE, gq[:, Q_C, :], g_i[:], g_b[:], ALU.subtract, [gi_b, gbb_b], [gq_b])
        c.copy(DVE, sm[:, RLAST:RLAST + 1], gq[:, Q_R, 127:128], [gq_b], [sm_b])
        c.act(gq[:, Q_INTER, :], gq[:, Q_R, :], AF.Exp, [gq_b, sm_b], [gq_b], bias=sm[:, MPREV:MPREV + 1], scale=1.0)
        c.act(gq[:, Q_W, :], gq[:, Q_C, :], AF.Exp, [gq_b, sm_b], [gq_b], bias=sm[:, RLAST:RLAST + 1], scale=1.0)
        c.act(gq[:, Q_EM, :], g_m[:], AF.Exp, [gm_b], [gq_b], scale=-1.0)
        c.act(sm[:, DEC:DEC + 1], sm[:, RLAST:RLAST + 1], AF.Exp, [sm_b], [sm_b], bias=sm[:, MPREV:MPREV + 1], scale=1.0)
        tq = c.sb("tq", [128, 5, 128], F32); tq_b = Buf("tq")
        for n in range(5):
            bk, bkb = c.next_bank()
            c.mm(bk[:, 0:128], gq[:, n, :], identf[:], True, True, [gq_b, identf_b], [bkb])
            c.copy(DVE, tq[:, n, :], bk[:, 0:128], [bkb], [tq_b])
        decm = c.sb("decm", [128, 128], F32); decm_b = Buf("decm")
        c.ts(DVE, decm[:], onesf[:], sm[:, DEC:DEC + 1], None, ALU.mult, None, [onesf_b, sm_b], [decm_b])
        bk, bkb = c.next_bank()
        c.mm(bk[:, 0:128], decm[:], identf[:], True, True, [decm_b, identf_b], [bkb])
        decb = c.sb("decb", [128, 128], F32); decb_b = Buf("decb")
        c.copy(DVE, decb[:], bk[:, 0:128], [bkb], [decb_b])

        lsm = c.sb("lsm", [128, 8], F32); lsm_b = Buf("lsm")
        lpr = c.sb("lpr", [128, 2, 128], F32); lpr_b = Buf("lpr")
        c.tt(DVE, lpr[:, 0, :], lamt[:, 0, :], lamt[:, 1, :], ALU.mult, [lamt_b], [lpr_b])
        c.tt(DVE, lpr[:, 1, :], lamt[:, 2, :], lamt[:, 3, :], ALU.mult, [lamt_b], [lpr_b])
        P.op(DVE, lambda e: e.reduce_sum(out=lsm[:, 0:2], in_=lpr[:], axis=AX.X), reads=[lpr_b], writes=[lsm_b])
        c.act(lsm[:, 2:4], lsm[:, 0:2], AF.Exp, [lsm_b], [lsm_b])
        c.tt(DVE, lsm[:, 4:5], lsm[:, 2:3], lsm[:, 3:4], ALU.subtract, [lsm_b], [lsm_b])
        c.ts(DVE, lsm[:, 5:6], lsm[:, 4:5], lami[:, 0:1], -1.0, ALU.add, ALU.mult, [lsm_b, lami_b], [lsm_b])
        NLAM = 5

        rawq = c.sb("rawq", [128, SEQ + 3], F32); rawq_b = Buf("rawq")
        rawk = c.sb("rawk", [128, SEQ + 3], F32); rawk_b = Buf("rawk")
        acc = c.sb("acc", [128, SEQ], F32); acc_b = Buf("acc")
        qT = c.sb("qT", [128, SEQ], BF16); qT_b = Buf("qT")
        kT = c.sb("kT", [128, SEQ], BF16); kT_b = Buf("kT")
        va = c.sb("va", [128, NCH, 257], BF16); va_b = Buf("va")
        P.op(POOL, lambda e: e.memset(rawq[:, 0:3], 0.0), writes=[rawq_b])
        P.op(POOL, lambda e: e.memset(rawk[:, 0:3], 0.0), writes=[rawk_b])
        P.op(POOL, lambda e: e.memset(va[:, :, 256:257], 1.0), writes=[va_b])
        CT = c.sb("CT", [128, 257], F32); CT_b = Buf("CT")
        CTb = c.sb("CTb", [128, 257], BF16); CTb_b = Buf("CTb")
        diagR = [(c.sb("diagR%d" % i, [128, 128], F32), Buf("diagR%d" % i)) for i in range(2)]
        Dm = [(c.sb("Dm%d" % i, [128, 128], F32), Buf("Dm%d" % i)) for i in range(2)]
        sdT = [(c.sb("sdT%d" % i, [128, 128], BF16), Buf("sdT%d" % i)) for i in range(2)]
        kw = [(c.sb("kw%d" % i, [128, 128], BF16), Buf("kw%d" % i)) for i in range(2)]
        numS = [(c.sb("numS%d" % i, [128, 257], F32), Buf("numS%d" % i)) for i in range(4)]
        tot = [(c.sb("tot%d" % i, [128, 257], F32), Buf("tot%d" % i)) for i in range(8)]
        CT2 = [(c.sb("CT2_%d" % i, [128, 257], F32), Buf("CT2_%d" % i)) for i in range(2)]
        CTb2 = [(c.sb("CTb2_%d" % i, [128, 257], BF16), Buf("CTb2_%d" % i)) for i in range(2)]
        junk = c.sb("junk", [128, 256], BF16); junk_b = Buf("junk")
        hs = [(c.sb("hs%d" % i, [128, 8], F32), Buf("hs%d" % i)) for i in range(8)]
        g2 = [(c.sb("g2_%d" % i, [128, 256], F32), Buf("g2_%d" % i)) for i in range(4)]
        hmt = [(c.sb("hm%d" % i, [128, 256], BF16), Buf("hm%d" % i)) for i in range(2)]
        mos = [(c.sb("mos%d" % i, [128, 4, 256], BF16), Buf("mos%d" % i)) for i in range(4)]
        hout = [(c.sb("hout%d" % i, [128, 2, 512], BF16), Buf("hout%d" % i)) for i in range(2)]
        kscale = 128.0 ** -0.5

        def conv_silu(raw, raw_b, idx, dst, dst_b, post_scale):
            c.ts(DVE, acc[:], raw[:, 0:SEQ], cw[:, idx, 0:1], cw[:, idx, 4:5], ALU.mult, ALU.add,
                 [raw_b, cw_b], [acc_b])
            for t in range(1, 4):
                c.stt(acc[:], raw[:, t:t + SEQ], cw[:, idx, t:t + 1], acc[:], ALU.mult, ALU.add,
                      [raw_b, cw_b, acc_b], [acc_b])
            if post_scale is None:
                c.act(dst[:], acc[:], AF.Silu, [acc_b], [dst_b])
            else:
                c.act(acc[:], acc[:], AF.Silu, [acc_b], [acc_b])
                c.ts(POOL, dst[:], acc[:], post_scale, None, ALU.mult, None, [acc_b], [dst_b])

        grp = 0
        for j in range(NH if do_m else 0):
            if j == 0:
                c.dma(SP, rawq[:, 3:SEQ + 3], d["qraw"](j), [], [rawq_b])
                c.dma(SP, rawk[:, 3:SEQ + 3], d["kraw"](j), [], [rawk_b])
            c.dma(SP, va[:, :, 0:256], mv_d[:, j * 256:(j + 1) * 256].rearrange("(ci p) v -> p ci v", p=128),
                  [], [va_b])
            conv_silu(rawq, rawq_b, j, qT, qT_b, None)
            conv_silu(rawk, rawk_b, NH + j, kT, kT_b, kscale)
            if j + 1 < NH:
                c.dma(SP, rawq[:, 3:SEQ + 3], d["qraw"](j + 1), [], [rawq_b])
                c.dma(SP, rawk[:, 3:SEQ + 3], d["kraw"](j + 1), [], [rawk_b])
            for k2 in range(2):
                P.op(POOL, lambda e, k2=k2: e.memset(CT2[k2][0][:], 0.0), writes=[CT2[k2][1]])
                P.op(POOL, lambda e, k2=k2: e.memset(CTb2[k2][0][:], 0.0), writes=[CTb2[k2][1]])

            def s0(ci):
                p = j * NCH + ci
                cs = slice(ci * 128, (ci + 1) * 128)
                bA, bA_b = c.banks[ci % 2], c.bank_bufs[ci % 2]
                dR, dR_b = diagR[ci % 2]
                c.ts(POOL, dR[:], identf[:], tq[:, Q_R, p:p + 1], None, ALU.mult, None, [identf_b, tq_b], [dR_b])
                c.mm(bA[:, 0:128], kT[:, cs], qT[:, cs], True, True, [kT_b, qT_b], [bA_b])
                c.mm(bA[:, 128:256], onesf[:], dR[:], True, False, [onesf_b, dR_b], [bA_b])
                c.mm(bA[:, 128:256], identf[:], maskf[:], False, True, [identf_b, maskf_b], [bA_b])
                c.mm(bA[:, 256:384], kT[:, cs], identb[:], True, True, [kT_b, identb_b], [bA_b])
                if ci % 4 == 0:
                    mo_t, mo_b = mos[(ci // 4) % 4]
                    c.dma(SP, mo_t[:], mo_d[ci * 128:(ci + 4) * 128, j * 256:(j + 1) * 256]
                          .rearrange("(cc p) v -> p cc v", p=128), [], [mo_b])

            def s1(ci):
                p = j * NCH + ci
                bA, bA_b = c.banks[ci % 2], c.bank_bufs[ci % 2]
                dm, dm_b = Dm[ci % 2]
                c.act(dm[:], bA[:, 128:256], AF.Exp, [bA_b, tq_b], [dm_b], bias=tq[:, Q_C, p:p + 1], scale=1.0)

            def s2(ci):
                p = j * NCH + ci
                bA, bA_b = c.banks[ci % 2], c.bank_bufs[ci % 2]
                dm, dm_b = Dm[ci % 2]
                sd, sd_b = sdT[ci % 2]
                c.tt(DVE, sd[:], bA[:, 0:128], dm[:], ALU.mult, [bA_b, dm_b], [sd_b])
                kwt, kw_b = kw[ci % 2]
                c.ts(DVE, kwt[:], bA[:, 256:384], tq[:, Q_W, p:p + 1], None, ALU.mult, None, [bA_b, tq_b], [kw_b])

            def s3(ci):
                bB, bB_b = c.banks[2 + ci % 2], c.bank_bufs[2 + ci % 2]
                bD, bD_b = c.banks[6], c.bank_bufs[6]
                sd, sd_b = sdT[ci % 2]
                kwt, kw_b = kw[ci % 2]
                c.mm(bB[:, 0:257], sd[:], va[:, ci, :], True, True, [sd_b, va_b], [bB_b])
                c.mm(bD[:, 0:257], kwt[:], va[:, ci, :], True, True, [kw_b, va_b], [bD_b])

            def s4(ci):
                p = j * NCH + ci
                bB, bB_b = c.banks[2 + ci % 2], c.bank_bufs[2 + ci % 2]
                bD, bD_b = c.banks[6], c.bank_bufs[6]
                ns, ns_b = numS[ci % 4]
                c.copy(ACT, ns[:], bB[:, 0:257], [bB_b], [ns_b])
                cn, cn_b = CT2[ci % 2]
                cp, cp_b = CT2[(ci + 1) % 2]
                c.stt(cn[:], cp[:], decb[:, p:p + 1], bD[:, 0:257], ALU.mult, ALU.add, [cp_b, decb_b, bD_b], [cn_b])

            def s5(ci):
                cs = slice(ci * 128, (ci + 1) * 128)
                cn, cn_b = CT2[ci % 2]
                cb, cb_b = CTb2[ci % 2]
                c.copy(POOL, cb[:], cn[:], [cn_b], [cb_b])
                cbp, cbp_b = CTb2[(ci + 1) % 2]
                bC, bC_b = c.banks[4 + ci % 2], c.bank_bufs[4 + ci % 2]
                c.mm(bC[:, 0:257], qT[:, cs], cbp[:], True, True, [qT_b, cbp_b], [bC_b])

            def s6(ci):
                p = j * NCH + ci
                bC, bC_b = c.banks[4 + ci % 2], c.bank_bufs[4 + ci % 2]
                ns, ns_b = numS[ci % 4]
                tt_, tt_b = tot[ci % 8]
                c.stt(tt_[:], bC[:, 0:257], tq[:, Q_INTER, p:p + 1], ns[:], ALU.mult, ALU.add,
                      [bC_b, tq_b, ns_b], [tt_b])

            def s7(ci):
                tt_, tt_b = tot[ci % 8]
                h_, h_b = hs[ci % 8]
                c.act(h_[:, 7:8], tt_[:, 256:257], AF.Abs, [tt_b], [h_b])
                c.act(junk[:], tt_[:, 0:256], AF.Square, [tt_b], [junk_b, h_b], accum_out=h_[:, 2:3])

            def s8(ci):
                p = j * NCH + ci
                h_, h_b = hs[ci % 8]
                c.ts(DVE, h_[:, 0:1], h_[:, 7:8], tq[:, Q_EM, p:p + 1], None, ALU.max, None, [h_b, tq_b], [h_b])
                mo_t, mo_b = mos[(ci // 4) % 4]
                g2t, g2_b = g2[ci % 4]
                c.tt(POOL, g2t[:], mnw[:, j * 256:(j + 1) * 256], mo_t[:, ci % 4, :], ALU.mult, [mnw_b, mo_b], [g2_b])

            def s9(ci):
                h_, h_b = hs[ci % 8]
                c.act(h_[:, 1:2], h_[:, 0:1], AF.Square, [h_b], [h_b], scale=EPS ** 0.5)
                c.act(h_[:, 4:5], h_[:, 2:3], AF.Sqrt, [h_b], [h_b], bias=h_[:, 1:2], scale=1.0 / 256)

            def s10(ci):
                h_, h_b = hs[ci % 8]
                tt_, tt_b = tot[ci % 8]
                g2t, g2_b = g2[ci % 4]
                P.op(DVE, lambda e, h_=h_: e.reciprocal(out=h_[:, 6:7], in_=h_[:, 4:5]), reads=[h_b], writes=[h_b])
                hm_, hm_b = hmt[ci % 2]
                c.stt(hm_[:], tt_[:, 0:256], h_[:, 6:7], g2t[:], ALU.mult, ALU.mult, [tt_b, h_b, g2_b], [hm_b])

            def s11(ci):
                bT, bT_b = c.banks[7], c.bank_bufs[7]
                hm_, hm_b = hmt[ci % 2]
                for vc in range(2):
                    c.mm(bT[:, vc * 128:(vc + 1) * 128], hm_[:, vc * 128:(vc + 1) * 128], identb[:], True, True,
                         [hm_b, identb_b], [bT_b])

            def s12(ci):
                bT, bT_b = c.banks[7], c.bank_bufs[7]
                ho_t, ho_b = hout[(ci // 4) % 2]
                c.copy(ACT, ho_t[:, :, (ci % 4) * 128:(ci % 4 + 1) * 128],
                       bT[:, 0:256].rearrange("p (a b) -> p a b", a=2), [bT_b], [ho_b])
                if ci % 4 == 3:
                    c.dma(SP, hmT_d[j * 256:(j + 1) * 256, (ci - 3) * 128:(ci + 1) * 128]
                          .rearrange("(a p) t -> p a t", p=128), ho_t[:], [ho_b], [])

            stages = [s0, s1, s2, s3, s4, s5, s6, s7, s8, s9, s10, s11, s12]
            for it in range(NCH + len(stages) - 1):
                for sidx in range(len(stages) - 1, -1, -1):
                    ci = it - sidx
                    if 0 <= ci < NCH:
                        stages[sidx](ci)

        qc = [(c.sb("dq%d" % i, [128, SEQ], BF16), Buf("dq%d" % i)) for i in range(2)]
        kc_ = [(c.sb("dk%d" % i, [128, SEQ], BF16), Buf("dk%d" % i)) for i in range(2)]
        sq = c.sb("sq", [128, SEQ], BF16); sq_b = Buf("sq")
        mx = c.sb("mx", [1, 64], F32); mx_b = Buf("mx")
        nG = c.sb("nG", [128, 1], F32); nG_b = Buf("nG")
        Et = [(c.sb("E%d" % i, [128, 512], BF16), Buf("E%d" % i)) for i in range(NSB + 2)]
        ds = [(c.sb("ds%d" % i, [128, 8], F32), Buf("ds%d" % i)) for i in range(4)]
        dtm = [(c.sb("dt%d" % i, [128, 256], F32), Buf("dt%d" % i)) for i in range(4)]
        dhd = [(c.sb("dhd%d" % i, [128, 256], F32), Buf("dhd%d" % i)) for i in range(4)]
        dhn = [(c.sb("dhn%d" % i, [128, 256], BF16), Buf("dhn%d" % i)) for i in range(4)]
        Os1 = [(c.sb("os1_%d" % i, [128, 257], F32), Buf("os1_%d" % i)) for i in range(4)]
        Os2 = [(c.sb("os2_%d" % i, [128, 257], F32), Buf("os2_%d" % i)) for i in range(4)]
        junkf = c.sb("junkf", [128, 256], F32); junkf_b = Buf("junkf")
        ascale = 128.0 ** -0.5
        Obanks = [(c.banks[i], c.bank_bufs[i]) for i in range(4)]
        Sbanks = [(c.banks[4 + i], c.bank_bufs[4 + i]) for i in range(NSB)]
        Tbanks = [(c.banks[4 + NSB + i], c.bank_bufs[4 + NSB + i]) for i in range(4 - NSB)]
        e_i = 0
        s_i = 0
        t_i = 0
        for j in range(NH if do_a else 0):
            for cc in range(2):
                c.dma(SP, qc[cc][0][:], d["dq"](j, cc), [], [qc[cc][1]])
                c.dma(SP, kc_[cc][0][:], d["dk"](j, cc), [], [kc_[cc][1]])
            c.dma(SP, va[:, :, 0:256], dv_d[:, j * 256:(j + 1) * 256].rearrange("(ci p) v -> p ci v", p=128),
                  [], [va_b])
            tb, tb_b = Tbanks[0]
            for ti, (tns, tns_b) in enumerate([qc[0], qc[1], kc_[0], kc_[1]]):
                c.act(sq[:], tns[:], AF.Square, [tns_b], [sq_b])
                for s8 in range(8):
                    c.mm(tb[0:1, 0:512], onesb[:, 0:1], sq[:, s8 * 512:(s8 + 1) * 512], True, True,
                         [onesb_b, sq_b], [tb_b])
                    P.op(DVE, lambda e, ti=ti, s8=s8: e.reduce_max(out=mx[0:1, ti * 8 + s8:ti * 8 + s8 + 1],
                                                                   in_=tb[0:1, 0:512], axis=AX.X),
                         reads=[tb_b], writes=[mx_b])
            P.op(DVE, lambda e: e.reduce_max(out=mx[0:1, 32:34], in_=mx[0:1, 0:32].rearrange("p (a b) -> p a b", a=2),
                                             axis=AX.X), reads=[mx_b], writes=[mx_b])
            c.tt(DVE, mx[0:1, 34:35], mx[0:1, 32:33], mx[0:1, 33:34], ALU.mult, [mx_b], [mx_b])
            c.act(mx[0:1, 35:36], mx[0:1, 34:35], AF.Sqrt, [mx_b], [mx_b], scale=ascale * ascale)
            c.ts(DVE, mx[0:1, 36:37], mx[0:1, 35:36], -1.0, None, ALU.mult, None, [mx_b], [mx_b])
            c.mm(tb[:, 0:1], onesf[0:1, :], mx[0:1, 36:37], True, True, [onesf_b, mx_b], [tb_b])
            c.copy(DVE, nG[:], tb[:, 0:1], [tb_b], [nG_b])
            if "dbg" in d:
                c.dma(SP, d["dbg"][j, 0:1, 0:64], mx[0:1, :], [mx_b], [])

            def qk_exp(g, kb):
                nonlocal s_i, e_i
                sb_, sb_b = Sbanks[s_i % NSB]; s_i += 1
                et, et_b = Et[e_i % (NSB + 2)]; e_i += 1
                ks = slice(kb * 128, (kb + 1) * 128)
                if kb <= 2 * g:
                    for cc in range(2):
                        diag = (kb == 2 * g)
                        c.mm(sb_[:, cc * 256:(cc + 1) * 256], kc_[cc][0][:, ks], qc[cc][0][:, g * 256:(g + 1) * 256],
                             True, not diag, [kc_[cc][1], qc[cc][1]], [sb_b])
                        if diag:
                            c.mm(sb_[:, cc * 256:cc * 256 + 128], identb[:], maskb[:], False, True,
                                 [identb_b, maskb_b], [sb_b])
                    c.act(et[:], sb_[:], AF.Exp, [sb_b, nG_b], [et_b], bias=nG[:, 0:1], scale=ascale)
                    ilist = (0, 1)
                else:
                    for cc in range(2):
                        c.mm(sb_[:, cc * 256 + 128:(cc + 1) * 256], kc_[cc][0][:, ks],
                             qc[cc][0][:, g * 256 + 128:(g + 1) * 256], True, False, [kc_[cc][1], qc[cc][1]], [sb_b])
                        c.mm(sb_[:, cc * 256 + 128:(cc + 1) * 256], identb[:], maskb[:], False, True,
                             [identb_b, maskb_b], [sb_b])
                    c.act(et[:].rearrange("p (a b) -> p a b", a=2)[:, :, 128:256],
                          sb_[:].rearrange("p (a b) -> p a b", a=2)[:, :, 128:256], AF.Exp,
                          [sb_b, nG_b], [et_b], bias=nG[:, 0:1], scale=ascale)
                    ilist = (1,)
                return (g, kb, et, et_b, ilist)

            def pv(st):
                g, kb, et, et_b, ilist = st
                for cc in range(2):
                    for i in ilist:
                        ob, ob_b = Obanks[cc * 2 + i]
                        last = (kb == 2 * g + i)
                        c.mm(ob[:, 0:257], et[:, cc * 256 + i * 128:cc * 256 + (i + 1) * 128], va[:, kb, :],
                             kb == 0, last, [et_b, va_b], [ob_b])
                if kb == 2 * g + 1:
                    epilogue(g)

            def epilogue(g):
                nonlocal t_i
                for i in range(2):
                    qb = 2 * g + i
                    r = qb % 4
                    o1, o1_b = Obanks[i]
                    o2, o2_b = Obanks[2 + i]
                    os1, os1_b = Os1[r]
                    os2, os2_b = Os2[r]
                    c.copy(DVE, os1[:], o1[:, 0:257], [o1_b], [os1_b])
                    c.copy(DVE, os2[:], o2[:, 0:257], [o2_b], [os2_b])
                R = [(2 * g + i) % 4 for i in range(2)]
                for r in R:
                    P.op(DVE, lambda e, d_=ds[r][0], os1=Os1[r][0]: e.reciprocal(out=d_[:, 0:1], in_=os1[:, 256:257]),
                         reads=[Os1[r][1]], writes=[ds[r][1]])
                for r in R:
                    P.op(DVE, lambda e, d_=ds[r][0], os2=Os2[r][0]: e.reciprocal(out=d_[:, 1:2], in_=os2[:, 256:257]),
                         reads=[Os2[r][1]], writes=[ds[r][1]])
                for r in R:
                    d_, d_b = ds[r]
                    c.ts(DVE, d_[:, 2:3], d_[:, 1:2], lsm[:, NLAM:NLAM + 1], None, ALU.mult, None, [d_b, lsm_b], [d_b])
                for r in R:
                    d_, d_b = ds[r]
                    c.ts(DVE, dtm[r][0][:], Os1[r][0][:, 0:256], d_[:, 0:1], None, ALU.mult, None,
                         [Os1[r][1], d_b], [dtm[r][1]])
                for r in R:
                    d_, d_b = ds[r]
                    c.stt(dhd[r][0][:], Os2[r][0][:, 0:256], d_[:, 2:3], dtm[r][0][:], ALU.mult, ALU.add,
                          [Os2[r][1], d_b, dtm[r][1]], [dhd[r][1]])
                for r in R:
                    P.op(DVE, lambda e, hd_=dhd[r][0], d_=ds[r][0]: e.scalar_tensor_tensor(
                        out=junkf[:], in0=hd_[:], scalar=1.0, in1=hd_[:], op0=ALU.mult, op1=ALU.mult,
                        accum_out=d_[:, 3:4]), reads=[dhd[r][1]], writes=[junkf_b, ds[r][1]])
                defer.append([it_no[0] + 3, stage_b, (g,)])

            def stage_b(g):
                R = [(2 * g + i) % 4 for i in range(2)]
                for r in R:
                    d_, d_b = ds[r]
                    c.act(d_[:, 4:5], d_[:, 3:4], AF.Sqrt, [d_b, eps_b], [d_b], bias=epst[:, 0:1], scale=1.0 / 256)
                for r in R:
                    P.op(DVE, lambda e, d_=ds[r][0]: e.reciprocal(out=d_[:, 5:6], in_=d_[:, 4:5]),
                         reads=[ds[r][1]], writes=[ds[r][1]])
                for r in R:
                    d_, d_b = ds[r]
                    c.ts(DVE, d_[:, 6:7], d_[:, 5:6], lami[:, 1:2], None, ALU.mult, None, [d_b, lami_b], [d_b])
                for r in R:
                    d_, d_b = ds[r]
                    c.stt(dhn[r][0][:], dhd[r][0][:], d_[:, 6:7], dnw[:, j * 256:(j + 1) * 256], ALU.mult, ALU.mult,
                          [dhd[r][1], d_b, dnw_b], [dhn[r][1]])
                for i in range(2):
                    defer.append([it_no[0] + 3, stage_c, (g, i)])

            def stage_c(g, i):
                nonlocal t_i
                qb = 2 * g + i
                r = qb % 4
                hn_, hn_b = dhn[r]
                tb, tb_b = Tbanks[t_i % (4 - NSB)]; t_i += 1
                for vc in range(2):
                    c.mm(tb[:, vc * 128:(vc + 1) * 128], hn_[:, vc * 128:(vc + 1) * 128], identb[:], True, True,
                         [hn_b, identb_b], [tb_b])
                ho_t, ho_b = hout[(qb // 4) % 2]
                c.copy(DVE, ho_t[:, :, (qb % 4) * 128:(qb % 4 + 1) * 128],
                       tb[:, 0:256].rearrange("p (a b) -> p a b", a=2), [tb_b], [ho_b])
                if qb % 4 == 3:
                    c.dma(SP, hdT_d[j * 256:(j + 1) * 256, (qb - 3) * 128:(qb + 1) * 128]
                          .rearrange("(a p) t -> p a t", p=128), ho_t[:], [ho_b], [])

            defer = []
            it_no = [0]

            def run_deferred(flush=False):
                k = 0
                while k < len(defer):
                    if flush or defer[k][0] <= it_no[0]:
                        _, fn, args = defer.pop(k)
                        fn(*args)
                    else:
                        k += 1

            pairs = [(g, kb) for g in range(NCH // 2) for kb in range(2 * g + 2)]
            pend = []
            for (g, kb) in pairs:
                pend.append(qk_exp(g, kb))
                if len(pend) > NSB - 1:
                    pv(pend.pop(0))
                it_no[0] += 1
                run_deferred()
            while pend:
                pv(pend.pop(0))
            while defer:
                run_deferred(flush=True)
        c.end()


IDENT = np.eye(128, dtype=np.float32)
MASKNEG_NP = np.where(np.arange(128)[:, None] <= np.arange(128)[None, :], 0.0, MASKNEG).astype(np.float32)


def lam_init_of(l):
    return 0.8 - 0.6 * math.exp(-0.3 * l)


TG = 512
NFC = D_FF // 128


def phase_D(c, d, final, last):
    x_d, hm_d, hd_d, gmd_d, xo_d = d["x"], d["hmT"], d["hdT"], d["gmd"], d["xo"]
    wbm_d, wbd_d, wout_d, wg_d, wu_d, wd_d = d["wbm"], d["wbd"], d["wout"], d["wg"], d["wu"], d["wd"]
    nw2_d, ident_d = d["nw2"], d["ident"]
    if final:
        fnw_d = d["fnw"]
    if True:
        c.begin()
        P = c.P
        ident = c.sb("ident_s", [128, 128], BF16); ident_b = Buf("ident")
        c.dma(POOL, ident[:], ident_d[:, :], [], [ident_b])
        nw2 = c.sb("nw2_s", [128, KC], F32); nw2_b = Buf("nw2")
        c.dma(SP, nw2[:], nw2_d, [], [nw2_b])
        if final:
            fnw = c.sb("fnw_s", [128, D_MODEL], F32); fnw_b = Buf("fnw")
            c.dma(SP, fnw[:], fnw_d, [], [fnw_b])
        hmg = c.sb("hmg", [128, KC, TG], BF16); hmg_b = Buf("hmg")
        hdg = c.sb("hdg", [128, KC, TG], BF16); hdg_b = Buf("hdg")
        yT = c.sb("yT", [128, KC, TG], BF16); yT_b = Buf("yT")
        xg = c.sb("xg", [128, TG // 128, D_MODEL], F32)
        xg_b = [Buf("xg%d" % i) for i in range(TG // 128)]
        aT = c.sb("aT", [128, NFC, TG], BF16); aT_b = Buf("aT")
        NW = 3
        wslots = [(c.sb("w%d" % i, [128, KC, 512], BF16), Buf("w%d" % i)) for i in range(NW)]
        t1 = [(c.sb("t1_%d" % i, [128, TG], F32), Buf("t1_%d" % i)) for i in range(2)]
        t2 = [(c.sb("t2_%d" % i, [128, TG], F32), Buf("t2_%d" % i)) for i in range(2)]
        sgm = [(c.sb("sgm%d" % i, [128, TG], BF16), Buf("sgm%d" % i)) for i in range(2)]
        sgd = [(c.sb("sgd%d" % i, [128, TG], BF16), Buf("sgd%d" % i)) for i in range(2)]
        scr = norm_scratch(c, "n_")

        wlist = []
        for g in range(TOK // TG):
            for blk in range(4):
                wlist.append((wbm_d, 0, KC, blk * 512, 512))
                wlist.append((wbd_d, 0, KC, blk * 512, 512))
            for cg in range(4):
                wlist.append((wout_d, 0, KC, cg * 512, 512))
            for blk in range(D_FF // 512):
                wlist.append((wg_d, 0, KC, blk * 512, 512))
                wlist.append((wu_d, 0, KC, blk * 512, 512))
            for cg in range(4):
                for fb in range(4):
                    wlist.append((wd_d, fb * 11 * 128, 11, cg * 512, 512))
        wstate = {"issued": 0, "used": 0, "done": 0}

        def issue_one():
            i = wstate["issued"]
            wdr, r0, nkc, c0, ncol = wlist[i]
            ws, wb = wslots[i % NW]
            load_wblock(c, ws, wb, wdr, r0, nkc, c0, ncol)
            wstate["issued"] += 1

        def next_w():
            i = wstate["used"]
            while wstate["issued"] <= i:
                assert wstate["issued"] < wstate["done"] + NW
                issue_one()
            wstate["used"] += 1
            return wslots[i % NW]

        def release_w():
            wstate["done"] = wstate["used"]
            while wstate["issued"] < len(wlist) and wstate["issued"] < wstate["done"] + NW:
                issue_one()

        k2 = 0
        for g in range(TOK // TG):
            ts0 = g * TG
            c.dma(SP, hmg[:], hm_d[:, ts0:ts0 + TG].rearrange("(kc p) t -> p kc t", p=128), [], [hmg_b])
            c.dma(SP, hdg[:], hd_d[:, ts0:ts0 + TG].rearrange("(kc p) t -> p kc t", p=128), [], [hdg_b])
            for tt in range(TG // 128):
                c.dma(SP, xg[:, tt, :], x_d[ts0 + tt * 128:ts0 + (tt + 1) * 128, :], [], [xg_b[tt]])
            for blk in range(4):
                wm, wm_b = next_w()
                wd_, wd_b = next_w()
                for cc in range(4):
                    col = blk * 4 + cc
                    sm_, sm_b = sgm[k2 % 2]
                    sd_, sd_b = sgd[k2 % 2]
                    c.dma(SP, sm_[:], gmd_d[col * 128:(col + 1) * 128, ts0:ts0 + TG], [], [sm_b])
                    c.dma(SP, sd_[:], gmd_d[D_MODEL + col * 128:D_MODEL + (col + 1) * 128, ts0:ts0 + TG], [], [sd_b])
                    bA, bA_b = c.next_bank()
                    for kc in range(KC):
                        c.mm(bA[:, :], wm[:, kc, cc * 128:(cc + 1) * 128], hmg[:, kc, :], kc == 0, kc == KC - 1,
                             [wm_b, hmg_b], [bA_b])
                    bB, bB_b = c.next_bank()
                    for kc in range(KC):
                        c.mm(bB[:, :], wd_[:, kc, cc * 128:(cc + 1) * 128], hdg[:, kc, :], kc == 0, kc == KC - 1,
                             [wd_b, hdg_b], [bB_b])
                    a1, a1_b = t1[k2 % 2]
                    a2, a2_b = t2[k2 % 2]
                    k2 += 1
                    c.tt(DVE, a1[:], bA[:, :], sm_[:], ALU.mult, [bA_b, sm_b], [a1_b])
                    c.tt(DVE, a2[:], bB[:, :], sd_[:], ALU.mult, [bB_b, sd_b], [a2_b])
                    c.tt(POOL, yT[:, col, :], a1[:], a2[:], ALU.add, [a1_b, a2_b], [yT_b])
                release_w()
            for cg in range(4):
                wo, wo_b = next_w()
                for tt in range(TG // 128):
                    bk, bk_b = c.next_bank()
                    for kc in range(KC):
                        c.mm(bk[:, :], yT[:, kc, tt * 128:(tt + 1) * 128], wo[:, kc, :], kc == 0, kc == KC - 1,
                             [yT_b, wo_b], [bk_b])
                    c.tt(DVE, xg[:, tt, cg * 512:(cg + 1) * 512], bk[:, :], xg[:, tt, cg * 512:(cg + 1) * 512], ALU.add,
                         [bk_b, xg_b[tt]], [xg_b[tt]])
                release_w()
            for tt in range(TG // 128):
                rmsnorm_to_T(c, xg[:, tt, :], xg_b[tt], scr, hmg, hmg_b, tt * 128, nw2, nw2_b, ident, ident_b, "n_")
            for blk in range(D_FF // 512):
                wg_, wg_b = next_w()
                wu_, wu_b = next_w()
                for cc in range(4):
                    fc = blk * 4 + cc
                    bG, bG_b = c.next_bank()
                    for kc in range(KC):
                        c.mm(bG[:, :], wg_[:, kc, cc * 128:(cc + 1) * 128], hmg[:, kc, :], kc == 0, kc == KC - 1,
                             [wg_b, hmg_b], [bG_b])
                    bU, bU_b = c.next_bank()
                    for kc in range(KC):
                        c.mm(bU[:, :], wu_[:, kc, cc * 128:(cc + 1) * 128], hmg[:, kc, :], kc == 0, kc == KC - 1,
                             [wu_b, hmg_b], [bU_b])
                    a1, a1_b = t1[k2 % 2]
                    k2 += 1
                    c.act(a1[:], bG[:, :], AF.Silu, [bG_b], [a1_b])
                    c.tt(DVE, aT[:, fc, :], bU[:, :], a1[:], ALU.mult, [bU_b, a1_b], [aT_b])
                release_w()
            for cg in range(4):
                bks = [c.next_bank() for _ in range(TG // 128)]
                for fb in range(4):
                    wdn, wdn_b = next_w()
                    for tt in range(TG // 128):
                        bk, bk_b = bks[tt]
                        for i in range(11):
                            fc = fb * 11 + i
                            c.mm(bk[:, :], aT[:, fc, tt * 128:(tt + 1) * 128], wdn[:, i, :], fc == 0, fc == NFC - 1,
                                 [aT_b, wdn_b], [bk_b])
                    release_w()
                for tt in range(TG // 128):
                    bk, bk_b = bks[tt]
                    c.tt(DVE, xg[:, tt, cg * 512:(cg + 1) * 512], bk[:, :], xg[:, tt, cg * 512:(cg + 1) * 512], ALU.add,
                         [bk_b, xg_b[tt]], [xg_b[tt]])
            for tt in range(TG // 128):
                if final:
                    junk, junk_b = scr["junk"]
                    ss, ss_b = scr["ss"]
                    c.act(junk[:], xg[:, tt, :], AF.Square, [xg_b[tt]], [junk_b, ss_b], accum_out=ss[:, 0:1])
                    c.act(ss[:, 1:2], ss[:, 0:1], AF.Sqrt, [ss_b, scr["eps_b"]], [ss_b], scale=1.0 / D_MODEL,
                          bias=scr["eps"][:, 0:1])
                    P.op(DVE, lambda e, ss=ss: e.reciprocal(out=ss[:, 2:3], in_=ss[:, 1:2]), reads=[ss_b], writes=[ss_b])
                    c.stt(xg[:, tt, :], xg[:, tt, :], ss[:, 2:3], fnw[:], ALU.mult, ALU.mult,
                          [xg_b[tt], ss_b, fnw_b], [xg_b[tt]])
                c.dma(SP, xo_d[ts0 + tt * 128:ts0 + (tt + 1) * 128, :], xg[:, tt, :], [xg_b[tt]], [], sem_key=xg_b[tt])
        c.end(last)


NUSED = 4


def build_fused(depth=DEPTH):
    nc = bass.Bass("TRN2", target_bir_lowering=False)
    di = lambda n, s, dt: nc.dram_tensor(n, s, dt, kind="ExternalInput").ap()
    x_in = di("x", [SEQ, D_MODEL], F32)
    w_in = di("w_in", [DEPTH, D_MODEL, N_IN], F32)
    w_bm = di("w_branch_m", [DEPTH, D_MODEL, D_MODEL], F32)
    w_bd = di("w_branch_d", [DEPTH, D_MODEL, D_MODEL], F32)
    w_out = di("w_out", [DEPTH, D_MODEL, D_MODEL], F32)
    w_g = di("w_ffn_gate", [DEPTH, D_MODEL, D_FF], F32)
    w_u = di("w_ffn_up", [DEPTH, D_MODEL, D_FF], F32)
    w_d = di("w_ffn_down", [DEPTH, D_FF, D_MODEL], F32)
    anw = di("anw", [DEPTH, 128, KC], F32)
    fnw2 = di("fnw2", [DEPTH, 128, KC], F32)
    fnw = di("fnw", [128, D_MODEL], F32)
    cw = di("cw", [DEPTH, 2, 128, 2 * NH, 5], F32)
    gb = di("gb", [DEPTH, 2, 128, 2], F32)
    mnw = di("mnw", [DEPTH, 128, D_MODEL], F32)
    dnw = di("dnw", [DEPTH, 128, D_MODEL], F32)
    lam = di("lam", [DEPTH, 128, 4, 128], F32)
    lami = di("lami", [DEPTH, 128, 2], F32)
    ident = di("ident", [128, 128], F32)
    maskneg = di("maskneg", [128, 128], F32)
    y_out = nc.dram_tensor("y", [SEQ, D_MODEL], F32, kind="ExternalOutput").ap()
    sc = lambda n, s, dt: nc.dram_tensor(n, s, dt).ap()
    qk_s = sc("qk_s", [2048, SEQ], F32)
    mv_s = sc("mv_s", [SEQ, 2048], BF16)
    mo_s = sc("mo_s", [SEQ, 2048], BF16)
    gt_s = sc("gt_s", [16, SEQ], F32)
    dqk_s = sc("dqk_s", [4096, SEQ], BF16)
    dv_s = sc("dv_s", [SEQ, 2048], BF16)
    gmd_s = sc("gmd_s", [4096, SEQ], BF16)
    hm_s = sc("hm_s", [2048, SEQ], BF16)
    hd_s = sc("hd_s", [2048, SEQ], BF16)
    x_s = sc("x_s", [SEQ, D_MODEL], F32)
    scr = {"qk": qk_s, "mv": mv_s, "mo": mo_s, "gt": gt_s, "dqk": dqk_s, "dv": dv_s, "gmd": gmd_s}
    wb = {"wbm": sc("wbm_b", [D_MODEL, D_MODEL], BF16), "wbd": sc("wbd_b", [D_MODEL, D_MODEL], BF16),
          "wout": sc("wout_b", [D_MODEL, D_MODEL], BF16), "wg": sc("wg_b", [D_MODEL, D_FF], BF16),
          "wu": sc("wu_b", [D_MODEL, D_FF], BF16), "wd": sc("wd_b", [D_FF, D_MODEL], BF16)}

    def make_cast(l):
        def pre(c):
            srcs = {"wbm": w_bm[l], "wbd": w_bd[l], "wout": w_out[l], "wg": w_g[l], "wu": w_u[l], "wd": w_d[l]}
            for k in ("wbm", "wbd", "wout", "wg", "wu", "wd"):
                src, dst = srcs[k], wb[k]
                if k in ("wg", "wu"):
                    src = src.rearrange("r (a b) -> r a b", b=1408)
                    dst = dst.rearrange("r (a b) -> r a b", b=1408)
                c.dma(POOL, dst, src, [], [Buf("cast_" + k)])
        return pre

    with contextlib.ExitStack() as es:
        c = Ctx(nc, es)
        c.alloc_banks(8)
        for l in range(depth):
            x_src = x_in if l == 0 else x_s
            final = (l == depth - 1)
            for th in range(2):
                ts = slice(th * TOK, (th + 1) * TOK)
                outs = {}
                for name, c0, n, mode, dt, sg in A_SECTIONS:
                    outs[name] = scr[name][:, ts] if mode == "F" else scr[name][ts, :]
                phase_A(c, x_src[ts, :], anw[l], w_in[l], ident, outs)
            for hh in range(2):
                hs = slice(hh * NH * 256, (hh + 1) * NH * 256)
                d = {
                    "cw": cw[l, hh], "gb": gb[l, hh], "mnw": mnw[l][:, hs], "dnw": dnw[l][:, hs],
                    "lam": lam[l], "lami": lami[l], "ident": ident, "maskneg": maskneg,
                    "mv": mv_s[:, hs], "mo": mo_s[:, hs], "dv": dv_s[:, hs],
                    "gi": gt_s[hh * NH:(hh + 1) * NH, :], "gf": gt_s[8 + hh * NH:8 + (hh + 1) * NH, :],
                    "hmT": hm_s[hs, :], "hdT": hd_s[hs, :],
                    "qraw": (lambda j, hh=hh: qk_s[(hh * NH + j) * 128:(hh * NH + j + 1) * 128, :]),
                    "kraw": (lambda j, hh=hh: qk_s[1024 + (hh * NH + j) * 128:1024 + (hh * NH + j + 1) * 128, :]),
                    "dq": (lambda j, cc, hh=hh: dqk_s[(hh * NH + j) * 256 + cc * 128:(hh * NH + j) * 256 + (cc + 1) * 128, :]),
                    "dk": (lambda j, cc, hh=hh: dqk_s[2048 + (hh * NH + j) * 256 + cc * 128:
                                                      2048 + (hh * NH + j) * 256 + (cc + 1) * 128, :]),
                }
                if hh == 0:
                    d["pre"] = make_cast(l)
                phase_BC(c, d)
            for th in range(2):
                ts = slice(th * TOK, (th + 1) * TOK)
                d = {"x": x_src[ts, :], "hmT": hm_s[:, ts], "hdT": hd_s[:, ts], "gmd": gmd_s[:, ts],
                     "xo": (y_out if final else x_s)[ts, :],
                     "wbm": wb["wbm"], "wbd": wb["wbd"], "wout": wb["wout"], "wg": wb["wg"], "wu": wb["wu"],
                     "wd": wb["wd"],
                     "nw2": fnw2[l], "ident": ident, "fnw": fnw}
                phase_D(c, d, final, last=(l == depth - 1 and th == 1))
        build_fused.stats = (c.P.n_total, dict(c.P.cnt))
    return nc


def host_params(prm):
    L = DEPTH
    anw = np.stack([nw_layout(prm["attn_norm_w"][l]) for l in range(L)])
    fnw2 = np.stack([nw_layout(prm["ffn_norm_w"][l]) for l in range(L)])
    fnw = np.ascontiguousarray(np.broadcast_to(prm["final_norm_w"][None], (128, D_MODEL))).astype(np.float32)
    cw = np.zeros((L, 2, 128, 2 * NH, 5), np.float32)
    gb = np.zeros((L, 2, 128, 2), np.float32)
    for l in range(L):
        cwl, cbl = prm["conv_w"][l], prm["conv_b"][l]
        for hh in range(2):
            for i in range(NH):
                h = hh * NH + i
                cw[l, hh, :, i, 0:4] = cwl[:, h * 128:(h + 1) * 128].T
                cw[l, hh, :, i, 4] = cbl[h * 128:(h + 1) * 128]
                cw[l, hh, :, NH + i, 0:4] = cwl[:, 1024 + h * 128:1024 + (h + 1) * 128].T
                cw[l, hh, :, NH + i, 4] = cbl[1024 + h * 128:1024 + (h + 1) * 128]
            heads = slice(hh * NH, (hh + 1) * NH)
            gb[l, hh, :, 0] = np.repeat(prm["b_igate"][l][heads], NCH)
            gb[l, hh, :, 1] = np.repeat(prm["b_fgate"][l][heads], NCH)
    mnw = np.ascontiguousarray(np.broadcast_to(prm["mlstm_norm_w"][:, None, :], (L, 128, D_MODEL))).astype(np.float32)
    dnw = np.ascontiguousarray(np.broadcast_to(prm["diff_norm_w"][:, None, :], (L, 128, D_MODEL))).astype(np.float32)
    lam = np.stack([np.stack([prm["lambda_q1"][l], prm["lambda_k1"][l], prm["lambda_q2"][l], prm["lambda_k2"][l]])
                    for l in range(L)])
    lam = np.ascontiguousarray(np.broadcast_to(lam[:, None], (L, 128, 4, 128))).astype(np.float32)
    lami = np.zeros((L, 128, 2), np.float32)
    for l in range(L):
        lami[l, :, 0] = lam_init_of(l)
        lami[l, :, 1] = 1.0 - lam_init_of(l)
    return {"anw": anw, "fnw2": fnw2, "fnw": fnw, "cw": cw, "gb": gb, "mnw": mnw, "dnw": dnw, "lam": lam,
            "lami": lami, "ident": IDENT, "maskneg": MASKNEG_NP}


_NC = {}


def kernel(**inputs):
    x = np.ascontiguousarray(inputs["x"], dtype=np.float32)
    prm = {k: np.asarray(v, dtype=np.float32) for k, v in inputs.items() if k != "x"}
    if "nc" not in _NC:
        _NC["nc"] = build_fused()
    hp = host_params(prm)
    big = {k: np.ascontiguousarray(prm[k]) for k in ("w_in", "w_branch_m", "w_branch_d", "w_out",
                                                     "w_ffn_gate", "w_ffn_up", "w_ffn_down")}
    maps = []
    for b in range(NUSED):
        m = {"x": np.ascontiguousarray(x[b])}
        m.update(big)
        m.update(hp)
        maps.append(m)
    res = run_bass_kernel_spmd(_NC["nc"], maps, core_ids=list(range(NUSED)))
    return np.stack([np.asarray(res.results[b]["y"]) for b in range(NUSED)]).astype(np.float32)
```

```python
import contextlib
import math
import numpy as np
import ml_dtypes
import concourse.bass as bass
import concourse.mybir as mybir
from concourse.bass_utils import run_bass_kernel_spmd

F32 = mybir.dt.float32
BF16 = mybir.dt.bfloat16
AF = mybir.ActivationFunctionType
ALU = mybir.AluOpType
AX = mybir.AxisListType
NPBF = ml_dtypes.bfloat16

D_MODEL = 2048
BATCH = 4
SEQ = 4096
DEPTH = 4
NCORES = 8
TOK = 2048
D_FF = 5632
N_IN = 16400
EPS = 1e-6
KC = D_MODEL // 128

PE, ACT, DVE, POOL, SP = "pe", "act", "dve", "pool", "sp"
COMPUTE = (PE, ACT, DVE, POOL)


class Buf:
    __slots__ = ("name", "last_write", "reads", "dma_sem")

    def __init__(self, name):
        self.name = name
        self.last_write = None
        self.reads = {}
        self.dma_sem = None


class Instr:
    __slots__ = ("eng", "fn", "deps", "is_dma", "sem_key", "sig_val", "needs_sig")

    def __init__(self, eng, fn, is_dma, sem_key):
        self.eng = eng
        self.fn = fn
        self.deps = []
        self.is_dma = is_dma
        self.sem_key = sem_key
        self.sig_val = None
        self.needs_sig = False


class Prog:
    NDMA = 72

    def __init__(self, nc, es):
        self.nc = nc
        self.esem = {e: es.enter_context(nc.semaphore("s_" + e)) for e in COMPUTE}
        self.dsem = [es.enter_context(nc.semaphore("d_%d" % i)) for i in range(self.NDMA)]
        self.cnt = {e: 0 for e in COMPUTE}
        self.dcnt = [0] * self.NDMA
        self.barrier = []
        self.n_total = 0
        self._reset()

    def _reset(self):
        self.q = {e: [] for e in (PE, ACT, DVE, POOL, SP)}
        self.started = set()

    def op(self, eng, fn, reads=(), writes=(), dma=False, sem_key=None, pe_accum=False):
        if dma and sem_key is None:
            sem_key = writes[0] if len(writes) else reads[0]
        ins = Instr(eng, fn, dma, sem_key)
        deps = []
        if eng not in self.started:
            self.started.add(eng)
            ins.deps.extend(self.barrier)
        for b in reads:
            if b.last_write is not None:
                deps.append(b.last_write)
        for b in writes:
            if b.last_write is not None:
                deps.append(b.last_write)
            deps.extend(b.reads.values())
        seen = set()
        for d in deps:
            if d is ins or id(d) in seen:
                continue
            seen.add(id(d))
            if (not d.is_dma) and d.eng == PE and eng == PE and not dma:
                continue
            ins.deps.append(d)
            d.needs_sig = True
        for b in reads:
            b.reads[("d", id(sem_key)) if dma else eng] = ins
        for b in writes:
            b.last_write = ins
            b.reads = {}
        self.q[eng].append(ins)
        return ins

    def end_phase(self, last=False):
        nc = self.nc
        bar = {}
        for e in self.q:
            if self.q[e]:
                bar[id(self.q[e][-1])] = self.q[e][-1]
            for ins in self.q[e]:
                if ins.is_dma:
                    bar["k%d" % id(ins.sem_key)] = ins
        barrier = []
        seen = set()
        for ins in bar.values():
            if id(ins) not in seen:
                seen.add(id(ins))
                barrier.append(ins)
                ins.needs_sig = True
        nkeys = 0
        for e in self.q:
            for ins in self.q[e]:
                if ins.is_dma and ins.needs_sig and ins.sem_key.dma_sem is None:
                    ins.sem_key.dma_sem = nkeys
                    nkeys += 1
        assert nkeys <= self.NDMA, nkeys
        for e in self.q:
            for ins in self.q[e]:
                self.n_total += 1
                if not ins.needs_sig:
                    continue
                if ins.is_dma:
                    k = ins.sem_key.dma_sem
                    self.dcnt[k] += 16
                    ins.sig_val = (self.dsem[k], self.dcnt[k], 16)
                else:
                    self.cnt[ins.eng] += 1
                    ins.sig_val = (self.esem[ins.eng], self.cnt[ins.eng], 1)
        q = self.q
        with nc.Block() as block:
            def run(e, eng_obj):
                waited = {}
                for ins in q[e]:
                    for d in ins.deps:
                        sem, val, _ = d.sig_val
                        key = id(sem)
                        if waited.get(key, 0) >= val:
                            continue
                        waited[key] = val
                        eng_obj.wait_ge(sem, val)
                    bi = ins.fn(eng_obj)
                    if ins.needs_sig:
                        sem, val, inc = ins.sig_val
                        bi.then_inc(sem, inc)
                if e == SP and last:
                    for ins in barrier:
                        sem, val, _ = ins.sig_val
                        if waited.get(id(sem), 0) >= val:
                            continue
                        waited[id(sem)] = val
                        eng_obj.wait_ge(sem, val)

            if q[PE]:
                @block.tensor
                def _(eng):
                    run(PE, eng)
            if q[ACT]:
                @block.scalar
                def _(eng):
                    run(ACT, eng)
            if q[DVE]:
                @block.vector
                def _(eng):
                    run(DVE, eng)
            if q[POOL]:
                @block.gpsimd
                def _(eng):
                    run(POOL, eng)
            if q[SP] or last:
                @block.sync
                def _(eng):
                    run(SP, eng)
        for ins in barrier:
            ins.fn = None
        for e in q:
            for ins in q[e]:
                ins.fn = None
                ins.deps = None
        self.barrier = barrier
        self._reset()


class Ctx:
    def __init__(self, nc, es):
        self.nc = nc
        self.ges = es
        self.es = None
        self.P = Prog(nc, es)
        self.uid = 0
        self.banks = []
        self.bank_bufs = []
        self.bank_i = 0
        self.evac_i = 0

    def sb(self, name, shape, dt):
        self.uid += 1
        return self.es.enter_context(self.nc.sbuf_tensor("%s_%d" % (name, self.uid), list(shape), dt))

    def begin(self):
        self.es = contextlib.ExitStack()
        self.es.__enter__()
        self.bank_bufs = [Buf("bank%d" % i) for i in range(len(self.banks))]

    def end(self, last=False):
        self.P.end_phase(last)
        self.es.__exit__(None, None, None)
        self.es = None

    def alloc_banks(self, n=8):
        for i in range(n):
            self.banks.append(self.ges.enter_context(self.nc.psum_tensor("bank%d" % i, [128, 512], F32)))
            self.bank_bufs.append(Buf("bank%d" % i))

    def next_bank(self):
        i = self.bank_i % len(self.banks)
        self.bank_i += 1
        return self.banks[i], self.bank_bufs[i]

    def mm(self, out, lhsT, rhs, start, stop, reads, writes):
        return self.P.op(PE, lambda e: e.matmul(out, lhsT, rhs, start=start, stop=stop),
                         reads=reads, writes=writes)

    def act(self, out, in_, func, reads, writes, bias=None, scale=None, accum_out=None):
        kw = {}
        if bias is not None:
            kw["bias"] = bias
        if scale is not None:
            kw["scale"] = scale
        if accum_out is not None:
            kw["accum_out"] = accum_out
        return self.P.op(ACT, lambda e: e.activation(out=out, in_=in_, func=func, **kw),
                         reads=reads, writes=writes)

    def dma(self, eng, out, in_, reads, writes, sem_key=None):
        return self.P.op(eng, lambda e: e.dma_start(out=out, in_=in_), reads=reads, writes=writes,
                         dma=True, sem_key=sem_key)

    def tt(self, eng, out, in0, in1, op, reads, writes):
        return self.P.op(eng, lambda e: e.tensor_tensor(out=out, in0=in0, in1=in1, op=op),
                         reads=reads, writes=writes)

    def ts(self, eng, out, in0, s1, s2, op0, op1, reads, writes, accum_out=None):
        if op1 is None:
            return self.P.op(eng, lambda e: e.tensor_scalar(out=out, in0=in0, scalar1=s1, scalar2=None, op0=op0),
                             reads=reads, writes=writes)
        if accum_out is not None:
            return self.P.op(eng, lambda e: e.tensor_scalar(out=out, in0=in0, scalar1=s1, scalar2=s2, op0=op0,
                                                            op1=op1, accum_out=accum_out),
                             reads=reads, writes=writes)
        return self.P.op(eng, lambda e: e.tensor_scalar(out=out, in0=in0, scalar1=s1, scalar2=s2, op0=op0, op1=op1),
                         reads=reads, writes=writes)

    def stt(self, out, in0, scalar, in1, op0, op1, reads, writes):
        return self.P.op(DVE, lambda e: e.scalar_tensor_tensor(out=out, in0=in0, scalar=scalar, in1=in1,
                                                               op0=op0, op1=op1),
                         reads=reads, writes=writes)

    def copy(self, eng, out, in_, reads, writes):
        if eng == ACT:
            return self.P.op(ACT, lambda e: e.copy(out=out, in_=in_), reads=reads, writes=writes)
        return self.P.op(eng, lambda e: e.tensor_copy(out=out, in_=in_), reads=reads, writes=writes)

    def evac_copy(self, out, in_, reads, writes):
        self.evac_i += 1
        return self.copy(ACT if self.evac_i % 2 else DVE, out, in_, reads, writes)


def rmsnorm_to_T(c, xt, xbuf, scratch, hT, hT_buf, tok0, nw, nw_buf, ident, ident_buf, pfx):
    junk, junk_b = scratch["junk"]
    ss, ss_b = scratch["ss"]
    hn, hn_b = scratch["hn"]
    c.act(junk[:], xt[:], AF.Square, reads=[xbuf], writes=[junk_b, ss_b], accum_out=ss[:, 0:1])
    c.act(ss[:, 1:2], ss[:, 0:1], AF.Sqrt, reads=[ss_b, scratch["eps_b"]], writes=[ss_b], scale=1.0 / D_MODEL, bias=scratch["eps"][:, 0:1])
    c.P.op(DVE, lambda e: e.reciprocal(out=ss[:, 2:3], in_=ss[:, 1:2]), reads=[ss_b], writes=[ss_b])
    c.act(hn[:, 0:1024], xt[:, 0:1024], AF.Copy, reads=[xbuf, ss_b], writes=[hn_b], scale=ss[:, 2:3])
    c.ts(DVE, hn[:, 1024:2048], xt[:, 1024:2048], ss[:, 2:3], None, ALU.mult, None, reads=[xbuf, ss_b], writes=[hn_b])
    for kq in range(KC // 4):
        bank, bb = c.next_bank()
        for j in range(4):
            kc = kq * 4 + j
            c.mm(bank[:, j * 128:(j + 1) * 128], hn[:, kc * 128:(kc + 1) * 128], ident[:], True, True,
                 reads=[hn_b, ident_buf], writes=[bb])
        for j in range(4):
            kc = kq * 4 + j
            if j % 2 == 0:
                c.act(hT[:, kc, tok0:tok0 + 128], bank[:, j * 128:(j + 1) * 128], AF.Copy,
                      reads=[bb, nw_buf], writes=[hT_buf], scale=nw[:, kc:kc + 1])
            else:
                c.ts(DVE, hT[:, kc, tok0:tok0 + 128], bank[:, j * 128:(j + 1) * 128], nw[:, kc:kc + 1], None,
                     ALU.mult, None, reads=[bb, nw_buf], writes=[hT_buf])


def norm_scratch(c, pfx):
    eps = c.sb(pfx + "eps", [128, 1], F32)
    eb = Buf(pfx + "eps")
    c.P.op(POOL, lambda e: e.memset(eps[:], EPS), writes=[eb])
    return {
        "junk": (c.sb(pfx + "junk", [128, 2048], BF16), Buf(pfx + "junk")),
        "ss": (c.sb(pfx + "ss", [128, 4], F32), Buf(pfx + "ss")),
        "hn": (c.sb(pfx + "hn", [128, 2048], BF16), Buf(pfx + "hn")),
        "eps": eps, "eps_b": eb,
    }


A_SECTIONS = [
    ("qk", 0, 2048, "F", F32, False),
    ("mv", 2048, 2048, "T", BF16, False),
    ("mo", 4096, 2048, "T", BF16, True),
    ("gt", 6144, 16, "F", F32, False),
    ("dqk", 6160, 4096, "F", BF16, False),
    ("dv", 10256, 2048, "T", BF16, False),
    ("gmd", 12304, 4096, "F", BF16, True),
]


def load_wblock(c, wslot, wbuf, w_dram, row0, nkc, col0, ncols):
    src = w_dram[row0:row0 + nkc * 128, col0:col0 + ncols].rearrange("(kc p) c -> p kc c", p=128)
    c.dma(POOL, wslot[:, 0:nkc, 0:ncols], src, reads=[], writes=[wbuf])


def phase_A(c, x, nw_d, w, ident_d, outs):
    if True:
        c.begin()
        hT = c.sb("hT", [128, KC, TOK], BF16)
        hT_b = Buf("hT")
        nw = c.sb("nw_s", [128, KC], F32)
        nw_b = Buf("nw")
        ident = c.sb("ident_s", [128, 128], BF16)
        ident_b = Buf("ident")
        c.dma(SP, nw[:], nw_d[:, :], [], [nw_b])
        c.dma(POOL, ident[:], ident_d[:, :], [], [ident_b])
        NW = 3
        wslots = [(c.sb("w%d" % i, [128, KC, 512], BF16), Buf("w%d" % i)) for i in range(NW)]
        xs = [(c.sb("x%d" % i, [128, D_MODEL], F32), Buf("x%d" % i)) for i in range(2)]
        scr = norm_scratch(c, "n_")
        of32 = [(c.sb("of%d" % i, [128, TOK], F32), Buf("of%d" % i)) for i in range(2)]
        obf = [(c.sb("ob%d" % i, [128, TOK], BF16), Buf("ob%d" % i)) for i in range(2)]
        otk = [(c.sb("ot%d" % i, [128, 512], BF16), Buf("ot%d" % i)) for i in range(3)]

        blocks = []
        for name, c0, n, mode, dt, sg in A_SECTIONS:
            for o in range(0, n, 512):
                blocks.append((name, c0, o, min(512, n - o), mode, dt, sg))
        PRE = NW - 1

        def issue_load(bi):
            name, c0, o, n, mode, dt, sg = blocks[bi]
            ws, wb = wslots[bi % NW]
            load_wblock(c, ws, wb, w, 0, KC, c0 + o, n)

        for bi in range(min(PRE, len(blocks))):
            issue_load(bi)

        for tt in range(TOK // 128):
            xt, xb = xs[tt % 2]
            c.dma(SP, xt[:], x[tt * 128:(tt + 1) * 128, :], [], [xb])
            rmsnorm_to_T(c, xt, xb, scr, hT, hT_b, tt * 128, nw, nw_b, ident, ident_b, "n_")

        finals = []
        cnt = {"f": 0, "b": 0, "t": 0}
        for bi, (name, c0, o, n, mode, dt, sg) in enumerate(blocks):
            if bi + PRE < len(blocks):
                issue_load(bi + PRE)
            ws, wb = wslots[bi % NW]
            od = outs[name]
            if mode == "F":
                for cc in range(0, n, 128):
                    m = min(128, n - cc)
                    if dt == F32:
                        ot, ob = of32[cnt["f"] % 2]
                        cnt["f"] += 1
                    else:
                        ot, ob = obf[cnt["b"] % 2]
                        cnt["b"] += 1
                    for tg in range(TOK // 512):
                        bank, bb = c.next_bank()
                        for kc in range(KC):
                            c.mm(bank[0:m, :], ws[:, kc, cc:cc + m], hT[:, kc, tg * 512:(tg + 1) * 512],
                                 kc == 0, kc == KC - 1, reads=[wb, hT_b], writes=[bb])
                        dst = ot[0:m, tg * 512:(tg + 1) * 512]
                        if sg:
                            c.act(dst, bank[0:m, :], AF.Sigmoid, reads=[bb], writes=[ob])
                        else:
                            c.evac_copy(dst, bank[0:m, :], reads=[bb], writes=[ob])
                    finals.append(c.dma(SP, od[o + cc:o + cc + m, :], ot[0:m, :], [ob], []))
            else:
                for tt in range(TOK // 128):
                    bank, bb = c.next_bank()
                    for kc in range(KC):
                        c.mm(bank[:, 0:n], hT[:, kc, tt * 128:(tt + 1) * 128], ws[:, kc, 0:n],
                             kc == 0, kc == KC - 1, reads=[wb, hT_b], writes=[bb])
                    ot, ob = otk[cnt["t"] % 3]
                    cnt["t"] += 1
                    if sg:
                        c.act(ot[:, 0:n], bank[:, 0:n], AF.Sigmoid, reads=[bb], writes=[ob])
                    else:
                        c.evac_copy(ot[:, 0:n], bank[:, 0:n], reads=[bb], writes=[ob])
                    finals.append(c.dma(SP, od[tt * 128:(tt + 1) * 128, o:o + n], ot[:, 0:n], [ob], []))
        c.end()


def nw_layout(v):
    return np.ascontiguousarray(v.reshape(KC, 128).T)


NH = 4
NCH = SEQ // 128
MASKNEG = -30000.0
Q_R, Q_C, Q_INTER, Q_W, Q_EM = 0, 1, 2, 3, 4
NSB = 3
MSKEW = 1


def phase_BC(c, d, do_m=True, do_a=True):
    cw_d, mv_d, mo_d, gb_d, mnw_d, dv_d, dnw_d = d["cw"], d["mv"], d["mo"], d["gb"], d["mnw"], d["dv"], d["dnw"]
    lam_d, lami_d, ident_d, mask_d, hmT_d, hdT_d = d["lam"], d["lami"], d["ident"], d["maskneg"], d["hmT"], d["hdT"]
    gi_d, gf_d = d["gi"], d["gf"]
    if True:
        c.begin()
        if "pre" in d:
            d["pre"](c)
        P = c.P
        identf = c.sb("identf", [128, 128], F32); identf_b = Buf("identf")
        identb = c.sb("identb", [128, 128], BF16); identb_b = Buf("identb")
        maskf = c.sb("maskf", [128, 128], F32); maskf_b = Buf("maskf")
        maskb = c.sb("maskb", [128, 128], BF16); maskb_b = Buf("maskb")
        onesf = c.sb("onesf", [128, 128], F32); onesf_b = Buf("onesf")
        onesb = c.sb("onesb", [128, 128], BF16); onesb_b = Buf("onesb")
        epst = c.sb("epst", [128, 1], F32); eps_b = Buf("epst")
        c.dma(SP, identf[:], ident_d[:, :], [], [identf_b])
        c.dma(POOL, identb[:], ident_d[:, :], [], [identb_b])
        c.dma(SP, maskf[:], mask_d[:, :], [], [maskf_b])
        c.dma(POOL, maskb[:], mask_d[:, :], [], [maskb_b])
        P.op(POOL, lambda e: e.memset(onesf[:], 1.0), writes=[onesf_b])
        P.op(POOL, lambda e: e.memset(onesb[:], 1.0), writes=[onesb_b])
        P.op(POOL, lambda e: e.memset(epst[:], EPS), writes=[eps_b])
        cw = c.sb("cw_s", [128, 2 * NH, 5], F32); cw_b = Buf("cw")
        c.dma(SP, cw[:], cw_d, [], [cw_b])
        gb = c.sb("gb_s", [128, 2], F32); gb_b = Buf("gb")
        c.dma(SP, gb[:], gb_d, [], [gb_b])
        mnw = c.sb("mnw_s", [128, NH * 256], F32); mnw_b = Buf("mnw")
        c.dma(SP, mnw[:], mnw_d, [], [mnw_b])
        dnw = c.sb("dnw_s", [128, NH * 256], F32); dnw_b = Buf("dnw")
        c.dma(SP, dnw[:], dnw_d, [], [dnw_b])
        lamt = c.sb("lamt", [128, 4, 128], F32); lamt_b = Buf("lamt")
        c.dma(SP, lamt[:], lam_d, [], [lamt_b])
        lami = c.sb("lami_s", [128, 2], F32); lami_b = Buf("lami")
        c.dma(SP, lami[:], lami_d, [], [lami_b])

        g_i = c.sb("g_i", [128, 128], F32); g_f = c.sb("g_f", [128, 128], F32)
        gi_b, gf_b = Buf("g_i"), Buf("g_f")
        c.dma(SP, g_i[:], gi_d.rearrange("j (ci l) -> (j ci) l", l=128), [], [gi_b])
        c.dma(SP, g_f[:], gf_d.rearrange("j (ci l) -> (j ci) l", l=128), [], [gf_b])
        sm = c.sb("gsm", [128, 16], F32); sm_b = Buf("gsm")
        NBF, MPREV, DEC, RLAST = 0, 1, 2, 3
        gq = c.sb("gq", [128, 5, 128], F32); gq_b = Buf("gq")
        g_b = c.sb("g_bb", [128, 128], F32); gbb_b = Buf("g_bb")
        g_ml = c.sb("g_ml", [128, 128], F32); gml_b = Buf("g_ml")
        g_m = c.sb("g_m", [128, 128], F32); gm_b = Buf("g_m")
        c.ts(DVE, g_i[:], g_i[:], gb[:, 0:1], None, ALU.add, None, [gi_b, gb_b], [gi_b])
        c.ts(DVE, sm[:, NBF:NBF + 1], gb[:, 1:2], -1.0, None, ALU.mult, None, [gb_b], [sm_b])
        c.act(g_f[:], g_f[:], AF.Exp, [gf_b, sm_b], [gf_b], bias=sm[:, NBF:NBF + 1], scale=-1.0)
        c.act(g_f[:], g_f[:], AF.Ln, [gf_b, onesf_b], [gf_b], bias=onesf[:, 0:1], scale=1.0)
        c.ts(DVE, g_f[:], g_f[:], -1.0, None, ALU.mult, None, [gf_b], [gf_b])
        P.op(DVE, lambda e: e.tensor_tensor_scan(out=g_b[:], data0=g_f[:], data1=g_f[:], initial=0.0,
                                                 op0=ALU.add, op1=ALU.min), reads=[gf_b], writes=[gbb_b])
        P.op(DVE, lambda e: e.tensor_tensor_scan(out=g_ml[:], data0=g_f[:], data1=g_i[:], initial=-1e30,
                                                 op0=ALU.add, op1=ALU.max), reads=[gf_b, gi_b], writes=[gml_b])
        bankr, bankr_b = c.next_bank()
        c.mm(bankr[0:1, 0:128], g_b[:, 127:128], identf[:], True, True, [gbb_b, identf_b], [bankr_b])
        c.mm(bankr[0:1, 128:256], g_ml[:, 127:128], identf[:], True, True, [gml_b, identf_b], [bankr_b])
        erow = c.sb("erow", [1, 512], F32); erow_b = Buf("erow")
        c.copy(DVE, erow[0:1, 0:256], bankr[0:1, 0:256], [bankr_b], [erow_b])
        P.op(POOL, lambda e: e.memset(erow[0:1, 256:512], 0.0), writes=[erow_b])
        for j in range(NH):
            P.op(DVE, lambda e, j=j: e.tensor_tensor_scan(
                out=erow[0:1, 384 + j * 32:384 + (j + 1) * 32], data0=erow[0:1, j * 32:(j + 1) * 32],
                data1=erow[0:1, 128 + j * 32:128 + (j + 1) * 32], initial=0.0, op0=ALU.add, op1=ALU.max),
                reads=[erow_b], writes=[erow_b])
            c.copy(DVE, erow[0:1, 256 + j * 32 + 1:256 + (j + 1) * 32], erow[0:1, 384 + j * 32:384 + (j + 1) * 32 - 1],
                   [erow_b], [erow_b])
        c.mm(bankr[:, 256:257], erow[0:1, 256:384], onesf[0:1, 0:1], True, True, [erow_b, onesf_b], [bankr_b])
        c.copy(DVE, sm[:, MPREV:MPREV + 1], bankr[:, 256:257], [bankr_b], [sm_b])
        c.stt(g_m[:], g_b[:], sm[:, MPREV:MPREV + 1], g_ml[:], ALU.add, ALU.max, [gbb_b, sm_b, gml_b], [gm_b])
        c.tt(DVE, gq[:, Q_R, :], g_b[:], g_m[:], ALU.subtract, [gbb_b, gm_b], [gq_b])
        c.tt(DVE, gq[:, Q_C, :], g_i[:], g_b[:], ALU.subtract, [gi_b, gbb_b], [gq_b])
        c.copy(DVE, sm[:, RLAST:RLAST + 1], gq[:, Q_R, 127:128], [gq_b], [sm_b])
        c.act(gq[:, Q_INTER, :], gq[:, Q_R, :], AF.Exp, [gq_b, sm_b], [gq_b], bias=sm[:, MPREV:MPREV + 1], scale=1.0)
        c.act(gq[:, Q_W, :], gq[:, Q_C, :], AF.Exp, [gq_b, sm_b], [gq_b], bias=sm[:, RLAST:RLAST + 1], scale=1.0)
        c.act(gq[:, Q_EM, :], g_m[:], AF.Exp, [gm_b], [gq_b], scale=-1.0)
        c.act(sm[:, DEC:DEC + 1], sm[:, RLAST:RLAST + 1], AF.Exp, [sm_b], [sm_b], bias=sm[:, MPREV:MPREV + 1], scale=1.0)
        tq = c.sb("tq", [128, 5, 128], F32); tq_b = Buf("tq")
        for n in range(5):
            bk, bkb = c.next_bank()
            c.mm(bk[:, 0:128], gq[:, n, :], identf[:], True, True, [gq_b, identf_b], [bkb])
            c.copy(DVE, tq[:, n, :], bk[:, 0:128], [bkb], [tq_b])
        decm = c.sb("decm", [128, 128], F32); decm_b = Buf("decm")
        c.ts(DVE, decm[:], onesf[:], sm[:, DEC:DEC + 1], None, ALU.mult, None, [onesf_b, sm_b], [decm_b])
        bk, bkb = c.next_bank()
        c.mm(bk[:, 0:128], decm[:], identf[:], True, True, [decm_b, identf_b], [bkb])
        decb = c.sb("decb", [128, 128], F32); decb_b = Buf("decb")
        c.copy(DVE, decb[:], bk[:, 0:128], [bkb], [decb_b])

        lsm = c.sb("lsm", [128, 8], F32); lsm_b = Buf("lsm")
        lpr = c.sb("lpr", [128, 2, 128], F32); lpr_b = Buf("lpr")
        c.tt(DVE, lpr[:, 0, :], lamt[:, 0, :], lamt[:, 1, :], ALU.mult, [lamt_b], [lpr_b])
        c.tt(DVE, lpr[:, 1, :], lamt[:, 2, :], lamt[:, 3, :], ALU.mult, [lamt_b], [lpr_b])
        P.op(DVE, lambda e: e.reduce_sum(out=lsm[:, 0:2], in_=lpr[:], axis=AX.X), reads=[lpr_b], writes=[lsm_b])
        c.act(lsm[:, 2:4], lsm[:, 0:2], AF.Exp, [lsm_b], [lsm_b])
        c.tt(DVE, lsm[:, 4:5], lsm[:, 2:3], lsm[:, 3:4], ALU.subtract, [lsm_b], [lsm_b])
        c.ts(DVE, lsm[:, 5:6], lsm[:, 4:5], lami[:, 0:1], -1.0, ALU.add, ALU.mult, [lsm_b, lami_b], [lsm_b])
        NLAM = 5

        rawq = c.sb("rawq", [128, SEQ + 3], F32); rawq_b = Buf("rawq")
        rawk = c.sb("rawk", [128, SEQ + 3], F32); rawk_b = Buf("rawk")
        acc = c.sb("acc", [128, SEQ], F32); acc_b = Buf("acc")
        qT = c.sb("qT", [128, SEQ], BF16); qT_b = Buf("qT")
        kT = c.sb("kT", [128, SEQ], BF16); kT_b = Buf("kT")
        va = c.sb("va", [128, NCH, 257], BF16); va_b = Buf("va")
        P.op(POOL, lambda e: e.memset(rawq[:, 0:3], 0.0), writes=[rawq_b])
        P.op(POOL, lambda e: e.memset(rawk[:, 0:3], 0.0), writes=[rawk_b])
        P.op(POOL, lambda e: e.memset(va[:, :, 256:257], 1.0), writes=[va_b])
        CT = c.sb("CT", [128, 257], F32); CT_b = Buf("CT")
        CTb = c.sb("CTb", [128, 257], BF16); CTb_b = Buf("CTb")
        diagR = [(c.sb("diagR%d" % i, [128, 128], F32), Buf("diagR%d" % i)) for i in range(2)]
        Dm = [(c.sb("Dm%d" % i, [128, 128], F32), Buf("Dm%d" % i)) for i in range(2)]
        sdT = [(c.sb("sdT%d" % i, [128, 128], BF16), Buf("sdT%d" % i)) for i in range(2)]
        kw = [(c.sb("kw%d" % i, [128, 128], BF16), Buf("kw%d" % i)) for i in range(2)]
        numS = [(c.sb("numS%d" % i, [128, 257], F32), Buf("numS%d" % i)) for i in range(4)]
        tot = [(c.sb("tot%d" % i, [128, 257], F32), Buf("tot%d" % i)) for i in range(8)]
        CT2 = [(c.sb("CT2_%d" % i, [128, 257], F32), Buf("CT2_%d" % i)) for i in range(2)]
        CTb2 = [(c.sb("CTb2_%d" % i, [128, 257], BF16), Buf("CTb2_%d" % i)) for i in range(2)]
        junk = c.sb("junk", [128, 256], BF16); junk_b = Buf("junk")
        hs = [(c.sb("hs%d" % i, [128, 8], F32), Buf("hs%d" % i)) for i in range(8)]
        g2 = [(c.sb("g2_%d" % i, [128, 256], F32), Buf("g2_%d" % i)) for i in range(4)]
        hmt = [(c.sb("hm%d" % i, [128, 256], BF16), Buf("hm%d" % i)) for i in range(2)]
        mos = [(c.sb("mos%d" % i, [128, 4, 256], BF16), Buf("mos%d" % i)) for i in range(4)]
        hout = [(c.sb("hout%d" % i, [128, 2, 512], BF16), Buf("hout%d" % i)) for i in range(2)]
        kscale = 128.0 ** -0.5

        def conv_silu(raw, raw_b, idx, dst, dst_b, post_scale):
            c.ts(DVE, acc[:], raw[:, 0:SEQ], cw[:, idx, 0:1], cw[:, idx, 4:5], ALU.mult, ALU.add,
                 [raw_b, cw_b], [acc_b])
            for t in range(1, 4):
                c.stt(acc[:], raw[:, t:t + SEQ], cw[:, idx, t:t + 1], acc[:], ALU.mult, ALU.add,
                      [raw_b, cw_b, acc_b], [acc_b])
            if post_scale is None:
                c.act(dst[:], acc[:], AF.Silu, [acc_b], [dst_b])
            else:
                c.act(acc[:], acc[:], AF.Silu, [acc_b], [acc_b])
                c.ts(POOL, dst[:], acc[:], post_scale, None, ALU.mult, None, [acc_b], [dst_b])

        grp = 0
        for j in range(NH if do_m else 0):
            if j == 0:
                c.dma(SP, rawq[:, 3:SEQ + 3], d["qraw"](j), [], [rawq_b])
                c.dma(SP, rawk[:, 3:SEQ + 3], d["kraw"](j), [], [rawk_b])
            c.dma(SP, va[:, :, 0:256], mv_d[:, j * 256:(j + 1) * 256].rearrange("(ci p) v -> p ci v", p=128),
                  [], [va_b])
            conv_silu(rawq, rawq_b, j, qT, qT_b, None)
            conv_silu(rawk, rawk_b, NH + j, kT, kT_b, kscale)
            if j + 1 < NH:
                c.dma(SP, rawq[:, 3:SEQ + 3], d["qraw"](j + 1), [], [rawq_b])
                c.dma(SP, rawk[:, 3:SEQ + 3], d["kraw"](j + 1), [], [rawk_b])
            for k2 in range(2):
                P.op(POOL, lambda e, k2=k2: e.memset(CT2[k2][0][:], 0.0), writes=[CT2[k2][1]])
                P.op(POOL, lambda e, k2=k2: e.memset(CTb2[k2][0][:], 0.0), writes=[CTb2[k2][1]])

            def s0(ci):
                p = j * NCH + ci
                cs = slice(ci * 128, (ci + 1) * 128)
                bA, bA_b = c.banks[ci % 2], c.bank_bufs[ci % 2]
                dR, dR_b = diagR[ci % 2]
                c.ts(POOL, dR[:], identf[:], tq[:, Q_R, p:p + 1], None, ALU.mult, None, [identf_b, tq_b], [dR_b])
                c.mm(bA[:, 0:128], kT[:, cs], qT[:, cs], True, True, [kT_b, qT_b], [bA_b])
                c.mm(bA[:, 128:256], onesf[:], dR[:], True, False, [onesf_b, dR_b], [bA_b])
                c.mm(bA[:, 128:256], identf[:], maskf[:], False, True, [identf_b, maskf_b], [bA_b])
                c.mm(bA[:, 256:384], kT[:, cs], identb[:], True, True, [kT_b, identb_b], [bA_b])
                if ci % 4 == 0:
                    mo_t, mo_b = mos[(ci // 4) % 4]
                    c.dma(SP, mo_t[:], mo_d[ci * 128:(ci + 4) * 128, j * 256:(j + 1) * 256]
                          .rearrange("(cc p) v -> p cc v", p=128), [], [mo_b])

            def s1(ci):
                p = j * NCH + ci
                bA, bA_b = c.banks[ci % 2], c.bank_bufs[ci % 2]
                dm, dm_b = Dm[ci % 2]
                c.act(dm[:], bA[:, 128:256], AF.Exp, [bA_b, tq_b], [dm_b], bias=tq[:, Q_C, p:p + 1], scale=1.0)

            def s2(ci):
                p = j * NCH + ci
                bA, bA_b = c.banks[ci % 2], c.bank_bufs[ci % 2]
                dm, dm_b = Dm[ci % 2]
                sd, sd_b = sdT[ci % 2]
                c.tt(DVE, sd[:], bA[:, 0:128], dm[:], ALU.mult, [bA_b, dm_b], [sd_b])
                kwt, kw_b = kw[ci % 2]
                c.ts(DVE, kwt[:], bA[:, 256:384], tq[:, Q_W, p:p + 1], None, ALU.mult, None, [bA_b, tq_b], [kw_b])

            def s3(ci):
                bB, bB_b = c.banks[2 + ci % 2], c.bank_bufs[2 + ci % 2]
                bD, bD_b = c.banks[6], c.bank_bufs[6]
                sd, sd_b = sdT[ci % 2]
                kwt, kw_b = kw[ci % 2]
                c.mm(bB[:, 0:257], sd[:], va[:, ci, :], True, True, [sd_b, va_b], [bB_b])
                c.mm(bD[:, 0:257], kwt[:], va[:, ci, :], True, True, [kw_b, va_b], [bD_b])

            def s4(ci):
                p = j * NCH + ci
                bB, bB_b = c.banks[2 + ci % 2], c.bank_bufs[2 + ci % 2]
                bD, bD_b = c.banks[6], c.bank_bufs[6]
                ns, ns_b = numS[ci % 4]
                c.copy(ACT, ns[:], bB[:, 0:257], [bB_b], [ns_b])
                cn, cn_b = CT2[ci % 2]
                cp, cp_b = CT2[(ci + 1) % 2]
                c.stt(cn[:], cp[:], decb[:, p:p + 1], bD[:, 0:257], ALU.mult, ALU.add, [cp_b, decb_b, bD_b], [cn_b])

            def s5(ci):
                cs = slice(ci * 128, (ci + 1) * 128)
                cn, cn_b = CT2[ci % 2]
                cb, cb_b = CTb2[ci % 2]
                c.copy(POOL, cb[:], cn[:], [cn_b], [cb_b])
                cbp, cbp_b = CTb2[(ci + 1) % 2]
                bC, bC_b = c.banks[4 + ci % 2], c.bank_bufs[4 + ci % 2]
                c.mm(bC[:, 0:257], qT[:, cs], cbp[:], True, True, [qT_b, cbp_b], [bC_b])

            def s6(ci):
                p = j * NCH + ci
                bC, bC_b = c.banks[4 + ci % 2], c.bank_bufs[4 + ci % 2]
                ns, ns_b = numS[ci % 4]
                tt_, tt_b = tot[ci % 8]
                c.stt(tt_[:], bC[:, 0:257], tq[:, Q_INTER, p:p + 1], ns[:], ALU.mult, ALU.add,
                      [bC_b, tq_b, ns_b], [tt_b])

            def s7(ci):
                tt_, tt_b = tot[ci % 8]
                h_, h_b = hs[ci % 8]
                c.act(h_[:, 7:8], tt_[:, 256:257], AF.Abs, [tt_b], [h_b])
                c.act(junk[:], tt_[:, 0:256], AF.Square, [tt_b], [junk_b, h_b], accum_out=h_[:, 2:3])

            def s8(ci):
                p = j * NCH + ci
                h_, h_b = hs[ci % 8]
                c.ts(DVE, h_[:, 0:1], h_[:, 7:8], tq[:, Q_EM, p:p + 1], None, ALU.max, None, [h_b, tq_b], [h_b])
                mo_t, mo_b = mos[(ci // 4) % 4]
                g2t, g2_b = g2[ci % 4]
                c.tt(POOL, g2t[:], mnw[:, j * 256:(j + 1) * 256], mo_t[:, ci % 4, :], ALU.mult, [mnw_b, mo_b], [g2_b])

            def s9(ci):
                h_, h_b = hs[ci % 8]
                c.act(h_[:, 1:2], h_[:, 0:1], AF.Square, [h_b], [h_b], scale=EPS ** 0.5)
                c.act(h_[:, 4:5], h_[:, 2:3], AF.Sqrt, [h_b], [h_b], bias=h_[:, 1:2], scale=1.0 / 256)

            def s10(ci):
                h_, h_b = hs[ci % 8]
                tt_, tt_b = tot[ci % 8]
                g2t, g2_b = g2[ci % 4]
                P.op(DVE, lambda e, h_=h_: e.reciprocal(out=h_[:, 6:7], in_=h_[:, 4:5]), reads=[h_b], writes=[h_b])
                hm_, hm_b = hmt[ci % 2]
                c.stt(hm_[:], tt_[:, 0:256], h_[:, 6:7], g2t[:], ALU.mult, ALU.mult, [tt_b, h_b, g2_b], [hm_b])

            def s11(ci):
                bT, bT_b = c.banks[7], c.bank_bufs[7]
                hm_, hm_b = hmt[ci % 2]
                for vc in range(2):
                    c.mm(bT[:, vc * 128:(vc + 1) * 128], hm_[:, vc * 128:(vc + 1) * 128], identb[:], True, True,
                         [hm_b, identb_b], [bT_b])

            def s12(ci):
                bT, bT_b = c.banks[7], c.bank_bufs[7]
                ho_t, ho_b = hout[(ci // 4) % 2]
                c.copy(ACT, ho_t[:, :, (ci % 4) * 128:(ci % 4 + 1) * 128],
                       bT[:, 0:256].rearrange("p (a b) -> p a b", a=2), [bT_b], [ho_b])
                if ci % 4 == 3:
                    c.dma(SP, hmT_d[j * 256:(j + 1) * 256, (ci - 3) * 128:(ci + 1) * 128]
                          .rearrange("(a p) t -> p a t", p=128), ho_t[:], [ho_b], [])

            stages = [s0, s1, s2, s3, s4, s5, s6, s7, s8, s9, s10, s11, s12]
            for it in range(NCH + len(stages) - 1):
                for sidx in range(len(stages) - 1, -1, -1):
                    ci = it - sidx
                    if 0 <= ci < NCH:
                        stages[sidx](ci)

        qc = [(c.sb("dq%d" % i, [128, SEQ], BF16), Buf("dq%d" % i)) for i in range(2)]
        kc_ = [(c.sb("dk%d" % i, [128, SEQ], BF16), Buf("dk%d" % i)) for i in range(2)]
        sq = c.sb("sq", [128, SEQ], BF16); sq_b = Buf("sq")
        mx = c.sb("mx", [1, 64], F32); mx_b = Buf("mx")
        nG = c.sb("nG", [128, 1], F32); nG_b = Buf("nG")
        Et = [(c.sb("E%d" % i, [128, 512], BF16), Buf("E%d" % i)) for i in range(NSB + 2)]
        ds = [(c.sb("ds%d" % i, [128, 8], F32), Buf("ds%d" % i)) for i in range(4)]
        dtm = [(c.sb("dt%d" % i, [128, 256], F32), Buf("dt%d" % i)) for i in range(4)]
        dhd = [(c.sb("dhd%d" % i, [128, 256], F32), Buf("dhd%d" % i)) for i in range(4)]
        dhn = [(c.sb("dhn%d" % i, [128, 256], BF16), Buf("dhn%d" % i)) for i in range(4)]
        Os1 = [(c.sb("os1_%d" % i, [128, 257], F32), Buf("os1_%d" % i)) for i in range(4)]
        Os2 = [(c.sb("os2_%d" % i, [128, 257], F32), Buf("os2_%d" % i)) for i in range(4)]
        junkf = c.sb("junkf", [128, 256], F32); junkf_b = Buf("junkf")
        ascale = 128.0 ** -0.5
        Obanks = [(c.banks[i], c.bank_bufs[i]) for i in range(4)]
        Sbanks = [(c.banks[4 + i], c.bank_bufs[4 + i]) for i in range(NSB)]
        Tbanks = [(c.banks[4 + NSB + i], c.bank_bufs[4 + NSB + i]) for i in range(4 - NSB)]
        e_i = 0
        s_i = 0
        t_i = 0
        for j in range(NH if do_a else 0):
            for cc in range(2):
                c.dma(SP, qc[cc][0][:], d["dq"](j, cc), [], [qc[cc][1]])
                c.dma(SP, kc_[cc][0][:], d["dk"](j, cc), [], [kc_[cc][1]])
            c.dma(SP, va[:, :, 0:256], dv_d[:, j * 256:(j + 1) * 256].rearrange("(ci p) v -> p ci v", p=128),
                  [], [va_b])
            tb, tb_b = Tbanks[0]
            for ti, (tns, tns_b) in enumerate([qc[0], qc[1], kc_[0], kc_[1]]):
                c.act(sq[:], tns[:], AF.Square, [tns_b], [sq_b])
                for s8 in range(8):
                    c.mm(tb[0:1, 0:512], onesb[:, 0:1], sq[:, s8 * 512:(s8 + 1) * 512], True, True,
                         [onesb_b, sq_b], [tb_b])
                    P.op(DVE, lambda e, ti=ti, s8=s8: e.reduce_max(out=mx[0:1, ti * 8 + s8:ti * 8 + s8 + 1],
                                                                   in_=tb[0:1, 0:512], axis=AX.X),
                         reads=[tb_b], writes=[mx_b])
            P.op(DVE, lambda e: e.reduce_max(out=mx[0:1, 32:34], in_=mx[0:1, 0:32].rearrange("p (a b) -> p a b", a=2),
                                             axis=AX.X), reads=[mx_b], writes=[mx_b])
            c.tt(DVE, mx[0:1, 34:35], mx[0:1, 32:33], mx[0:1, 33:34], ALU.mult, [mx_b], [mx_b])
            c.act(mx[0:1, 35:36], mx[0:1, 34:35], AF.Sqrt, [mx_b], [mx_b], scale=ascale * ascale)
            c.ts(DVE, mx[0:1, 36:37], mx[0:1, 35:36], -1.0, None, ALU.mult, None, [mx_b], [mx_b])
            c.mm(tb[:, 0:1], onesf[0:1, :], mx[0:1, 36:37], True, True, [onesf_b, mx_b], [tb_b])
            c.copy(DVE, nG[:], tb[:, 0:1], [tb_b], [nG_b])
            if "dbg" in d:
                c.dma(SP, d["dbg"][j, 0:1, 0:64], mx[0:1, :], [mx_b], [])

            def qk_exp(g, kb):
                nonlocal s_i, e_i
                sb_, sb_b = Sbanks[s_i % NSB]; s_i += 1
                et, et_b = Et[e_i % (NSB + 2)]; e_i += 1
                ks = slice(kb * 128, (kb + 1) * 128)
                if kb <= 2 * g:
                    for cc in range(2):
                        diag = (kb == 2 * g)
                        c.mm(sb_[:, cc * 256:(cc + 1) * 256], kc_[cc][0][:, ks], qc[cc][0][:, g * 256:(g + 1) * 256],
                             True, not diag, [kc_[cc][1], qc[cc][1]], [sb_b])
                        if diag:
                            c.mm(sb_[:, cc * 256:cc * 256 + 128], identb[:], maskb[:], False, True,
                                 [identb_b, maskb_b], [sb_b])
                    c.act(et[:], sb_[:], AF.Exp, [sb_b, nG_b], [et_b], bias=nG[:, 0:1], scale=ascale)
                    ilist = (0, 1)
                else:
                    for cc in range(2):
                        c.mm(sb_[:, cc * 256 + 128:(cc + 1) * 256], kc_[cc][0][:, ks],
                             qc[cc][0][:, g * 256 + 128:(g + 1) * 256], True, False, [kc_[cc][1], qc[cc][1]], [sb_b])
                        c.mm(sb_[:, cc * 256 + 128:(cc + 1) * 256], identb[:], maskb[:], False, True,
                             [identb_b, maskb_b], [sb_b])
                    c.act(et[:].rearrange("p (a b) -> p a b", a=2)[:, :, 128:256],
                          sb_[:].rearrange("p (a b) -> p a b", a=2)[:, :, 128:256], AF.Exp,
                          [sb_b, nG_b], [et_b], bias=nG[:, 0:1], scale=ascale)
                    ilist = (1,)
                return (g, kb, et, et_b, ilist)

            def pv(st):
                g, kb, et, et_b, ilist = st
                for cc in range(2):
                    for i in ilist:
                        ob, ob_b = Obanks[cc * 2 + i]
                        last = (kb == 2 * g + i)
                        c.mm(ob[:, 0:257], et[:, cc * 256 + i * 128:cc * 256 + (i + 1) * 128], va[:, kb, :],
                             kb == 0, last, [et_b, va_b], [ob_b])
                if kb == 2 * g + 1:
                    epilogue(g)

            def epilogue(g):
                nonlocal t_i
                for i in range(2):
                    qb = 2 * g + i
                    r = qb % 4
                    o1, o1_b = Obanks[i]
                    o2, o2_b = Obanks[2 + i]
                    os1, os1_b = Os1[r]
                    os2, os2_b = Os2[r]
                    c.copy(DVE, os1[:], o1[:, 0:257], [o1_b], [os1_b])
                    c.copy(DVE, os2[:], o2[:, 0:257], [o2_b], [os2_b])
                R = [(2 * g + i) % 4 for i in range(2)]
                for r in R:
                    P.op(DVE, lambda e, d_=ds[r][0], os1=Os1[r][0]: e.reciprocal(out=d_[:, 0:1], in_=os1[:, 256:257]),
                         reads=[Os1[r][1]], writes=[ds[r][1]])
                for r in R:
                    P.op(DVE, lambda e, d_=ds[r][0], os2=Os2[r][0]: e.reciprocal(out=d_[:, 1:2], in_=os2[:, 256:257]),
                         reads=[Os2[r][1]], writes=[ds[r][1]])
                for r in R:
                    d_, d_b = ds[r]
                    c.ts(DVE, d_[:, 2:3], d_[:, 1:2], lsm[:, NLAM:NLAM + 1], None, ALU.mult, None, [d_b, lsm_b], [d_b])
                for r in R:
                    d_, d_b = ds[r]
                    c.ts(DVE, dtm[r][0][:], Os1[r][0][:, 0:256], d_[:, 0:1], None, ALU.mult, None,
                         [Os1[r][1], d_b], [dtm[r][1]])
                for r in R:
                    d_, d_b = ds[r]
                    c.stt(dhd[r][0][:], Os2[r][0][:, 0:256], d_[:, 2:3], dtm[r][0][:], ALU.mult, ALU.add,
                          [Os2[r][1], d_b, dtm[r][1]], [dhd[r][1]])
                for r in R:
                    P.op(DVE, lambda e, hd_=dhd[r][0], d_=ds[r][0]: e.scalar_tensor_tensor(
                        out=junkf[:], in0=hd_[:], scalar=1.0, in1=hd_[:], op0=ALU.mult, op1=ALU.mult,
                        accum_out=d_[:, 3:4]), reads=[dhd[r][1]], writes=[junkf_b, ds[r][1]])
                defer.append([it_no[0] + 3, stage_b, (g,)])

            def stage_b(g):
                R = [(2 * g + i) % 4 for i in range(2)]
                for r in R:
                    d_, d_b = ds[r]
                    c.act(d_[:, 4:5], d_[:, 3:4], AF.Sqrt, [d_b, eps_b], [d_b], bias=epst[:, 0:1], scale=1.0 / 256)
                for r in R:
                    P.op(DVE, lambda e, d_=ds[r][0]: e.reciprocal(out=d_[:, 5:6], in_=d_[:, 4:5]),
                         reads=[ds[r][1]], writes=[ds[r][1]])
                for r in R:
                    d_, d_b = ds[r]
                    c.ts(DVE, d_[:, 6:7], d_[:, 5:6], lami[:, 1:2], None, ALU.mult, None, [d_b, lami_b], [d_b])
                for r in R:
                    d_, d_b = ds[r]
                    c.stt(dhn[r][0][:], dhd[r][0][:], d_[:, 6:7], dnw[:, j * 256:(j + 1) * 256], ALU.mult, ALU.mult,
                          [dhd[r][1], d_b, dnw_b], [dhn[r][1]])
                for i in range(2):
                    defer.append([it_no[0] + 3, stage_c, (g, i)])

            def stage_c(g, i):
                nonlocal t_i
                qb = 2 * g + i
                r = qb % 4
                hn_, hn_b = dhn[r]
                tb, tb_b = Tbanks[t_i % (4 - NSB)]; t_i += 1
                for vc in range(2):
                    c.mm(tb[:, vc * 128:(vc + 1) * 128], hn_[:, vc * 128:(vc + 1) * 128], identb[:], True, True,
                         [hn_b, identb_b], [tb_b])
                ho_t, ho_b = hout[(qb // 4) % 2]
                c.copy(DVE, ho_t[:, :, (qb % 4) * 128:(qb % 4 + 1) * 128],
                       tb[:, 0:256].rearrange("p (a b) -> p a b", a=2), [tb_b], [ho_b])
                if qb % 4 == 3:
                    c.dma(SP, hdT_d[j * 256:(j + 1) * 256, (qb - 3) * 128:(qb + 1) * 128]
                          .rearrange("(a p) t -> p a t", p=128), ho_t[:], [ho_b], [])

            defer = []
            it_no = [0]

            def run_deferred(flush=False):
                k = 0
                while k < len(defer):
                    if flush or defer[k][0] <= it_no[0]:
                        _, fn, args = defer.pop(k)
                        fn(*args)
                    else:
                        k += 1

            pairs = [(g, kb) for g in range(NCH // 2) for kb in range(2 * g + 2)]
            pend = []
            for (g, kb) in pairs:
                pend.append(qk_exp(g, kb))
                if len(pend) > NSB - 1:
                    pv(pend.pop(0))
                it_no[0] += 1
                run_deferred()
            while pend:
                pv(pend.pop(0))
            while defer:
                run_deferred(flush=True)
        c.end()


IDENT = np.eye(128, dtype=np.float32)
MASKNEG_NP = np.where(np.arange(128)[:, None] <= np.arange(128)[None, :], 0.0, MASKNEG).astype(np.float32)


def lam_init_of(l):
    return 0.8 - 0.6 * math.exp(-0.3 * l)


TG = 512
NFC = D_FF // 128


def phase_D(c, d, final, last):
    x_d, hm_d, hd_d, gmd_d, xo_d = d["x"], d["hmT"], d["hdT"], d["gmd"], d["xo"]
    wbm_d, wbd_d, wout_d, wg_d, wu_d, wd_d = d["wbm"], d["wbd"], d["wout"], d["wg"], d["wu"], d["wd"]
    nw2_d, ident_d = d["nw2"], d["ident"]
    if final:
        fnw_d = d["fnw"]
    if True:
        c.begin()
        P = c.P
        ident = c.sb("ident_s", [128, 128], BF16); ident_b = Buf("ident")
        c.dma(POOL, ident[:], ident_d[:, :], [], [ident_b])
        nw2 = c.sb("nw2_s", [128, KC], F32); nw2_b = Buf("nw2")
        c.dma(SP, nw2[:], nw2_d, [], [nw2_b])
        if final:
            fnw = c.sb("fnw_s", [128, D_MODEL], F32); fnw_b = Buf("fnw")
            c.dma(SP, fnw[:], fnw_d, [], [fnw_b])
        hmg = c.sb("hmg", [128, KC, TG], BF16); hmg_b = Buf("hmg")
        hdg = c.sb("hdg", [128, KC, TG], BF16); hdg_b = Buf("hdg")
        yT = c.sb("yT", [128, KC, TG], BF16); yT_b = Buf("yT")
        xg = c.sb("xg", [128, TG // 128, D_MODEL], F32)
        xg_b = [Buf("xg%d" % i) for i in range(TG // 128)]
        aT = c.sb("aT", [128, NFC, TG], BF16); aT_b = Buf("aT")
        NW = 3
        wslots = [(c.sb("w%d" % i, [128, KC, 512], BF16), Buf("w%d" % i)) for i in range(NW)]
        t1 = [(c.sb("t1_%d" % i, [128, TG], F32), Buf("t1_%d" % i)) for i in range(4)]
        t2 = [(c.sb("t2_%d" % i, [128, TG], F32), Buf("t2_%d" % i)) for i in range(2)]
        sgm = [(c.sb("sgm%d" % i, [128, TG], BF16), Buf("sgm%d" % i)) for i in range(2)]
        sgd = [(c.sb("sgd%d" % i, [128, TG], BF16), Buf("sgd%d" % i)) for i in range(2)]
        scr = norm_scratch(c, "n_")

        wlist = []
        for g in range(TOK // TG):
            for blk in range(4):
                wlist.append((wbm_d, 0, KC, blk * 512, 512))
                wlist.append((wbd_d, 0, KC, blk * 512, 512))
            for cg in range(4):
                wlist.append((wout_d, 0, KC, cg * 512, 512))
            for blk in range(D_FF // 512):
                wlist.append((wg_d, 0, KC, blk * 512, 512))
                wlist.append((wu_d, 0, KC, blk * 512, 512))
            for cg in range(4):
                for fb in range(4):
                    wlist.append((wd_d, fb * 11 * 128, 11, cg * 512, 512))
        wstate = {"issued": 0, "used": 0, "done": 0}

        def issue_one():
            i = wstate["issued"]
            wdr, r0, nkc, c0, ncol = wlist[i]
            ws, wb = wslots[i % NW]
            load_wblock(c, ws, wb, wdr, r0, nkc, c0, ncol)
            wstate["issued"] += 1

        def next_w():
            i = wstate["used"]
            while wstate["issued"] <= i:
                assert wstate["issued"] < wstate["done"] + NW
                issue_one()
            wstate["used"] += 1
            return wslots[i % NW]

        def release_w():
            wstate["done"] = wstate["used"]
            while wstate["issued"] < len(wlist) and wstate["issued"] < wstate["done"] + NW:
                issue_one()

        k2 = 0
        for g in range(TOK // TG):
            ts0 = g * TG
            c.dma(SP, hmg[:], hm_d[:, ts0:ts0 + TG].rearrange("(kc p) t -> p kc t", p=128), [], [hmg_b])
            c.dma(SP, hdg[:], hd_d[:, ts0:ts0 + TG].rearrange("(kc p) t -> p kc t", p=128), [], [hdg_b])
            for tt in range(TG // 128):
                c.dma(SP, xg[:, tt, :], x_d[ts0 + tt * 128:ts0 + (tt + 1) * 128, :], [], [xg_b[tt]])
            for blk in range(4):
                wm, wm_b = next_w()
                for cc in range(4):
                    col = blk * 4 + cc
                    sm_, sm_b = sgm[cc % 2]
                    c.dma(SP, sm_[:], gmd_d[col * 128:(col + 1) * 128, ts0:ts0 + TG], [], [sm_b])
                    bA, bA_b = c.next_bank()
                    for kc in range(KC):
                        c.mm(bA[:, :], wm[:, kc, cc * 128:(cc + 1) * 128], hmg[:, kc, :], kc == 0, kc == KC - 1,
                             [wm_b, hmg_b], [bA_b])
                    a1, a1_b = t1[cc]
                    c.tt(DVE, a1[:], bA[:, :], sm_[:], ALU.mult, [bA_b, sm_b], [a1_b])
                release_w()
                wd_, wd_b = next_w()
                for cc in range(4):
                    col = blk * 4 + cc
                    sd_, sd_b = sgd[cc % 2]
                    c.dma(SP, sd_[:], gmd_d[D_MODEL + col * 128:D_MODEL + (col + 1) * 128, ts0:ts0 + TG], [], [sd_b])
                    bB, bB_b = c.next_bank()
                    for kc in range(KC):
                        c.mm(bB[:, :], wd_[:, kc, cc * 128:(cc + 1) * 128], hdg[:, kc, :], kc == 0, kc == KC - 1,
                             [wd_b, hdg_b], [bB_b])
                    a1, a1_b = t1[cc]
                    a2, a2_b = t2[cc % 2]
                    c.tt(DVE, a2[:], bB[:, :], sd_[:], ALU.mult, [bB_b, sd_b], [a2_b])
                    c.tt(POOL, yT[:, col, :], a1[:], a2[:], ALU.add, [a1_b, a2_b], [yT_b])
                release_w()
            for cg in range(4):
                wo, wo_b = next_w()
                for tt in range(TG // 128):
                    bk, bk_b = c.next_bank()
                    for kc in range(KC):
                        c.mm(bk[:, :], yT[:, kc, tt * 128:(tt + 1) * 128], wo[:, kc, :], kc == 0, kc == KC - 1,
                             [yT_b, wo_b], [bk_b])
                    c.tt(DVE, xg[:, tt, cg * 512:(cg + 1) * 512], bk[:, :], xg[:, tt, cg * 512:(cg + 1) * 512], ALU.add,
                         [bk_b, xg_b[tt]], [xg_b[tt]])
                release_w()
            for tt in range(TG // 128):
                rmsnorm_to_T(c, xg[:, tt, :], xg_b[tt], scr, hmg, hmg_b, tt * 128, nw2, nw2_b, ident, ident_b, "n_")
            for blk in range(D_FF // 512):
                wg_, wg_b = next_w()
                for cc in range(4):
                    bG, bG_b = c.next_bank()
                    for kc in range(KC):
                        c.mm(bG[:, :], wg_[:, kc, cc * 128:(cc + 1) * 128], hmg[:, kc, :], kc == 0, kc == KC - 1,
                             [wg_b, hmg_b], [bG_b])
                    a1, a1_b = t1[cc]
                    c.act(a1[:], bG[:, :], AF.Silu, [bG_b], [a1_b])
                release_w()
                wu_, wu_b = next_w()
                for cc in range(4):
                    fc = blk * 4 + cc
                    bU, bU_b = c.next_bank()
                    for kc in range(KC):
                        c.mm(bU[:, :], wu_[:, kc, cc * 128:(cc + 1) * 128], hmg[:, kc, :], kc == 0, kc == KC - 1,
                             [wu_b, hmg_b], [bU_b])
                    a1, a1_b = t1[cc]
                    c.tt(DVE, aT[:, fc, :], bU[:, :], a1[:], ALU.mult, [bU_b, a1_b], [aT_b])
                release_w()
            for cg in range(4):
                bks = [c.next_bank() for _ in range(TG // 128)]
                for fb in range(4):
                    wdn, wdn_b = next_w()
                    for tt in range(TG // 128):
                        bk, bk_b = bks[tt]
                        for i in range(11):
                            fc = fb * 11 + i
                            c.mm(bk[:, :], aT[:, fc, tt * 128:(tt + 1) * 128], wdn[:, i, :], fc == 0, fc == NFC - 1,
                                 [aT_b, wdn_b], [bk_b])
                    release_w()
                for tt in range(TG // 128):
                    bk, bk_b = bks[tt]
                    c.tt(DVE, xg[:, tt, cg * 512:(cg + 1) * 512], bk[:, :], xg[:, tt, cg * 512:(cg + 1) * 512], ALU.add,
                         [bk_b, xg_b[tt]], [xg_b[tt]])
            for tt in range(TG // 128):
                if final:
                    junk, junk_b = scr["junk"]
                    ss, ss_b = scr["ss"]
                    c.act(junk[:], xg[:, tt, :], AF.Square, [xg_b[tt]], [junk_b, ss_b], accum_out=ss[:, 0:1])
                    c.act(ss[:, 1:2], ss[:, 0:1], AF.Sqrt, [ss_b, scr["eps_b"]], [ss_b], scale=1.0 / D_MODEL,
                          bias=scr["eps"][:, 0:1])
                    P.op(DVE, lambda e, ss=ss: e.reciprocal(out=ss[:, 2:3], in_=ss[:, 1:2]), reads=[ss_b], writes=[ss_b])
                    c.stt(xg[:, tt, :], xg[:, tt, :], ss[:, 2:3], fnw[:], ALU.mult, ALU.mult,
                          [xg_b[tt], ss_b, fnw_b], [xg_b[tt]])
                c.dma(SP, xo_d[ts0 + tt * 128:ts0 + (tt + 1) * 128, :], xg[:, tt, :], [xg_b[tt]], [], sem_key=xg_b[tt])
        c.end(last)


NUSED = 4


def build_fused(depth=DEPTH):
    nc = bass.Bass("TRN2", target_bir_lowering=False)
    di = lambda n, s, dt: nc.dram_tensor(n, s, dt, kind="ExternalInput").ap()
    x_in = di("x", [SEQ, D_MODEL], F32)
    w_in = di("w_in", [DEPTH, D_MODEL, N_IN], F32)
    w_bm = di("w_branch_m", [DEPTH, D_MODEL, D_MODEL], F32)
    w_bd = di("w_branch_d", [DEPTH, D_MODEL, D_MODEL], F32)
    w_out = di("w_out", [DEPTH, D_MODEL, D_MODEL], F32)
    w_g = di("w_ffn_gate", [DEPTH, D_MODEL, D_FF], F32)
    w_u = di("w_ffn_up", [DEPTH, D_MODEL, D_FF], F32)
    w_d = di("w_ffn_down", [DEPTH, D_FF, D_MODEL], F32)
    anw = di("anw", [DEPTH, 128, KC], F32)
    fnw2 = di("fnw2", [DEPTH, 128, KC], F32)
    fnw = di("fnw", [128, D_MODEL], F32)
    cw = di("cw", [DEPTH, 2, 128, 2 * NH, 5], F32)
    gb = di("gb", [DEPTH, 2, 128, 2], F32)
    mnw = di("mnw", [DEPTH, 128, D_MODEL], F32)
    dnw = di("dnw", [DEPTH, 128, D_MODEL], F32)
    lam = di("lam", [DEPTH, 128, 4, 128], F32)
    lami = di("lami", [DEPTH, 128, 2], F32)
    ident = di("ident", [128, 128], F32)
    maskneg = di("maskneg", [128, 128], F32)
    y_out = nc.dram_tensor("y", [SEQ, D_MODEL], F32, kind="ExternalOutput").ap()
    sc = lambda n, s, dt: nc.dram_tensor(n, s, dt).ap()
    qk_s = sc("qk_s", [2048, SEQ], F32)
    mv_s = sc("mv_s", [SEQ, 2048], BF16)
    mo_s = sc("mo_s", [SEQ, 2048], BF16)
    gt_s = sc("gt_s", [16, SEQ], F32)
    dqk_s = sc("dqk_s", [4096, SEQ], BF16)
    dv_s = sc("dv_s", [SEQ, 2048], BF16)
    gmd_s = sc("gmd_s", [4096, SEQ], BF16)
    hm_s = sc("hm_s", [2048, SEQ], BF16)
    hd_s = sc("hd_s", [2048, SEQ], BF16)
    x_s = sc("x_s", [SEQ, D_MODEL], F32)
    scr = {"qk": qk_s, "mv": mv_s, "mo": mo_s, "gt": gt_s, "dqk": dqk_s, "dv": dv_s, "gmd": gmd_s}
    wb = {"wbm": sc("wbm_b", [D_MODEL, D_MODEL], BF16), "wbd": sc("wbd_b", [D_MODEL, D_MODEL], BF16),
          "wout": sc("wout_b", [D_MODEL, D_MODEL], BF16), "wg": sc("wg_b", [D_MODEL, D_FF], BF16),
          "wu": sc("wu_b", [D_MODEL, D_FF], BF16), "wd": sc("wd_b", [D_FF, D_MODEL], BF16)}

    def make_cast(l):
        def pre(c):
            srcs = {"wbm": w_bm[l], "wbd": w_bd[l], "wout": w_out[l], "wg": w_g[l], "wu": w_u[l], "wd": w_d[l]}
            for k in ("wbm", "wbd", "wout", "wg", "wu", "wd"):
                src, dst = srcs[k], wb[k]
                if k in ("wg", "wu"):
                    src = src.rearrange("r (a b) -> r a b", b=1408)
                    dst = dst.rearrange("r (a b) -> r a b", b=1408)
                c.dma(POOL, dst, src, [], [Buf("cast_" + k)])
        return pre

    with contextlib.ExitStack() as es:
        c = Ctx(nc, es)
        c.alloc_banks(8)
        for l in range(depth):
            x_src = x_in if l == 0 else x_s
            final = (l == depth - 1)
            for th in range(2):
                ts = slice(th * TOK, (th + 1) * TOK)
                outs = {}
                for name, c0, n, mode, dt, sg in A_SECTIONS:
                    outs[name] = scr[name][:, ts] if mode == "F" else scr[name][ts, :]
                phase_A(c, x_src[ts, :], anw[l], w_in[l], ident, outs)
            for hh in range(2):
                hs = slice(hh * NH * 256, (hh + 1) * NH * 256)
                d = {
                    "cw": cw[l, hh], "gb": gb[l, hh], "mnw": mnw[l][:, hs], "dnw": dnw[l][:, hs],
                    "lam": lam[l], "lami": lami[l], "ident": ident, "maskneg": maskneg,
                    "mv": mv_s[:, hs], "mo": mo_s[:, hs], "dv": dv_s[:, hs],
                    "gi": gt_s[hh * NH:(hh + 1) * NH, :], "gf": gt_s[8 + hh * NH:8 + (hh + 1) * NH, :],
                    "hmT": hm_s[hs, :], "hdT": hd_s[hs, :],
                    "qraw": (lambda j, hh=hh: qk_s[(hh * NH + j) * 128:(hh * NH + j + 1) * 128, :]),
                    "kraw": (lambda j, hh=hh: qk_s[1024 + (hh * NH + j) * 128:1024 + (hh * NH + j + 1) * 128, :]),
                    "dq": (lambda j, cc, hh=hh: dqk_s[(hh * NH + j) * 256 + cc * 128:(hh * NH + j) * 256 + (cc + 1) * 128, :]),
                    "dk": (lambda j, cc, hh=hh: dqk_s[2048 + (hh * NH + j) * 256 + cc * 128:
                                                      2048 + (hh * NH + j) * 256 + (cc + 1) * 128, :]),
                }
                if hh == 0:
                    d["pre"] = make_cast(l)
                phase_BC(c, d)
            for th in range(2):
                ts = slice(th * TOK, (th + 1) * TOK)
                d = {"x": x_src[ts, :], "hmT": hm_s[:, ts], "hdT": hd_s[:, ts], "gmd": gmd_s[:, ts],
                     "xo": (y_out if final else x_s)[ts, :],
                     "wbm": wb["wbm"], "wbd": wb["wbd"], "wout": wb["wout"], "wg": wb["wg"], "wu": wb["wu"],
                     "wd": wb["wd"],
                     "nw2": fnw2[l], "ident": ident, "fnw": fnw}
                phase_D(c, d, final, last=(l == depth - 1 and th == 1))
        build_fused.stats = (c.P.n_total, dict(c.P.cnt))
    return nc


def host_params(prm):
    L = DEPTH
    anw = np.stack([nw_layout(prm["attn_norm_w"][l]) for l in range(L)])
    fnw2 = np.stack([nw_layout(prm["ffn_norm_w"][l]) for l in range(L)])
    fnw = np.ascontiguousarray(np.broadcast_to(prm["final_norm_w"][None], (128, D_MODEL))).astype(np.float32)
    cw = np.zeros((L, 2, 128, 2 * NH, 5), np.float32)
    gb = np.zeros((L, 2, 128, 2), np.float32)
    for l in range(L):
        cwl, cbl = prm["conv_w"][l], prm["conv_b"][l]
        for hh in range(2):
            for i in range(NH):
                h = hh * NH + i
                cw[l, hh, :, i, 0:4] = cwl[:, h * 128:(h + 1) * 128].T
                cw[l, hh, :, i, 4] = cbl[h * 128:(h + 1) * 128]
                cw[l, hh, :, NH + i, 0:4] = cwl[:, 1024 + h * 128:1024 + (h + 1) * 128].T
                cw[l, hh, :, NH + i, 4] = cbl[1024 + h * 128:1024 + (h + 1) * 128]
            heads = slice(hh * NH, (hh + 1) * NH)
            gb[l, hh, :, 0] = np.repeat(prm["b_igate"][l][heads], NCH)
            gb[l, hh, :, 1] = np.repeat(prm["b_fgate"][l][heads], NCH)
    mnw = np.ascontiguousarray(np.broadcast_to(prm["mlstm_norm_w"][:, None, :], (L, 128, D_MODEL))).astype(np.float32)
    dnw = np.ascontiguousarray(np.broadcast_to(prm["diff_norm_w"][:, None, :], (L, 128, D_MODEL))).astype(np.float32)
    lam = np.stack([np.stack([prm["lambda_q1"][l], prm["lambda_k1"][l], prm["lambda_q2"][l], prm["lambda_k2"][l]])
                    for l in range(L)])
    lam = np.ascontiguousarray(np.broadcast_to(lam[:, None], (L, 128, 4, 128))).astype(np.float32)
    lami = np.zeros((L, 128, 2), np.float32)
    for l in range(L):
        lami[l, :, 0] = lam_init_of(l)
        lami[l, :, 1] = 1.0 - lam_init_of(l)
    return {"anw": anw, "fnw2": fnw2, "fnw": fnw, "cw": cw, "gb": gb, "mnw": mnw, "dnw": dnw, "lam": lam,
            "lami": lami, "ident": IDENT, "maskneg": MASKNEG_NP}


_NC = {}


def kernel(**inputs):
    x = np.ascontiguousarray(inputs["x"], dtype=np.float32)
    prm = {k: np.asarray(v, dtype=np.float32) for k, v in inputs.items() if k != "x"}
    if "nc" not in _NC:
        _NC["nc"] = build_fused()
    hp = host_params(prm)
    big = {k: np.ascontiguousarray(prm[k]) for k in ("w_in", "w_branch_m", "w_branch_d", "w_out",
                                                     "w_ffn_gate", "w_ffn_up", "w_ffn_down")}
    maps = []
    for b in range(NUSED):
        m = {"x": np.ascontiguousarray(x[b])}
        m.update(big)
        m.update(hp)
        maps.append(m)
    res = run_bass_kernel_spmd(_NC["nc"], maps, core_ids=list(range(NUSED)))
    return np.stack([np.asarray(res.results[b]["y"]) for b in range(NUSED)]).astype(np.float32)
```

```python
import contextlib
import math
import numpy as np
import ml_dtypes
import concourse.bass as bass
import concourse.mybir as mybir
from concourse.bass_utils import run_bass_kernel_spmd

F32 = mybir.dt.float32
BF16 = mybir.dt.bfloat16
AF = mybir.ActivationFunctionType
ALU = mybir.AluOpType
AX = mybir.AxisListType
NPBF = ml_dtypes.bfloat16

D_MODEL = 2048
BATCH = 4
SEQ = 4096
DEPTH = 4
NCORES = 8
TOK = 2048
D_FF = 5632
N_IN = 16400
EPS = 1e-6
KC = D_MODEL // 128

PE, ACT, DVE, POOL, SP = "pe", "act", "dve", "pool", "sp"
COMPUTE = (PE, ACT, DVE, POOL)


class Buf:
    __slots__ = ("name", "last_write", "reads", "dma_sem")

    def __init__(self, name):
        self.name = name
        self.last_write = None
        self.reads = {}
        self.dma_sem = None


class Instr:
    __slots__ = ("eng", "fn", "deps", "is_dma", "sem_key", "sig_val", "needs_sig")

    def __init__(self, eng, fn, is_dma, sem_key):
        self.eng = eng
        self.fn = fn
        self.deps = []
        self.is_dma = is_dma
        self.sem_key = sem_key
        self.sig_val = None
        self.needs_sig = False


class Prog:
    NDMA = 72

    def __init__(self, nc, es):
        self.nc = nc
        self.esem = {e: es.enter_context(nc.semaphore("s_" + e)) for e in COMPUTE}
        self.dsem = [es.enter_context(nc.semaphore("d_%d" % i)) for i in range(self.NDMA)]
        self.cnt = {e: 0 for e in COMPUTE}
        self.dcnt = [0] * self.NDMA
        self.barrier = []
        self.n_total = 0
        self._reset()

    def _reset(self):
        self.q = {e: [] for e in (PE, ACT, DVE, POOL, SP)}
        self.started = set()

    def op(self, eng, fn, reads=(), writes=(), dma=False, sem_key=None, pe_accum=False):
        if dma and sem_key is None:
            sem_key = writes[0] if len(writes) else reads[0]
        ins = Instr(eng, fn, dma, sem_key)
        deps = []
        if eng not in self.started:
            self.started.add(eng)
            ins.deps.extend(self.barrier)
        for b in reads:
            if b.last_write is not None:
                deps.append(b.last_write)
        for b in writes:
            if b.last_write is not None:
                deps.append(b.last_write)
            deps.extend(b.reads.values())
        seen = set()
        for d in deps:
            if d is ins or id(d) in seen:
                continue
            seen.add(id(d))
            if (not d.is_dma) and d.eng == PE and eng == PE and not dma:
                continue
            ins.deps.append(d)
            d.needs_sig = True
        for b in reads:
            b.reads[("d", id(sem_key)) if dma else eng] = ins
        for b in writes:
            b.last_write = ins
            b.reads = {}
        self.q[eng].append(ins)
        return ins

    def end_phase(self, last=False):
        nc = self.nc
        bar = {}
        for e in self.q:
            if self.q[e]:
                bar[id(self.q[e][-1])] = self.q[e][-1]
            for ins in self.q[e]:
                if ins.is_dma:
                    bar["k%d" % id(ins.sem_key)] = ins
        barrier = []
        seen = set()
        for ins in bar.values():
            if id(ins) not in seen:
                seen.add(id(ins))
                barrier.append(ins)
                ins.needs_sig = True
        nkeys = 0
        for e in self.q:
            for ins in self.q[e]:
                if ins.is_dma and ins.needs_sig and ins.sem_key.dma_sem is None:
                    ins.sem_key.dma_sem = nkeys
                    nkeys += 1
        assert nkeys <= self.NDMA, nkeys
        for e in self.q:
            for ins in self.q[e]:
                self.n_total += 1
                if not ins.needs_sig:
                    continue
                if ins.is_dma:
                    k = ins.sem_key.dma_sem
                    self.dcnt[k] += 16
                    ins.sig_val = (self.dsem[k], self.dcnt[k], 16)
                else:
                    self.cnt[ins.eng] += 1
                    ins.sig_val = (self.esem[ins.eng], self.cnt[ins.eng], 1)
        q = self.q
        with nc.Block() as block:
            def run(e, eng_obj):
                waited = {}
                for ins in q[e]:
                    for d in ins.deps:
                        sem, val, _ = d.sig_val
                        key = id(sem)
                        if waited.get(key, 0) >= val:
                            continue
                        waited[key] = val
                        eng_obj.wait_ge(sem, val)
                    bi = ins.fn(eng_obj)
                    if ins.needs_sig:
                        sem, val, inc = ins.sig_val
                        bi.then_inc(sem, inc)
                if e == SP and last:
                    for ins in barrier:
                        sem, val, _ = ins.sig_val
                        if waited.get(id(sem), 0) >= val:
                            continue
                        waited[id(sem)] = val
                        eng_obj.wait_ge(sem, val)

            if q[PE]:
                @block.tensor
                def _(eng):
                    run(PE, eng)
            if q[ACT]:
                @block.scalar
                def _(eng):
                    run(ACT, eng)
            if q[DVE]:
                @block.vector
                def _(eng):
                    run(DVE, eng)
            if q[POOL]:
                @block.gpsimd
                def _(eng):
                    run(POOL, eng)
            if q[SP] or last:
                @block.sync
                def _(eng):
                    run(SP, eng)
        for ins in barrier:
            ins.fn = None
        for e in q:
            for ins in q[e]:
                ins.fn = None
                ins.deps = None
        self.barrier = barrier
        self._reset()


class Ctx:
    def __init__(self, nc, es):
        self.nc = nc
        self.ges = es
        self.es = None
        self.P = Prog(nc, es)
        self.uid = 0
        self.banks = []
        self.bank_bufs = []
        self.bank_i = 0
        self.evac_i = 0

    def sb(self, name, shape, dt):
        self.uid += 1
        return self.es.enter_context(self.nc.sbuf_tensor("%s_%d" % (name, self.uid), list(shape), dt))

    def begin(self):
        self.es = contextlib.ExitStack()
        self.es.__enter__()
        self.bank_bufs = [Buf("bank%d" % i) for i in range(len(self.banks))]

    def end(self, last=False):
        self.P.end_phase(last)
        self.es.__exit__(None, None, None)
        self.es = None

    def alloc_banks(self, n=8):
        for i in range(n):
            self.banks.append(self.ges.enter_context(self.nc.psum_tensor("bank%d" % i, [128, 512], F32)))
            self.bank_bufs.append(Buf("bank%d" % i))

    def next_bank(self):
        i = self.bank_i % len(self.banks)
        self.bank_i += 1
        return self.banks[i], self.bank_bufs[i]

    def mm(self, out, lhsT, rhs, start, stop, reads, writes):
        return self.P.op(PE, lambda e: e.matmul(out, lhsT, rhs, start=start, stop=stop),
                         reads=reads, writes=writes)

    def act(self, out, in_, func, reads, writes, bias=None, scale=None, accum_out=None):
        kw = {}
        if bias is not None:
            kw["bias"] = bias
        if scale is not None:
            kw["scale"] = scale
        if accum_out is not None:
            kw["accum_out"] = accum_out
        return self.P.op(ACT, lambda e: e.activation(out=out, in_=in_, func=func, **kw),
                         reads=reads, writes=writes)

    def dma(self, eng, out, in_, reads, writes, sem_key=None):
        return self.P.op(eng, lambda e: e.dma_start(out=out, in_=in_), reads=reads, writes=writes,
                         dma=True, sem_key=sem_key)

    def tt(self, eng, out, in0, in1, op, reads, writes):
        return self.P.op(eng, lambda e: e.tensor_tensor(out=out, in0=in0, in1=in1, op=op),
                         reads=reads, writes=writes)

    def ts(self, eng, out, in0, s1, s2, op0, op1, reads, writes, accum_out=None):
        if op1 is None:
            return self.P.op(eng, lambda e: e.tensor_scalar(out=out, in0=in0, scalar1=s1, scalar2=None, op0=op0),
                             reads=reads, writes=writes)
        if accum_out is not None:
            return self.P.op(eng, lambda e: e.tensor_scalar(out=out, in0=in0, scalar1=s1, scalar2=s2, op0=op0,
                                                            op1=op1, accum_out=accum_out),
                             reads=reads, writes=writes)
        return self.P.op(eng, lambda e: e.tensor_scalar(out=out, in0=in0, scalar1=s1, scalar2=s2, op0=op0, op1=op1),
                         reads=reads, writes=writes)

    def stt(self, out, in0, scalar, in1, op0, op1, reads, writes):
        return self.P.op(DVE, lambda e: e.scalar_tensor_tensor(out=out, in0=in0, scalar=scalar, in1=in1,
                                                               op0=op0, op1=op1),
                         reads=reads, writes=writes)

    def copy(self, eng, out, in_, reads, writes):
        if eng == ACT:
            return self.P.op(ACT, lambda e: e.copy(out=out, in_=in_), reads=reads, writes=writes)
        return self.P.op(eng, lambda e: e.tensor_copy(out=out, in_=in_), reads=reads, writes=writes)

    def evac_copy(self, out, in_, reads, writes):
        self.evac_i += 1
        return self.copy(ACT if self.evac_i % 2 else DVE, out, in_, reads, writes)


def rmsnorm_to_T(c, xt, xbuf, scratch, hT, hT_buf, tok0, nw, nw_buf, ident, ident_buf, pfx):
    junk, junk_b = scratch["junk"]
    ss, ss_b = scratch["ss"]
    hn, hn_b = scratch["hn"]
    c.act(junk[:], xt[:], AF.Square, reads=[xbuf], writes=[junk_b, ss_b], accum_out=ss[:, 0:1])
    c.act(ss[:, 1:2], ss[:, 0:1], AF.Sqrt, reads=[ss_b, scratch["eps_b"]], writes=[ss_b], scale=1.0 / D_MODEL, bias=scratch["eps"][:, 0:1])
    c.P.op(DVE, lambda e: e.reciprocal(out=ss[:, 2:3], in_=ss[:, 1:2]), reads=[ss_b], writes=[ss_b])
    c.act(hn[:, 0:1024], xt[:, 0:1024], AF.Copy, reads=[xbuf, ss_b], writes=[hn_b], scale=ss[:, 2:3])
    c.ts(DVE, hn[:, 1024:2048], xt[:, 1024:2048], ss[:, 2:3], None, ALU.mult, None, reads=[xbuf, ss_b], writes=[hn_b])
    for kq in range(KC // 4):
        bank, bb = c.next_bank()
        for j in range(4):
            kc = kq * 4 + j
            c.mm(bank[:, j * 128:(j + 1) * 128], hn[:, kc * 128:(kc + 1) * 128], ident[:], True, True,
                 reads=[hn_b, ident_buf], writes=[bb])
        for j in range(4):
            kc = kq * 4 + j
            if j % 2 == 0:
                c.act(hT[:, kc, tok0:tok0 + 128], bank[:, j * 128:(j + 1) * 128], AF.Copy,
                      reads=[bb, nw_buf], writes=[hT_buf], scale=nw[:, kc:kc + 1])
            else:
                c.ts(DVE, hT[:, kc, tok0:tok0 + 128], bank[:, j * 128:(j + 1) * 128], nw[:, kc:kc + 1], None,
                     ALU.mult, None, reads=[bb, nw_buf], writes=[hT_buf])


def norm_scratch(c, pfx):
    eps = c.sb(pfx + "eps", [128, 1], F32)
    eb = Buf(pfx + "eps")
    c.P.op(POOL, lambda e: e.memset(eps[:], EPS), writes=[eb])
    return {
        "junk": (c.sb(pfx + "junk", [128, 2048], BF16), Buf(pfx + "junk")),
        "ss": (c.sb(pfx + "ss", [128, 4], F32), Buf(pfx + "ss")),
        "hn": (c.sb(pfx + "hn", [128, 2048], BF16), Buf(pfx + "hn")),
        "eps": eps, "eps_b": eb,
    }


A_SECTIONS = [
    ("qk", 0, 2048, "F", F32, False),
    ("mv", 2048, 2048, "T", BF16, False),
    ("mo", 4096, 2048, "T", BF16, True),
    ("gt", 6144, 16, "F", F32, False),
    ("dqk", 6160, 4096, "F", BF16, False),
    ("dv", 10256, 2048, "T", BF16, False),
    ("gmd", 12304, 4096, "F", BF16, True),
]


def load_wblock(c, wslot, wbuf, w_dram, row0, nkc, col0, ncols):
    src = w_dram[row0:row0 + nkc * 128, col0:col0 + ncols].rearrange("(kc p) c -> p kc c", p=128)
    c.dma(POOL, wslot[:, 0:nkc, 0:ncols], src, reads=[], writes=[wbuf])


def phase_A(c, x, nw_d, w, ident_d, outs):
    if True:
        c.begin()
        hT = c.sb("hT", [128, KC, TOK], BF16)
        hT_b = Buf("hT")
        nw = c.sb("nw_s", [128, KC], F32)
        nw_b = Buf("nw")
        ident = c.sb("ident_s", [128, 128], BF16)
        ident_b = Buf("ident")
        c.dma(SP, nw[:], nw_d[:, :], [], [nw_b])
        c.dma(POOL, ident[:], ident_d[:, :], [], [ident_b])
        NW = 5
        wslots = [(c.sb("w%d" % i, [128, KC, 512], BF16), Buf("w%d" % i)) for i in range(NW)]
        xs = [(c.sb("x%d" % i, [128, D_MODEL], F32), Buf("x%d" % i)) for i in range(2)]
        scr = norm_scratch(c, "n_")
        of32 = [(c.sb("of%d" % i, [128, TOK], F32), Buf("of%d" % i)) for i in range(2)]
        obf = [(c.sb("ob%d" % i, [128, TOK], BF16), Buf("ob%d" % i)) for i in range(2)]
        otk = [(c.sb("ot%d" % i, [128, 512], BF16), Buf("ot%d" % i)) for i in range(3)]

        blocks = []
        for name, c0, n, mode, dt, sg in A_SECTIONS:
            for o in range(0, n, 512):
                blocks.append((name, c0, o, min(512, n - o), mode, dt, sg))
        PRE = NW - 1

        def issue_load(bi):
            name, c0, o, n, mode, dt, sg = blocks[bi]
            ws, wb = wslots[bi % NW]
            load_wblock(c, ws, wb, w, 0, KC, c0 + o, n)

        for bi in range(min(PRE, len(blocks))):
            issue_load(bi)

        for tt in range(TOK // 128):
            xt, xb = xs[tt % 2]
            c.dma(SP, xt[:], x[tt * 128:(tt + 1) * 128, :], [], [xb])
            rmsnorm_to_T(c, xt, xb, scr, hT, hT_b, tt * 128, nw, nw_b, ident, ident_b, "n_")

        finals = []
        cnt = {"f": 0, "b": 0, "t": 0}
        for bi, (name, c0, o, n, mode, dt, sg) in enumerate(blocks):
            if bi + PRE < len(blocks):
                issue_load(bi + PRE)
            ws, wb = wslots[bi % NW]
            od = outs[name]
            if mode == "F":
                for cc in range(0, n, 128):
                    m = min(128, n - cc)
                    if dt == F32:
                        ot, ob = of32[cnt["f"] % 2]
                        cnt["f"] += 1
                    else:
                        ot, ob = obf[cnt["b"] % 2]
                        cnt["b"] += 1
                    for tg in range(TOK // 512):
                        bank, bb = c.next_bank()
                        for kc in range(KC):
                            c.mm(bank[0:m, :], ws[:, kc, cc:cc + m], hT[:, kc, tg * 512:(tg + 1) * 512],
                                 kc == 0, kc == KC - 1, reads=[wb, hT_b], writes=[bb])
                        dst = ot[0:m, tg * 512:(tg + 1) * 512]
                        if sg:
                            c.act(dst, bank[0:m, :], AF.Sigmoid, reads=[bb], writes=[ob])
                        else:
                            c.evac_copy(dst, bank[0:m, :], reads=[bb], writes=[ob])
                    finals.append(c.dma(SP, od[o + cc:o + cc + m, :], ot[0:m, :], [ob], []))
            else:
                for tt in range(TOK // 128):
                    bank, bb = c.next_bank()
                    for kc in range(KC):
                        c.mm(bank[:, 0:n], hT[:, kc, tt * 128:(tt + 1) * 128], ws[:, kc, 0:n],
                             kc == 0, kc == KC - 1, reads=[wb, hT_b], writes=[bb])
                    ot, ob = otk[cnt["t"] % 3]
                    cnt["t"] += 1
                    if sg:
                        c.act(ot[:, 0:n], bank[:, 0:n], AF.Sigmoid, reads=[bb], writes=[ob])
                    else:
                        c.evac_copy(ot[:, 0:n], bank[:, 0:n], reads=[bb], writes=[ob])
                    finals.append(c.dma(SP, od[tt * 128:(tt + 1) * 128, o:o + n], ot[:, 0:n], [ob], []))
        c.end()


def nw_layout(v):
    return np.ascontiguousarray(v.reshape(KC, 128).T)


NH = 4
NCH = SEQ // 128
MASKNEG = -30000.0
Q_R, Q_C, Q_INTER, Q_W, Q_EM = 0, 1, 2, 3, 4
NSB = 3
MSKEW = 1


def phase_BC(c, d, do_m=True, do_a=True):
    cw_d, mv_d, mo_d, gb_d, mnw_d, dv_d, dnw_d = d["cw"], d["mv"], d["mo"], d["gb"], d["mnw"], d["dv"], d["dnw"]
    lam_d, lami_d, ident_d, mask_d, hmT_d, hdT_d = d["lam"], d["lami"], d["ident"], d["maskneg"], d["hmT"], d["hdT"]
    gi_d, gf_d = d["gi"], d["gf"]
    if True:
        c.begin()
        if "pre" in d:
            d["pre"](c)
        P = c.P
        identf = c.sb("identf", [128, 128], F32); identf_b = Buf("identf")
        identb = c.sb("identb", [128, 128], BF16); identb_b = Buf("identb")
        maskf = c.sb("maskf", [128, 128], F32); maskf_b = Buf("maskf")
        maskb = c.sb("maskb", [128, 128], BF16); maskb_b = Buf("maskb")
        onesf = c.sb("onesf", [128, 128], F32); onesf_b = Buf("onesf")
        onesb = c.sb("onesb", [128, 128], BF16); onesb_b = Buf("onesb")
        epst = c.sb("epst", [128, 1], F32); eps_b = Buf("epst")
        c.dma(SP, identf[:], ident_d[:, :], [], [identf_b])
        c.dma(POOL, identb[:], ident_d[:, :], [], [identb_b])
        c.dma(SP, maskf[:], mask_d[:, :], [], [maskf_b])
        c.dma(POOL, maskb[:], mask_d[:, :], [], [maskb_b])
        P.op(POOL, lambda e: e.memset(onesf[:], 1.0), writes=[onesf_b])
        P.op(POOL, lambda e: e.memset(onesb[:], 1.0), writes=[onesb_b])
        P.op(POOL, lambda e: e.memset(epst[:], EPS), writes=[eps_b])
        cw = c.sb("cw_s", [128, 2 * NH, 5], F32); cw_b = Buf("cw")
        c.dma(SP, cw[:], cw_d, [], [cw_b])
        gb = c.sb("gb_s", [128, 2], F32); gb_b = Buf("gb")
        c.dma(SP, gb[:], gb_d, [], [gb_b])
        mnw = c.sb("mnw_s", [128, NH * 256], F32); mnw_b = Buf("mnw")
        c.dma(SP, mnw[:], mnw_d, [], [mnw_b])
        dnw = c.sb("dnw_s", [128, NH * 256], F32); dnw_b = Buf("dnw")
        c.dma(SP, dnw[:], dnw_d, [], [dnw_b])
        lamt = c.sb("lamt", [128, 4, 128], F32); lamt_b = Buf("lamt")
        c.dma(SP, lamt[:], lam_d, [], [lamt_b])
        lami = c.sb("lami_s", [128, 2], F32); lami_b = Buf("lami")
        c.dma(SP, lami[:], lami_d, [], [lami_b])

        g_i = c.sb("g_i", [128, 128], F32); g_f = c.sb("g_f", [128, 128], F32)
        gi_b, gf_b = Buf("g_i"), Buf("g_f")
        c.dma(SP, g_i[:], gi_d.rearrange("j (ci l) -> (j ci) l", l=128), [], [gi_b])
        c.dma(SP, g_f[:], gf_d.rearrange("j (ci l) -> (j ci) l", l=128), [], [gf_b])
        sm = c.sb("gsm", [128, 16], F32); sm_b = Buf("gsm")
        NBF, MPREV, DEC, RLAST = 0, 1, 2, 3
        gq = c.sb("gq", [128, 5, 128], F32); gq_b = Buf("gq")
        g_b = c.sb("g_bb", [128, 128], F32); gbb_b = Buf("g_bb")
        g_ml = c.sb("g_ml", [128, 128], F32); gml_b = Buf("g_ml")
        g_m = c.sb("g_m", [128, 128], F32); gm_b = Buf("g_m")
        c.ts(DVE, g_i[:], g_i[:], gb[:, 0:1], None, ALU.add, None, [gi_b, gb_b], [gi_b])
        c.ts(DVE, sm[:, NBF:NBF + 1], gb[:, 1:2], -1.0, None, ALU.mult, None, [gb_b], [sm_b])
        c.act(g_f[:], g_f[:], AF.Exp, [gf_b, sm_b], [gf_b], bias=sm[:, NBF:NBF + 1], scale=-1.0)
        c.act(g_f[:], g_f[:], AF.Ln, [gf_b, onesf_b], [gf_b], bias=onesf[:, 0:1], scale=1.0)
        c.ts(DVE, g_f[:], g_f[:], -1.0, None, ALU.mult, None, [gf_b], [gf_b])
        P.op(DVE, lambda e: e.tensor_tensor_scan(out=g_b[:], data0=g_f[:], data1=g_f[:], initial=0.0,
                                                 op0=ALU.add, op1=ALU.min), reads=[gf_b], writes=[gbb_b])
        P.op(DVE, lambda e: e.tensor_tensor_scan(out=g_ml[:], data0=g_f[:], data1=g_i[:], initial=-1e30,
                                                 op0=ALU.add, op1=ALU.max), reads=[gf_b, gi_b], writes=[gml_b])
        bankr, bankr_b = c.next_bank()
        c.mm(bankr[0:1, 0:128], g_b[:, 127:128], identf[:], True, True, [gbb_b, identf_b], [bankr_b])
        c.mm(bankr[0:1, 128:256], g_ml[:, 127:128], identf[:], True, True, [gml_b, identf_b], [bankr_b])
        erow = c.sb("erow", [1, 512], F32); erow_b = Buf("erow")
        c.copy(DVE, erow[0:1, 0:256], bankr[0:1, 0:256], [bankr_b], [erow_b])
        P.op(POOL, lambda e: e.memset(erow[0:1, 256:512], 0.0), writes=[erow_b])
        for j in range(NH):
            P.op(DVE, lambda e, j=j: e.tensor_tensor_scan(
                out=erow[0:1, 384 + j * 32:384 + (j + 1) * 32], data0=erow[0:1, j * 32:(j + 1) * 32],
                data1=erow[0:1, 128 + j * 32:128 + (j + 1) * 32], initial=0.0, op0=ALU.add, op1=ALU.max),
                reads=[erow_b], writes=[erow_b])
            c.copy(DVE, erow[0:1, 256 + j * 32 + 1:256 + (j + 1) * 32], erow[0:1, 384 + j * 32:384 + (j + 1) * 32 - 1],
                   [erow_b], [erow_b])
        c.mm(bankr[:, 256:257], erow[0:1, 256:384], onesf[0:1, 0:1], True, True, [erow_b, onesf_b], [bankr_b])
        c.copy(DVE, sm[:, MPREV:MPREV + 1], bankr[:, 256:257], [bankr_b], [sm_b])
        c.stt(g_m[:], g_b[:], sm[:, MPREV:MPREV + 1], g_ml[:], ALU.add, ALU.max, [gbb_b, sm_b, gml_b], [gm_b])
        c.tt(DVE, gq[:, Q_R, :], g_b[:], g_m[:], ALU.subtract, [gbb_b, gm_b], [gq_b])
        c.tt(DVE, gq[:, Q_C, :], g_i[:], g_b[:], ALU.subtract, [gi_b, gbb_b], [gq_b])
        c.copy(DVE, sm[:, RLAST:RLAST + 1], gq[:, Q_R, 127:128], [gq_b], [sm_b])
        c.act(gq[:, Q_INTER, :], gq[:, Q_R, :], AF.Exp, [gq_b, sm_b], [gq_b], bias=sm[:, MPREV:MPREV + 1], scale=1.0)
        c.act(gq[:, Q_W, :], gq[:, Q_C, :], AF.Exp, [gq_b, sm_b], [gq_b], bias=sm[:, RLAST:RLAST + 1], scale=1.0)
        c.act(gq[:, Q_EM, :], g_m[:], AF.Exp, [gm_b], [gq_b], scale=-1.0)
        c.act(sm[:, DEC:DEC + 1], sm[:, RLAST:RLAST + 1], AF.Exp, [sm_b], [sm_b], bias=sm[:, MPREV:MPREV + 1], scale=1.0)
        tq = c.sb("tq", [128, 5, 128], F32); tq_b = Buf("tq")
        for n in range(5):
            bk, bkb = c.next_bank()
            c.mm(bk[:, 0:128], gq[:, n, :], identf[:], True, True, [gq_b, identf_b], [bkb])
            c.copy(DVE, tq[:, n, :], bk[:, 0:128], [bkb], [tq_b])
        decm = c.sb("decm", [128, 128], F32); decm_b = Buf("decm")
        c.ts(DVE, decm[:], onesf[:], sm[:, DEC:DEC + 1], None, ALU.mult, None, [onesf_b, sm_b], [decm_b])
        bk, bkb = c.next_bank()
        c.mm(bk[:, 0:128], decm[:], identf[:], True, True, [decm_b, identf_b], [bkb])
        decb = c.sb("decb", [128, 128], F32); decb_b = Buf("decb")
        c.copy(DVE, decb[:], bk[:, 0:128], [bkb], [decb_b])

        lsm = c.sb("lsm", [128, 8], F32); lsm_b = Buf("lsm")
        lpr = c.sb("lpr", [128, 2, 128], F32); lpr_b = Buf("lpr")
        c.tt(DVE, lpr[:, 0, :], lamt[:, 0, :], lamt[:, 1, :], ALU.mult, [lamt_b], [lpr_b])
        c.tt(DVE, lpr[:, 1, :], lamt[:, 2, :], lamt[:, 3, :], ALU.mult, [lamt_b], [lpr_b])
        P.op(DVE, lambda e: e.reduce_sum(out=lsm[:, 0:2], in_=lpr[:], axis=AX.X), reads=[lpr_b], writes=[lsm_b])
        c.act(lsm[:, 2:4], lsm[:, 0:2], AF.Exp, [lsm_b], [lsm_b])
        c.tt(DVE, lsm[:, 4:5], lsm[:, 2:3], lsm[:, 3:4], ALU.subtract, [lsm_b], [lsm_b])
        c.ts(DVE, lsm[:, 5:6], lsm[:, 4:5], lami[:, 0:1], -1.0, ALU.add, ALU.mult, [lsm_b, lami_b], [lsm_b])
        NLAM = 5

        rawq = c.sb("rawq", [128, SEQ + 3], F32); rawq_b = Buf("rawq")
        rawk = c.sb("rawk", [128, SEQ + 3], F32); rawk_b = Buf("rawk")
        acc = c.sb("acc", [128, SEQ], F32); acc_b = Buf("acc")
        qT = c.sb("qT", [128, SEQ], BF16); qT_b = Buf("qT")
        kT = c.sb("kT", [128, SEQ], BF16); kT_b = Buf("kT")
        va = c.sb("va", [128, NCH, 257], BF16); va_b = Buf("va")
        P.op(POOL, lambda e: e.memset(rawq[:, 0:3], 0.0), writes=[rawq_b])
        P.op(POOL, lambda e: e.memset(rawk[:, 0:3], 0.0), writes=[rawk_b])
        P.op(POOL, lambda e: e.memset(va[:, :, 256:257], 1.0), writes=[va_b])
        CT = c.sb("CT", [128, 257], F32); CT_b = Buf("CT")
        CTb = c.sb("CTb", [128, 257], BF16); CTb_b = Buf("CTb")
        diagR = [(c.sb("diagR%d" % i, [128, 128], F32), Buf("diagR%d" % i)) for i in range(2)]
        Dm = [(c.sb("Dm%d" % i, [128, 128], F32), Buf("Dm%d" % i)) for i in range(2)]
        sdT = [(c.sb("sdT%d" % i, [128, 128], BF16), Buf("sdT%d" % i)) for i in range(2)]
        kw = [(c.sb("kw%d" % i, [128, 128], BF16), Buf("kw%d" % i)) for i in range(2)]
        numS = [(c.sb("numS%d" % i, [128, 257], F32), Buf("numS%d" % i)) for i in range(4)]
        tot = [(c.sb("tot%d" % i, [128, 257], F32), Buf("tot%d" % i)) for i in range(8)]
        CT2 = [(c.sb("CT2_%d" % i, [128, 257], F32), Buf("CT2_%d" % i)) for i in range(2)]
        CTb2 = [(c.sb("CTb2_%d" % i, [128, 257], BF16), Buf("CTb2_%d" % i)) for i in range(2)]
        junk = c.sb("junk", [128, 256], BF16); junk_b = Buf("junk")
        hs = [(c.sb("hs%d" % i, [128, 8], F32), Buf("hs%d" % i)) for i in range(8)]
        g2 = [(c.sb("g2_%d" % i, [128, 256], F32), Buf("g2_%d" % i)) for i in range(4)]
        hmt = [(c.sb("hm%d" % i, [128, 256], BF16), Buf("hm%d" % i)) for i in range(2)]
        mos = [(c.sb("mos%d" % i, [128, 4, 256], BF16), Buf("mos%d" % i)) for i in range(4)]
        hout = [(c.sb("hout%d" % i, [128, 2, 512], BF16), Buf("hout%d" % i)) for i in range(2)]
        kscale = 128.0 ** -0.5

        def conv_silu(raw, raw_b, idx, dst, dst_b, post_scale):
            c.ts(DVE, acc[:], raw[:, 0:SEQ], cw[:, idx, 0:1], cw[:, idx, 4:5], ALU.mult, ALU.add,
                 [raw_b, cw_b], [acc_b])
            for t in range(1, 4):
                c.stt(acc[:], raw[:, t:t + SEQ], cw[:, idx, t:t + 1], acc[:], ALU.mult, ALU.add,
                      [raw_b, cw_b, acc_b], [acc_b])
            if post_scale is None:
                c.act(dst[:], acc[:], AF.Silu, [acc_b], [dst_b])
            else:
                c.act(acc[:], acc[:], AF.Silu, [acc_b], [acc_b])
                c.ts(POOL, dst[:], acc[:], post_scale, None, ALU.mult, None, [acc_b], [dst_b])

        grp = 0
        for j in range(NH if do_m else 0):
            if j == 0:
                c.dma(SP, rawq[:, 3:SEQ + 3], d["qraw"](j), [], [rawq_b])
                c.dma(SP, rawk[:, 3:SEQ + 3], d["kraw"](j), [], [rawk_b])
            c.dma(SP, va[:, :, 0:256], mv_d[:, j * 256:(j + 1) * 256].rearrange("(ci p) v -> p ci v", p=128),
                  [], [va_b])
            conv_silu(rawq, rawq_b, j, qT, qT_b, None)
            conv_silu(rawk, rawk_b, NH + j, kT, kT_b, kscale)
            if j + 1 < NH:
                c.dma(SP, rawq[:, 3:SEQ + 3], d["qraw"](j + 1), [], [rawq_b])
                c.dma(SP, rawk[:, 3:SEQ + 3], d["kraw"](j + 1), [], [rawk_b])
            for k2 in range(2):
                P.op(POOL, lambda e, k2=k2: e.memset(CT2[k2][0][:], 0.0), writes=[CT2[k2][1]])
                P.op(POOL, lambda e, k2=k2: e.memset(CTb2[k2][0][:], 0.0), writes=[CTb2[k2][1]])

            def s0(ci):
                p = j * NCH + ci
                cs = slice(ci * 128, (ci + 1) * 128)
                bA, bA_b = c.banks[ci % 2], c.bank_bufs[ci % 2]
                dR, dR_b = diagR[ci % 2]
                c.ts(POOL, dR[:], identf[:], tq[:, Q_R, p:p + 1], None, ALU.mult, None, [identf_b, tq_b], [dR_b])
                c.mm(bA[:, 0:128], kT[:, cs], qT[:, cs], True, True, [kT_b, qT_b], [bA_b])
                c.mm(bA[:, 128:256], onesf[:], dR[:], True, False, [onesf_b, dR_b], [bA_b])
                c.mm(bA[:, 128:256], identf[:], maskf[:], False, True, [identf_b, maskf_b], [bA_b])
                c.mm(bA[:, 256:384], kT[:, cs], identb[:], True, True, [kT_b, identb_b], [bA_b])
                if ci % 4 == 0:
                    mo_t, mo_b = mos[(ci // 4) % 4]
                    c.dma(SP, mo_t[:], mo_d[ci * 128:(ci + 4) * 128, j * 256:(j + 1) * 256]
                          .rearrange("(cc p) v -> p cc v", p=128), [], [mo_b])

            def s1(ci):
                p = j * NCH + ci
                bA, bA_b = c.banks[ci % 2], c.bank_bufs[ci % 2]
                dm, dm_b = Dm[ci % 2]
                c.act(dm[:], bA[:, 128:256], AF.Exp, [bA_b, tq_b], [dm_b], bias=tq[:, Q_C, p:p + 1], scale=1.0)

            def s2(ci):
                p = j * NCH + ci
                bA, bA_b = c.banks[ci % 2], c.bank_bufs[ci % 2]
                dm, dm_b = Dm[ci % 2]
                sd, sd_b = sdT[ci % 2]
                c.tt(DVE, sd[:], bA[:, 0:128], dm[:], ALU.mult, [bA_b, dm_b], [sd_b])
                kwt, kw_b = kw[ci % 2]
                c.ts(DVE, kwt[:], bA[:, 256:384], tq[:, Q_W, p:p + 1], None, ALU.mult, None, [bA_b, tq_b], [kw_b])

            def s3(ci):
                bB, bB_b = c.banks[2 + ci % 2], c.bank_bufs[2 + ci % 2]
                bD, bD_b = c.banks[6], c.bank_bufs[6]
                sd, sd_b = sdT[ci % 2]
                kwt, kw_b = kw[ci % 2]
                c.mm(bB[:, 0:257], sd[:], va[:, ci, :], True, True, [sd_b, va_b], [bB_b])
                c.mm(bD[:, 0:257], kwt[:], va[:, ci, :], True, True, [kw_b, va_b], [bD_b])

            def s4(ci):
                p = j * NCH + ci
                bB, bB_b = c.banks[2 + ci % 2], c.bank_bufs[2 + ci % 2]
                bD, bD_b = c.banks[6], c.bank_bufs[6]
                ns, ns_b = numS[ci % 4]
                c.copy(ACT, ns[:], bB[:, 0:257], [bB_b], [ns_b])
                cn, cn_b = CT2[ci % 2]
                cp, cp_b = CT2[(ci + 1) % 2]
                c.stt(cn[:], cp[:], decb[:, p:p + 1], bD[:, 0:257], ALU.mult, ALU.add, [cp_b, decb_b, bD_b], [cn_b])

            def s5(ci):
                cs = slice(ci * 128, (ci + 1) * 128)
                cn, cn_b = CT2[ci % 2]
                cb, cb_b = CTb2[ci % 2]
                c.copy(POOL, cb[:], cn[:], [cn_b], [cb_b])
                cbp, cbp_b = CTb2[(ci + 1) % 2]
                bC, bC_b = c.banks[4 + ci % 2], c.bank_bufs[4 + ci % 2]
                c.mm(bC[:, 0:257], qT[:, cs], cbp[:], True, True, [qT_b, cbp_b], [bC_b])

            def s6(ci):
                p = j * NCH + ci
                bC, bC_b = c.banks[4 + ci % 2], c.bank_bufs[4 + ci % 2]
                ns, ns_b = numS[ci % 4]
                tt_, tt_b = tot[ci % 8]
                c.stt(tt_[:], bC[:, 0:257], tq[:, Q_INTER, p:p + 1], ns[:], ALU.mult, ALU.add,
                      [bC_b, tq_b, ns_b], [tt_b])

            def s7(ci):
                tt_, tt_b = tot[ci % 8]
                h_, h_b = hs[ci % 8]
                c.act(h_[:, 7:8], tt_[:, 256:257], AF.Abs, [tt_b], [h_b])
                c.act(junk[:], tt_[:, 0:256], AF.Square, [tt_b], [junk_b, h_b], accum_out=h_[:, 2:3])

            def s8(ci):
                p = j * NCH + ci
                h_, h_b = hs[ci % 8]
                c.ts(DVE, h_[:, 0:1], h_[:, 7:8], tq[:, Q_EM, p:p + 1], None, ALU.max, None, [h_b, tq_b], [h_b])
                mo_t, mo_b = mos[(ci // 4) % 4]
                g2t, g2_b = g2[ci % 4]
                c.tt(POOL, g2t[:], mnw[:, j * 256:(j + 1) * 256], mo_t[:, ci % 4, :], ALU.mult, [mnw_b, mo_b], [g2_b])

            def s9(ci):
                h_, h_b = hs[ci % 8]
                c.act(h_[:, 1:2], h_[:, 0:1], AF.Square, [h_b], [h_b], scale=EPS ** 0.5)
                c.act(h_[:, 4:5], h_[:, 2:3], AF.Sqrt, [h_b], [h_b], bias=h_[:, 1:2], scale=1.0 / 256)

            def s10(ci):
                h_, h_b = hs[ci % 8]
                tt_, tt_b = tot[ci % 8]
                g2t, g2_b = g2[ci % 4]
                P.op(DVE, lambda e, h_=h_: e.reciprocal(out=h_[:, 6:7], in_=h_[:, 4:5]), reads=[h_b], writes=[h_b])
                hm_, hm_b = hmt[ci % 2]
                c.stt(hm_[:], tt_[:, 0:256], h_[:, 6:7], g2t[:], ALU.mult, ALU.mult, [tt_b, h_b, g2_b], [hm_b])

            def s11(ci):
                bT, bT_b = c.banks[7], c.bank_bufs[7]
                hm_, hm_b = hmt[ci % 2]
                for vc in range(2):
                    c.mm(bT[:, vc * 128:(vc + 1) * 128], hm_[:, vc * 128:(vc + 1) * 128], identb[:], True, True,
                         [hm_b, identb_b], [bT_b])

            def s12(ci):
                bT, bT_b = c.banks[7], c.bank_bufs[7]
                ho_t, ho_b = hout[(ci // 4) % 2]
                c.copy(ACT, ho_t[:, :, (ci % 4) * 128:(ci % 4 + 1) * 128],
                       bT[:, 0:256].rearrange("p (a b) -> p a b", a=2), [bT_b], [ho_b])
                if ci % 4 == 3:
                    c.dma(SP, hmT_d[j * 256:(j + 1) * 256, (ci - 3) * 128:(ci + 1) * 128]
                          .rearrange("(a p) t -> p a t", p=128), ho_t[:], [ho_b], [])

            stages = [s0, s1, s2, s3, s4, s5, s6, s7, s8, s9, s10, s11, s12]
            for it in range(NCH + len(stages) - 1):
                for sidx in range(len(stages) - 1, -1, -1):
                    ci = it - sidx
                    if 0 <= ci < NCH:
                        stages[sidx](ci)

        qc = [(c.sb("dq%d" % i, [128, SEQ], BF16), Buf("dq%d" % i)) for i in range(2)]
        kc_ = [(c.sb("dk%d" % i, [128, SEQ], BF16), Buf("dk%d" % i)) for i in range(2)]
        sq = c.sb("sq", [128, SEQ], BF16); sq_b = Buf("sq")
        mx = c.sb("mx", [1, 64], F32); mx_b = Buf("mx")
        nG = c.sb("nG", [128, 1], F32); nG_b = Buf("nG")
        Et = [(c.sb("E%d" % i, [128, 512], BF16), Buf("E%d" % i)) for i in range(NSB + 2)]
        ds = [(c.sb("ds%d" % i, [128, 8], F32), Buf("ds%d" % i)) for i in range(4)]
        dtm = [(c.sb("dt%d" % i, [128, 256], F32), Buf("dt%d" % i)) for i in range(4)]
        dhd = [(c.sb("dhd%d" % i, [128, 256], F32), Buf("dhd%d" % i)) for i in range(4)]
        dhn = [(c.sb("dhn%d" % i, [128, 256], BF16), Buf("dhn%d" % i)) for i in range(4)]
        Os1 = [(c.sb("os1_%d" % i, [128, 257], F32), Buf("os1_%d" % i)) for i in range(4)]
        Os2 = [(c.sb("os2_%d" % i, [128, 257], F32), Buf("os2_%d" % i)) for i in range(4)]
        junkf = c.sb("junkf", [128, 256], F32); junkf_b = Buf("junkf")
        ascale = 128.0 ** -0.5
        Obanks = [(c.banks[i], c.bank_bufs[i]) for i in range(4)]
        Sbanks = [(c.banks[4 + i], c.bank_bufs[4 + i]) for i in range(NSB)]
        Tbanks = [(c.banks[4 + NSB + i], c.bank_bufs[4 + NSB + i]) for i in range(4 - NSB)]
        e_i = 0
        s_i = 0
        t_i = 0
        for j in range(NH if do_a else 0):
            for cc in range(2):
                c.dma(SP, qc[cc][0][:], d["dq"](j, cc), [], [qc[cc][1]])
                c.dma(SP, kc_[cc][0][:], d["dk"](j, cc), [], [kc_[cc][1]])
            c.dma(SP, va[:, :, 0:256], dv_d[:, j * 256:(j + 1) * 256].rearrange("(ci p) v -> p ci v", p=128),
                  [], [va_b])
            tb, tb_b = Tbanks[0]
            for ti, (tns, tns_b) in enumerate([qc[0], qc[1], kc_[0], kc_[1]]):
                c.act(sq[:], tns[:], AF.Square, [tns_b], [sq_b])
                for s8 in range(8):
                    c.mm(tb[0:1, 0:512], onesb[:, 0:1], sq[:, s8 * 512:(s8 + 1) * 512], True, True,
                         [onesb_b, sq_b], [tb_b])
                    P.op(DVE, lambda e, ti=ti, s8=s8: e.reduce_max(out=mx[0:1, ti * 8 + s8:ti * 8 + s8 + 1],
                                                                   in_=tb[0:1, 0:512], axis=AX.X),
                         reads=[tb_b], writes=[mx_b])
            P.op(DVE, lambda e: e.reduce_max(out=mx[0:1, 32:34], in_=mx[0:1, 0:32].rearrange("p (a b) -> p a b", a=2),
                                             axis=AX.X), reads=[mx_b], writes=[mx_b])
            c.tt(DVE, mx[0:1, 34:35], mx[0:1, 32:33], mx[0:1, 33:34], ALU.mult, [mx_b], [mx_b])
            c.act(mx[0:1, 35:36], mx[0:1, 34:35], AF.Sqrt, [mx_b], [mx_b], scale=ascale * ascale)
            c.ts(DVE, mx[0:1, 36:37], mx[0:1, 35:36], -1.0, None, ALU.mult, None, [mx_b], [mx_b])
            c.mm(tb[:, 0:1], onesf[0:1, :], mx[0:1, 36:37], True, True, [onesf_b, mx_b], [tb_b])
            c.copy(DVE, nG[:], tb[:, 0:1], [tb_b], [nG_b])
            if "dbg" in d:
                c.dma(SP, d["dbg"][j, 0:1, 0:64], mx[0:1, :], [mx_b], [])

            def qk_exp(g, kb):
                nonlocal s_i, e_i
                sb_, sb_b = Sbanks[s_i % NSB]; s_i += 1
                et, et_b = Et[e_i % (NSB + 2)]; e_i += 1
                ks = slice(kb * 128, (kb + 1) * 128)
                if kb <= 2 * g:
                    for cc in range(2):
                        diag = (kb == 2 * g)
                        c.mm(sb_[:, cc * 256:(cc + 1) * 256], kc_[cc][0][:, ks], qc[cc][0][:, g * 256:(g + 1) * 256],
                             True, not diag, [kc_[cc][1], qc[cc][1]], [sb_b])
                        if diag:
                            c.mm(sb_[:, cc * 256:cc * 256 + 128], identb[:], maskb[:], False, True,
                                 [identb_b, maskb_b], [sb_b])
                    c.act(et[:], sb_[:], AF.Exp, [sb_b, nG_b], [et_b], bias=nG[:, 0:1], scale=ascale)
                    ilist = (0, 1)
                else:
                    for cc in range(2):
                        c.mm(sb_[:, cc * 256 + 128:(cc + 1) * 256], kc_[cc][0][:, ks],
                             qc[cc][0][:, g * 256 + 128:(g + 1) * 256], True, False, [kc_[cc][1], qc[cc][1]], [sb_b])
                        c.mm(sb_[:, cc * 256 + 128:(cc + 1) * 256], identb[:], maskb[:], False, True,
                             [identb_b, maskb_b], [sb_b])
                    c.act(et[:].rearrange("p (a b) -> p a b", a=2)[:, :, 128:256],
                          sb_[:].rearrange("p (a b) -> p a b", a=2)[:, :, 128:256], AF.Exp,
                          [sb_b, nG_b], [et_b], bias=nG[:, 0:1], scale=ascale)
                    ilist = (1,)
                return (g, kb, et, et_b, ilist)

            def pv(st):
                g, kb, et, et_b, ilist = st
                for cc in range(2):
                    for i in ilist:
                        ob, ob_b = Obanks[cc * 2 + i]
                        last = (kb == 2 * g + i)
                        c.mm(ob[:, 0:257], et[:, cc * 256 + i * 128:cc * 256 + (i + 1) * 128], va[:, kb, :],
                             kb == 0, last, [et_b, va_b], [ob_b])
                if kb == 2 * g + 1:
                    epilogue(g)

            def epilogue(g):
                nonlocal t_i
                for i in range(2):
                    qb = 2 * g + i
                    r = qb % 4
                    o1, o1_b = Obanks[i]
                    o2, o2_b = Obanks[2 + i]
                    os1, os1_b = Os1[r]
                    os2, os2_b = Os2[r]
                    c.copy(DVE, os1[:], o1[:, 0:257], [o1_b], [os1_b])
                    c.copy(DVE, os2[:], o2[:, 0:257], [o2_b], [os2_b])
                R = [(2 * g + i) % 4 for i in range(2)]
                for r in R:
                    P.op(DVE, lambda e, d_=ds[r][0], os1=Os1[r][0]: e.reciprocal(out=d_[:, 0:1], in_=os1[:, 256:257]),
                         reads=[Os1[r][1]], writes=[ds[r][1]])
                for r in R:
                    P.op(DVE, lambda e, d_=ds[r][0], os2=Os2[r][0]: e.reciprocal(out=d_[:, 1:2], in_=os2[:, 256:257]),
                         reads=[Os2[r][1]], writes=[ds[r][1]])
                for r in R:
                    d_, d_b = ds[r]
                    c.ts(DVE, d_[:, 2:3], d_[:, 1:2], lsm[:, NLAM:NLAM + 1], None, ALU.mult, None, [d_b, lsm_b], [d_b])
                for r in R:
                    d_, d_b = ds[r]
                    c.ts(DVE, dtm[r][0][:], Os1[r][0][:, 0:256], d_[:, 0:1], None, ALU.mult, None,
                         [Os1[r][1], d_b], [dtm[r][1]])
                for r in R:
                    d_, d_b = ds[r]
                    c.stt(dhd[r][0][:], Os2[r][0][:, 0:256], d_[:, 2:3], dtm[r][0][:], ALU.mult, ALU.add,
                          [Os2[r][1], d_b, dtm[r][1]], [dhd[r][1]])
                for r in R:
                    P.op(DVE, lambda e, hd_=dhd[r][0], d_=ds[r][0]: e.scalar_tensor_tensor(
                        out=junkf[:], in0=hd_[:], scalar=1.0, in1=hd_[:], op0=ALU.mult, op1=ALU.mult,
                        accum_out=d_[:, 3:4]), reads=[dhd[r][1]], writes=[junkf_b, ds[r][1]])
                defer.append([it_no[0] + 3, stage_b, (g,)])

            def stage_b(g):
                R = [(2 * g + i) % 4 for i in range(2)]
                for r in R:
                    d_, d_b = ds[r]
                    c.act(d_[:, 4:5], d_[:, 3:4], AF.Sqrt, [d_b, eps_b], [d_b], bias=epst[:, 0:1], scale=1.0 / 256)
                for r in R:
                    P.op(DVE, lambda e, d_=ds[r][0]: e.reciprocal(out=d_[:, 5:6], in_=d_[:, 4:5]),
                         reads=[ds[r][1]], writes=[ds[r][1]])
                for r in R:
                    d_, d_b = ds[r]
                    c.ts(DVE, d_[:, 6:7], d_[:, 5:6], lami[:, 1:2], None, ALU.mult, None, [d_b, lami_b], [d_b])
                for r in R:
                    d_, d_b = ds[r]
                    c.stt(dhn[r][0][:], dhd[r][0][:], d_[:, 6:7], dnw[:, j * 256:(j + 1) * 256], ALU.mult, ALU.mult,
                          [dhd[r][1], d_b, dnw_b], [dhn[r][1]])
                for i in range(2):
                    defer.append([it_no[0] + 3, stage_c, (g, i)])

            def stage_c(g, i):
                nonlocal t_i
                qb = 2 * g + i
                r = qb % 4
                hn_, hn_b = dhn[r]
                tb, tb_b = Tbanks[t_i % (4 - NSB)]; t_i += 1
                for vc in range(2):
                    c.mm(tb[:, vc * 128:(vc + 1) * 128], hn_[:, vc * 128:(vc + 1) * 128], identb[:], True, True,
                         [hn_b, identb_b], [tb_b])
                ho_t, ho_b = hout[(qb // 4) % 2]
                c.copy(DVE, ho_t[:, :, (qb % 4) * 128:(qb % 4 + 1) * 128],
                       tb[:, 0:256].rearrange("p (a b) -> p a b", a=2), [tb_b], [ho_b])
                if qb % 4 == 3:
                    c.dma(SP, hdT_d[j * 256:(j + 1) * 256, (qb - 3) * 128:(qb + 1) * 128]
                          .rearrange("(a p) t -> p a t", p=128), ho_t[:], [ho_b], [])

            defer = []
            it_no = [0]

            def run_deferred(flush=False):
                k = 0
                while k < len(defer):
                    if flush or defer[k][0] <= it_no[0]:
                        _, fn, args = defer.pop(k)
                        fn(*args)
                    else:
                        k += 1

            pairs = [(g, kb) for g in range(NCH // 2) for kb in range(2 * g + 2)]
            pend = []
            for (g, kb) in pairs:
                pend.append(qk_exp(g, kb))
                if len(pend) > NSB - 1:
                    pv(pend.pop(0))
                it_no[0] += 1
                run_deferred()
            while pend:
                pv(pend.pop(0))
            while defer:
                run_deferred(flush=True)
        c.end()


IDENT = np.eye(128, dtype=np.float32)
MASKNEG_NP = np.where(np.arange(128)[:, None] <= np.arange(128)[None, :], 0.0, MASKNEG).astype(np.float32)


def lam_init_of(l):
    return 0.8 - 0.6 * math.exp(-0.3 * l)


TG = 512
NFC = D_FF // 128


def phase_D(c, d, final, last):
    x_d, hm_d, hd_d, gmd_d, xo_d = d["x"], d["hmT"], d["hdT"], d["gmd"], d["xo"]
    wbm_d, wbd_d, wout_d, wg_d, wu_d, wd_d = d["wbm"], d["wbd"], d["wout"], d["wg"], d["wu"], d["wd"]
    nw2_d, ident_d = d["nw2"], d["ident"]
    if final:
        fnw_d = d["fnw"]
    if True:
        c.begin()
        P = c.P
        ident = c.sb("ident_s", [128, 128], BF16); ident_b = Buf("ident")
        c.dma(POOL, ident[:], ident_d[:, :], [], [ident_b])
        nw2 = c.sb("nw2_s", [128, KC], F32); nw2_b = Buf("nw2")
        c.dma(SP, nw2[:], nw2_d, [], [nw2_b])
        if final:
            fnw = c.sb("fnw_s", [128, D_MODEL], F32); fnw_b = Buf("fnw")
            c.dma(SP, fnw[:], fnw_d, [], [fnw_b])
        hmg = c.sb("hmg", [128, KC, TG], BF16); hmg_b = Buf("hmg")
        hdg = c.sb("hdg", [128, KC, TG], BF16); hdg_b = Buf("hdg")
        yT = c.sb("yT", [128, KC, TG], BF16); yT_b = Buf("yT")
        xg = c.sb("xg", [128, TG // 128, D_MODEL], F32)
        xg_b = [Buf("xg%d" % i) for i in range(TG // 128)]
        aT = c.sb("aT", [128, NFC, TG], BF16); aT_b = Buf("aT")
        NW = 3
        wslots = [(c.sb("w%d" % i, [128, KC, 512], BF16), Buf("w%d" % i)) for i in range(NW)]
        t1 = [(c.sb("t1_%d" % i, [128, TG], F32), Buf("t1_%d" % i)) for i in range(4)]
        t2 = [(c.sb("t2_%d" % i, [128, TG], F32), Buf("t2_%d" % i)) for i in range(2)]
        sgm = [(c.sb("sgm%d" % i, [128, TG], BF16), Buf("sgm%d" % i)) for i in range(2)]
        sgd = [(c.sb("sgd%d" % i, [128, TG], BF16), Buf("sgd%d" % i)) for i in range(2)]
        scr = norm_scratch(c, "n_")

        wlist = []
        for g in range(TOK // TG):
            for blk in range(4):
                wlist.append((wbm_d, 0, KC, blk * 512, 512))
                wlist.append((wbd_d, 0, KC, blk * 512, 512))
            for cg in range(4):
                wlist.append((wout_d, 0, KC, cg * 512, 512))
            for blk in range(D_FF // 512):
                wlist.append((wg_d, 0, KC, blk * 512, 512))
                wlist.append((wu_d, 0, KC, blk * 512, 512))
            for cg in range(4):
                for fb in range(4):
                    wlist.append((wd_d, fb * 11 * 128, 11, cg * 512, 512))
        wstate = {"issued": 0, "used": 0, "done": 0}

        def issue_one():
            i = wstate["issued"]
            wdr, r0, nkc, c0, ncol = wlist[i]
            ws, wb = wslots[i % NW]
            load_wblock(c, ws, wb, wdr, r0, nkc, c0, ncol)
            wstate["issued"] += 1

        def next_w():
            i = wstate["used"]
            while wstate["issued"] <= i:
                assert wstate["issued"] < wstate["done"] + NW
                issue_one()
            wstate["used"] += 1
            return wslots[i % NW]

        def release_w():
            wstate["done"] = wstate["used"]
            while wstate["issued"] < len(wlist) and wstate["issued"] < wstate["done"] + NW:
                issue_one()

        k2 = 0
        for g in range(TOK // TG):
            ts0 = g * TG
            if g == 0:
                c.dma(SP, hmg[:], hm_d[:, ts0:ts0 + TG].rearrange("(kc p) t -> p kc t", p=128), [], [hmg_b])
                c.dma(SP, hdg[:], hd_d[:, ts0:ts0 + TG].rearrange("(kc p) t -> p kc t", p=128), [], [hdg_b])
            tsn = ts0 + TG
            for tt in range(TG // 128):
                c.dma(SP, xg[:, tt, :], x_d[ts0 + tt * 128:ts0 + (tt + 1) * 128, :], [], [xg_b[tt]])
            for blk in range(4):
                wm, wm_b = next_w()
                for cc in range(4):
                    col = blk * 4 + cc
                    sm_, sm_b = sgm[cc % 2]
                    c.dma(SP, sm_[:], gmd_d[col * 128:(col + 1) * 128, ts0:ts0 + TG], [], [sm_b])
                    bA, bA_b = c.next_bank()
                    for kc in range(KC):
                        c.mm(bA[:, :], wm[:, kc, cc * 128:(cc + 1) * 128], hmg[:, kc, :], kc == 0, kc == KC - 1,
                             [wm_b, hmg_b], [bA_b])
                    a1, a1_b = t1[cc]
                    c.tt(DVE, a1[:], bA[:, :], sm_[:], ALU.mult, [bA_b, sm_b], [a1_b])
                release_w()
                wd_, wd_b = next_w()
                for cc in range(4):
                    col = blk * 4 + cc
                    sd_, sd_b = sgd[cc % 2]
                    c.dma(SP, sd_[:], gmd_d[D_MODEL + col * 128:D_MODEL + (col + 1) * 128, ts0:ts0 + TG], [], [sd_b])
                    bB, bB_b = c.next_bank()
                    for kc in range(KC):
                        c.mm(bB[:, :], wd_[:, kc, cc * 128:(cc + 1) * 128], hdg[:, kc, :], kc == 0, kc == KC - 1,
                             [wd_b, hdg_b], [bB_b])
                    a1, a1_b = t1[cc]
                    a2, a2_b = t2[cc % 2]
                    c.tt(DVE, a2[:], bB[:, :], sd_[:], ALU.mult, [bB_b, sd_b], [a2_b])
                    c.tt(POOL, yT[:, col, :], a1[:], a2[:], ALU.add, [a1_b, a2_b], [yT_b])
                release_w()
            if g + 1 < TOK // TG:
                c.dma(SP, hdg[:], hd_d[:, tsn:tsn + TG].rearrange("(kc p) t -> p kc t", p=128), [], [hdg_b])
            for cg in range(4):
                wo, wo_b = next_w()
                for tt in range(TG // 128):
                    bk, bk_b = c.next_bank()
                    for kc in range(KC):
                        c.mm(bk[:, :], yT[:, kc, tt * 128:(tt + 1) * 128], wo[:, kc, :], kc == 0, kc == KC - 1,
                             [yT_b, wo_b], [bk_b])
                    c.tt(DVE, xg[:, tt, cg * 512:(cg + 1) * 512], bk[:, :], xg[:, tt, cg * 512:(cg + 1) * 512], ALU.add,
                         [bk_b, xg_b[tt]], [xg_b[tt]])
                release_w()
            for tt in range(TG // 128):
                rmsnorm_to_T(c, xg[:, tt, :], xg_b[tt], scr, hmg, hmg_b, tt * 128, nw2, nw2_b, ident, ident_b, "n_")
            for blk in range(D_FF // 512):
                wg_, wg_b = next_w()
                for cc in range(4):
                    bG, bG_b = c.next_bank()
                    for kc in range(KC):
                        c.mm(bG[:, :], wg_[:, kc, cc * 128:(cc + 1) * 128], hmg[:, kc, :], kc == 0, kc == KC - 1,
                             [wg_b, hmg_b], [bG_b])
                    a1, a1_b = t1[cc]
                    c.act(a1[:], bG[:, :], AF.Silu, [bG_b], [a1_b])
                release_w()
                wu_, wu_b = next_w()
                for cc in range(4):
                    fc = blk * 4 + cc
                    bU, bU_b = c.next_bank()
                    for kc in range(KC):
                        c.mm(bU[:, :], wu_[:, kc, cc * 128:(cc + 1) * 128], hmg[:, kc, :], kc == 0, kc == KC - 1,
                             [wu_b, hmg_b], [bU_b])
                    a1, a1_b = t1[cc]
                    c.tt(DVE, aT[:, fc, :], bU[:, :], a1[:], ALU.mult, [bU_b, a1_b], [aT_b])
                release_w()
            if g + 1 < TOK // TG:
                c.dma(SP, hmg[:], hm_d[:, tsn:tsn + TG].rearrange("(kc p) t -> p kc t", p=128), [], [hmg_b])
            for cg in range(4):
                bks = [c.next_bank() for _ in range(TG // 128)]
                for fb in range(4):
                    wdn, wdn_b = next_w()
                    for tt in range(TG // 128):
                        bk, bk_b = bks[tt]
                        for i in range(11):
                            fc = fb * 11 + i
                            c.mm(bk[:, :], aT[:, fc, tt * 128:(tt + 1) * 128], wdn[:, i, :], fc == 0, fc == NFC - 1,
                                 [aT_b, wdn_b], [bk_b])
                    release_w()
                for tt in range(TG // 128):
                    bk, bk_b = bks[tt]
                    c.tt(DVE, xg[:, tt, cg * 512:(cg + 1) * 512], bk[:, :], xg[:, tt, cg * 512:(cg + 1) * 512], ALU.add,
                         [bk_b, xg_b[tt]], [xg_b[tt]])
            for tt in range(TG // 128):
                if final:
                    junk, junk_b = scr["junk"]
                    ss, ss_b = scr["ss"]
                    c.act(junk[:], xg[:, tt, :], AF.Square, [xg_b[tt]], [junk_b, ss_b], accum_out=ss[:, 0:1])
                    c.act(ss[:, 1:2], ss[:, 0:1], AF.Sqrt, [ss_b, scr["eps_b"]], [ss_b], scale=1.0 / D_MODEL,
                          bias=scr["eps"][:, 0:1])
                    P.op(DVE, lambda e, ss=ss: e.reciprocal(out=ss[:, 2:3], in_=ss[:, 1:2]), reads=[ss_b], writes=[ss_b])
                    c.stt(xg[:, tt, :], xg[:, tt, :], ss[:, 2:3], fnw[:], ALU.mult, ALU.mult,
                          [xg_b[tt], ss_b, fnw_b], [xg_b[tt]])
                c.dma(SP, xo_d[ts0 + tt * 128:ts0 + (tt + 1) * 128, :], xg[:, tt, :], [xg_b[tt]], [], sem_key=xg_b[tt])
        c.end(last)


NUSED = 4


def build_fused(depth=DEPTH):
    nc = bass.Bass("TRN2", target_bir_lowering=False)
    di = lambda n, s, dt: nc.dram_tensor(n, s, dt, kind="ExternalInput").ap()
    x_in = di("x", [SEQ, D_MODEL], F32)
    w_in = di("w_in", [DEPTH, D_MODEL, N_IN], F32)
    w_bm = di("w_branch_m", [DEPTH, D_MODEL, D_MODEL], F32)
    w_bd = di("w_branch_d", [DEPTH, D_MODEL, D_MODEL], F32)
    w_out = di("w_out", [DEPTH, D_MODEL, D_MODEL], F32)
    w_g = di("w_ffn_gate", [DEPTH, D_MODEL, D_FF], F32)
    w_u = di("w_ffn_up", [DEPTH, D_MODEL, D_FF], F32)
    w_d = di("w_ffn_down", [DEPTH, D_FF, D_MODEL], F32)
    anw = di("anw", [DEPTH, 128, KC], F32)
    fnw2 = di("fnw2", [DEPTH, 128, KC], F32)
    fnw = di("fnw", [128, D_MODEL], F32)
    cw = di("cw", [DEPTH, 2, 128, 2 * NH, 5], F32)
    gb = di("gb", [DEPTH, 2, 128, 2], F32)
    mnw = di("mnw", [DEPTH, 128, D_MODEL], F32)
    dnw = di("dnw", [DEPTH, 128, D_MODEL], F32)
    lam = di("lam", [DEPTH, 128, 4, 128], F32)
    lami = di("lami", [DEPTH, 128, 2], F32)
    ident = di("ident", [128, 128], F32)
    maskneg = di("maskneg", [128, 128], F32)
    y_out = nc.dram_tensor("y", [SEQ, D_MODEL], F32, kind="ExternalOutput").ap()
    sc = lambda n, s, dt: nc.dram_tensor(n, s, dt).ap()
    qk_s = sc("qk_s", [2048, SEQ], F32)
    mv_s = sc("mv_s", [SEQ, 2048], BF16)
    mo_s = sc("mo_s", [SEQ, 2048], BF16)
    gt_s = sc("gt_s", [16, SEQ], F32)
    dqk_s = sc("dqk_s", [4096, SEQ], BF16)
    dv_s = sc("dv_s", [SEQ, 2048], BF16)
    gmd_s = sc("gmd_s", [4096, SEQ], BF16)
    hm_s = sc("hm_s", [2048, SEQ], BF16)
    hd_s = sc("hd_s", [2048, SEQ], BF16)
    x_s = sc("x_s", [SEQ, D_MODEL], F32)
    scr = {"qk": qk_s, "mv": mv_s, "mo": mo_s, "gt": gt_s, "dqk": dqk_s, "dv": dv_s, "gmd": gmd_s}
    wb = {"wbm": sc("wbm_b", [D_MODEL, D_MODEL], BF16), "wbd": sc("wbd_b", [D_MODEL, D_MODEL], BF16),
          "wout": sc("wout_b", [D_MODEL, D_MODEL], BF16), "wg": sc("wg_b", [D_MODEL, D_FF], BF16),
          "wu": sc("wu_b", [D_MODEL, D_FF], BF16), "wd": sc("wd_b", [D_FF, D_MODEL], BF16)}

    def make_cast(l):
        def pre(c):
            srcs = {"wbm": w_bm[l], "wbd": w_bd[l], "wout": w_out[l], "wg": w_g[l], "wu": w_u[l], "wd": w_d[l]}
            for k in ("wbm", "wbd", "wout", "wg", "wu", "wd"):
                src, dst = srcs[k], wb[k]
                if k in ("wg", "wu"):
                    src = src.rearrange("r (a b) -> r a b", b=1408)
                    dst = dst.rearrange("r (a b) -> r a b", b=1408)
                c.dma(POOL, dst, src, [], [Buf("cast_" + k)])
        return pre

    with contextlib.ExitStack() as es:
        c = Ctx(nc, es)
        c.alloc_banks(8)
        for l in range(depth):
            x_src = x_in if l == 0 else x_s
            final = (l == depth - 1)
            for th in range(2):
                ts = slice(th * TOK, (th + 1) * TOK)
                outs = {}
                for name, c0, n, mode, dt, sg in A_SECTIONS:
                    outs[name] = scr[name][:, ts] if mode == "F" else scr[name][ts, :]
                phase_A(c, x_src[ts, :], anw[l], w_in[l], ident, outs)
            for hh in range(2):
                hs = slice(hh * NH * 256, (hh + 1) * NH * 256)
                d = {
                    "cw": cw[l, hh], "gb": gb[l, hh], "mnw": mnw[l][:, hs], "dnw": dnw[l][:, hs],
                    "lam": lam[l], "lami": lami[l], "ident": ident, "maskneg": maskneg,
                    "mv": mv_s[:, hs], "mo": mo_s[:, hs], "dv": dv_s[:, hs],
                    "gi": gt_s[hh * NH:(hh + 1) * NH, :], "gf": gt_s[8 + hh * NH:8 + (hh + 1) * NH, :],
                    "hmT": hm_s[hs, :], "hdT": hd_s[hs, :],
                    "qraw": (lambda j, hh=hh: qk_s[(hh * NH + j) * 128:(hh * NH + j + 1) * 128, :]),
                    "kraw": (lambda j, hh=hh: qk_s[1024 + (hh * NH + j) * 128:1024 + (hh * NH + j + 1) * 128, :]),
                    "dq": (lambda j, cc, hh=hh: dqk_s[(hh * NH + j) * 256 + cc * 128:(hh * NH + j) * 256 + (cc + 1) * 128, :]),
                    "dk": (lambda j, cc, hh=hh: dqk_s[2048 + (hh * NH + j) * 256 + cc * 128:
                                                      2048 + (hh * NH + j) * 256 + (cc + 1) * 128, :]),
                }
                if hh == 0:
                    d["pre"] = make_cast(l)
                phase_BC(c, d)
            for th in range(2):
                ts = slice(th * TOK, (th + 1) * TOK)
                d = {"x": x_src[ts, :], "hmT": hm_s[:, ts], "hdT": hd_s[:, ts], "gmd": gmd_s[:, ts],
                     "xo": (y_out if final else x_s)[ts, :],
                     "wbm": wb["wbm"], "wbd": wb["wbd"], "wout": wb["wout"], "wg": wb["wg"], "wu": wb["wu"],
                     "wd": wb["wd"],
                     "nw2": fnw2[l], "ident": ident, "fnw": fnw}
                phase_D(c, d, final, last=(l == depth - 1 and th == 1))
        build_fused.stats = (c.P.n_total, dict(c.P.cnt))
    return nc


def host_params(prm):
    L = DEPTH
    anw = np.stack([nw_layout(prm["attn_norm_w"][l]) for l in range(L)])
    fnw2 = np.stack([nw_layout(prm["ffn_norm_w"][l]) for l in range(L)])
    fnw = np.ascontiguousarray(np.broadcast_to(prm["final_norm_w"][None], (128, D_MODEL))).astype(np.float32)
    cw = np.zeros((L, 2, 128, 2 * NH, 5), np.float32)
    gb = np.zeros((L, 2, 128, 2), np.float32)
    for l in range(L):
        cwl, cbl = prm["conv_w"][l], prm["conv_b"][l]
        for hh in range(2):
            for i in range(NH):
                h = hh * NH + i
                cw[l, hh, :, i, 0:4] = cwl[:, h * 128:(h + 1) * 128].T
                cw[l, hh, :, i, 4] = cbl[h * 128:(h + 1) * 128]
                cw[l, hh, :, NH + i, 0:4] = cwl[:, 1024 + h * 128:1024 + (h + 1) * 128].T
                cw[l, hh, :, NH + i, 4] = cbl[1024 + h * 128:1024 + (h + 1) * 128]
            heads = slice(hh * NH, (hh + 1) * NH)
            gb[l, hh, :, 0] = np.repeat(prm["b_igate"][l][heads], NCH)
            gb[l, hh, :, 1] = np.repeat(prm["b_fgate"][l][heads], NCH)
    mnw = np.ascontiguousarray(np.broadcast_to(prm["mlstm_norm_w"][:, None, :], (L, 128, D_MODEL))).astype(np.float32)
    dnw = np.ascontiguousarray(np.broadcast_to(prm["diff_norm_w"][:, None, :], (L, 128, D_MODEL))).astype(np.float32)
    lam = np.stack([np.stack([prm["lambda_q1"][l], prm["lambda_k1"][l], prm["lambda_q2"][l], prm["lambda_k2"][l]])
                    for l in range(L)])
    lam = np.ascontiguousarray(np.broadcast_to(lam[:, None], (L, 128, 4, 128))).astype(np.float32)
    lami = np.zeros((L, 128, 2), np.float32)
    for l in range(L):
        lami[l, :, 0] = lam_init_of(l)
        lami[l, :, 1] = 1.0 - lam_init_of(l)
    return {"anw": anw, "fnw2": fnw2, "fnw": fnw, "cw": cw, "gb": gb, "mnw": mnw, "dnw": dnw, "lam": lam,
            "lami": lami, "ident": IDENT, "maskneg": MASKNEG_NP}


_NC = {}


def kernel(**inputs):
    x = np.ascontiguousarray(inputs["x"], dtype=np.float32)
    prm = {k: np.asarray(v, dtype=np.float32) for k, v in inputs.items() if k != "x"}
    if "nc" not in _NC:
        _NC["nc"] = build_fused()
    hp = host_params(prm)
    big = {k: np.ascontiguousarray(prm[k]) for k in ("w_in", "w_branch_m", "w_branch_d", "w_out",
                                                     "w_ffn_gate", "w_ffn_up", "w_ffn_down")}
    maps = []
    for b in range(NUSED):
        m = {"x": np.ascontiguousarray(x[b])}
        m.update(big)
        m.update(hp)
        maps.append(m)
    res = run_bass_kernel_spmd(_NC["nc"], maps, core_ids=list(range(NUSED)))
    return np.stack([np.asarray(res.results[b]["y"]) for b in range(NUSED)]).astype(np.float32)
```

```python
import contextlib
import math
import numpy as np
import ml_dtypes
import concourse.bass as bass
import concourse.mybir as mybir
from concourse.bass_utils import run_bass_kernel_spmd

F32 = mybir.dt.float32
BF16 = mybir.dt.bfloat16
AF = mybir.ActivationFunctionType
ALU = mybir.AluOpType
AX = mybir.AxisListType
NPBF = ml_dtypes.bfloat16

D_MODEL = 2048
BATCH = 4
SEQ = 4096
DEPTH = 4
NCORES = 8
TOK = 2048
D_FF = 5632
N_IN = 16400
EPS = 1e-6
KC = D_MODEL // 128

PE, ACT, DVE, POOL, SP = "pe", "act", "dve", "pool", "sp"
COMPUTE = (PE, ACT, DVE, POOL)


class Buf:
    __slots__ = ("name", "last_write", "reads", "dma_sem")

    def __init__(self, name):
        self.name = name
        self.last_write = None
        self.reads = {}
        self.dma_sem = None


class Instr:
    __slots__ = ("eng", "fn", "deps", "is_dma", "sem_key", "sig_val", "needs_sig")

    def __init__(self, eng, fn, is_dma, sem_key):
        self.eng = eng
        self.fn = fn
        self.deps = []
        self.is_dma = is_dma
        self.sem_key = sem_key
        self.sig_val = None
        self.needs_sig = False


class Prog:
    NDMA = 72

    def __init__(self, nc, es):
        self.nc = nc
        self.esem = {e: es.enter_context(nc.semaphore("s_" + e)) for e in COMPUTE}
        self.dsem = [es.enter_context(nc.semaphore("d_%d" % i)) for i in range(self.NDMA)]
        self.cnt = {e: 0 for e in COMPUTE}
        self.dcnt = [0] * self.NDMA
        self.barrier = []
        self.n_total = 0
        self._reset()

    def _reset(self):
        self.q = {e: [] for e in (PE, ACT, DVE, POOL, SP)}
        self.started = set()

    def op(self, eng, fn, reads=(), writes=(), dma=False, sem_key=None, pe_accum=False):
        if dma and sem_key is None:
            sem_key = writes[0] if len(writes) else reads[0]
        ins = Instr(eng, fn, dma, sem_key)
        deps = []
        if eng not in self.started:
            self.started.add(eng)
            ins.deps.extend(self.barrier)
        for b in reads:
            if b.last_write is not None:
                deps.append(b.last_write)
        for b in writes:
            if b.last_write is not None:
                deps.append(b.last_write)
            deps.extend(b.reads.values())
        seen = set()
        for d in deps:
            if d is ins or id(d) in seen:
                continue
            seen.add(id(d))
            if (not d.is_dma) and d.eng == PE and eng == PE and not dma:
                continue
            ins.deps.append(d)
            d.needs_sig = True
        for b in reads:
            b.reads[("d", id(sem_key)) if dma else eng] = ins
        for b in writes:
            b.last_write = ins
            b.reads = {}
        self.q[eng].append(ins)
        return ins

    def end_phase(self, last=False):
        nc = self.nc
        bar = {}
        for e in self.q:
            if self.q[e]:
                bar[id(self.q[e][-1])] = self.q[e][-1]
            for ins in self.q[e]:
                if ins.is_dma:
                    bar["k%d" % id(ins.sem_key)] = ins
        barrier = []
        seen = set()
        for ins in bar.values():
            if id(ins) not in seen:
                seen.add(id(ins))
                barrier.append(ins)
                ins.needs_sig = True
        nkeys = 0
        for e in self.q:
            for ins in self.q[e]:
                if ins.is_dma and ins.needs_sig and ins.sem_key.dma_sem is None:
                    ins.sem_key.dma_sem = nkeys
                    nkeys += 1
        assert nkeys <= self.NDMA, nkeys
        for e in self.q:
            for ins in self.q[e]:
                self.n_total += 1
                if not ins.needs_sig:
                    continue
                if ins.is_dma:
                    k = ins.sem_key.dma_sem
                    self.dcnt[k] += 16
                    ins.sig_val = (self.dsem[k], self.dcnt[k], 16)
                else:
                    self.cnt[ins.eng] += 1
                    ins.sig_val = (self.esem[ins.eng], self.cnt[ins.eng], 1)
        q = self.q
        with nc.Block() as block:
            def run(e, eng_obj):
                waited = {}
                for ins in q[e]:
                    for d in ins.deps:
                        sem, val, _ = d.sig_val
                        key = id(sem)
                        if waited.get(key, 0) >= val:
                            continue
                        waited[key] = val
                        eng_obj.wait_ge(sem, val)
                    bi = ins.fn(eng_obj)
                    if ins.needs_sig:
                        sem, val, inc = ins.sig_val
                        bi.then_inc(sem, inc)
                if e == SP and last:
                    for ins in barrier:
                        sem, val, _ = ins.sig_val
                        if waited.get(id(sem), 0) >= val:
                            continue
                        waited[id(sem)] = val
                        eng_obj.wait_ge(sem, val)

            if q[PE]:
                @block.tensor
                def _(eng):
                    run(PE, eng)
            if q[ACT]:
                @block.scalar
                def _(eng):
                    run(ACT, eng)
            if q[DVE]:
                @block.vector
                def _(eng):
                    run(DVE, eng)
            if q[POOL]:
                @block.gpsimd
                def _(eng):
                    run(POOL, eng)
            if q[SP] or last:
                @block.sync
                def _(eng):
                    run(SP, eng)
        for ins in barrier:
            ins.fn = None
        for e in q:
            for ins in q[e]:
                ins.fn = None
                ins.deps = None
        self.barrier = barrier
        self._reset()


class Ctx:
    def __init__(self, nc, es):
        self.nc = nc
        self.ges = es
        self.es = None
        self.P = Prog(nc, es)
        self.uid = 0
        self.banks = []
        self.bank_bufs = []
        self.bank_i = 0
        self.evac_i = 0

    def sb(self, name, shape, dt):
        self.uid += 1
        return self.es.enter_context(self.nc.sbuf_tensor("%s_%d" % (name, self.uid), list(shape), dt))

    def begin(self):
        self.es = contextlib.ExitStack()
        self.es.__enter__()
        self.bank_bufs = [Buf("bank%d" % i) for i in range(len(self.banks))]

    def end(self, last=False):
        self.P.end_phase(last)
        self.es.__exit__(None, None, None)
        self.es = None

    def alloc_banks(self, n=8):
        for i in range(n):
            self.banks.append(self.ges.enter_context(self.nc.psum_tensor("bank%d" % i, [128, 512], F32)))
            self.bank_bufs.append(Buf("bank%d" % i))

    def next_bank(self):
        i = self.bank_i % len(self.banks)
        self.bank_i += 1
        return self.banks[i], self.bank_bufs[i]

    def mm(self, out, lhsT, rhs, start, stop, reads, writes):
        return self.P.op(PE, lambda e: e.matmul(out, lhsT, rhs, start=start, stop=stop),
                         reads=reads, writes=writes)

    def act(self, out, in_, func, reads, writes, bias=None, scale=None, accum_out=None):
        kw = {}
        if bias is not None:
            kw["bias"] = bias
        if scale is not None:
            kw["scale"] = scale
        if accum_out is not None:
            kw["accum_out"] = accum_out
        return self.P.op(ACT, lambda e: e.activation(out=out, in_=in_, func=func, **kw),
                         reads=reads, writes=writes)

    def dma(self, eng, out, in_, reads, writes, sem_key=None):
        return self.P.op(eng, lambda e: e.dma_start(out=out, in_=in_), reads=reads, writes=writes,
                         dma=True, sem_key=sem_key)

    def tt(self, eng, out, in0, in1, op, reads, writes):
        return self.P.op(eng, lambda e: e.tensor_tensor(out=out, in0=in0, in1=in1, op=op),
                         reads=reads, writes=writes)

    def ts(self, eng, out, in0, s1, s2, op0, op1, reads, writes, accum_out=None):
        if op1 is None:
            return self.P.op(eng, lambda e: e.tensor_scalar(out=out, in0=in0, scalar1=s1, scalar2=None, op0=op0),
                             reads=reads, writes=writes)
        if accum_out is not None:
            return self.P.op(eng, lambda e: e.tensor_scalar(out=out, in0=in0, scalar1=s1, scalar2=s2, op0=op0,
                                                            op1=op1, accum_out=accum_out),
                             reads=reads, writes=writes)
        return self.P.op(eng, lambda e: e.tensor_scalar(out=out, in0=in0, scalar1=s1, scalar2=s2, op0=op0, op1=op1),
                         reads=reads, writes=writes)

    def stt(self, out, in0, scalar, in1, op0, op1, reads, writes):
        return self.P.op(DVE, lambda e: e.scalar_tensor_tensor(out=out, in0=in0, scalar=scalar, in1=in1,
                                                               op0=op0, op1=op1),
                         reads=reads, writes=writes)

    def copy(self, eng, out, in_, reads, writes):
        if eng == ACT:
            return self.P.op(ACT, lambda e: e.copy(out=out, in_=in_), reads=reads, writes=writes)
        return self.P.op(eng, lambda e: e.tensor_copy(out=out, in_=in_), reads=reads, writes=writes)

    def evac_copy(self, out, in_, reads, writes):
        self.evac_i += 1
        return self.copy(ACT if self.evac_i % 2 else DVE, out, in_, reads, writes)


def rmsnorm_to_T(c, xt, xbuf, scratch, hT, hT_buf, tok0, nw, nw_buf, ident, ident_buf, pfx):
    junk, junk_b = scratch["junk"]
    ss, ss_b = scratch["ss"]
    hn, hn_b = scratch["hn"]
    c.act(junk[:], xt[:], AF.Square, reads=[xbuf], writes=[junk_b, ss_b], accum_out=ss[:, 0:1])
    c.act(ss[:, 1:2], ss[:, 0:1], AF.Sqrt, reads=[ss_b, scratch["eps_b"]], writes=[ss_b], scale=1.0 / D_MODEL, bias=scratch["eps"][:, 0:1])
    c.P.op(DVE, lambda e: e.reciprocal(out=ss[:, 2:3], in_=ss[:, 1:2]), reads=[ss_b], writes=[ss_b])
    c.act(hn[:, 0:1024], xt[:, 0:1024], AF.Copy, reads=[xbuf, ss_b], writes=[hn_b], scale=ss[:, 2:3])
    c.ts(DVE, hn[:, 1024:2048], xt[:, 1024:2048], ss[:, 2:3], None, ALU.mult, None, reads=[xbuf, ss_b], writes=[hn_b])
    for kq in range(KC // 4):
        bank, bb = c.next_bank()
        for j in range(4):
            kc = kq * 4 + j
            c.mm(bank[:, j * 128:(j + 1) * 128], hn[:, kc * 128:(kc + 1) * 128], ident[:], True, True,
                 reads=[hn_b, ident_buf], writes=[bb])
        for j in range(4):
            kc = kq * 4 + j
            if j % 2 == 0:
                c.act(hT[:, kc, tok0:tok0 + 128], bank[:, j * 128:(j + 1) * 128], AF.Copy,
                      reads=[bb, nw_buf], writes=[hT_buf], scale=nw[:, kc:kc + 1])
            else:
                c.ts(DVE, hT[:, kc, tok0:tok0 + 128], bank[:, j * 128:(j + 1) * 128], nw[:, kc:kc + 1], None,
                     ALU.mult, None, reads=[bb, nw_buf], writes=[hT_buf])


def norm_scratch(c, pfx):
    eps = c.sb(pfx + "eps", [128, 1], F32)
    eb = Buf(pfx + "eps")
    c.P.op(POOL, lambda e: e.memset(eps[:], EPS), writes=[eb])
    return {
        "junk": (c.sb(pfx + "junk", [128, 2048], BF16), Buf(pfx + "junk")),
        "ss": (c.sb(pfx + "ss", [128, 4], F32), Buf(pfx + "ss")),
        "hn": (c.sb(pfx + "hn", [128, 2048], BF16), Buf(pfx + "hn")),
        "eps": eps, "eps_b": eb,
    }


A_SECTIONS = [
    ("qk", 0, 2048, "F", F32, False),
    ("mv", 2048, 2048, "T", BF16, False),
    ("mo", 4096, 2048, "T", BF16, True),
    ("gt", 6144, 16, "F", F32, False),
    ("dqk", 6160, 4096, "F", BF16, False),
    ("dv", 10256, 2048, "T", BF16, False),
    ("gmd", 12304, 4096, "F", BF16, True),
]


def load_wblock(c, wslot, wbuf, w_dram, row0, nkc, col0, ncols):
    src = w_dram[row0:row0 + nkc * 128, col0:col0 + ncols].rearrange("(kc p) c -> p kc c", p=128)
    c.dma(POOL, wslot[:, 0:nkc, 0:ncols], src, reads=[], writes=[wbuf])


def phase_A(c, x, nw_d, w, ident_d, outs):
    if True:
        c.begin()
        hT = c.sb("hT", [128, KC, TOK], BF16)
        hT_b = Buf("hT")
        nw = c.sb("nw_s", [128, KC], F32)
        nw_b = Buf("nw")
        ident = c.sb("ident_s", [128, 128], BF16)
        ident_b = Buf("ident")
        c.dma(SP, nw[:], nw_d[:, :], [], [nw_b])
        c.dma(POOL, ident[:], ident_d[:, :], [], [ident_b])
        NW = 3
        wslots = [(c.sb("w%d" % i, [128, KC, 512], BF16), Buf("w%d" % i)) for i in range(NW)]
        xs = [(c.sb("x%d" % i, [128, D_MODEL], F32), Buf("x%d" % i)) for i in range(2)]
        scr = norm_scratch(c, "n_")
        of32 = [(c.sb("of%d" % i, [128, TOK], F32), Buf("of%d" % i)) for i in range(2)]
        obf = [(c.sb("ob%d" % i, [128, TOK], BF16), Buf("ob%d" % i)) for i in range(2)]
        otk = [(c.sb("ot%d" % i, [128, 512], BF16), Buf("ot%d" % i)) for i in range(3)]

        blocks = []
        for name, c0, n, mode, dt, sg in A_SECTIONS:
            for o in range(0, n, 512):
                blocks.append((name, c0, o, min(512, n - o), mode, dt, sg))
        PRE = NW - 1

        def issue_load(bi):
            name, c0, o, n, mode, dt, sg = blocks[bi]
            ws, wb = wslots[bi % NW]
            load_wblock(c, ws, wb, w, 0, KC, c0 + o, n)

        for bi in range(min(PRE, len(blocks))):
            issue_load(bi)

        for tt in range(TOK // 128):
            xt, xb = xs[tt % 2]
            c.dma(SP, xt[:], x[tt * 128:(tt + 1) * 128, :], [], [xb])
            rmsnorm_to_T(c, xt, xb, scr, hT, hT_b, tt * 128, nw, nw_b, ident, ident_b, "n_")

        finals = []
        cnt = {"f": 0, "b": 0, "t": 0}
        for bi, (name, c0, o, n, mode, dt, sg) in enumerate(blocks):
            if bi + PRE < len(blocks):
                issue_load(bi + PRE)
            ws, wb = wslots[bi % NW]
            od = outs[name]
            if mode == "F":
                for cc in range(0, n, 128):
                    m = min(128, n - cc)
                    if dt == F32:
                        ot, ob = of32[cnt["f"] % 2]
                        cnt["f"] += 1
                    else:
                        ot, ob = obf[cnt["b"] % 2]
                        cnt["b"] += 1
                    for tg in range(TOK // 512):
                        bank, bb = c.next_bank()
                        for kc in range(KC):
                            c.mm(bank[0:m, :], ws[:, kc, cc:cc + m], hT[:, kc, tg * 512:(tg + 1) * 512],
                                 kc == 0, kc == KC - 1, reads=[wb, hT_b], writes=[bb])
                        dst = ot[0:m, tg * 512:(tg + 1) * 512]
                        if sg:
                            c.act(dst, bank[0:m, :], AF.Sigmoid, reads=[bb], writes=[ob])
                        else:
                            c.evac_copy(dst, bank[0:m, :], reads=[bb], writes=[ob])
                    finals.append(c.dma(SP, od[o + cc:o + cc + m, :], ot[0:m, :], [ob], []))
            else:
                for tt in range(TOK // 128):
                    bank, bb = c.next_bank()
                    for kc in range(KC):
                        c.mm(bank[:, 0:n], hT[:, kc, tt * 128:(tt + 1) * 128], ws[:, kc, 0:n],
                             kc == 0, kc == KC - 1, reads=[wb, hT_b], writes=[bb])
                    ot, ob = otk[cnt["t"] % 3]
                    cnt["t"] += 1
                    if sg:
                        c.act(ot[:, 0:n], bank[:, 0:n], AF.Sigmoid, reads=[bb], writes=[ob])
                    else:
                        c.evac_copy(ot[:, 0:n], bank[:, 0:n], reads=[bb], writes=[ob])
                    finals.append(c.dma(SP, od[tt * 128:(tt + 1) * 128, o:o + n], ot[:, 0:n], [ob], []))
        c.end()


def nw_layout(v):
    return np.ascontiguousarray(v.reshape(KC, 128).T)


NH = 4
NCH = SEQ // 128
MASKNEG = -30000.0
Q_R, Q_C, Q_INTER, Q_W, Q_EM = 0, 1, 2, 3, 4
NSB = 3
MSKEW = 1


def phase_BC(c, d, do_m=True, do_a=True):
    cw_d, mv_d, mo_d, gb_d, mnw_d, dv_d, dnw_d = d["cw"], d["mv"], d["mo"], d["gb"], d["mnw"], d["dv"], d["dnw"]
    lam_d, lami_d, ident_d, mask_d, hmT_d, hdT_d = d["lam"], d["lami"], d["ident"], d["maskneg"], d["hmT"], d["hdT"]
    gi_d, gf_d = d["gi"], d["gf"]
    if True:
        c.begin()
        if "pre" in d:
            d["pre"](c)
        P = c.P
        identf = c.sb("identf", [128, 128], F32); identf_b = Buf("identf")
        identb = c.sb("identb", [128, 128], BF16); identb_b = Buf("identb")
        maskf = c.sb("maskf", [128, 128], F32); maskf_b = Buf("maskf")
        maskb = c.sb("maskb", [128, 128], BF16); maskb_b = Buf("maskb")
        onesf = c.sb("onesf", [128, 128], F32); onesf_b = Buf("onesf")
        onesb = c.sb("onesb", [128, 128], BF16); onesb_b = Buf("onesb")
        epst = c.sb("epst", [128, 1], F32); eps_b = Buf("epst")
        c.dma(SP, identf[:], ident_d[:, :], [], [identf_b])
        c.dma(POOL, identb[:], ident_d[:, :], [], [identb_b])
        c.dma(SP, maskf[:], mask_d[:, :], [], [maskf_b])
        c.dma(POOL, maskb[:], mask_d[:, :], [], [maskb_b])
        P.op(POOL, lambda e: e.memset(onesf[:], 1.0), writes=[onesf_b])
        P.op(POOL, lambda e: e.memset(onesb[:], 1.0), writes=[onesb_b])
        P.op(POOL, lambda e: e.memset(epst[:], EPS), writes=[eps_b])
        cw = c.sb("cw_s", [128, 2 * NH, 5], F32); cw_b = Buf("cw")
        c.dma(SP, cw[:], cw_d, [], [cw_b])
        gb = c.sb("gb_s", [128, 2], F32); gb_b = Buf("gb")
        c.dma(SP, gb[:], gb_d, [], [gb_b])
        mnw = c.sb("mnw_s", [128, NH * 256], F32); mnw_b = Buf("mnw")
        c.dma(SP, mnw[:], mnw_d, [], [mnw_b])
        dnw = c.sb("dnw_s", [128, NH * 256], F32); dnw_b = Buf("dnw")
        c.dma(SP, dnw[:], dnw_d, [], [dnw_b])
        lamt = c.sb("lamt", [128, 4, 128], F32); lamt_b = Buf("lamt")
        c.dma(SP, lamt[:], lam_d, [], [lamt_b])
        lami = c.sb("lami_s", [128, 2], F32); lami_b = Buf("lami")
        c.dma(SP, lami[:], lami_d, [], [lami_b])

        g_i = c.sb("g_i", [128, 128], F32); g_f = c.sb("g_f", [128, 128], F32)
        gi_b, gf_b = Buf("g_i"), Buf("g_f")
        c.dma(SP, g_i[:], gi_d.rearrange("j (ci l) -> (j ci) l", l=128), [], [gi_b])
        c.dma(SP, g_f[:], gf_d.rearrange("j (ci l) -> (j ci) l", l=128), [], [gf_b])
        sm = c.sb("gsm", [128, 16], F32); sm_b = Buf("gsm")
        NBF, MPREV, DEC, RLAST = 0, 1, 2, 3
        gq = c.sb("gq", [128, 5, 128], F32); gq_b = Buf("gq")
        g_b = c.sb("g_bb", [128, 128], F32); gbb_b = Buf("g_bb")
        g_ml = c.sb("g_ml", [128, 128], F32); gml_b = Buf("g_ml")
        g_m = c.sb("g_m", [128, 128], F32); gm_b = Buf("g_m")
        c.ts(DVE, g_i[:], g_i[:], gb[:, 0:1], None, ALU.add, None, [gi_b, gb_b], [gi_b])
        c.ts(DVE, sm[:, NBF:NBF + 1], gb[:, 1:2], -1.0, None, ALU.mult, None, [gb_b], [sm_b])
        c.act(g_f[:], g_f[:], AF.Exp, [gf_b, sm_b], [gf_b], bias=sm[:, NBF:NBF + 1], scale=-1.0)
        c.act(g_f[:], g_f[:], AF.Ln, [gf_b, onesf_b], [gf_b], bias=onesf[:, 0:1], scale=1.0)
        c.ts(DVE, g_f[:], g_f[:], -1.0, None, ALU.mult, None, [gf_b], [gf_b])
        P.op(DVE, lambda e: e.tensor_tensor_scan(out=g_b[:], data0=g_f[:], data1=g_f[:], initial=0.0,
                                                 op0=ALU.add, op1=ALU.min), reads=[gf_b], writes=[gbb_b])
        P.op(DVE, lambda e: e.tensor_tensor_scan(out=g_ml[:], data0=g_f[:], data1=g_i[:], initial=-1e30,
                                                 op0=ALU.add, op1=ALU.max), reads=[gf_b, gi_b], writes=[gml_b])
        bankr, bankr_b = c.next_bank()
        c.mm(bankr[0:1, 0:128], g_b[:, 127:128], identf[:], True, True, [gbb_b, identf_b], [bankr_b])
        c.mm(bankr[0:1, 128:256], g_ml[:, 127:128], identf[:], True, True, [gml_b, identf_b], [bankr_b])
        erow = c.sb("erow", [1, 512], F32); erow_b = Buf("erow")
        c.copy(DVE, erow[0:1, 0:256], bankr[0:1, 0:256], [bankr_b], [erow_b])
        P.op(POOL, lambda e: e.memset(erow[0:1, 256:512], 0.0), writes=[erow_b])
        for j in range(NH):
            P.op(DVE, lambda e, j=j: e.tensor_tensor_scan(
                out=erow[0:1, 384 + j * 32:384 + (j + 1) * 32], data0=erow[0:1, j * 32:(j + 1) * 32],
                data1=erow[0:1, 128 + j * 32:128 + (j + 1) * 32], initial=0.0, op0=ALU.add, op1=ALU.max),
                reads=[erow_b], writes=[erow_b])
            c.copy(DVE, erow[0:1, 256 + j * 32 + 1:256 + (j + 1) * 32], erow[0:1, 384 + j * 32:384 + (j + 1) * 32 - 1],
                   [erow_b], [erow_b])
        c.mm(bankr[:, 256:257], erow[0:1, 256:384], onesf[0:1, 0:1], True, True, [erow_b, onesf_b], [bankr_b])
        c.copy(DVE, sm[:, MPREV:MPREV + 1], bankr[:, 256:257], [bankr_b], [sm_b])
        c.stt(g_m[:], g_b[:], sm[:, MPREV:MPREV + 1], g_ml[:], ALU.add, ALU.max, [gbb_b, sm_b, gml_b], [gm_b])
        c.tt(DVE, gq[:, Q_R, :], g_b[:], g_m[:], ALU.subtract, [gbb_b, gm_b], [gq_b])
        c.tt(DVE, gq[:, Q_C, :], g_i[:], g_b[:], ALU.subtract, [gi_b, gbb_b], [gq_b])
        c.copy(DVE, sm[:, RLAST:RLAST + 1], gq[:, Q_R, 127:128], [gq_b], [sm_b])
        c.act(gq[:, Q_INTER, :], gq[:, Q_R, :], AF.Exp, [gq_b, sm_b], [gq_b], bias=sm[:, MPREV:MPREV + 1], scale=1.0)
        c.act(gq[:, Q_W, :], gq[:, Q_C, :], AF.Exp, [gq_b, sm_b], [gq_b], bias=sm[:, RLAST:RLAST + 1], scale=1.0)
        c.act(gq[:, Q_EM, :], g_m[:], AF.Exp, [gm_b], [gq_b], scale=-1.0)
        c.act(sm[:, DEC:DEC + 1], sm[:, RLAST:RLAST + 1], AF.Exp, [sm_b], [sm_b], bias=sm[:, MPREV:MPREV + 1], scale=1.0)
        tq = c.sb("tq", [128, 5, 128], F32); tq_b = Buf("tq")
        for n in range(5):
            bk, bkb = c.next_bank()
            c.mm(bk[:, 0:128], gq[:, n, :], identf[:], True, True, [gq_b, identf_b], [bkb])
            c.copy(DVE, tq[:, n, :], bk[:, 0:128], [bkb], [tq_b])
        decm = c.sb("decm", [128, 128], F32); decm_b = Buf("decm")
        c.ts(DVE, decm[:], onesf[:], sm[:, DEC:DEC + 1], None, ALU.mult, None, [onesf_b, sm_b], [decm_b])
        bk, bkb = c.next_bank()
        c.mm(bk[:, 0:128], decm[:], identf[:], True, True, [decm_b, identf_b], [bkb])
        decb = c.sb("decb", [128, 128], F32); decb_b = Buf("decb")
        c.copy(DVE, decb[:], bk[:, 0:128], [bkb], [decb_b])

        lsm = c.sb("lsm", [128, 8], F32); lsm_b = Buf("lsm")
        lpr = c.sb("lpr", [128, 2, 128], F32); lpr_b = Buf("lpr")
        c.tt(DVE, lpr[:, 0, :], lamt[:, 0, :], lamt[:, 1, :], ALU.mult, [lamt_b], [lpr_b])
        c.tt(DVE, lpr[:, 1, :], lamt[:, 2, :], lamt[:, 3, :], ALU.mult, [lamt_b], [lpr_b])
        P.op(DVE, lambda e: e.reduce_sum(out=lsm[:, 0:2], in_=lpr[:], axis=AX.X), reads=[lpr_b], writes=[lsm_b])
        c.act(lsm[:, 2:4], lsm[:, 0:2], AF.Exp, [lsm_b], [lsm_b])
        c.tt(DVE, lsm[:, 4:5], lsm[:, 2:3], lsm[:, 3:4], ALU.subtract, [lsm_b], [lsm_b])
        c.ts(DVE, lsm[:, 5:6], lsm[:, 4:5], lami[:, 0:1], -1.0, ALU.add, ALU.mult, [lsm_b, lami_b], [lsm_b])
        NLAM = 5

        rawq = c.sb("rawq", [128, SEQ + 3], F32); rawq_b = Buf("rawq")
        rawk = c.sb("rawk", [128, SEQ + 3], F32); rawk_b = Buf("rawk")
        acc = c.sb("acc", [128, SEQ], F32); acc_b = Buf("acc")
        qT = c.sb("qT", [128, SEQ], BF16); qT_b = Buf("qT")
        kT = c.sb("kT", [128, SEQ], BF16); kT_b = Buf("kT")
        va = c.sb("va", [128, NCH, 257], BF16); va_b = Buf("va")
        P.op(POOL, lambda e: e.memset(rawq[:, 0:3], 0.0), writes=[rawq_b])
        P.op(POOL, lambda e: e.memset(rawk[:, 0:3], 0.0), writes=[rawk_b])
        P.op(POOL, lambda e: e.memset(va[:, :, 256:257], 1.0), writes=[va_b])
        CT = c.sb("CT", [128, 257], F32); CT_b = Buf("CT")
        CTb = c.sb("CTb", [128, 257], BF16); CTb_b = Buf("CTb")
        diagR = [(c.sb("diagR%d" % i, [128, 128], F32), Buf("diagR%d" % i)) for i in range(2)]
        Dm = [(c.sb("Dm%d" % i, [128, 128], F32), Buf("Dm%d" % i)) for i in range(2)]
        sdT = [(c.sb("sdT%d" % i, [128, 128], BF16), Buf("sdT%d" % i)) for i in range(2)]
        kw = [(c.sb("kw%d" % i, [128, 128], BF16), Buf("kw%d" % i)) for i in range(2)]
        numS = [(c.sb("numS%d" % i, [128, 257], F32), Buf("numS%d" % i)) for i in range(4)]
        tot = [(c.sb("tot%d" % i, [128, 257], F32), Buf("tot%d" % i)) for i in range(8)]
        CT2 = [(c.sb("CT2_%d" % i, [128, 257], F32), Buf("CT2_%d" % i)) for i in range(2)]
        CTb2 = [(c.sb("CTb2_%d" % i, [128, 257], BF16), Buf("CTb2_%d" % i)) for i in range(2)]
        junk = c.sb("junk", [128, 256], BF16); junk_b = Buf("junk")
        hs = [(c.sb("hs%d" % i, [128, 8], F32), Buf("hs%d" % i)) for i in range(8)]
        g2 = [(c.sb("g2_%d" % i, [128, 256], F32), Buf("g2_%d" % i)) for i in range(4)]
        hmt = [(c.sb("hm%d" % i, [128, 256], BF16), Buf("hm%d" % i)) for i in range(2)]
        mos = [(c.sb("mos%d" % i, [128, 4, 256], BF16), Buf("mos%d" % i)) for i in range(4)]
        hout = [(c.sb("hout%d" % i, [128, 2, 512], BF16), Buf("hout%d" % i)) for i in range(2)]
        kscale = 128.0 ** -0.5

        def conv_silu(raw, raw_b, idx, dst, dst_b, post_scale):
            c.ts(DVE, acc[:], raw[:, 0:SEQ], cw[:, idx, 0:1], cw[:, idx, 4:5], ALU.mult, ALU.add,
                 [raw_b, cw_b], [acc_b])
            for t in range(1, 4):
                c.stt(acc[:], raw[:, t:t + SEQ], cw[:, idx, t:t + 1], acc[:], ALU.mult, ALU.add,
                      [raw_b, cw_b, acc_b], [acc_b])
            if post_scale is None:
                c.act(dst[:], acc[:], AF.Silu, [acc_b], [dst_b])
            else:
                c.act(acc[:], acc[:], AF.Silu, [acc_b], [acc_b])
                c.ts(POOL, dst[:], acc[:], post_scale, None, ALU.mult, None, [acc_b], [dst_b])

        grp = 0
        for j in range(NH if do_m else 0):
            if j == 0:
                c.dma(SP, rawq[:, 3:SEQ + 3], d["qraw"](j), [], [rawq_b])
                c.dma(SP, rawk[:, 3:SEQ + 3], d["kraw"](j), [], [rawk_b])
            c.dma(SP, va[:, :, 0:256], mv_d[:, j * 256:(j + 1) * 256].rearrange("(ci p) v -> p ci v", p=128),
                  [], [va_b])
            conv_silu(rawq, rawq_b, j, qT, qT_b, None)
            conv_silu(rawk, rawk_b, NH + j, kT, kT_b, kscale)
            if j + 1 < NH:
                c.dma(SP, rawq[:, 3:SEQ + 3], d["qraw"](j + 1), [], [rawq_b])
                c.dma(SP, rawk[:, 3:SEQ + 3], d["kraw"](j + 1), [], [rawk_b])
            for k2 in range(2):
                P.op(POOL, lambda e, k2=k2: e.memset(CT2[k2][0][:], 0.0), writes=[CT2[k2][1]])
                P.op(POOL, lambda e, k2=k2: e.memset(CTb2[k2][0][:], 0.0), writes=[CTb2[k2][1]])

            def s0(ci):
                p = j * NCH + ci
                cs = slice(ci * 128, (ci + 1) * 128)
                bA, bA_b = c.banks[ci % 2], c.bank_bufs[ci % 2]
                dR, dR_b = diagR[ci % 2]
                c.ts(POOL, dR[:], identf[:], tq[:, Q_R, p:p + 1], None, ALU.mult, None, [identf_b, tq_b], [dR_b])
                c.mm(bA[:, 0:128], kT[:, cs], qT[:, cs], True, True, [kT_b, qT_b], [bA_b])
                c.mm(bA[:, 128:256], onesf[:], dR[:], True, False, [onesf_b, dR_b], [bA_b])
                c.mm(bA[:, 128:256], identf[:], maskf[:], False, True, [identf_b, maskf_b], [bA_b])
                c.mm(bA[:, 256:384], kT[:, cs], identb[:], True, True, [kT_b, identb_b], [bA_b])
                if ci % 4 == 0:
                    mo_t, mo_b = mos[(ci // 4) % 4]
                    c.dma(SP, mo_t[:], mo_d[ci * 128:(ci + 4) * 128, j * 256:(j + 1) * 256]
                          .rearrange("(cc p) v -> p cc v", p=128), [], [mo_b])

            def s1(ci):
                p = j * NCH + ci
                bA, bA_b = c.banks[ci % 2], c.bank_bufs[ci % 2]
                dm, dm_b = Dm[ci % 2]
                c.act(dm[:], bA[:, 128:256], AF.Exp, [bA_b, tq_b], [dm_b], bias=tq[:, Q_C, p:p + 1], scale=1.0)

            def s2(ci):
                p = j * NCH + ci
                bA, bA_b = c.banks[ci % 2], c.bank_bufs[ci % 2]
                dm, dm_b = Dm[ci % 2]
                sd, sd_b = sdT[ci % 2]
                c.tt(DVE, sd[:], bA[:, 0:128], dm[:], ALU.mult, [bA_b, dm_b], [sd_b])
                kwt, kw_b = kw[ci % 2]
                c.ts(DVE, kwt[:], bA[:, 256:384], tq[:, Q_W, p:p + 1], None, ALU.mult, None, [bA_b, tq_b], [kw_b])

            def s3(ci):
                bB, bB_b = c.banks[2 + ci % 2], c.bank_bufs[2 + ci % 2]
                bD, bD_b = c.banks[6], c.bank_bufs[6]
                sd, sd_b = sdT[ci % 2]
                kwt, kw_b = kw[ci % 2]
                c.mm(bB[:, 0:257], sd[:], va[:, ci, :], True, True, [sd_b, va_b], [bB_b])
                c.mm(bD[:, 0:257], kwt[:], va[:, ci, :], True, True, [kw_b, va_b], [bD_b])

            def s4(ci):
                p = j * NCH + ci
                bB, bB_b = c.banks[2 + ci % 2], c.bank_bufs[2 + ci % 2]
                bD, bD_b = c.banks[6], c.bank_bufs[6]
                ns, ns_b = numS[ci % 4]
                c.copy(ACT, ns[:], bB[:, 0:257], [bB_b], [ns_b])
                cn, cn_b = CT2[ci % 2]
                cp, cp_b = CT2[(ci + 1) % 2]
                c.stt(cn[:], cp[:], decb[:, p:p + 1], bD[:, 0:257], ALU.mult, ALU.add, [cp_b, decb_b, bD_b], [cn_b])

            def s5(ci):
                cs = slice(ci * 128, (ci + 1) * 128)
                cn, cn_b = CT2[ci % 2]
                cb, cb_b = CTb2[ci % 2]
                c.copy(POOL, cb[:], cn[:], [cn_b], [cb_b])
                cbp, cbp_b = CTb2[(ci + 1) % 2]
                bC, bC_b = c.banks[4 + ci % 2], c.bank_bufs[4 + ci % 2]
                c.mm(bC[:, 0:257], qT[:, cs], cbp[:], True, True, [qT_b, cbp_b], [bC_b])

            def s6(ci):
                p = j * NCH + ci
                bC, bC_b = c.banks[4 + ci % 2], c.bank_bufs[4 + ci % 2]
                ns, ns_b = numS[ci % 4]
                tt_, tt_b = tot[ci % 8]
                c.stt(tt_[:], bC[:, 0:257], tq[:, Q_INTER, p:p + 1], ns[:], ALU.mult, ALU.add,
                      [bC_b, tq_b, ns_b], [tt_b])

            def s7(ci):
                tt_, tt_b = tot[ci % 8]
                h_, h_b = hs[ci % 8]
                c.act(h_[:, 7:8], tt_[:, 256:257], AF.Abs, [tt_b], [h_b])
                c.act(junk[:], tt_[:, 0:256], AF.Square, [tt_b], [junk_b, h_b], accum_out=h_[:, 2:3])

            def s8(ci):
                p = j * NCH + ci
                h_, h_b = hs[ci % 8]
                c.ts(DVE, h_[:, 0:1], h_[:, 7:8], tq[:, Q_EM, p:p + 1], None, ALU.max, None, [h_b, tq_b], [h_b])
                mo_t, mo_b = mos[(ci // 4) % 4]
                g2t, g2_b = g2[ci % 4]
                c.tt(POOL, g2t[:], mnw[:, j * 256:(j + 1) * 256], mo_t[:, ci % 4, :], ALU.mult, [mnw_b, mo_b], [g2_b])

            def s9(ci):
                h_, h_b = hs[ci % 8]
                c.act(h_[:, 1:2], h_[:, 0:1], AF.Square, [h_b], [h_b], scale=EPS ** 0.5)
                c.act(h_[:, 4:5], h_[:, 2:3], AF.Sqrt, [h_b], [h_b], bias=h_[:, 1:2], scale=1.0 / 256)

            def s10(ci):
                h_, h_b = hs[ci % 8]
                tt_, tt_b = tot[ci % 8]
                g2t, g2_b = g2[ci % 4]
                P.op(DVE, lambda e, h_=h_: e.reciprocal(out=h_[:, 6:7], in_=h_[:, 4:5]), reads=[h_b], writes=[h_b])
                hm_, hm_b = hmt[ci % 2]
                c.stt(hm_[:], tt_[:, 0:256], h_[:, 6:7], g2t[:], ALU.mult, ALU.mult, [tt_b, h_b, g2_b], [hm_b])

            def s11(ci):
                bT, bT_b = c.banks[7], c.bank_bufs[7]
                hm_, hm_b = hmt[ci % 2]
                for vc in range(2):
                    c.mm(bT[:, vc * 128:(vc + 1) * 128], hm_[:, vc * 128:(vc + 1) * 128], identb[:], True, True,
                         [hm_b, identb_b], [bT_b])

            def s12(ci):
                bT, bT_b = c.banks[7], c.bank_bufs[7]
                ho_t, ho_b = hout[(ci // 4) % 2]
                c.copy(ACT, ho_t[:, :, (ci % 4) * 128:(ci % 4 + 1) * 128],
                       bT[:, 0:256].rearrange("p (a b) -> p a b", a=2), [bT_b], [ho_b])
                if ci % 4 == 3:
                    c.dma(SP, hmT_d[j * 256:(j + 1) * 256, (ci - 3) * 128:(ci + 1) * 128]
                          .rearrange("(a p) t -> p a t", p=128), ho_t[:], [ho_b], [])

            stages = [s0, s1, s2, s3, s4, s5, s6, s7, s8, s9, s10, s11, s12]
            for it in range(NCH + len(stages) - 1):
                for sidx in range(len(stages) - 1, -1, -1):
                    ci = it - sidx
                    if 0 <= ci < NCH:
                        stages[sidx](ci)

        qc = [(c.sb("dq%d" % i, [128, SEQ], BF16), Buf("dq%d" % i)) for i in range(2)]
        kc_ = [(c.sb("dk%d" % i, [128, SEQ], BF16), Buf("dk%d" % i)) for i in range(2)]
        sq = c.sb("sq", [128, SEQ], BF16); sq_b = Buf("sq")
        mx = c.sb("mx", [1, 64], F32); mx_b = Buf("mx")
        nG = c.sb("nG", [128, 1], F32); nG_b = Buf("nG")
        Et = [(c.sb("E%d" % i, [128, 512], BF16), Buf("E%d" % i)) for i in range(NSB + 2)]
        ds = [(c.sb("ds%d" % i, [128, 8], F32), Buf("ds%d" % i)) for i in range(4)]
        dtm = [(c.sb("dt%d" % i, [128, 256], F32), Buf("dt%d" % i)) for i in range(4)]
        dhd = [(c.sb("dhd%d" % i, [128, 256], F32), Buf("dhd%d" % i)) for i in range(4)]
        dhn = [(c.sb("dhn%d" % i, [128, 256], BF16), Buf("dhn%d" % i)) for i in range(4)]
        Os1 = [(c.sb("os1_%d" % i, [128, 257], F32), Buf("os1_%d" % i)) for i in range(4)]
        Os2 = [(c.sb("os2_%d" % i, [128, 257], F32), Buf("os2_%d" % i)) for i in range(4)]
        junkf = c.sb("junkf", [128, 256], F32); junkf_b = Buf("junkf")
        ascale = 128.0 ** -0.5
        Obanks = [(c.banks[i], c.bank_bufs[i]) for i in range(4)]
        Sbanks = [(c.banks[4 + i], c.bank_bufs[4 + i]) for i in range(NSB)]
        Tbanks = [(c.banks[4 + NSB + i], c.bank_bufs[4 + NSB + i]) for i in range(4 - NSB)]
        e_i = 0
        s_i = 0
        t_i = 0
        for j in range(NH if do_a else 0):
            for cc in range(2):
                c.dma(SP, qc[cc][0][:], d["dq"](j, cc), [], [qc[cc][1]])
                c.dma(SP, kc_[cc][0][:], d["dk"](j, cc), [], [kc_[cc][1]])
            c.dma(SP, va[:, :, 0:256], dv_d[:, j * 256:(j + 1) * 256].rearrange("(ci p) v -> p ci v", p=128),
                  [], [va_b])
            tb, tb_b = Tbanks[0]
            for ti, (tns, tns_b) in enumerate([qc[0], qc[1], kc_[0], kc_[1]]):
                c.act(sq[:], tns[:], AF.Square, [tns_b], [sq_b])
                for s8 in range(8):
                    c.mm(tb[0:1, 0:512], onesb[:, 0:1], sq[:, s8 * 512:(s8 + 1) * 512], True, True,
                         [onesb_b, sq_b], [tb_b])
                    P.op(DVE, lambda e, ti=ti, s8=s8: e.reduce_max(out=mx[0:1, ti * 8 + s8:ti * 8 + s8 + 1],
                                                                   in_=tb[0:1, 0:512], axis=AX.X),
                         reads=[tb_b], writes=[mx_b])
            P.op(DVE, lambda e: e.reduce_max(out=mx[0:1, 32:34], in_=mx[0:1, 0:32].rearrange("p (a b) -> p a b", a=2),
                                             axis=AX.X), reads=[mx_b], writes=[mx_b])
            c.tt(DVE, mx[0:1, 34:35], mx[0:1, 32:33], mx[0:1, 33:34], ALU.mult, [mx_b], [mx_b])
            c.act(mx[0:1, 35:36], mx[0:1, 34:35], AF.Sqrt, [mx_b], [mx_b], scale=ascale * ascale)
            c.ts(DVE, mx[0:1, 36:37], mx[0:1, 35:36], -1.0, None, ALU.mult, None, [mx_b], [mx_b])
            c.mm(tb[:, 0:1], onesf[0:1, :], mx[0:1, 36:37], True, True, [onesf_b, mx_b], [tb_b])
            c.copy(DVE, nG[:], tb[:, 0:1], [tb_b], [nG_b])
            if "dbg" in d:
                c.dma(SP, d["dbg"][j, 0:1, 0:64], mx[0:1, :], [mx_b], [])

            def qk_exp(g, kb):
                nonlocal s_i, e_i
                sb_, sb_b = Sbanks[s_i % NSB]; s_i += 1
                et, et_b = Et[e_i % (NSB + 2)]; e_i += 1
                ks = slice(kb * 128, (kb + 1) * 128)
                if kb <= 2 * g:
                    for cc in range(2):
                        diag = (kb == 2 * g)
                        c.mm(sb_[:, cc * 256:(cc + 1) * 256], kc_[cc][0][:, ks], qc[cc][0][:, g * 256:(g + 1) * 256],
                             True, not diag, [kc_[cc][1], qc[cc][1]], [sb_b])
                        if diag:
                            c.mm(sb_[:, cc * 256:cc * 256 + 128], identb[:], maskb[:], False, True,
                                 [identb_b, maskb_b], [sb_b])
                    c.act(et[:], sb_[:], AF.Exp, [sb_b, nG_b], [et_b], bias=nG[:, 0:1], scale=ascale)
                    ilist = (0, 1)
                else:
                    for cc in range(2):
                        c.mm(sb_[:, cc * 256 + 128:(cc + 1) * 256], kc_[cc][0][:, ks],
                             qc[cc][0][:, g * 256 + 128:(g + 1) * 256], True, False, [kc_[cc][1], qc[cc][1]], [sb_b])
                        c.mm(sb_[:, cc * 256 + 128:(cc + 1) * 256], identb[:], maskb[:], False, True,
                             [identb_b, maskb_b], [sb_b])
                    c.act(et[:].rearrange("p (a b) -> p a b", a=2)[:, :, 128:256],
                          sb_[:].rearrange("p (a b) -> p a b", a=2)[:, :, 128:256], AF.Exp,
                          [sb_b, nG_b], [et_b], bias=nG[:, 0:1], scale=ascale)
                    ilist = (1,)
                return (g, kb, et, et_b, ilist)

            def pv(st):
                g, kb, et, et_b, ilist = st
                for cc in range(2):
                    for i in ilist:
                        ob, ob_b = Obanks[cc * 2 + i]
                        last = (kb == 2 * g + i)
                        c.mm(ob[:, 0:257], et[:, cc * 256 + i * 128:cc * 256 + (i + 1) * 128], va[:, kb, :],
                             kb == 0, last, [et_b, va_b], [ob_b])
                if kb == 2 * g + 1:
                    epilogue(g)

            def epilogue(g):
                nonlocal t_i
                for i in range(2):
                    qb = 2 * g + i
                    r = qb % 4
                    o1, o1_b = Obanks[i]
                    o2, o2_b = Obanks[2 + i]
                    os1, os1_b = Os1[r]
                    os2, os2_b = Os2[r]
                    c.copy(DVE, os1[:], o1[:, 0:257], [o1_b], [os1_b])
                    c.copy(DVE, os2[:], o2[:, 0:257], [o2_b], [os2_b])
                R = [(2 * g + i) % 4 for i in range(2)]
                for r in R:
                    P.op(DVE, lambda e, d_=ds[r][0], os1=Os1[r][0]: e.reciprocal(out=d_[:, 0:1], in_=os1[:, 256:257]),
                         reads=[Os1[r][1]], writes=[ds[r][1]])
                for r in R:
                    P.op(DVE, lambda e, d_=ds[r][0], os2=Os2[r][0]: e.reciprocal(out=d_[:, 1:2], in_=os2[:, 256:257]),
                         reads=[Os2[r][1]], writes=[ds[r][1]])
                for r in R:
                    d_, d_b = ds[r]
                    c.ts(DVE, d_[:, 2:3], d_[:, 1:2], lsm[:, NLAM:NLAM + 1], None, ALU.mult, None, [d_b, lsm_b], [d_b])
                for r in R:
                    d_, d_b = ds[r]
                    c.ts(DVE, dtm[r][0][:], Os1[r][0][:, 0:256], d_[:, 0:1], None, ALU.mult, None,
                         [Os1[r][1], d_b], [dtm[r][1]])
                for r in R:
                    d_, d_b = ds[r]
                    c.stt(dhd[r][0][:], Os2[r][0][:, 0:256], d_[:, 2:3], dtm[r][0][:], ALU.mult, ALU.add,
                          [Os2[r][1], d_b, dtm[r][1]], [dhd[r][1]])
                for r in R:
                    P.op(DVE, lambda e, hd_=dhd[r][0], d_=ds[r][0]: e.scalar_tensor_tensor(
                        out=junkf[:], in0=hd_[:], scalar=1.0, in1=hd_[:], op0=ALU.mult, op1=ALU.mult,
                        accum_out=d_[:, 3:4]), reads=[dhd[r][1]], writes=[junkf_b, ds[r][1]])
                defer.append([it_no[0] + 3, stage_b, (g,)])

            def stage_b(g):
                R = [(2 * g + i) % 4 for i in range(2)]
                for r in R:
                    d_, d_b = ds[r]
                    c.act(d_[:, 4:5], d_[:, 3:4], AF.Sqrt, [d_b, eps_b], [d_b], bias=epst[:, 0:1], scale=1.0 / 256)
                for r in R:
                    P.op(DVE, lambda e, d_=ds[r][0]: e.reciprocal(out=d_[:, 5:6], in_=d_[:, 4:5]),
                         reads=[ds[r][1]], writes=[ds[r][1]])
                for r in R:
                    d_, d_b = ds[r]
                    c.ts(DVE, d_[:, 6:7], d_[:, 5:6], lami[:, 1:2], None, ALU.mult, None, [d_b, lami_b], [d_b])
                for r in R:
                    d_, d_b = ds[r]
                    c.stt(dhn[r][0][:], dhd[r][0][:], d_[:, 6:7], dnw[:, j * 256:(j + 1) * 256], ALU.mult, ALU.mult,
                          [dhd[r][1], d_b, dnw_b], [dhn[r][1]])
                for i in range(2):
                    defer.append([it_no[0] + 3, stage_c, (g, i)])

            def stage_c(g, i):
                nonlocal t_i
                qb = 2 * g + i
                r = qb % 4
                hn_, hn_b = dhn[r]
                tb, tb_b = Tbanks[t_i % (4 - NSB)]; t_i += 1
                for vc in range(2):
                    c.mm(tb[:, vc * 128:(vc + 1) * 128], hn_[:, vc * 128:(vc + 1) * 128], identb[:], True, True,
                         [hn_b, identb_b], [tb_b])
                ho_t, ho_b = hout[(qb // 4) % 2]
                c.copy(DVE, ho_t[:, :, (qb % 4) * 128:(qb % 4 + 1) * 128],
                       tb[:, 0:256].rearrange("p (a b) -> p a b", a=2), [tb_b], [ho_b])
                if qb % 4 == 3:
                    c.dma(SP, hdT_d[j * 256:(j + 1) * 256, (qb - 3) * 128:(qb + 1) * 128]
                          .rearrange("(a p) t -> p a t", p=128), ho_t[:], [ho_b], [])

            defer = []
            it_no = [0]

            def run_deferred(flush=False):
                k = 0
                while k < len(defer):
                    if flush or defer[k][0] <= it_no[0]:
                        _, fn, args = defer.pop(k)
                        fn(*args)
                    else:
                        k += 1

            pairs = [(g, kb) for g in range(NCH // 2) for kb in range(2 * g + 2)]
            pend = []
            for (g, kb) in pairs:
                pend.append(qk_exp(g, kb))
                if len(pend) > NSB - 1:
                    pv(pend.pop(0))
                it_no[0] += 1
                run_deferred()
            while pend:
                pv(pend.pop(0))
            while defer:
                run_deferred(flush=True)
        c.end()


IDENT = np.eye(128, dtype=np.float32)
MASKNEG_NP = np.where(np.arange(128)[:, None] <= np.arange(128)[None, :], 0.0, MASKNEG).astype(np.float32)


def lam_init_of(l):
    return 0.8 - 0.6 * math.exp(-0.3 * l)


TG = 512
NFC = D_FF // 128


def phase_D(c, d, final, last):
    x_d, hm_d, hd_d, gmd_d, xo_d = d["x"], d["hmT"], d["hdT"], d["gmd"], d["xo"]
    wbm_d, wbd_d, wout_d, wg_d, wu_d, wd_d = d["wbm"], d["wbd"], d["wout"], d["wg"], d["wu"], d["wd"]
    nw2_d, ident_d = d["nw2"], d["ident"]
    if final:
        fnw_d = d["fnw"]
    if True:
        c.begin()
        P = c.P
        ident = c.sb("ident_s", [128, 128], BF16); ident_b = Buf("ident")
        c.dma(POOL, ident[:], ident_d[:, :], [], [ident_b])
        nw2 = c.sb("nw2_s", [128, KC], F32); nw2_b = Buf("nw2")
        c.dma(SP, nw2[:], nw2_d, [], [nw2_b])
        if final:
            fnw = c.sb("fnw_s", [128, D_MODEL], F32); fnw_b = Buf("fnw")
            c.dma(SP, fnw[:], fnw_d, [], [fnw_b])
        hmg = c.sb("hmg", [128, KC, TG], BF16); hmg_b = Buf("hmg")
        hdg = c.sb("hdg", [128, KC, TG], BF16); hdg_b = Buf("hdg")
        yT = c.sb("yT", [128, KC, TG], BF16); yT_b = Buf("yT")
        xg = c.sb("xg", [128, TG // 128, D_MODEL], F32)
        xg_b = [Buf("xg%d" % i) for i in range(TG // 128)]
        aT = c.sb("aT", [128, NFC, TG], BF16); aT_b = Buf("aT")
        NW = 3
        wslots = [(c.sb("w%d" % i, [128, KC, 512], BF16), Buf("w%d" % i)) for i in range(NW)]
        t1 = [(c.sb("t1_%d" % i, [128, TG], F32), Buf("t1_%d" % i)) for i in range(4)]
        t2 = [(c.sb("t2_%d" % i, [128, TG], F32), Buf("t2_%d" % i)) for i in range(2)]
        sgm = [(c.sb("sgm%d" % i, [128, TG], BF16), Buf("sgm%d" % i)) for i in range(2)]
        sgd = [(c.sb("sgd%d" % i, [128, TG], BF16), Buf("sgd%d" % i)) for i in range(2)]
        scr = norm_scratch(c, "n_")

        wlist = []
        for g in range(TOK // TG):
            for blk in range(4):
                wlist.append((wbm_d, 0, KC, blk * 512, 512))
                wlist.append((wbd_d, 0, KC, blk * 512, 512))
            for cg in range(4):
                wlist.append((wout_d, 0, KC, cg * 512, 512))
            for blk in range(D_FF // 512):
                wlist.append((wg_d, 0, KC, blk * 512, 512))
                wlist.append((wu_d, 0, KC, blk * 512, 512))
            for cg in range(4):
                for fb in range(4):
                    wlist.append((wd_d, fb * 11 * 128, 11, cg * 512, 512))
        wstate = {"issued": 0, "used": 0, "done": 0}

        def issue_one():
            i = wstate["issued"]
            wdr, r0, nkc, c0, ncol = wlist[i]
            ws, wb = wslots[i % NW]
            load_wblock(c, ws, wb, wdr, r0, nkc, c0, ncol)
            wstate["issued"] += 1

        def next_w():
            i = wstate["used"]
            while wstate["issued"] <= i:
                assert wstate["issued"] < wstate["done"] + NW
                issue_one()
            wstate["used"] += 1
            return wslots[i % NW]

        def release_w():
            wstate["done"] = wstate["used"]
            while wstate["issued"] < len(wlist) and wstate["issued"] < wstate["done"] + NW:
                issue_one()

        k2 = 0
        for g in range(TOK // TG):
            ts0 = g * TG
            if g == 0:
                c.dma(SP, hmg[:], hm_d[:, ts0:ts0 + TG].rearrange("(kc p) t -> p kc t", p=128), [], [hmg_b])
                c.dma(SP, hdg[:], hd_d[:, ts0:ts0 + TG].rearrange("(kc p) t -> p kc t", p=128), [], [hdg_b])
            tsn = ts0 + TG
            for tt in range(TG // 128):
                c.dma(SP, xg[:, tt, :], x_d[ts0 + tt * 128:ts0 + (tt + 1) * 128, :], [], [xg_b[tt]])
            for blk in range(4):
                wm, wm_b = next_w()
                for cc in range(4):
                    col = blk * 4 + cc
                    sm_, sm_b = sgm[cc % 2]
                    c.dma(SP, sm_[:], gmd_d[col * 128:(col + 1) * 128, ts0:ts0 + TG], [], [sm_b])
                    bA, bA_b = c.next_bank()
                    for kc in range(KC):
                        c.mm(bA[:, :], wm[:, kc, cc * 128:(cc + 1) * 128], hmg[:, kc, :], kc == 0, kc == KC - 1,
                             [wm_b, hmg_b], [bA_b])
                    a1, a1_b = t1[cc]
                    c.tt(DVE, a1[:], bA[:, :], sm_[:], ALU.mult, [bA_b, sm_b], [a1_b])
                release_w()
                wd_, wd_b = next_w()
                for cc in range(4):
                    col = blk * 4 + cc
                    sd_, sd_b = sgd[cc % 2]
                    c.dma(SP, sd_[:], gmd_d[D_MODEL + col * 128:D_MODEL + (col + 1) * 128, ts0:ts0 + TG], [], [sd_b])
                    bB, bB_b = c.next_bank()
                    for kc in range(KC):
                        c.mm(bB[:, :], wd_[:, kc, cc * 128:(cc + 1) * 128], hdg[:, kc, :], kc == 0, kc == KC - 1,
                             [wd_b, hdg_b], [bB_b])
                    a1, a1_b = t1[cc]
                    a2, a2_b = t2[cc % 2]
                    c.tt(DVE, a2[:], bB[:, :], sd_[:], ALU.mult, [bB_b, sd_b], [a2_b])
                    c.tt(POOL, yT[:, col, :], a1[:], a2[:], ALU.add, [a1_b, a2_b], [yT_b])
                release_w()
            if g + 1 < TOK // TG:
                c.dma(SP, hdg[:], hd_d[:, tsn:tsn + TG].rearrange("(kc p) t -> p kc t", p=128), [], [hdg_b])
            for cg in range(4):
                wo, wo_b = next_w()
                for tt in range(TG // 128):
                    bk, bk_b = c.next_bank()
                    for kc in range(KC):
                        c.mm(bk[:, :], yT[:, kc, tt * 128:(tt + 1) * 128], wo[:, kc, :], kc == 0, kc == KC - 1,
                             [yT_b, wo_b], [bk_b])
                    c.tt(DVE, xg[:, tt, cg * 512:(cg + 1) * 512], bk[:, :], xg[:, tt, cg * 512:(cg + 1) * 512], ALU.add,
                         [bk_b, xg_b[tt]], [xg_b[tt]])
                release_w()
            for tt in range(TG // 128):
                rmsnorm_to_T(c, xg[:, tt, :], xg_b[tt], scr, hmg, hmg_b, tt * 128, nw2, nw2_b, ident, ident_b, "n_")
            for blk in range(D_FF // 512):
                wg_, wg_b = next_w()
                for cc in range(4):
                    bG, bG_b = c.next_bank()
                    for kc in range(KC):
                        c.mm(bG[:, :], wg_[:, kc, cc * 128:(cc + 1) * 128], hmg[:, kc, :], kc == 0, kc == KC - 1,
                             [wg_b, hmg_b], [bG_b])
                    a1, a1_b = t1[cc]
                    c.act(a1[:], bG[:, :], AF.Silu, [bG_b], [a1_b])
                release_w()
                wu_, wu_b = next_w()
                for cc in range(4):
                    fc = blk * 4 + cc
                    bU, bU_b = c.next_bank()
                    for kc in range(KC):
                        c.mm(bU[:, :], wu_[:, kc, cc * 128:(cc + 1) * 128], hmg[:, kc, :], kc == 0, kc == KC - 1,
                             [wu_b, hmg_b], [bU_b])
                    a1, a1_b = t1[cc]
                    c.tt(DVE, aT[:, fc, :], bU[:, :], a1[:], ALU.mult, [bU_b, a1_b], [aT_b])
                release_w()
            if g + 1 < TOK // TG:
                c.dma(SP, hmg[:], hm_d[:, tsn:tsn + TG].rearrange("(kc p) t -> p kc t", p=128), [], [hmg_b])
            for cg in range(4):
                bks = [c.next_bank() for _ in range(TG // 128)]
                for fb in range(4):
                    wdn, wdn_b = next_w()
                    for tt in range(TG // 128):
                        bk, bk_b = bks[tt]
                        for i in range(11):
                            fc = fb * 11 + i
                            c.mm(bk[:, :], aT[:, fc, tt * 128:(tt + 1) * 128], wdn[:, i, :], fc == 0, fc == NFC - 1,
                                 [aT_b, wdn_b], [bk_b])
                    release_w()
                for tt in range(TG // 128):
                    bk, bk_b = bks[tt]
                    c.tt(DVE, xg[:, tt, cg * 512:(cg + 1) * 512], bk[:, :], xg[:, tt, cg * 512:(cg + 1) * 512], ALU.add,
                         [bk_b, xg_b[tt]], [xg_b[tt]])
            for tt in range(TG // 128):
                if final:
                    junk, junk_b = scr["junk"]
                    ss, ss_b = scr["ss"]
                    c.act(junk[:], xg[:, tt, :], AF.Square, [xg_b[tt]], [junk_b, ss_b], accum_out=ss[:, 0:1])
                    c.act(ss[:, 1:2], ss[:, 0:1], AF.Sqrt, [ss_b, scr["eps_b"]], [ss_b], scale=1.0 / D_MODEL,
                          bias=scr["eps"][:, 0:1])
                    P.op(DVE, lambda e, ss=ss: e.reciprocal(out=ss[:, 2:3], in_=ss[:, 1:2]), reads=[ss_b], writes=[ss_b])
                    c.stt(xg[:, tt, :], xg[:, tt, :], ss[:, 2:3], fnw[:], ALU.mult, ALU.mult,
                          [xg_b[tt], ss_b, fnw_b], [xg_b[tt]])
                c.dma(SP, xo_d[ts0 + tt * 128:ts0 + (tt + 1) * 128, :], xg[:, tt, :], [xg_b[tt]], [], sem_key=xg_b[tt])
        c.end(last)


NUSED = 4


def build_fused(depth=DEPTH):
    nc = bass.Bass("TRN2", target_bir_lowering=False)
    di = lambda n, s, dt: nc.dram_tensor(n, s, dt, kind="ExternalInput").ap()
    x_in = di("x", [SEQ, D_MODEL], F32)
    w_in = di("w_in", [DEPTH, D_MODEL, N_IN], F32)
    w_bm = di("w_branch_m", [DEPTH, D_MODEL, D_MODEL], F32)
    w_bd = di("w_branch_d", [DEPTH, D_MODEL, D_MODEL], F32)
    w_out = di("w_out", [DEPTH, D_MODEL, D_MODEL], F32)
    w_g = di("w_ffn_gate", [DEPTH, D_MODEL, D_FF], F32)
    w_u = di("w_ffn_up", [DEPTH, D_MODEL, D_FF], F32)
    w_d = di("w_ffn_down", [DEPTH, D_FF, D_MODEL], F32)
    anw = di("anw", [DEPTH, 128, KC], F32)
    fnw2 = di("fnw2", [DEPTH, 128, KC], F32)
    fnw = di("fnw", [128, D_MODEL], F32)
    cw = di("cw", [DEPTH, 2, 128, 2 * NH, 5], F32)
    gb = di("gb", [DEPTH, 2, 128, 2], F32)
    mnw = di("mnw", [DEPTH, 128, D_MODEL], F32)
    dnw = di("dnw", [DEPTH, 128, D_MODEL], F32)
    lam = di("lam", [DEPTH, 128, 4, 128], F32)
    lami = di("lami", [DEPTH, 128, 2], F32)
    ident = di("ident", [128, 128], F32)
    maskneg = di("maskneg", [128, 128], F32)
    y_out = nc.dram_tensor("y", [SEQ, D_MODEL], F32, kind="ExternalOutput").ap()
    sc = lambda n, s, dt: nc.dram_tensor(n, s, dt).ap()
    qk_s = sc("qk_s", [2048, SEQ], F32)
    mv_s = sc("mv_s", [SEQ, 2048], BF16)
    mo_s = sc("mo_s", [SEQ, 2048], BF16)
    gt_s = sc("gt_s", [16, SEQ], F32)
    dqk_s = sc("dqk_s", [4096, SEQ], BF16)
    dv_s = sc("dv_s", [SEQ, 2048], BF16)
    gmd_s = sc("gmd_s", [4096, SEQ], BF16)
    hm_s = sc("hm_s", [2048, SEQ], BF16)
    hd_s = sc("hd_s", [2048, SEQ], BF16)
    x_s = sc("x_s", [SEQ, D_MODEL], F32)
    scr = {"qk": qk_s, "mv": mv_s, "mo": mo_s, "gt": gt_s, "dqk": dqk_s, "dv": dv_s, "gmd": gmd_s}
    wb = {"wbm": sc("wbm_b", [D_MODEL, D_MODEL], BF16), "wbd": sc("wbd_b", [D_MODEL, D_MODEL], BF16),
          "wout": sc("wout_b", [D_MODEL, D_MODEL], BF16), "wg": sc("wg_b", [D_MODEL, D_FF], BF16),
          "wu": sc("wu_b", [D_MODEL, D_FF], BF16), "wd": sc("wd_b", [D_FF, D_MODEL], BF16)}

    def make_cast(l):
        def pre(c):
            srcs = {"wbm": w_bm[l], "wbd": w_bd[l], "wout": w_out[l], "wg": w_g[l], "wu": w_u[l], "wd": w_d[l]}
            for k in ("wbm", "wbd", "wout", "wg", "wu", "wd"):
                src, dst = srcs[k], wb[k]
                if k in ("wg", "wu"):
                    src = src.rearrange("r (a b) -> r a b", b=1408)
                    dst = dst.rearrange("r (a b) -> r a b", b=1408)
                c.dma(POOL, dst, src, [], [Buf("cast_" + k)])
        return pre

    with contextlib.ExitStack() as es:
        c = Ctx(nc, es)
        c.alloc_banks(8)
        for l in range(depth):
            x_src = x_in if l == 0 else x_s
            final = (l == depth - 1)
            for th in range(2):
                ts = slice(th * TOK, (th + 1) * TOK)
                outs = {}
                for name, c0, n, mode, dt, sg in A_SECTIONS:
                    outs[name] = scr[name][:, ts] if mode == "F" else scr[name][ts, :]
                phase_A(c, x_src[ts, :], anw[l], w_in[l], ident, outs)
            for hh in range(2):
                hs = slice(hh * NH * 256, (hh + 1) * NH * 256)
                d = {
                    "cw": cw[l, hh], "gb": gb[l, hh], "mnw": mnw[l][:, hs], "dnw": dnw[l][:, hs],
                    "lam": lam[l], "lami": lami[l], "ident": ident, "maskneg": maskneg,
                    "mv": mv_s[:, hs], "mo": mo_s[:, hs], "dv": dv_s[:, hs],
                    "gi": gt_s[hh * NH:(hh + 1) * NH, :], "gf": gt_s[8 + hh * NH:8 + (hh + 1) * NH, :],
                    "hmT": hm_s[hs, :], "hdT": hd_s[hs, :],
                    "qraw": (lambda j, hh=hh: qk_s[(hh * NH + j) * 128:(hh * NH + j + 1) * 128, :]),
                    "kraw": (lambda j, hh=hh: qk_s[1024 + (hh * NH + j) * 128:1024 + (hh * NH + j + 1) * 128, :]),
                    "dq": (lambda j, cc, hh=hh: dqk_s[(hh * NH + j) * 256 + cc * 128:(hh * NH + j) * 256 + (cc + 1) * 128, :]),
                    "dk": (lambda j, cc, hh=hh: dqk_s[2048 + (hh * NH + j) * 256 + cc * 128:
                                                      2048 + (hh * NH + j) * 256 + (cc + 1) * 128, :]),
                }
                if hh == 0:
                    d["pre"] = make_cast(l)
                phase_BC(c, d)
            for th in range(2):
                ts = slice(th * TOK, (th + 1) * TOK)
                d = {"x": x_src[ts, :], "hmT": hm_s[:, ts], "hdT": hd_s[:, ts], "gmd": gmd_s[:, ts],
                     "xo": (y_out if final else x_s)[ts, :],
                     "wbm": wb["wbm"], "wbd": wb["wbd"], "wout": wb["wout"], "wg": wb["wg"], "wu": wb["wu"],
                     "wd": wb["wd"],
                     "nw2": fnw2[l], "ident": ident, "fnw": fnw}
                phase_D(c, d, final, last=(l == depth - 1 and th == 1))
        build_fused.stats = (c.P.n_total, dict(c.P.cnt))
    return nc


def host_params(prm):
    L = DEPTH
    anw = np.stack([nw_layout(prm["attn_norm_w"][l]) for l in range(L)])
    fnw2 = np.stack([nw_layout(prm["ffn_norm_w"][l]) for l in range(L)])
    fnw = np.ascontiguousarray(np.broadcast_to(prm["final_norm_w"][None], (128, D_MODEL))).astype(np.float32)
    cw = np.zeros((L, 2, 128, 2 * NH, 5), np.float32)
    gb = np.zeros((L, 2, 128, 2), np.float32)
    for l in range(L):
        cwl, cbl = prm["conv_w"][l], prm["conv_b"][l]
        for hh in range(2):
            for i in range(NH):
                h = hh * NH + i
                cw[l, hh, :, i, 0:4] = cwl[:, h * 128:(h + 1) * 128].T
                cw[l, hh, :, i, 4] = cbl[h * 128:(h + 1) * 128]
                cw[l, hh, :, NH + i, 0:4] = cwl[:, 1024 + h * 128:1024 + (h + 1) * 128].T
                cw[l, hh, :, NH + i, 4] = cbl[1024 + h * 128:1024 + (h + 1) * 128]
            heads = slice(hh * NH, (hh + 1) * NH)
            gb[l, hh, :, 0] = np.repeat(prm["b_igate"][l][heads], NCH)
            gb[l, hh, :, 1] = np.repeat(prm["b_fgate"][l][heads], NCH)
    mnw = np.ascontiguousarray(np.broadcast_to(prm["mlstm_norm_w"][:, None, :], (L, 128, D_MODEL))).astype(np.float32)
    dnw = np.ascontiguousarray(np.broadcast_to(prm["diff_norm_w"][:, None, :], (L, 128, D_MODEL))).astype(np.float32)
    lam = np.stack([np.stack([prm["lambda_q1"][l], prm["lambda_k1"][l], prm["lambda_q2"][l], prm["lambda_k2"][l]])
                    for l in range(L)])
    lam = np.ascontiguousarray(np.broadcast_to(lam[:, None], (L, 128, 4, 128))).astype(np.float32)
    lami = np.zeros((L, 128, 2), np.float32)
    for l in range(L):
        lami[l, :, 0] = lam_init_of(l)
        lami[l, :, 1] = 1.0 - lam_init_of(l)
    return {"anw": anw, "fnw2": fnw2, "fnw": fnw, "cw": cw, "gb": gb, "mnw": mnw, "dnw": dnw, "lam": lam,
            "lami": lami, "ident": IDENT, "maskneg": MASKNEG_NP}


_NC = {}


def kernel(**inputs):
    x = np.ascontiguousarray(inputs["x"], dtype=np.float32)
    prm = {k: np.asarray(v, dtype=np.float32) for k, v in inputs.items() if k != "x"}
    if "nc" not in _NC:
        _NC["nc"] = build_fused()
    hp = host_params(prm)
    big = {k: np.ascontiguousarray(prm[k]) for k in ("w_in", "w_branch_m", "w_branch_d", "w_out",
                                                     "w_ffn_gate", "w_ffn_up", "w_ffn_down")}
    maps = []
    for b in range(NUSED):
        m = {"x": np.ascontiguousarray(x[b])}
        m.update(big)
        m.update(hp)
        maps.append(m)
    res = run_bass_kernel_spmd(_NC["nc"], maps, core_ids=list(range(NUSED)))
    return np.stack([np.asarray(res.results[b]["y"]) for b in range(NUSED)]).astype(np.float32)
```

```python
import contextlib
import math
import numpy as np
import ml_dtypes
import concourse.bass as bass
import concourse.mybir as mybir
from concourse.bass_utils import run_bass_kernel_spmd

F32 = mybir.dt.float32
BF16 = mybir.dt.bfloat16
AF = mybir.ActivationFunctionType
ALU = mybir.AluOpType
AX = mybir.AxisListType
NPBF = ml_dtypes.bfloat16

D_MODEL = 2048
BATCH = 4
SEQ = 4096
DEPTH = 4
NCORES = 8
TOK = 2048
D_FF = 5632
N_IN = 16400
EPS = 1e-6
KC = D_MODEL // 128

PE, ACT, DVE, POOL, SP = "pe", "act", "dve", "pool", "sp"
COMPUTE = (PE, ACT, DVE, POOL)


class Buf:
    __slots__ = ("name", "last_write", "reads", "dma_sem")

    def __init__(self, name):
        self.name = name
        self.last_write = None
        self.reads = {}
        self.dma_sem = None


class Instr:
    __slots__ = ("eng", "fn", "deps", "is_dma", "sem_key", "sig_val", "needs_sig")

    def __init__(self, eng, fn, is_dma, sem_key):
        self.eng = eng
        self.fn = fn
        self.deps = []
        self.is_dma = is_dma
        self.sem_key = sem_key
        self.sig_val = None
        self.needs_sig = False


class Prog:
    NDMA = 72

    def __init__(self, nc, es):
        self.nc = nc
        self.esem = {e: es.enter_context(nc.semaphore("s_" + e)) for e in COMPUTE}
        self.dsem = [es.enter_context(nc.semaphore("d_%d" % i)) for i in range(self.NDMA)]
        self.cnt = {e: 0 for e in COMPUTE}
        self.dcnt = [0] * self.NDMA
        self.barrier = []
        self.n_total = 0
        self._reset()

    def _reset(self):
        self.q = {e: [] for e in (PE, ACT, DVE, POOL, SP)}
        self.started = set()

    def op(self, eng, fn, reads=(), writes=(), dma=False, sem_key=None, pe_accum=False):
        if dma and sem_key is None:
            sem_key = writes[0] if len(writes) else reads[0]
        ins = Instr(eng, fn, dma, sem_key)
        deps = []
        if eng not in self.started:
            self.started.add(eng)
            ins.deps.extend(self.barrier)
        for b in reads:
            if b.last_write is not None:
                deps.append(b.last_write)
        for b in writes:
            if b.last_write is not None:
                deps.append(b.last_write)
            deps.extend(b.reads.values())
        seen = set()
        for d in deps:
            if d is ins or id(d) in seen:
                continue
            seen.add(id(d))
            if (not d.is_dma) and d.eng == PE and eng == PE and not dma:
                continue
            ins.deps.append(d)
            d.needs_sig = True
        for b in reads:
            b.reads[("d", id(sem_key)) if dma else eng] = ins
        for b in writes:
            b.last_write = ins
            b.reads = {}
        self.q[eng].append(ins)
        return ins

    def end_phase(self, last=False):
        nc = self.nc
        bar = {}
        for e in self.q:
            if self.q[e]:
                bar[id(self.q[e][-1])] = self.q[e][-1]
            for ins in self.q[e]:
                if ins.is_dma:
                    bar["k%d" % id(ins.sem_key)] = ins
        barrier = []
        seen = set()
        for ins in bar.values():
            if id(ins) not in seen:
                seen.add(id(ins))
                barrier.append(ins)
                ins.needs_sig = True
        nkeys = 0
        for e in self.q:
            for ins in self.q[e]:
                if ins.is_dma and ins.needs_sig and ins.sem_key.dma_sem is None:
                    ins.sem_key.dma_sem = nkeys
                    nkeys += 1
        assert nkeys <= self.NDMA, nkeys
        for e in self.q:
            for ins in self.q[e]:
                self.n_total += 1
                if not ins.needs_sig:
                    continue
                if ins.is_dma:
                    k = ins.sem_key.dma_sem
                    self.dcnt[k] += 16
                    ins.sig_val = (self.dsem[k], self.dcnt[k], 16)
                else:
                    self.cnt[ins.eng] += 1
                    ins.sig_val = (self.esem[ins.eng], self.cnt[ins.eng], 1)
        q = self.q
        with nc.Block() as block:
            def run(e, eng_obj):
                waited = {}
                for ins in q[e]:
                    for d in ins.deps:
                        sem, val, _ = d.sig_val
                        key = id(sem)
                        if waited.get(key, 0) >= val:
                            continue
                        waited[key] = val
                        eng_obj.wait_ge(sem, val)
                    bi = ins.fn(eng_obj)
                    if ins.needs_sig:
                        sem, val, inc = ins.sig_val
                        bi.then_inc(sem, inc)
                if e == SP and last:
                    for ins in barrier:
                        sem, val, _ = ins.sig_val
                        if waited.get(id(sem), 0) >= val:
                            continue
                        waited[id(sem)] = val
                        eng_obj.wait_ge(sem, val)

            if q[PE]:
                @block.tensor
                def _(eng):
                    run(PE, eng)
            if q[ACT]:
                @block.scalar
                def _(eng):
                    run(ACT, eng)
            if q[DVE]:
                @block.vector
                def _(eng):
                    run(DVE, eng)
            if q[POOL]:
                @block.gpsimd
                def _(eng):
                    run(POOL, eng)
            if q[SP] or last:
                @block.sync
                def _(eng):
                    run(SP, eng)
        for ins in barrier:
            ins.fn = None
        for e in q:
            for ins in q[e]:
                ins.fn = None
                ins.deps = None
        self.barrier = barrier
        self._reset()


class Ctx:
    def __init__(self, nc, es):
        self.nc = nc
        self.ges = es
        self.es = None
        self.P = Prog(nc, es)
        self.uid = 0
        self.banks = []
        self.bank_bufs = []
        self.bank_i = 0
        self.evac_i = 0

    def sb(self, name, shape, dt):
        self.uid += 1
        return self.es.enter_context(self.nc.sbuf_tensor("%s_%d" % (name, self.uid), list(shape), dt))

    def begin(self):
        self.es = contextlib.ExitStack()
        self.es.__enter__()
        self.bank_bufs = [Buf("bank%d" % i) for i in range(len(self.banks))]

    def end(self, last=False):
        self.P.end_phase(last)
        self.es.__exit__(None, None, None)
        self.es = None

    def alloc_banks(self, n=8):
        for i in range(n):
            self.banks.append(self.ges.enter_context(self.nc.psum_tensor("bank%d" % i, [128, 512], F32)))
            self.bank_bufs.append(Buf("bank%d" % i))

    def next_bank(self):
        i = self.bank_i % len(self.banks)
        self.bank_i += 1
        return self.banks[i], self.bank_bufs[i]

    def mm(self, out, lhsT, rhs, start, stop, reads, writes):
        return self.P.op(PE, lambda e: e.matmul(out, lhsT, rhs, start=start, stop=stop),
                         reads=reads, writes=writes)

    def act(self, out, in_, func, reads, writes, bias=None, scale=None, accum_out=None):
        kw = {}
        if bias is not None:
            kw["bias"] = bias
        if scale is not None:
            kw["scale"] = scale
        if accum_out is not None:
            kw["accum_out"] = accum_out
        return self.P.op(ACT, lambda e: e.activation(out=out, in_=in_, func=func, **kw),
                         reads=reads, writes=writes)

    def dma(self, eng, out, in_, reads, writes, sem_key=None):
        return self.P.op(eng, lambda e: e.dma_start(out=out, in_=in_), reads=reads, writes=writes,
                         dma=True, sem_key=sem_key)

    def tt(self, eng, out, in0, in1, op, reads, writes):
        return self.P.op(eng, lambda e: e.tensor_tensor(out=out, in0=in0, in1=in1, op=op),
                         reads=reads, writes=writes)

    def ts(self, eng, out, in0, s1, s2, op0, op1, reads, writes, accum_out=None):
        if op1 is None:
            return self.P.op(eng, lambda e: e.tensor_scalar(out=out, in0=in0, scalar1=s1, scalar2=None, op0=op0),
                             reads=reads, writes=writes)
        if accum_out is not None:
            return self.P.op(eng, lambda e: e.tensor_scalar(out=out, in0=in0, scalar1=s1, scalar2=s2, op0=op0,
                                                            op1=op1, accum_out=accum_out),
                             reads=reads, writes=writes)
        return self.P.op(eng, lambda e: e.tensor_scalar(out=out, in0=in0, scalar1=s1, scalar2=s2, op0=op0, op1=op1),
                         reads=reads, writes=writes)

    def stt(self, out, in0, scalar, in1, op0, op1, reads, writes):
        return self.P.op(DVE, lambda e: e.scalar_tensor_tensor(out=out, in0=in0, scalar=scalar, in1=in1,
                                                               op0=op0, op1=op1),
                         reads=reads, writes=writes)

    def copy(self, eng, out, in_, reads, writes):
        if eng == ACT:
            return self.P.op(ACT, lambda e: e.copy(out=out, in_=in_), reads=reads, writes=writes)
        return self.P.op(eng, lambda e: e.tensor_copy(out=out, in_=in_), reads=reads, writes=writes)

    def evac_copy(self, out, in_, reads, writes):
        self.evac_i += 1
        return self.copy(ACT if self.evac_i % 2 else DVE, out, in_, reads, writes)


def rmsnorm_to_T(c, xt, xbuf, scratch, hT, hT_buf, tok0, nw, nw_buf, ident, ident_buf, pfx):
    junk, junk_b = scratch["junk"]
    ss, ss_b = scratch["ss"]
    hn, hn_b = scratch["hn"]
    c.act(junk[:], xt[:], AF.Square, reads=[xbuf], writes=[junk_b, ss_b], accum_out=ss[:, 0:1])
    c.act(ss[:, 1:2], ss[:, 0:1], AF.Sqrt, reads=[ss_b, scratch["eps_b"]], writes=[ss_b], scale=1.0 / D_MODEL, bias=scratch["eps"][:, 0:1])
    c.P.op(DVE, lambda e: e.reciprocal(out=ss[:, 2:3], in_=ss[:, 1:2]), reads=[ss_b], writes=[ss_b])
    c.act(hn[:, 0:1024], xt[:, 0:1024], AF.Copy, reads=[xbuf, ss_b], writes=[hn_b], scale=ss[:, 2:3])
    c.ts(DVE, hn[:, 1024:2048], xt[:, 1024:2048], ss[:, 2:3], None, ALU.mult, None, reads=[xbuf, ss_b], writes=[hn_b])
    for kq in range(KC // 4):
        bank, bb = c.next_bank()
        for j in range(4):
            kc = kq * 4 + j
            c.mm(bank[:, j * 128:(j + 1) * 128], hn[:, kc * 128:(kc + 1) * 128], ident[:], True, True,
                 reads=[hn_b, ident_buf], writes=[bb])
        for j in range(4):
            kc = kq * 4 + j
            if j % 2 == 0:
                c.act(hT[:, kc, tok0:tok0 + 128], bank[:, j * 128:(j + 1) * 128], AF.Copy,
                      reads=[bb, nw_buf], writes=[hT_buf], scale=nw[:, kc:kc + 1])
            else:
                c.ts(DVE, hT[:, kc, tok0:tok0 + 128], bank[:, j * 128:(j + 1) * 128], nw[:, kc:kc + 1], None,
                     ALU.mult, None, reads=[bb, nw_buf], writes=[hT_buf])


def norm_scratch(c, pfx):
    eps = c.sb(pfx + "eps", [128, 1], F32)
    eb = Buf(pfx + "eps")
    c.P.op(POOL, lambda e: e.memset(eps[:], EPS), writes=[eb])
    return {
        "junk": (c.sb(pfx + "junk", [128, 2048], BF16), Buf(pfx + "junk")),
        "ss": (c.sb(pfx + "ss", [128, 4], F32), Buf(pfx + "ss")),
        "hn": (c.sb(pfx + "hn", [128, 2048], BF16), Buf(pfx + "hn")),
        "eps": eps, "eps_b": eb,
    }


A_SECTIONS = [
    ("qk", 0, 2048, "F", F32, False),
    ("mv", 2048, 2048, "T", BF16, False),
    ("mo", 4096, 2048, "T", BF16, True),
    ("gt", 6144, 16, "F", F32, False),
    ("dqk", 6160, 4096, "F", BF16, False),
    ("dv", 10256, 2048, "T", BF16, False),
    ("gmd", 12304, 4096, "F", BF16, True),
]


def load_wblock(c, wslot, wbuf, w_dram, row0, nkc, col0, ncols):
    src = w_dram[row0:row0 + nkc * 128, col0:col0 + ncols].rearrange("(kc p) c -> p kc c", p=128)
    c.dma(POOL, wslot[:, 0:nkc, 0:ncols], src, reads=[], writes=[wbuf])


def phase_A(c, x, nw_d, w, ident_d, outs):
    if True:
        c.begin()
        hT = c.sb("hT", [128, KC, TOK], BF16)
        hT_b = Buf("hT")
        nw = c.sb("nw_s", [128, KC], F32)
        nw_b = Buf("nw")
        ident = c.sb("ident_s", [128, 128], BF16)
        ident_b = Buf("ident")
        c.dma(SP, nw[:], nw_d[:, :], [], [nw_b])
        c.dma(POOL, ident[:], ident_d[:, :], [], [ident_b])
        NW = 5
        wslots = [(c.sb("w%d" % i, [128, KC, 512], BF16), Buf("w%d" % i)) for i in range(NW)]
        xs = [(c.sb("x%d" % i, [128, D_MODEL], F32), Buf("x%d" % i)) for i in range(2)]
        scr = norm_scratch(c, "n_")
        of32 = [(c.sb("of%d" % i, [128, TOK], F32), Buf("of%d" % i)) for i in range(2)]
        obf = [(c.sb("ob%d" % i, [128, TOK], BF16), Buf("ob%d" % i)) for i in range(2)]
        otk = [(c.sb("ot%d" % i, [128, 512], BF16), Buf("ot%d" % i)) for i in range(3)]

        blocks = []
        for name, c0, n, mode, dt, sg in A_SECTIONS:
            for o in range(0, n, 512):
                blocks.append((name, c0, o, min(512, n - o), mode, dt, sg))
        PRE = NW - 1

        def issue_load(bi):
            name, c0, o, n, mode, dt, sg = blocks[bi]
            ws, wb = wslots[bi % NW]
            load_wblock(c, ws, wb, w, 0, KC, c0 + o, n)

        for bi in range(min(PRE, len(blocks))):
            issue_load(bi)

        for tt in range(TOK // 128):
            xt, xb = xs[tt % 2]
            c.dma(SP, xt[:], x[tt * 128:(tt + 1) * 128, :], [], [xb])
            rmsnorm_to_T(c, xt, xb, scr, hT, hT_b, tt * 128, nw, nw_b, ident, ident_b, "n_")

        finals = []
        cnt = {"f": 0, "b": 0, "t": 0}
        for bi, (name, c0, o, n, mode, dt, sg) in enumerate(blocks):
            if bi + PRE < len(blocks):
                issue_load(bi + PRE)
            ws, wb = wslots[bi % NW]
            od = outs[name]
            if mode == "F":
                for cc in range(0, n, 128):
                    m = min(128, n - cc)
                    if dt == F32:
                        ot, ob = of32[cnt["f"] % 2]
                        cnt["f"] += 1
                    else:
                        ot, ob = obf[cnt["b"] % 2]
                        cnt["b"] += 1
                    for tg in range(TOK // 512):
                        bank, bb = c.next_bank()
                        for kc in range(KC):
                            c.mm(bank[0:m, :], ws[:, kc, cc:cc + m], hT[:, kc, tg * 512:(tg + 1) * 512],
                                 kc == 0, kc == KC - 1, reads=[wb, hT_b], writes=[bb])
                        dst = ot[0:m, tg * 512:(tg + 1) * 512]
                        if sg:
                            c.act(dst, bank[0:m, :], AF.Sigmoid, reads=[bb], writes=[ob])
                        else:
                            c.evac_copy(dst, bank[0:m, :], reads=[bb], writes=[ob])
                    finals.append(c.dma(SP, od[o + cc:o + cc + m, :], ot[0:m, :], [ob], []))
            else:
                for tt in range(TOK // 128):
                    bank, bb = c.next_bank()
                    for kc in range(KC):
                        c.mm(bank[:, 0:n], hT[:, kc, tt * 128:(tt + 1) * 128], ws[:, kc, 0:n],
                             kc == 0, kc == KC - 1, reads=[wb, hT_b], writes=[bb])
                    ot, ob = otk[cnt["t"] % 3]
                    cnt["t"] += 1
                    if sg:
                        c.act(ot[:, 0:n], bank[:, 0:n], AF.Sigmoid, reads=[bb], writes=[ob])
                    else:
                        c.evac_copy(ot[:, 0:n], bank[:, 0:n], reads=[bb], writes=[ob])
                    finals.append(c.dma(SP, od[tt * 128:(tt + 1) * 128, o:o + n], ot[:, 0:n], [ob], []))
        c.end()


def nw_layout(v):
    return np.ascontiguousarray(v.reshape(KC, 128).T)


NH = 4
NCH = SEQ // 128
MASKNEG = -30000.0
Q_R, Q_C, Q_INTER, Q_W, Q_EM = 0, 1, 2, 3, 4
NSB = 3
MSKEW = 1


def phase_BC(c, d, do_m=True, do_a=True):
    cw_d, mv_d, mo_d, gb_d, mnw_d, dv_d, dnw_d = d["cw"], d["mv"], d["mo"], d["gb"], d["mnw"], d["dv"], d["dnw"]
    lam_d, lami_d, ident_d, mask_d, hmT_d, hdT_d = d["lam"], d["lami"], d["ident"], d["maskneg"], d["hmT"], d["hdT"]
    gi_d, gf_d = d["gi"], d["gf"]
    if True:
        c.begin()
        if "pre" in d:
            d["pre"](c)
        P = c.P
        identf = c.sb("identf", [128, 128], F32); identf_b = Buf("identf")
        identb = c.sb("identb", [128, 128], BF16); identb_b = Buf("identb")
        maskf = c.sb("maskf", [128, 128], F32); maskf_b = Buf("maskf")
        maskb = c.sb("maskb", [128, 128], BF16); maskb_b = Buf("maskb")
        onesf = c.sb("onesf", [128, 128], F32); onesf_b = Buf("onesf")
        onesb = c.sb("onesb", [128, 128], BF16); onesb_b = Buf("onesb")
        epst = c.sb("epst", [128, 1], F32); eps_b = Buf("epst")
        c.dma(SP, identf[:], ident_d[:, :], [], [identf_b])
        c.dma(POOL, identb[:], ident_d[:, :], [], [identb_b])
        c.dma(SP, maskf[:], mask_d[:, :], [], [maskf_b])
        c.dma(POOL, maskb[:], mask_d[:, :], [], [maskb_b])
        P.op(POOL, lambda e: e.memset(onesf[:], 1.0), writes=[onesf_b])
        P.op(POOL, lambda e: e.memset(onesb[:], 1.0), writes=[onesb_b])
        P.op(POOL, lambda e: e.memset(epst[:], EPS), writes=[eps_b])
        cw = c.sb("cw_s", [128, 2 * NH, 5], F32); cw_b = Buf("cw")
        c.dma(SP, cw[:], cw_d, [], [cw_b])
        gb = c.sb("gb_s", [128, 2], F32); gb_b = Buf("gb")
        c.dma(SP, gb[:], gb_d, [], [gb_b])
        mnw = c.sb("mnw_s", [128, NH * 256], F32); mnw_b = Buf("mnw")
        c.dma(SP, mnw[:], mnw_d, [], [mnw_b])
        dnw = c.sb("dnw_s", [128, NH * 256], F32); dnw_b = Buf("dnw")
        c.dma(SP, dnw[:], dnw_d, [], [dnw_b])
        lamt = c.sb("lamt", [128, 4, 128], F32); lamt_b = Buf("lamt")
        c.dma(SP, lamt[:], lam_d, [], [lamt_b])
        lami = c.sb("lami_s", [128, 2], F32); lami_b = Buf("lami")
        c.dma(SP, lami[:], lami_d, [], [lami_b])

        g_i = c.sb("g_i", [128, 128], F32); g_f = c.sb("g_f", [128, 128], F32)
        gi_b, gf_b = Buf("g_i"), Buf("g_f")
        c.dma(SP, g_i[:], gi_d.rearrange("j (ci l) -> (j ci) l", l=128), [], [gi_b])
        c.dma(SP, g_f[:], gf_d.rearrange("j (ci l) -> (j ci) l", l=128), [], [gf_b])
        sm = c.sb("gsm", [128, 16], F32); sm_b = Buf("gsm")
        NBF, MPREV, DEC, RLAST = 0, 1, 2, 3
        gq = c.sb("gq", [128, 5, 128], F32); gq_b = Buf("gq")
        g_b = c.sb("g_bb", [128, 128], F32); gbb_b = Buf("g_bb")
        g_ml = c.sb("g_ml", [128, 128], F32); gml_b = Buf("g_ml")
        g_m = c.sb("g_m", [128, 128], F32); gm_b = Buf("g_m")
        c.ts(DVE, g_i[:], g_i[:], gb[:, 0:1], None, ALU.add, None, [gi_b, gb_b], [gi_b])
        c.ts(DVE, sm[:, NBF:NBF + 1], gb[:, 1:2], -1.0, None, ALU.mult, None, [gb_b], [sm_b])
        c.act(g_f[:], g_f[:], AF.Exp, [gf_b, sm_b], [gf_b], bias=sm[:, NBF:NBF + 1], scale=-1.0)
        c.act(g_f[:], g_f[:], AF.Ln, [gf_b, onesf_b], [gf_b], bias=onesf[:, 0:1], scale=1.0)
        c.ts(DVE, g_f[:], g_f[:], -1.0, None, ALU.mult, None, [gf_b], [gf_b])
        P.op(DVE, lambda e: e.tensor_tensor_scan(out=g_b[:], data0=g_f[:], data1=g_f[:], initial=0.0,
                                                 op0=ALU.add, op1=ALU.min), reads=[gf_b], writes=[gbb_b])
        P.op(DVE, lambda e: e.tensor_tensor_scan(out=g_ml[:], data0=g_f[:], data1=g_i[:], initial=-1e30,
                                                 op0=ALU.add, op1=ALU.max), reads=[gf_b, gi_b], writes=[gml_b])
        bankr, bankr_b = c.next_bank()
        c.mm(bankr[0:1, 0:128], g_b[:, 127:128], identf[:], True, True, [gbb_b, identf_b], [bankr_b])
        c.mm(bankr[0:1, 128:256], g_ml[:, 127:128], identf[:], True, True, [gml_b, identf_b], [bankr_b])
        erow = c.sb("erow", [1, 512], F32); erow_b = Buf("erow")
        c.copy(DVE, erow[0:1, 0:256], bankr[0:1, 0:256], [bankr_b], [erow_b])
        P.op(POOL, lambda e: e.memset(erow[0:1, 256:512], 0.0), writes=[erow_b])
        for j in range(NH):
            P.op(DVE, lambda e, j=j: e.tensor_tensor_scan(
                out=erow[0:1, 384 + j * 32:384 + (j + 1) * 32], data0=erow[0:1, j * 32:(j + 1) * 32],
                data1=erow[0:1, 128 + j * 32:128 + (j + 1) * 32], initial=0.0, op0=ALU.add, op1=ALU.max),
                reads=[erow_b], writes=[erow_b])
            c.copy(DVE, erow[0:1, 256 + j * 32 + 1:256 + (j + 1) * 32], erow[0:1, 384 + j * 32:384 + (j + 1) * 32 - 1],
                   [erow_b], [erow_b])
        c.mm(bankr[:, 256:257], erow[0:1, 256:384], onesf[0:1, 0:1], True, True, [erow_b, onesf_b], [bankr_b])
        c.copy(DVE, sm[:, MPREV:MPREV + 1], bankr[:, 256:257], [bankr_b], [sm_b])
        c.stt(g_m[:], g_b[:], sm[:, MPREV:MPREV + 1], g_ml[:], ALU.add, ALU.max, [gbb_b, sm_b, gml_b], [gm_b])
        c.tt(DVE, gq[:, Q_R, :], g_b[:], g_m[:], ALU.subtract, [gbb_b, gm_b], [gq_b])
        c.tt(DVE, gq[:, Q_C, :], g_i[:], g_b[:], ALU.subtract, [gi_b, gbb_b], [gq_b])
        c.copy(DVE, sm[:, RLAST:RLAST + 1], gq[:, Q_R, 127:128], [gq_b], [sm_b])
        c.act(gq[:, Q_INTER, :], gq[:, Q_R, :], AF.Exp, [gq_b, sm_b], [gq_b], bias=sm[:, MPREV:MPREV + 1], scale=1.0)
        c.act(gq[:, Q_W, :], gq[:, Q_C, :], AF.Exp, [gq_b, sm_b], [gq_b], bias=sm[:, RLAST:RLAST + 1], scale=1.0)
        c.act(gq[:, Q_EM, :], g_m[:], AF.Exp, [gm_b], [gq_b], scale=-1.0)
        c.act(sm[:, DEC:DEC + 1], sm[:, RLAST:RLAST + 1], AF.Exp, [sm_b], [sm_b], bias=sm[:, MPREV:MPREV + 1], scale=1.0)
        tq = c.sb("tq", [128, 5, 128], F32); tq_b = Buf("tq")
        for n in range(5):
            bk, bkb = c.next_bank()
            c.mm(bk[:, 0:128], gq[:, n, :], identf[:], True, True, [gq_b, identf_b], [bkb])
            c.copy(DVE, tq[:, n, :], bk[:, 0:128], [bkb], [tq_b])
        decm = c.sb("decm", [128, 128], F32); decm_b = Buf("decm")
        c.ts(DVE, decm[:], onesf[:], sm[:, DEC:DEC + 1], None, ALU.mult, None, [onesf_b, sm_b], [decm_b])
        bk, bkb = c.next_bank()
        c.mm(bk[:, 0:128], decm[:], identf[:], True, True, [decm_b, identf_b], [bkb])
        decb = c.sb("decb", [128, 128], F32); decb_b = Buf("decb")
        c.copy(DVE, decb[:], bk[:, 0:128], [bkb], [decb_b])

        lsm = c.sb("lsm", [128, 8], F32); lsm_b = Buf("lsm")
        lpr = c.sb("lpr", [128, 2, 128], F32); lpr_b = Buf("lpr")
        c.tt(DVE, lpr[:, 0, :], lamt[:, 0, :], lamt[:, 1, :], ALU.mult, [lamt_b], [lpr_b])
        c.tt(DVE, lpr[:, 1, :], lamt[:, 2, :], lamt[:, 3, :], ALU.mult, [lamt_b], [lpr_b])
        P.op(DVE, lambda e: e.reduce_sum(out=lsm[:, 0:2], in_=lpr[:], axis=AX.X), reads=[lpr_b], writes=[lsm_b])
        c.act(lsm[:, 2:4], lsm[:, 0:2], AF.Exp, [lsm_b], [lsm_b])
        c.tt(DVE, lsm[:, 4:5], lsm[:, 2:3], lsm[:, 3:4], ALU.subtract, [lsm_b], [lsm_b])
        c.ts(DVE, lsm[:, 5:6], lsm[:, 4:5], lami[:, 0:1], -1.0, ALU.add, ALU.mult, [lsm_b, lami_b], [lsm_b])
        NLAM = 5

        rawq = c.sb("rawq", [128, SEQ + 3], F32); rawq_b = Buf("rawq")
        rawk = c.sb("rawk", [128, SEQ + 3], F32); rawk_b = Buf("rawk")
        acc = c.sb("acc", [128, SEQ], F32); acc_b = Buf("acc")
        qT = c.sb("qT", [128, SEQ], BF16); qT_b = Buf("qT")
        kT = c.sb("kT", [128, SEQ], BF16); kT_b = Buf("kT")
        va = c.sb("va", [128, NCH, 257], BF16); va_b = Buf("va")
        P.op(POOL, lambda e: e.memset(rawq[:, 0:3], 0.0), writes=[rawq_b])
        P.op(POOL, lambda e: e.memset(rawk[:, 0:3], 0.0), writes=[rawk_b])
        P.op(POOL, lambda e: e.memset(va[:, :, 256:257], 1.0), writes=[va_b])
        CT = c.sb("CT", [128, 257], F32); CT_b = Buf("CT")
        CTb = c.sb("CTb", [128, 257], BF16); CTb_b = Buf("CTb")
        diagR = [(c.sb("diagR%d" % i, [128, 128], F32), Buf("diagR%d" % i)) for i in range(2)]
        Dm = [(c.sb("Dm%d" % i, [128, 128], F32), Buf("Dm%d" % i)) for i in range(2)]
        sdT = [(c.sb("sdT%d" % i, [128, 128], BF16), Buf("sdT%d" % i)) for i in range(2)]
        kw = [(c.sb("kw%d" % i, [128, 128], BF16), Buf("kw%d" % i)) for i in range(2)]
        numS = [(c.sb("numS%d" % i, [128, 257], F32), Buf("numS%d" % i)) for i in range(4)]
        tot = [(c.sb("tot%d" % i, [128, 257], F32), Buf("tot%d" % i)) for i in range(8)]
        CT2 = [(c.sb("CT2_%d" % i, [128, 257], F32), Buf("CT2_%d" % i)) for i in range(2)]
        CTb2 = [(c.sb("CTb2_%d" % i, [128, 257], BF16), Buf("CTb2_%d" % i)) for i in range(2)]
        junk = c.sb("junk", [128, 256], BF16); junk_b = Buf("junk")
        hs = [(c.sb("hs%d" % i, [128, 8], F32), Buf("hs%d" % i)) for i in range(8)]
        g2 = [(c.sb("g2_%d" % i, [128, 256], F32), Buf("g2_%d" % i)) for i in range(4)]
        hmt = [(c.sb("hm%d" % i, [128, 256], BF16), Buf("hm%d" % i)) for i in range(2)]
        mos = [(c.sb("mos%d" % i, [128, 4, 256], BF16), Buf("mos%d" % i)) for i in range(4)]
        hout = [(c.sb("hout%d" % i, [128, 2, 512], BF16), Buf("hout%d" % i)) for i in range(2)]
        kscale = 128.0 ** -0.5

        def conv_silu(raw, raw_b, idx, dst, dst_b, post_scale):
            c.ts(DVE, acc[:], raw[:, 0:SEQ], cw[:, idx, 0:1], cw[:, idx, 4:5], ALU.mult, ALU.add,
                 [raw_b, cw_b], [acc_b])
            for t in range(1, 4):
                c.stt(acc[:], raw[:, t:t + SEQ], cw[:, idx, t:t + 1], acc[:], ALU.mult, ALU.add,
                      [raw_b, cw_b, acc_b], [acc_b])
            if post_scale is None:
                c.act(dst[:], acc[:], AF.Silu, [acc_b], [dst_b])
            else:
                c.act(acc[:], acc[:], AF.Silu, [acc_b], [acc_b])
                c.ts(POOL, dst[:], acc[:], post_scale, None, ALU.mult, None, [acc_b], [dst_b])

        grp = 0
        for j in range(NH if do_m else 0):
            if j == 0:
                c.dma(SP, rawq[:, 3:SEQ + 3], d["qraw"](j), [], [rawq_b])
                c.dma(SP, rawk[:, 3:SEQ + 3], d["kraw"](j), [], [rawk_b])
            c.dma(SP, va[:, :, 0:256], mv_d[:, j * 256:(j + 1) * 256].rearrange("(ci p) v -> p ci v", p=128),
                  [], [va_b])
            conv_silu(rawq, rawq_b, j, qT, qT_b, None)
            conv_silu(rawk, rawk_b, NH + j, kT, kT_b, kscale)
            if j + 1 < NH:
                c.dma(SP, rawq[:, 3:SEQ + 3], d["qraw"](j + 1), [], [rawq_b])
                c.dma(SP, rawk[:, 3:SEQ + 3], d["kraw"](j + 1), [], [rawk_b])
            for k2 in range(2):
                P.op(POOL, lambda e, k2=k2: e.memset(CT2[k2][0][:], 0.0), writes=[CT2[k2][1]])
                P.op(POOL, lambda e, k2=k2: e.memset(CTb2[k2][0][:], 0.0), writes=[CTb2[k2][1]])

            def s0(ci):
                p = j * NCH + ci
                cs = slice(ci * 128, (ci + 1) * 128)
                bA, bA_b = c.banks[ci % 2], c.bank_bufs[ci % 2]
                dR, dR_b = diagR[ci % 2]
                c.ts(POOL, dR[:], identf[:], tq[:, Q_R, p:p + 1], None, ALU.mult, None, [identf_b, tq_b], [dR_b])
                c.mm(bA[:, 0:128], kT[:, cs], qT[:, cs], True, True, [kT_b, qT_b], [bA_b])
                c.mm(bA[:, 128:256], onesf[:], dR[:], True, False, [onesf_b, dR_b], [bA_b])
                c.mm(bA[:, 128:256], identf[:], maskf[:], False, True, [identf_b, maskf_b], [bA_b])
                c.mm(bA[:, 256:384], kT[:, cs], identb[:], True, True, [kT_b, identb_b], [bA_b])
                if ci % 4 == 0:
                    mo_t, mo_b = mos[(ci // 4) % 4]
                    c.dma(SP, mo_t[:], mo_d[ci * 128:(ci + 4) * 128, j * 256:(j + 1) * 256]
                          .rearrange("(cc p) v -> p cc v", p=128), [], [mo_b])

            def s1(ci):
                p = j * NCH + ci
                bA, bA_b = c.banks[ci % 2], c.bank_bufs[ci % 2]
                dm, dm_b = Dm[ci % 2]
                c.act(dm[:], bA[:, 128:256], AF.Exp, [bA_b, tq_b], [dm_b], bias=tq[:, Q_C, p:p + 1], scale=1.0)

            def s2(ci):
                p = j * NCH + ci
                bA, bA_b = c.banks[ci % 2], c.bank_bufs[ci % 2]
                dm, dm_b = Dm[ci % 2]
                sd, sd_b = sdT[ci % 2]
                c.tt(DVE, sd[:], bA[:, 0:128], dm[:], ALU.mult, [bA_b, dm_b], [sd_b])
                kwt, kw_b = kw[ci % 2]
                c.ts(DVE, kwt[:], bA[:, 256:384], tq[:, Q_W, p:p + 1], None, ALU.mult, None, [bA_b, tq_b], [kw_b])

            def s3(ci):
                bB, bB_b = c.banks[2 + ci % 2], c.bank_bufs[2 + ci % 2]
                bD, bD_b = c.banks[6], c.bank_bufs[6]
                sd, sd_b = sdT[ci % 2]
                kwt, kw_b = kw[ci % 2]
                c.mm(bB[:, 0:257], sd[:], va[:, ci, :], True, True, [sd_b, va_b], [bB_b])
                c.mm(bD[:, 0:257], kwt[:], va[:, ci, :], True, True, [kw_b, va_b], [bD_b])

            def s4(ci):
                p = j * NCH + ci
                bB, bB_b = c.banks[2 + ci % 2], c.bank_bufs[2 + ci % 2]
                bD, bD_b = c.banks[6], c.bank_bufs[6]
                ns, ns_b = numS[ci % 4]
                c.copy(ACT, ns[:], bB[:, 0:257], [bB_b], [ns_b])
                cn, cn_b = CT2[ci % 2]
                cp, cp_b = CT2[(ci + 1) % 2]
                c.stt(cn[:], cp[:], decb[:, p:p + 1], bD[:, 0:257], ALU.mult, ALU.add, [cp_b, decb_b, bD_b], [cn_b])

            def s5(ci):
                cs = slice(ci * 128, (ci + 1) * 128)
                cn, cn_b = CT2[ci % 2]
                cb, cb_b = CTb2[ci % 2]
                c.copy(POOL, cb[:], cn[:], [cn_b], [cb_b])
                cbp, cbp_b = CTb2[(ci + 1) % 2]
                bC, bC_b = c.banks[4 + ci % 2], c.bank_bufs[4 + ci % 2]
                c.mm(bC[:, 0:257], qT[:, cs], cbp[:], True, True, [qT_b, cbp_b], [bC_b])

            def s6(ci):
                p = j * NCH + ci
                bC, bC_b = c.banks[4 + ci % 2], c.bank_bufs[4 + ci % 2]
                ns, ns_b = numS[ci % 4]
                tt_, tt_b = tot[ci % 8]
                c.stt(tt_[:], bC[:, 0:257], tq[:, Q_INTER, p:p + 1], ns[:], ALU.mult, ALU.add,
                      [bC_b, tq_b, ns_b], [tt_b])

            def s7(ci):
                tt_, tt_b = tot[ci % 8]
                h_, h_b = hs[ci % 8]
                c.act(h_[:, 7:8], tt_[:, 256:257], AF.Abs, [tt_b], [h_b])
                c.act(junk[:], tt_[:, 0:256], AF.Square, [tt_b], [junk_b, h_b], accum_out=h_[:, 2:3])

            def s8(ci):
                p = j * NCH + ci
                h_, h_b = hs[ci % 8]
                c.ts(DVE, h_[:, 0:1], h_[:, 7:8], tq[:, Q_EM, p:p + 1], None, ALU.max, None, [h_b, tq_b], [h_b])
                mo_t, mo_b = mos[(ci // 4) % 4]
                g2t, g2_b = g2[ci % 4]
                c.tt(POOL, g2t[:], mnw[:, j * 256:(j + 1) * 256], mo_t[:, ci % 4, :], ALU.mult, [mnw_b, mo_b], [g2_b])

            def s9(ci):
                h_, h_b = hs[ci % 8]
                c.act(h_[:, 1:2], h_[:, 0:1], AF.Square, [h_b], [h_b], scale=EPS ** 0.5)
                c.act(h_[:, 4:5], h_[:, 2:3], AF.Sqrt, [h_b], [h_b], bias=h_[:, 1:2], scale=1.0 / 256)

            def s10(ci):
                h_, h_b = hs[ci % 8]
                tt_, tt_b = tot[ci % 8]
                g2t, g2_b = g2[ci % 4]
                P.op(DVE, lambda e, h_=h_: e.reciprocal(out=h_[:, 6:7], in_=h_[:, 4:5]), reads=[h_b], writes=[h_b])
                hm_, hm_b = hmt[ci % 2]
                c.stt(hm_[:], tt_[:, 0:256], h_[:, 6:7], g2t[:], ALU.mult, ALU.mult, [tt_b, h_b, g2_b], [hm_b])

            def s11(ci):
                bT, bT_b = c.banks[7], c.bank_bufs[7]
                hm_, hm_b = hmt[ci % 2]
                for vc in range(2):
                    c.mm(bT[:, vc * 128:(vc + 1) * 128], hm_[:, vc * 128:(vc + 1) * 128], identb[:], True, True,
                         [hm_b, identb_b], [bT_b])

            def s12(ci):
                bT, bT_b = c.banks[7], c.bank_bufs[7]
                ho_t, ho_b = hout[(ci // 4) % 2]
                c.copy(ACT, ho_t[:, :, (ci % 4) * 128:(ci % 4 + 1) * 128],
                       bT[:, 0:256].rearrange("p (a b) -> p a b", a=2), [bT_b], [ho_b])
                if ci % 4 == 3:
                    c.dma(SP, hmT_d[j * 256:(j + 1) * 256, (ci - 3) * 128:(ci + 1) * 128]
                          .rearrange("(a p) t -> p a t", p=128), ho_t[:], [ho_b], [])

            stages = [s0, s1, s2, s3, s4, s5, s6, s7, s8, s9, s10, s11, s12]
            for it in range(NCH + len(stages) - 1):
                for sidx in range(len(stages) - 1, -1, -1):
                    ci = it - sidx
                    if 0 <= ci < NCH:
                        stages[sidx](ci)

        qc = [(c.sb("dq%d" % i, [128, SEQ], BF16), Buf("dq%d" % i)) for i in range(2)]
        kc_ = [(c.sb("dk%d" % i, [128, SEQ], BF16), Buf("dk%d" % i)) for i in range(2)]
        sq = c.sb("sq", [128, SEQ], BF16); sq_b = Buf("sq")
        mx = c.sb("mx", [1, 64], F32); mx_b = Buf("mx")
        nG = c.sb("nG", [128, 1], F32); nG_b = Buf("nG")
        Et = [(c.sb("E%d" % i, [128, 512], BF16), Buf("E%d" % i)) for i in range(NSB + 2)]
        ds = [(c.sb("ds%d" % i, [128, 8], F32), Buf("ds%d" % i)) for i in range(4)]
        dtm = [(c.sb("dt%d" % i, [128, 256], F32), Buf("dt%d" % i)) for i in range(4)]
        dhd = [(c.sb("dhd%d" % i, [128, 256], F32), Buf("dhd%d" % i)) for i in range(4)]
        dhn = [(c.sb("dhn%d" % i, [128, 256], BF16), Buf("dhn%d" % i)) for i in range(4)]
        Os1 = [(c.sb("os1_%d" % i, [128, 257], F32), Buf("os1_%d" % i)) for i in range(4)]
        Os2 = [(c.sb("os2_%d" % i, [128, 257], F32), Buf("os2_%d" % i)) for i in range(4)]
        junkf = c.sb("junkf", [128, 256], F32); junkf_b = Buf("junkf")
        ascale = 128.0 ** -0.5
        Obanks = [(c.banks[i], c.bank_bufs[i]) for i in range(4)]
        Sbanks = [(c.banks[4 + i], c.bank_bufs[4 + i]) for i in range(NSB)]
        Tbanks = [(c.banks[4 + NSB + i], c.bank_bufs[4 + NSB + i]) for i in range(4 - NSB)]
        e_i = 0
        s_i = 0
        t_i = 0
        for j in range(NH if do_a else 0):
            for cc in range(2):
                c.dma(SP, qc[cc][0][:], d["dq"](j, cc), [], [qc[cc][1]])
                c.dma(SP, kc_[cc][0][:], d["dk"](j, cc), [], [kc_[cc][1]])
            c.dma(SP, va[:, :, 0:256], dv_d[:, j * 256:(j + 1) * 256].rearrange("(ci p) v -> p ci v", p=128),
                  [], [va_b])
            tb, tb_b = Tbanks[0]
            for ti, (tns, tns_b) in enumerate([qc[0], qc[1], kc_[0], kc_[1]]):
                c.act(sq[:], tns[:], AF.Square, [tns_b], [sq_b])
                for s8 in range(8):
                    c.mm(tb[0:1, 0:512], onesb[:, 0:1], sq[:, s8 * 512:(s8 + 1) * 512], True, True,
                         [onesb_b, sq_b], [tb_b])
                    P.op(DVE, lambda e, ti=ti, s8=s8: e.reduce_max(out=mx[0:1, ti * 8 + s8:ti * 8 + s8 + 1],
                                                                   in_=tb[0:1, 0:512], axis=AX.X),
                         reads=[tb_b], writes=[mx_b])
            P.op(DVE, lambda e: e.reduce_max(out=mx[0:1, 32:34], in_=mx[0:1, 0:32].rearrange("p (a b) -> p a b", a=2),
                                             axis=AX.X), reads=[mx_b], writes=[mx_b])
            c.tt(DVE, mx[0:1, 34:35], mx[0:1, 32:33], mx[0:1, 33:34], ALU.mult, [mx_b], [mx_b])
            c.act(mx[0:1, 35:36], mx[0:1, 34:35], AF.Sqrt, [mx_b], [mx_b], scale=ascale * ascale)
            c.ts(DVE, mx[0:1, 36:37], mx[0:1, 35:36], -1.0, None, ALU.mult, None, [mx_b], [mx_b])
            c.mm(tb[:, 0:1], onesf[0:1, :], mx[0:1, 36:37], True, True, [onesf_b, mx_b], [tb_b])
            c.copy(DVE, nG[:], tb[:, 0:1], [tb_b], [nG_b])
            if "dbg" in d:
                c.dma(SP, d["dbg"][j, 0:1, 0:64], mx[0:1, :], [mx_b], [])

            def qk_exp(g, kb):
                nonlocal s_i, e_i
                sb_, sb_b = Sbanks[s_i % NSB]; s_i += 1
                et, et_b = Et[e_i % (NSB + 2)]; e_i += 1
                ks = slice(kb * 128, (kb + 1) * 128)
                if kb <= 2 * g:
                    for cc in range(2):
                        diag = (kb == 2 * g)
                        c.mm(sb_[:, cc * 256:(cc + 1) * 256], kc_[cc][0][:, ks], qc[cc][0][:, g * 256:(g + 1) * 256],
                             True, not diag, [kc_[cc][1], qc[cc][1]], [sb_b])
                        if diag:
                            c.mm(sb_[:, cc * 256:cc * 256 + 128], identb[:], maskb[:], False, True,
                                 [identb_b, maskb_b], [sb_b])
                    c.act(et[:], sb_[:], AF.Exp, [sb_b, nG_b], [et_b], bias=nG[:, 0:1], scale=ascale)
                    ilist = (0, 1)
                else:
                    for cc in range(2):
                        c.mm(sb_[:, cc * 256 + 128:(cc + 1) * 256], kc_[cc][0][:, ks],
                             qc[cc][0][:, g * 256 + 128:(g + 1) * 256], True, False, [kc_[cc][1], qc[cc][1]], [sb_b])
                        c.mm(sb_[:, cc * 256 + 128:(cc + 1) * 256], identb[:], maskb[:], False, True,
                             [identb_b, maskb_b], [sb_b])
                    c.act(et[:].rearrange("p (a b) -> p a b", a=2)[:, :, 128:256],
                          sb_[:].rearrange("p (a b) -> p a b", a=2)[:, :, 128:256], AF.Exp,
                          [sb_b, nG_b], [et_b], bias=nG[:, 0:1], scale=ascale)
                    ilist = (1,)
                return (g, kb, et, et_b, ilist)

            def pv(st):
                g, kb, et, et_b, ilist = st
                for cc in range(2):
                    for i in ilist:
                        ob, ob_b = Obanks[cc * 2 + i]
                        last = (kb == 2 * g + i)
                        c.mm(ob[:, 0:257], et[:, cc * 256 + i * 128:cc * 256 + (i + 1) * 128], va[:, kb, :],
                             kb == 0, last, [et_b, va_b], [ob_b])
                if kb == 2 * g + 1:
                    epilogue(g)

            def epilogue(g):
                nonlocal t_i
                for i in range(2):
                    qb = 2 * g + i
                    r = qb % 4
                    o1, o1_b = Obanks[i]
                    o2, o2_b = Obanks[2 + i]
                    os1, os1_b = Os1[r]
                    os2, os2_b = Os2[r]
                    c.copy(DVE, os1[:], o1[:, 0:257], [o1_b], [os1_b])
                    c.copy(DVE, os2[:], o2[:, 0:257], [o2_b], [os2_b])
                R = [(2 * g + i) % 4 for i in range(2)]
                for r in R:
                    P.op(DVE, lambda e, d_=ds[r][0], os1=Os1[r][0]: e.reciprocal(out=d_[:, 0:1], in_=os1[:, 256:257]),
                         reads=[Os1[r][1]], writes=[ds[r][1]])
                for r in R:
                    P.op(DVE, lambda e, d_=ds[r][0], os2=Os2[r][0]: e.reciprocal(out=d_[:, 1:2], in_=os2[:, 256:257]),
                         reads=[Os2[r][1]], writes=[ds[r][1]])
                for r in R:
                    d_, d_b = ds[r]
                    c.ts(DVE, d_[:, 2:3], d_[:, 1:2], lsm[:, NLAM:NLAM + 1], None, ALU.mult, None, [d_b, lsm_b], [d_b])
                for r in R:
                    d_, d_b = ds[r]
                    c.ts(DVE, dtm[r][0][:], Os1[r][0][:, 0:256], d_[:, 0:1], None, ALU.mult, None,
                         [Os1[r][1], d_b], [dtm[r][1]])
                for r in R:
                    d_, d_b = ds[r]
                    c.stt(dhd[r][0][:], Os2[r][0][:, 0:256], d_[:, 2:3], dtm[r][0][:], ALU.mult, ALU.add,
                          [Os2[r][1], d_b, dtm[r][1]], [dhd[r][1]])
                for r in R:
                    P.op(DVE, lambda e, hd_=dhd[r][0], d_=ds[r][0]: e.scalar_tensor_tensor(
                        out=junkf[:], in0=hd_[:], scalar=1.0, in1=hd_[:], op0=ALU.mult, op1=ALU.mult,
                        accum_out=d_[:, 3:4]), reads=[dhd[r][1]], writes=[junkf_b, ds[r][1]])
                defer.append([it_no[0] + 3, stage_b, (g,)])

            def stage_b(g):
                R = [(2 * g + i) % 4 for i in range(2)]
                for r in R:
                    d_, d_b = ds[r]
                    c.act(d_[:, 4:5], d_[:, 3:4], AF.Sqrt, [d_b, eps_b], [d_b], bias=epst[:, 0:1], scale=1.0 / 256)
                for r in R:
                    P.op(DVE, lambda e, d_=ds[r][0]: e.reciprocal(out=d_[:, 5:6], in_=d_[:, 4:5]),
                         reads=[ds[r][1]], writes=[ds[r][1]])
                for r in R:
                    d_, d_b = ds[r]
                    c.ts(DVE, d_[:, 6:7], d_[:, 5:6], lami[:, 1:2], None, ALU.mult, None, [d_b, lami_b], [d_b])
                for r in R:
                    d_, d_b = ds[r]
                    c.stt(dhn[r][0][:], dhd[r][0][:], d_[:, 6:7], dnw[:, j * 256:(j + 1) * 256], ALU.mult, ALU.mult,
                          [dhd[r][1], d_b, dnw_b], [dhn[r][1]])
                for i in range(2):
                    defer.append([it_no[0] + 3, stage_c, (g, i)])

            def stage_c(g, i):
                nonlocal t_i
                qb = 2 * g + i
                r = qb % 4
                hn_, hn_b = dhn[r]
                tb, tb_b = Tbanks[t_i % (4 - NSB)]; t_i += 1
                for vc in range(2):
                    c.mm(tb[:, vc * 128:(vc + 1) * 128], hn_[:, vc * 128:(vc + 1) * 128], identb[:], True, True,
                         [hn_b, identb_b], [tb_b])
                ho_t, ho_b = hout[(qb // 4) % 2]
                c.copy(DVE, ho_t[:, :, (qb % 4) * 128:(qb % 4 + 1) * 128],
                       tb[:, 0:256].rearrange("p (a b) -> p a b", a=2), [tb_b], [ho_b])
                if qb % 4 == 3:
                    c.dma(SP, hdT_d[j * 256:(j + 1) * 256, (qb - 3) * 128:(qb + 1) * 128]
                          .rearrange("(a p) t -> p a t", p=128), ho_t[:], [ho_b], [])

            defer = []
            it_no = [0]

            def run_deferred(flush=False):
                k = 0
                while k < len(defer):
                    if flush or defer[k][0] <= it_no[0]:
                        _, fn, args = defer.pop(k)
                        fn(*args)
                    else:
                        k += 1

            pairs = [(g, kb) for g in range(NCH // 2) for kb in range(2 * g + 2)]
            pend = []
            for (g, kb) in pairs:
                pend.append(qk_exp(g, kb))
                if len(pend) > NSB - 1:
                    pv(pend.pop(0))
                it_no[0] += 1
                run_deferred()
            while pend:
                pv(pend.pop(0))
            while defer:
                run_deferred(flush=True)
        c.end()


IDENT = np.eye(128, dtype=np.float32)
MASKNEG_NP = np.where(np.arange(128)[:, None] <= np.arange(128)[None, :], 0.0, MASKNEG).astype(np.float32)


def lam_init_of(l):
    return 0.8 - 0.6 * math.exp(-0.3 * l)


TG = 512
NFC = D_FF // 128


def phase_D(c, d, final, last):
    x_d, hm_d, hd_d, gmd_d, xo_d = d["x"], d["hmT"], d["hdT"], d["gmd"], d["xo"]
    wbm_d, wbd_d, wout_d, wg_d, wu_d, wd_d = d["wbm"], d["wbd"], d["wout"], d["wg"], d["wu"], d["wd"]
    nw2_d, ident_d = d["nw2"], d["ident"]
    if final:
        fnw_d = d["fnw"]
    if True:
        c.begin()
        P = c.P
        ident = c.sb("ident_s", [128, 128], BF16); ident_b = Buf("ident")
        c.dma(POOL, ident[:], ident_d[:, :], [], [ident_b])
        nw2 = c.sb("nw2_s", [128, KC], F32); nw2_b = Buf("nw2")
        c.dma(SP, nw2[:], nw2_d, [], [nw2_b])
        if final:
            fnw = c.sb("fnw_s", [128, D_MODEL], F32); fnw_b = Buf("fnw")
            c.dma(SP, fnw[:], fnw_d, [], [fnw_b])
        hmg = c.sb("hmg", [128, KC, TG], BF16); hmg_b = Buf("hmg")
        hdg = c.sb("hdg", [128, KC, TG], BF16); hdg_b = Buf("hdg")
        yT = c.sb("yT", [128, KC, TG], BF16); yT_b = Buf("yT")
        xg = c.sb("xg", [128, TG // 128, D_MODEL], F32)
        xg_b = [Buf("xg%d" % i) for i in range(TG // 128)]
        aT = c.sb("aT", [128, NFC, TG], BF16); aT_b = Buf("aT")
        NW = 3
        wslots = [(c.sb("w%d" % i, [128, KC, 512], BF16), Buf("w%d" % i)) for i in range(NW)]
        t1 = [(c.sb("t1_%d" % i, [128, TG], F32), Buf("t1_%d" % i)) for i in range(4)]
        t2 = [(c.sb("t2_%d" % i, [128, TG], F32), Buf("t2_%d" % i)) for i in range(2)]
        sgm = [(c.sb("sgm%d" % i, [128, TG], BF16), Buf("sgm%d" % i)) for i in range(2)]
        sgd = [(c.sb("sgd%d" % i, [128, TG], BF16), Buf("sgd%d" % i)) for i in range(2)]
        scr = norm_scratch(c, "n_")

        wlist = []
        for g in range(TOK // TG):
            for blk in range(4):
                wlist.append((wbm_d, 0, KC, blk * 512, 512))
                wlist.append((wbd_d, 0, KC, blk * 512, 512))
            for cg in range(4):
                wlist.append((wout_d, 0, KC, cg * 512, 512))
            for blk in range(D_FF // 512):
                wlist.append((wg_d, 0, KC, blk * 512, 512))
                wlist.append((wu_d, 0, KC, blk * 512, 512))
            for cg in range(4):
                for fb in range(4):
                    wlist.append((wd_d, fb * 11 * 128, 11, cg * 512, 512))
        wstate = {"issued": 0, "used": 0, "done": 0}

        def issue_one():
            i = wstate["issued"]
            wdr, r0, nkc, c0, ncol = wlist[i]
            ws, wb = wslots[i % NW]
            load_wblock(c, ws, wb, wdr, r0, nkc, c0, ncol)
            wstate["issued"] += 1

        def next_w():
            i = wstate["used"]
            while wstate["issued"] <= i:
                assert wstate["issued"] < wstate["done"] + NW
                issue_one()
            wstate["used"] += 1
            return wslots[i % NW]

        def release_w():
            wstate["done"] = wstate["used"]
            while wstate["issued"] < len(wlist) and wstate["issued"] < wstate["done"] + NW:
                issue_one()

        k2 = 0
        for g in range(TOK // TG):
            ts0 = g * TG
            c.dma(SP, hmg[:], hm_d[:, ts0:ts0 + TG].rearrange("(kc p) t -> p kc t", p=128), [], [hmg_b])
            c.dma(SP, hdg[:], hd_d[:, ts0:ts0 + TG].rearrange("(kc p) t -> p kc t", p=128), [], [hdg_b])
            for tt in range(TG // 128):
                c.dma(SP, xg[:, tt, :], x_d[ts0 + tt * 128:ts0 + (tt + 1) * 128, :], [], [xg_b[tt]])
            for blk in range(4):
                wm, wm_b = next_w()
                for cc in range(4):
                    col = blk * 4 + cc
                    sm_, sm_b = sgm[cc % 2]
                    c.dma(SP, sm_[:], gmd_d[col * 128:(col + 1) * 128, ts0:ts0 + TG], [], [sm_b])
                    bA, bA_b = c.next_bank()
                    for kc in range(KC):
                        c.mm(bA[:, :], wm[:, kc, cc * 128:(cc + 1) * 128], hmg[:, kc, :], kc == 0, kc == KC - 1,
                             [wm_b, hmg_b], [bA_b])
                    a1, a1_b = t1[cc]
                    c.tt(DVE, a1[:], bA[:, :], sm_[:], ALU.mult, [bA_b, sm_b], [a1_b])
                release_w()
                wd_, wd_b = next_w()
                for cc in range(4):
                    col = blk * 4 + cc
                    sd_, sd_b = sgd[cc % 2]
                    c.dma(SP, sd_[:], gmd_d[D_MODEL + col * 128:D_MODEL + (col + 1) * 128, ts0:ts0 + TG], [], [sd_b])
                    bB, bB_b = c.next_bank()
                    for kc in range(KC):
                        c.mm(bB[:, :], wd_[:, kc, cc * 128:(cc + 1) * 128], hdg[:, kc, :], kc == 0, kc == KC - 1,
                             [wd_b, hdg_b], [bB_b])
                    a1, a1_b = t1[cc]
                    a2, a2_b = t2[cc % 2]
                    c.tt(DVE, a2[:], bB[:, :], sd_[:], ALU.mult, [bB_b, sd_b], [a2_b])
                    c.tt(POOL, yT[:, col, :], a1[:], a2[:], ALU.add, [a1_b, a2_b], [yT_b])
                release_w()
            for cg in range(4):
                wo, wo_b = next_w()
                for tt in range(TG // 128):
                    bk, bk_b = c.next_bank()
                    for kc in range(KC):
                        c.mm(bk[:, :], yT[:, kc, tt * 128:(tt + 1) * 128], wo[:, kc, :], kc == 0, kc == KC - 1,
                             [yT_b, wo_b], [bk_b])
                    c.tt(DVE, xg[:, tt, cg * 512:(cg + 1) * 512], bk[:, :], xg[:, tt, cg * 512:(cg + 1) * 512], ALU.add,
                         [bk_b, xg_b[tt]], [xg_b[tt]])
                release_w()
            for tt in range(TG // 128):
                rmsnorm_to_T(c, xg[:, tt, :], xg_b[tt], scr, hmg, hmg_b, tt * 128, nw2, nw2_b, ident, ident_b, "n_")
            for blk in range(D_FF // 512):
                wg_, wg_b = next_w()
                for cc in range(4):
                    bG, bG_b = c.next_bank()
                    for kc in range(KC):
                        c.mm(bG[:, :], wg_[:, kc, cc * 128:(cc + 1) * 128], hmg[:, kc, :], kc == 0, kc == KC - 1,
                             [wg_b, hmg_b], [bG_b])
                    a1, a1_b = t1[cc]
                    c.act(a1[:], bG[:, :], AF.Silu, [bG_b], [a1_b])
                release_w()
                wu_, wu_b = next_w()
                for cc in range(4):
                    fc = blk * 4 + cc
                    bU, bU_b = c.next_bank()
                    for kc in range(KC):
                        c.mm(bU[:, :], wu_[:, kc, cc * 128:(cc + 1) * 128], hmg[:, kc, :], kc == 0, kc == KC - 1,
                             [wu_b, hmg_b], [bU_b])
                    a1, a1_b = t1[cc]
                    c.tt(DVE, aT[:, fc, :], bU[:, :], a1[:], ALU.mult, [bU_b, a1_b], [aT_b])
                release_w()
            for cg in range(4):
                bks = [c.next_bank() for _ in range(TG // 128)]
                for fb in range(4):
                    wdn, wdn_b = next_w()
                    for tt in range(TG // 128):
                        bk, bk_b = bks[tt]
                        for i in range(11):
                            fc = fb * 11 + i
                            c.mm(bk[:, :], aT[:, fc, tt * 128:(tt + 1) * 128], wdn[:, i, :], fc == 0, fc == NFC - 1,
                                 [aT_b, wdn_b], [bk_b])
                    release_w()
                for tt in range(TG // 128):
                    bk, bk_b = bks[tt]
                    c.tt(DVE, xg[:, tt, cg * 512:(cg + 1) * 512], bk[:, :], xg[:, tt, cg * 512:(cg + 1) * 512], ALU.add,
                         [bk_b, xg_b[tt]], [xg_b[tt]])
            for tt in range(TG // 128):
                if final:
                    junk, junk_b = scr["junk"]
                    ss, ss_b = scr["ss"]
                    c.act(junk[:], xg[:, tt, :], AF.Square, [xg_b[tt]], [junk_b, ss_b], accum_out=ss[:, 0:1])
                    c.act(ss[:, 1:2], ss[:, 0:1], AF.Sqrt, [ss_b, scr["eps_b"]], [ss_b], scale=1.0 / D_MODEL,
                          bias=scr["eps"][:, 0:1])
                    P.op(DVE, lambda e, ss=ss: e.reciprocal(out=ss[:, 2:3], in_=ss[:, 1:2]), reads=[ss_b], writes=[ss_b])
                    c.stt(xg[:, tt, :], xg[:, tt, :], ss[:, 2:3], fnw[:], ALU.mult, ALU.mult,
                          [xg_b[tt], ss_b, fnw_b], [xg_b[tt]])
                c.dma(SP, xo_d[ts0 + tt * 128:ts0 + (tt + 1) * 128, :], xg[:, tt, :], [xg_b[tt]], [], sem_key=xg_b[tt])
        c.end(last)


NUSED = 4


def build_fused(depth=DEPTH):
    nc = bass.Bass("TRN2", target_bir_lowering=False)
    di = lambda n, s, dt: nc.dram_tensor(n, s, dt, kind="ExternalInput").ap()
    x_in = di("x", [SEQ, D_MODEL], F32)
    w_in = di("w_in", [DEPTH, D_MODEL, N_IN], F32)
    w_bm = di("w_branch_m", [DEPTH, D_MODEL, D_MODEL], F32)
    w_bd = di("w_branch_d", [DEPTH, D_MODEL, D_MODEL], F32)
    w_out = di("w_out", [DEPTH, D_MODEL, D_MODEL], F32)
    w_g = di("w_ffn_gate", [DEPTH, D_MODEL, D_FF], F32)
    w_u = di("w_ffn_up", [DEPTH, D_MODEL, D_FF], F32)
    w_d = di("w_ffn_down", [DEPTH, D_FF, D_MODEL], F32)
    anw = di("anw", [DEPTH, 128, KC], F32)
    fnw2 = di("fnw2", [DEPTH, 128, KC], F32)
    fnw = di("fnw", [128, D_MODEL], F32)
    cw = di("cw", [DEPTH, 2, 128, 2 * NH, 5], F32)
    gb = di("gb", [DEPTH, 2, 128, 2], F32)
    mnw = di("mnw", [DEPTH, 128, D_MODEL], F32)
    dnw = di("dnw", [DEPTH, 128, D_MODEL], F32)
    lam = di("lam", [DEPTH, 128, 4, 128], F32)
    lami = di("lami", [DEPTH, 128, 2], F32)
    ident = di("ident", [128, 128], F32)
    maskneg = di("maskneg", [128, 128], F32)
    y_out = nc.dram_tensor("y", [SEQ, D_MODEL], F32, kind="ExternalOutput").ap()
    sc = lambda n, s, dt: nc.dram_tensor(n, s, dt).ap()
    qk_s = sc("qk_s", [2048, SEQ], F32)
    mv_s = sc("mv_s", [SEQ, 2048], BF16)
    mo_s = sc("mo_s", [SEQ, 2048], BF16)
    gt_s = sc("gt_s", [16, SEQ], F32)
    dqk_s = sc("dqk_s", [4096, SEQ], BF16)
    dv_s = sc("dv_s", [SEQ, 2048], BF16)
    gmd_s = sc("gmd_s", [4096, SEQ], BF16)
    hm_s = sc("hm_s", [2048, SEQ], BF16)
    hd_s = sc("hd_s", [2048, SEQ], BF16)
    x_s = sc("x_s", [SEQ, D_MODEL], F32)
    scr = {"qk": qk_s, "mv": mv_s, "mo": mo_s, "gt": gt_s, "dqk": dqk_s, "dv": dv_s, "gmd": gmd_s}
    wb = {"wbm": sc("wbm_b", [D_MODEL, D_MODEL], BF16), "wbd": sc("wbd_b", [D_MODEL, D_MODEL], BF16),
          "wout": sc("wout_b", [D_MODEL, D_MODEL], BF16), "wg": sc("wg_b", [D_MODEL, D_FF], BF16),
          "wu": sc("wu_b", [D_MODEL, D_FF], BF16), "wd": sc("wd_b", [D_FF, D_MODEL], BF16)}

    def make_cast(l):
        def pre(c):
            srcs = {"wbm": w_bm[l], "wbd": w_bd[l], "wout": w_out[l], "wg": w_g[l], "wu": w_u[l], "wd": w_d[l]}
            for k in ("wbm", "wbd", "wout", "wg", "wu", "wd"):
                src, dst = srcs[k], wb[k]
                if k in ("wg", "wu"):
                    src = src.rearrange("r (a b) -> r a b", b=1408)
                    dst = dst.rearrange("r (a b) -> r a b", b=1408)
                c.dma(POOL, dst, src, [], [Buf("cast_" + k)])
        return pre

    with contextlib.ExitStack() as es:
        c = Ctx(nc, es)
        c.alloc_banks(8)
        for l in range(depth):
            x_src = x_in if l == 0 else x_s
            final = (l == depth - 1)
            for th in range(2):
                ts = slice(th * TOK, (th + 1) * TOK)
                outs = {}
                for name, c0, n, mode, dt, sg in A_SECTIONS:
                    outs[name] = scr[name][:, ts] if mode == "F" else scr[name][ts, :]
                phase_A(c, x_src[ts, :], anw[l], w_in[l], ident, outs)
            for hh in range(2):
                hs = slice(hh * NH * 256, (hh + 1) * NH * 256)
                d = {
                    "cw": cw[l, hh], "gb": gb[l, hh], "mnw": mnw[l][:, hs], "dnw": dnw[l][:, hs],
                    "lam": lam[l], "lami": lami[l], "ident": ident, "maskneg": maskneg,
                    "mv": mv_s[:, hs], "mo": mo_s[:, hs], "dv": dv_s[:, hs],
                    "gi": gt_s[hh * NH:(hh + 1) * NH, :], "gf": gt_s[8 + hh * NH:8 + (hh + 1) * NH, :],
                    "hmT": hm_s[hs, :], "hdT": hd_s[hs, :],
                    "qraw": (lambda j, hh=hh: qk_s[(hh * NH + j) * 128:(hh * NH + j + 1) * 128, :]),
                    "kraw": (lambda j, hh=hh: qk_s[1024 + (hh * NH + j) * 128:1024 + (hh * NH + j + 1) * 128, :]),
                    "dq": (lambda j, cc, hh=hh: dqk_s[(hh * NH + j) * 256 + cc * 128:(hh * NH + j) * 256 + (cc + 1) * 128, :]),
                    "dk": (lambda j, cc, hh=hh: dqk_s[2048 + (hh * NH + j) * 256 + cc * 128:
                                                      2048 + (hh * NH + j) * 256 + (cc + 1) * 128, :]),
                }
                if hh == 0:
                    d["pre"] = make_cast(l)
                phase_BC(c, d)
            for th in range(2):
                ts = slice(th * TOK, (th + 1) * TOK)
                d = {"x": x_src[ts, :], "hmT": hm_s[:, ts], "hdT": hd_s[:, ts], "gmd": gmd_s[:, ts],
                     "xo": (y_out if final else x_s)[ts, :],
                     "wbm": wb["wbm"], "wbd": wb["wbd"], "wout": wb["wout"], "wg": wb["wg"], "wu": wb["wu"],
                     "wd": wb["wd"],
                     "nw2": fnw2[l], "ident": ident, "fnw": fnw}
                phase_D(c, d, final, last=(l == depth - 1 and th == 1))
        build_fused.stats = (c.P.n_total, dict(c.P.cnt))
    return nc


def host_params(prm):
    L = DEPTH
    anw = np.stack([nw_layout(prm["attn_norm_w"][l]) for l in range(L)])
    fnw2 = np.stack([nw_layout(prm["ffn_norm_w"][l]) for l in range(L)])
    fnw = np.ascontiguousarray(np.broadcast_to(prm["final_norm_w"][None], (128, D_MODEL))).astype(np.float32)
    cw = np.zeros((L, 2, 128, 2 * NH, 5), np.float32)
    gb = np.zeros((L, 2, 128, 2), np.float32)
    for l in range(L):
        cwl, cbl = prm["conv_w"][l], prm["conv_b"][l]
        for hh in range(2):
            for i in range(NH):
                h = hh * NH + i
                cw[l, hh, :, i, 0:4] = cwl[:, h * 128:(h + 1) * 128].T
                cw[l, hh, :, i, 4] = cbl[h * 128:(h + 1) * 128]
                cw[l, hh, :, NH + i, 0:4] = cwl[:, 1024 + h * 128:1024 + (h + 1) * 128].T
                cw[l, hh, :, NH + i, 4] = cbl[1024 + h * 128:1024 + (h + 1) * 128]
            heads = slice(hh * NH, (hh + 1) * NH)
            gb[l, hh, :, 0] = np.repeat(prm["b_igate"][l][heads], NCH)
            gb[l, hh, :, 1] = np.repeat(prm["b_fgate"][l][heads], NCH)
    mnw = np.ascontiguousarray(np.broadcast_to(prm["mlstm_norm_w"][:, None, :], (L, 128, D_MODEL))).astype(np.float32)
    dnw = np.ascontiguousarray(np.broadcast_to(prm["diff_norm_w"][:, None, :], (L, 128, D_MODEL))).astype(np.float32)
    lam = np.stack([np.stack([prm["lambda_q1"][l], prm["lambda_k1"][l], prm["lambda_q2"][l], prm["lambda_k2"][l]])
                    for l in range(L)])
    lam = np.ascontiguousarray(np.broadcast_to(lam[:, None], (L, 128, 4, 128))).astype(np.float32)
    lami = np.zeros((L, 128, 2), np.float32)
    for l in range(L):
        lami[l, :, 0] = lam_init_of(l)
        lami[l, :, 1] = 1.0 - lam_init_of(l)
    return {"anw": anw, "fnw2": fnw2, "fnw": fnw, "cw": cw, "gb": gb, "mnw": mnw, "dnw": dnw, "lam": lam,
            "lami": lami, "ident": IDENT, "maskneg": MASKNEG_NP}


_NC = {}


def kernel(**inputs):
    x = np.ascontiguousarray(inputs["x"], dtype=np.float32)
    prm = {k: np.asarray(v, dtype=np.float32) for k, v in inputs.items() if k != "x"}
    if "nc" not in _NC:
        _NC["nc"] = build_fused()
    hp = host_params(prm)
    big = {k: np.ascontiguousarray(prm[k]) for k in ("w_in", "w_branch_m", "w_branch_d", "w_out",
                                                     "w_ffn_gate", "w_ffn_up", "w_ffn_down")}
    maps = []
    for b in range(NUSED):
        m = {"x": np.ascontiguousarray(x[b])}
        m.update(big)
        m.update(hp)
        maps.append(m)
    res = run_bass_kernel_spmd(_NC["nc"], maps, core_ids=list(range(NUSED)))
    return np.stack([np.asarray(res.results[b]["y"]) for b in range(NUSED)]).astype(np.float32)
```
